# Optimizing a Trainium2 kernel written in Bass

```python
import math
import jax, jax.numpy as jnp
from jax import lax
import numpy as np

D_MODEL = 1024
BATCH = 2
SEQ = 8192
DEPTH = 2

CTX_LEN = 256
GRID_W = 64
Q_BLOCK = 128
RET_CHUNK = 128
ROPE_THETA = 10000.0
NORM_EPS = 1e-6
RWKV_LN_EPS = 64e-5

RET_HEADS = 4
RET_DK = 128
RET_DV = 128
GQA_Q_HEADS = 4
GQA_KV_HEADS = 2
GQA_GROUP = GQA_Q_HEADS // GQA_KV_HEADS
GQA_HEAD_DIM = 128
DIFF_HEADS = 4
DIFF_HEAD_DIM = 64
DIFF_V_DIM = 2 * DIFF_HEAD_DIM
RWKV_HEADS = 8
RWKV_HEAD_DIM = 64
RWKV_W = RWKV_HEADS * RWKV_HEAD_DIM
RWKV_W_LORA = 64
RWKV_A_LORA = 64
RWKV_G_LORA = 128
FFN_HIDDEN = 2816
N_EXPERTS = 8
TOP_K = 2
EXPERT_HIDDEN = 3584

N_EVEN = (DEPTH + 1) // 2
N_ODD = DEPTH // 2

EVEN_WIDTHS = (RET_HEADS * RET_DK, RET_HEADS * RET_DK, RET_HEADS * RET_DV, RET_HEADS * RET_DV,
               GQA_Q_HEADS * GQA_HEAD_DIM, GQA_KV_HEADS * GQA_HEAD_DIM, GQA_KV_HEADS * GQA_HEAD_DIM)
EVEN_IN = sum(EVEN_WIDTHS)
EVEN_MIX = RET_HEADS * RET_DV + GQA_Q_HEADS * GQA_HEAD_DIM
RWKV_WIDTHS = (RWKV_W, RWKV_W, RWKV_W, 2 * RWKV_W_LORA, 2 * RWKV_A_LORA, RWKV_G_LORA)
RWKV_SHIFT_W = sum(RWKV_WIDTHS)
ODD_WIDTHS = (DIFF_HEADS * 2 * DIFF_HEAD_DIM, DIFF_HEADS * 2 * DIFF_HEAD_DIM, DIFF_HEADS * DIFF_V_DIM, RWKV_SHIFT_W)
ODD_IN = sum(ODD_WIDTHS)
ODD_MIX = DIFF_HEADS * DIFF_V_DIM + RWKV_W

kernel_name = 'hybrid_retention_gqa_diffattn_rwkv7_moe_dit'


def rmsnorm(x, g, eps=NORM_EPS):
    xf = x.astype(jnp.float32)
    y = xf * lax.rsqrt(jnp.mean(xf * xf, axis=-1, keepdims=True) + eps)
    return (y * g.astype(jnp.float32)).astype(x.dtype)


def head_layernorm(y, g, b, eps):
    yf = y.astype(jnp.float32)
    mu = jnp.mean(yf, axis=-1, keepdims=True)
    var = jnp.mean(jnp.square(yf - mu), axis=-1, keepdims=True)
    h, n = y.shape[-2:]
    return (yf - mu) * lax.rsqrt(var + eps) * g.reshape(h, n).astype(jnp.float32) + b.reshape(h, n).astype(jnp.float32)


def modulate(h, shift, scale):
    return h * (1.0 + scale) + shift


def split_cols(p, widths):
    out, start = [], 0
    for w in widths:
        out.append(p[..., start:start + w])
        start += w
    return out


def axial_rope_tables(rows, dim):
    row = jnp.repeat(jnp.arange(rows, dtype=jnp.float32), GRID_W)
    col = jnp.tile(jnp.arange(GRID_W, dtype=jnp.float32), rows)
    n_freq = dim // 4
    freqs = ROPE_THETA ** (-jnp.arange(n_freq, dtype=jnp.float32) / n_freq)
    ang = jnp.concatenate([row[:, None] * freqs, col[:, None] * freqs], axis=-1)
    return jnp.cos(ang), jnp.sin(ang)


def apply_rope(x, cos, sin):
    half = x.shape[-1] // 2
    xf = x.astype(jnp.float32)
    x1, x2 = xf[..., :half], xf[..., half:]
    return jnp.concatenate([x1 * cos - x2 * sin, x1 * sin + x2 * cos], axis=-1).astype(x.dtype)


def sweep_query_blocks(block_fn, q):
    lead = q.shape[:-2]
    n, d = q.shape[-2:]
    nb = n // Q_BLOCK
    qb = jnp.moveaxis(q.reshape(lead + (nb, Q_BLOCK, d)), -3, 0)
    out = jnp.moveaxis(lax.map(block_fn, qb), 0, -3)
    return out.reshape(out.shape[:-3] + (n, out.shape[-1]))


def gqa_attend(q, k, v):
    scale = q.shape[-1] ** -0.5
    def block(qb):
        s = jnp.einsum('bkgqd,bksd->bkgqs', qb, k).astype(jnp.float32) * scale
        p = jax.nn.softmax(s, axis=-1).astype(v.dtype)
        return jnp.einsum('bkgqs,bksd->bkgqd', p, v)
    return sweep_query_blocks(block, q)


def diff_attend(q, k, v, lam):
    scale = q.shape[-1] ** -0.5
    def block(qb):
        s = jnp.einsum('bhmqd,bhmsd->bhmqs', qb, k).astype(jnp.float32) * scale
        p = jax.nn.softmax(s, axis=-1)
        p = p[:, :, 0] - lam * p[:, :, 1]
        return jnp.einsum('bhqs,bhsv->bhqv', p.astype(v.dtype), v)
    return sweep_query_blocks(block, q)


def retention_scan(q, k, v, log_gamma, state0, strict):
    bsz, nh, n, dk = q.shape
    dv = v.shape[-1]
    nc = n // RET_CHUNK
    pos = jnp.arange(RET_CHUNK, dtype=jnp.float32)
    diff = pos[:, None] - pos[None, :]
    mask = diff > 0 if strict else diff >= 0
    inner_decay = jnp.where(mask, jnp.exp(log_gamma[:, None, None] * jnp.maximum(diff, 0.0)), 0.0)
    q_decay = jnp.exp(log_gamma[:, None] * (pos + 1.0))[..., None]
    k_decay = jnp.exp(log_gamma[:, None] * (RET_CHUNK - 1.0 - pos))[..., None]
    chunk_decay = jnp.exp(log_gamma * RET_CHUNK)[:, None, None]

    def to_chunks(t):
        return jnp.moveaxis(t.astype(jnp.float32).reshape(bsz, nh, nc, RET_CHUNK, t.shape[-1]), 2, 0)

    def step(r_state, qkv):
        qc, kc, vc = qkv
        inner = jnp.einsum('bhnd,bhmd->bhnm', qc, kc) * inner_decay
        y = jnp.einsum('bhnm,bhmv->bhnv', inner, vc) + jnp.einsum('bhnd,bhdv->bhnv', qc * q_decay, r_state)
        r_state = r_state * chunk_decay + jnp.einsum('bhmd,bhmv->bhdv', kc * k_decay, vc)
        return r_state, y

    r_final, ys = lax.scan(step, state0, (to_chunks(q), to_chunks(k), to_chunks(v)))
    return jnp.moveaxis(ys, 0, 2).reshape(bsz, nh, n, dv), r_final


def bidirectional_retention(ctx_qkv, lat_qkv, log_gamma):
    bsz, nh, _, dk = ctx_qkv[0].shape
    dv = ctx_qkv[2].shape[-1]
    zero = jnp.zeros((bsz, nh, dk, dv), jnp.float32)
    y_ctx, y_lat = 0.0, 0.0
    for d in range(2):
        f = (lambda t: t[..., ::-1, :]) if d else (lambda t: t)
        yc, state = retention_scan(*[f(t) for t in ctx_qkv], log_gamma[d], zero, strict=bool(d))
        yl, _ = retention_scan(*[f(t) for t in lat_qkv], log_gamma[d], state, strict=bool(d))
        y_ctx = y_ctx + f(yc)
        y_lat = y_lat + f(yl)
    return y_ctx, y_lat


def centred_token_shift(p, mu):
    prev = jnp.pad(p[:, :-1], ((0, 0), (1, 0), (0, 0)))
    nxt = jnp.pad(p[:, 1:], ((0, 0), (0, 1), (0, 0)))
    return p + mu * (0.5 * (prev + nxt) - p)


def rwkv_inputs(p, w0, w2, a0, a2, k_k, k_a):
    bsz, n, _ = p.shape
    r, k, v, wd, ad, gd = split_cols(p, RWKV_WIDTHS)
    def heads(t):
        return t.reshape(bsz, n, RWKV_HEADS, RWKV_HEAD_DIM)
    wd = wd.reshape(bsz, n, 2, RWKV_W_LORA)
    ad = ad.reshape(bsz, n, 2, RWKV_A_LORA)
    kk = heads(k * k_k).astype(jnp.float32)
    kk = kk / jnp.maximum(jnp.sqrt(jnp.sum(kk * kk, axis=-1, keepdims=True)), 1e-12)
    per_dir = []
    for d in range(2):
        wl = (w0[d] + jnp.tanh(wd[:, :, d]) @ w2[d]).astype(jnp.float32)
        decay = jnp.exp(-jnp.exp(-jax.nn.softplus(-wl) - 0.5))
        a = jax.nn.sigmoid(a0[d] + ad[:, :, d] @ a2[d])
        kd = k * (1.0 + (a - 1.0) * k_a)
        per_dir.append((heads(r), heads(decay), heads(kd), heads(v), -kk, kk * heads(a).astype(jnp.float32)))
    return per_dir, (heads(r), heads(k), heads(v), gd)


def rwkv_scan(r, w, k, v, a, b, state0, read_before):
    def step(s, inp):
        rt, wt, kt, vt, at, bt = inp
        s_new = (s * wt[:, :, None, :]
                 + jnp.einsum('bhvk,bhk->bhv', s, at)[..., None] * bt[:, :, None, :]
                 + vt[..., None] * kt[:, :, None, :])
        y = jnp.einsum('bhvk,bhk->bhv', s if read_before else s_new, rt)
        return s_new, y
    xs = tuple(jnp.moveaxis(t.astype(jnp.float32), 1, 0) for t in (r, w, k, v, a, b))
    s_final, ys = lax.scan(step, state0, xs)
    return jnp.moveaxis(ys, 0, 1), s_final


def rwkv_branch(pc, pl, w0, w2, a0, a2, g2, k_k, k_a, r_k, ln_g, ln_b, need_ctx):
    ctx_dirs, ctx_extra = rwkv_inputs(pc, w0, w2, a0, a2, k_k, k_a)
    lat_dirs, lat_extra = rwkv_inputs(pl, w0, w2, a0, a2, k_k, k_a)
    bsz = pl.shape[0]
    zero = jnp.zeros((bsz, RWKV_HEADS, RWKV_HEAD_DIM, RWKV_HEAD_DIM), jnp.float32)
    y_ctx, y_lat = 0.0, 0.0
    for d in range(2):
        f = (lambda t: t[:, ::-1]) if d else (lambda t: t)
        yc, state = rwkv_scan(*[f(t) for t in ctx_dirs[d]], zero, read_before=bool(d))
        yl, _ = rwkv_scan(*[f(t) for t in lat_dirs[d]], state, read_before=bool(d))
        y_ctx = y_ctx + f(yc)
        y_lat = y_lat + f(yl)

    def finish(y, extra):
        r, k, v, gd = extra
        n = r.shape[1]
        y = head_layernorm(y, ln_g, ln_b, RWKV_LN_EPS).astype(r.dtype)
        bonus = jnp.sum(r * k * r_k, axis=-1, keepdims=True) * v
        return (y + bonus).reshape(bsz, n, RWKV_W) * (jax.nn.sigmoid(gd) @ g2)

    return (finish(y_ctx, ctx_extra) if need_ctx else None), finish(y_lat, lat_extra)


def even_mixer(hc, hl, rope_ret, rope_gqa, w_in, w_out, decay_exp, ret_g, q_g, k_g, need_ctx):
    bsz = hl.shape[0]
    log_gamma = jnp.log1p(-jnp.exp2(-decay_exp.astype(jnp.float32)))

    def project(h, rope_r, rope_a):
        n = h.shape[1]
        rq, rk, rv, rg, aq, ak, av = split_cols(h @ w_in, EVEN_WIDTHS)
        def heads(t, nh, d):
            return t.reshape(bsz, n, nh, d).transpose(0, 2, 1, 3)
        rq = heads(rq, RET_HEADS, RET_DK)
        rk = heads(rk, RET_HEADS, RET_DK) * RET_DK ** -0.5
        rv = heads(rv, RET_HEADS, RET_DV)
        aq = heads(rmsnorm(aq.reshape(bsz, n, GQA_Q_HEADS, GQA_HEAD_DIM), q_g), GQA_Q_HEADS, GQA_HEAD_DIM)
        ak = heads(rmsnorm(ak.reshape(bsz, n, GQA_KV_HEADS, GQA_HEAD_DIM), k_g), GQA_KV_HEADS, GQA_HEAD_DIM)
        av = heads(av, GQA_KV_HEADS, GQA_HEAD_DIM)
        if rope_r is not None:
            rq, rk = apply_rope(rq, *rope_r), apply_rope(rk, *rope_r)
            aq, ak = apply_rope(aq, *rope_a), apply_rope(ak, *rope_a)
        aq = aq.reshape(bsz, GQA_KV_HEADS, GQA_GROUP, n, GQA_HEAD_DIM)
        return (rq, rk, rv), rg, (aq, ak, av)

    ctx_ret, ctx_gate, (cq, ck, cv) = project(hc, None, None)
    lat_ret, lat_gate, (lq, lk, lv) = project(hl, rope_ret, rope_gqa)
    ret_ctx, ret_lat = bidirectional_retention(ctx_ret, lat_ret, log_gamma)

    def ret_out(y, gate):
        n = y.shape[2]
        y = rmsnorm(y.transpose(0, 2, 1, 3), ret_g.reshape(RET_HEADS, RET_DV))
        return y.reshape(bsz, n, RET_HEADS * RET_DV).astype(gate.dtype) * jax.nn.silu(gate)

    def gqa_out(o):
        n = o.shape[3]
        o = o.reshape(bsz, GQA_Q_HEADS, n, GQA_HEAD_DIM).transpose(0, 2, 1, 3)
        return o.reshape(bsz, n, GQA_Q_HEADS * GQA_HEAD_DIM)

    attn_lat = gqa_attend(lq, jnp.concatenate([ck, lk], axis=2), jnp.concatenate([cv, lv], axis=2))
    y_lat = jnp.concatenate([ret_out(ret_lat, lat_gate), gqa_out(attn_lat)], axis=-1) @ w_out
    y_ctx = None
    if need_ctx:
        y_ctx = jnp.concatenate([ret_out(ret_ctx, ctx_gate), gqa_out(gqa_attend(cq, ck, cv))], axis=-1) @ w_out
    return y_ctx, y_lat


def odd_mixer(hc, hl, rope_diff, w_in, w_out, lam_vec, subln_g, lam_init, mu, w0, w2, a0, a2, g2,
              k_k, k_a, r_k, ln_g, ln_b, need_ctx):
    bsz = hl.shape[0]
    lamv = lam_vec.astype(jnp.float32)
    lam = jnp.exp(jnp.sum(lamv[0] * lamv[1])) - jnp.exp(jnp.sum(lamv[2] * lamv[3])) + lam_init

    def project(h, rope):
        n = h.shape[1]
        dq, dk, dv, rw = split_cols(h @ w_in, ODD_WIDTHS)
        q = dq.reshape(bsz, n, DIFF_HEADS, 2, DIFF_HEAD_DIM).transpose(0, 2, 3, 1, 4)
        k = dk.reshape(bsz, n, DIFF_HEADS, 2, DIFF_HEAD_DIM).transpose(0, 2, 3, 1, 4)
        v = dv.reshape(bsz, n, DIFF_HEADS, DIFF_V_DIM).transpose(0, 2, 1, 3)
        if rope is not None:
            q, k = apply_rope(q, *rope), apply_rope(k, *rope)
        return (q, k, v), centred_token_shift(rw, mu)

    (cq, ck, cv), c_rw = project(hc, None)
    (lq, lk, lv), l_rw = project(hl, rope_diff)
    rwkv_ctx, rwkv_lat = rwkv_branch(c_rw, l_rw, w0, w2, a0, a2, g2, k_k, k_a, r_k, ln_g, ln_b, need_ctx)

    def diff_out(o):
        n = o.shape[2]
        o = rmsnorm(o.transpose(0, 2, 1, 3), subln_g) * (1.0 - lam_init)
        return o.reshape(bsz, n, DIFF_HEADS * DIFF_V_DIM)

    attn_lat = diff_attend(lq, jnp.concatenate([ck, lk], axis=3), jnp.concatenate([cv, lv], axis=2), lam)
    y_lat = jnp.concatenate([diff_out(attn_lat), rwkv_lat], axis=-1) @ w_out
    y_ctx = None
    if need_ctx:
        y_ctx = jnp.concatenate([diff_out(diff_attend(cq, ck, cv, lam)), rwkv_ctx], axis=-1) @ w_out
    return y_ctx, y_lat


def swiglu(h, wg, wu, wd):
    return (jax.nn.silu(h @ wg) * (h @ wu)) @ wd


def moe_swiglu(h, router, wg, wu, wd):
    logits = (h @ router).astype(jnp.float32)
    top_logits, top_idx = lax.top_k(logits, TOP_K)
    top_w = jax.nn.softmax(top_logits, axis=-1)
    gates = jnp.einsum('bske,bsk->bse', jax.nn.one_hot(top_idx, N_EXPERTS, dtype=jnp.float32), top_w).astype(h.dtype)
    out = jnp.zeros_like(h)
    for e in range(N_EXPERTS):
        out = out + gates[..., e:e + 1] * swiglu(h, wg[e], wu[e], wd[e])
    return out


def setup_inputs(seed: int = 0) -> dict:
    key = jax.random.key(seed)
    ks = iter(jax.random.split(key, 40))
    f32 = jnp.float32
    def nrm(shape, scale):
        return scale * jax.random.normal(next(ks), shape, f32)
    D = D_MODEL
    ramp = jnp.arange(RWKV_W, dtype=f32) / (RWKV_W - 1)
    return {
        'x': nrm((BATCH, SEQ, D), 1.0),
        'c': nrm((BATCH, D), 1.0),
        'ctx': nrm((BATCH, CTX_LEN, D), 1.0),
        'c_ctx': nrm((D,), 1.0),
        'mod_w': nrm((DEPTH, D, 6 * D), 0.5 * D ** -0.5),
        'mod_b': nrm((DEPTH, 6 * D), 0.02),
        'norm_g': 1.0 + nrm((DEPTH, 4, D), 0.05),
        'even_w_in': nrm((N_EVEN, D, EVEN_IN), D ** -0.5),
        'even_w_out': nrm((N_EVEN, EVEN_MIX, D), EVEN_MIX ** -0.5),
        'ret_decay_exp': 5.0 + jnp.arange(RET_HEADS, dtype=f32) + nrm((N_EVEN, 2, RET_HEADS), 0.1),
        'ret_norm_g': 1.0 + nrm((N_EVEN, RET_HEADS * RET_DV), 0.05),
        'gqa_q_norm': 1.0 + nrm((N_EVEN, GQA_HEAD_DIM), 0.05),
        'gqa_k_norm': 1.0 + nrm((N_EVEN, GQA_HEAD_DIM), 0.05),
        'ffn_w_gate': nrm((N_EVEN, D, FFN_HIDDEN), D ** -0.5),
        'ffn_w_up': nrm((N_EVEN, D, FFN_HIDDEN), D ** -0.5),
        'ffn_w_down': nrm((N_EVEN, FFN_HIDDEN, D), FFN_HIDDEN ** -0.5),
        'odd_w_in': nrm((N_ODD, D, ODD_IN), D ** -0.5),
        'odd_w_out': nrm((N_ODD, ODD_MIX, D), ODD_MIX ** -0.5),
        'diff_lambda': nrm((N_ODD, 4, DIFF_HEAD_DIM), 0.1),
        'diff_subln_g': 1.0 + nrm((N_ODD, DIFF_V_DIM), 0.05),
        'rwkv_mu': jax.random.uniform(next(ks), (N_ODD, RWKV_SHIFT_W), f32),
        'rwkv_w0': -6.0 + 5.0 * ramp ** 0.85 + nrm((N_ODD, 2, RWKV_W), 0.1),
        'rwkv_w2': nrm((N_ODD, 2, RWKV_W_LORA, RWKV_W), 0.5 * RWKV_W_LORA ** -0.5),
        'rwkv_a0': nrm((N_ODD, 2, RWKV_W), 0.1),
        'rwkv_a2': nrm((N_ODD, 2, RWKV_A_LORA, RWKV_W), 0.5 * RWKV_A_LORA ** -0.5),
        'rwkv_g2': nrm((N_ODD, RWKV_G_LORA, RWKV_W), RWKV_G_LORA ** -0.5),
        'rwkv_k_k': 0.85 + nrm((N_ODD, RWKV_W), 0.05),
        'rwkv_k_a': 1.0 + nrm((N_ODD, RWKV_W), 0.05),
        'rwkv_r_k': nrm((N_ODD, RWKV_HEADS, RWKV_HEAD_DIM), 0.1),
        'rwkv_ln_g': 1.0 + nrm((N_ODD, RWKV_W), 0.05),
        'rwkv_ln_b': nrm((N_ODD, RWKV_W), 0.02),
        'moe_router': nrm((N_ODD, D, N_EXPERTS), D ** -0.5),
        'moe_w_gate': nrm((N_ODD, N_EXPERTS, D, EXPERT_HIDDEN), D ** -0.5),
        'moe_w_up': nrm((N_ODD, N_EXPERTS, D, EXPERT_HIDDEN), D ** -0.5),
        'moe_w_down': nrm((N_ODD, N_EXPERTS, EXPERT_HIDDEN, D), EXPERT_HIDDEN ** -0.5),
    }


def reference(x, c, ctx, c_ctx, mod_w, mod_b, norm_g, even_w_in, even_w_out, ret_decay_exp, ret_norm_g,
              gqa_q_norm, gqa_k_norm, ffn_w_gate, ffn_w_up, ffn_w_down, odd_w_in, odd_w_out, diff_lambda,
              diff_subln_g, rwkv_mu, rwkv_w0, rwkv_w2, rwkv_a0, rwkv_a2, rwkv_g2, rwkv_k_k, rwkv_k_a, rwkv_r_k,
              rwkv_ln_g, rwkv_ln_b, moe_router, moe_w_gate, moe_w_up, moe_w_down):
    rows = x.shape[1] // GRID_W
    rope_ret = axial_rope_tables(rows, RET_DK)
    rope_gqa = axial_rope_tables(rows, GQA_HEAD_DIM)
    rope_diff = axial_rope_tables(rows, DIFF_HEAD_DIM)
    cond_lat = jax.nn.silu(c)
    cond_ctx = jax.nn.silu(c_ctx)

    for layer in range(DEPTH):
        i = layer // 2
        need_ctx = layer < DEPTH - 1
        l_sh1, l_sc1, l_g1, l_sh2, l_sc2, l_g2 = jnp.split((cond_lat @ mod_w[layer] + mod_b[layer])[:, None, :], 6, axis=-1)
        c_sh1, c_sc1, c_g1, c_sh2, c_sc2, c_g2 = jnp.split(cond_ctx @ mod_w[layer] + mod_b[layer], 6, axis=-1)

        hl = modulate(rmsnorm(x, norm_g[layer, 0]), l_sh1, l_sc1)
        hc = modulate(rmsnorm(ctx, norm_g[layer, 0]), c_sh1, c_sc1)
        if layer % 2 == 0:
            y_ctx, y_lat = even_mixer(hc, hl, rope_ret, rope_gqa, even_w_in[i], even_w_out[i], ret_decay_exp[i],
                                      ret_norm_g[i], gqa_q_norm[i], gqa_k_norm[i], need_ctx)
            ffn = lambda h: swiglu(h, ffn_w_gate[i], ffn_w_up[i], ffn_w_down[i])
        else:
            lam_init = 0.8 - 0.6 * math.exp(-0.3 * layer)
            y_ctx, y_lat = odd_mixer(hc, hl, rope_diff, odd_w_in[i], odd_w_out[i], diff_lambda[i], diff_subln_g[i],
                                     lam_init, rwkv_mu[i], rwkv_w0[i], rwkv_w2[i], rwkv_a0[i], rwkv_a2[i], rwkv_g2[i],
                                     rwkv_k_k[i], rwkv_k_a[i], rwkv_r_k[i], rwkv_ln_g[i], rwkv_ln_b[i], need_ctx)
            ffn = lambda h: moe_swiglu(h, moe_router[i], moe_w_gate[i], moe_w_up[i], moe_w_down[i])

        x = x + l_g1 * rmsnorm(y_lat, norm_g[layer, 1])
        x = x + l_g2 * rmsnorm(ffn(modulate(rmsnorm(x, norm_g[layer, 2]), l_sh2, l_sc2)), norm_g[layer, 3])
        if need_ctx:
            ctx = ctx + c_g1 * rmsnorm(y_ctx, norm_g[layer, 1])
            ctx = ctx + c_g2 * rmsnorm(ffn(modulate(rmsnorm(ctx, norm_g[layer, 2]), c_sh2, c_sc2)), norm_g[layer, 3])
    return x
```

```python
import contextlib
import math
import numpy as np
import concourse.bass as bass
import concourse.mybir as mybir
from concourse.bass_utils import run_bass_kernel_spmd

ALU = mybir.AluOpType
AF = mybir.ActivationFunctionType
AX = mybir.AxisListType
F32 = mybir.dt.float32
BF16 = mybir.dt.bfloat16

SAME_ENGINE_SYNC = True
ENGS = ('pe', 'act', 'dve', 'pool', 'sp')


class Res:
    __slots__ = ('name', 'w', 'r', 'dsem', 'dcount')

    def __init__(self, name):
        self.name = name
        self.w = None
        self.r = []
        self.dsem = None
        self.dcount = 0


class T:
    __slots__ = ('ap', 'res')

    def __init__(self, ap, res):
        self.ap = ap
        self.res = res

    def __getitem__(self, idx):
        return T(self.ap[idx], self.res)

    def with_ap(self, ap):
        return T(ap, self.res)


class Prog:
    def __init__(self, nc):
        self.nc = nc
        self.stack = contextlib.ExitStack()
        self.ops = {e: [] for e in ENGS}
        self.nops = {e: 0 for e in ENGS}
        self.seen = {e: {} for e in ENGS}
        self.signal = {e: set() for e in ENGS}
        self.dma_res = []
        self.all_res = []
        self.nsb = 0

    def _res(self, name):
        r = Res(name)
        self.all_res.append(r)
        return r

    def sb(self, name, shape, dt, stack=None):
        t = (stack or self.stack).enter_context(self.nc.sbuf_tensor('sb_' + name, list(shape), dt))
        return T(t[tuple(slice(None) for _ in shape)], self._res(name))

    def ps(self, name, shape, dt=F32, stack=None):
        t = (stack or self.stack).enter_context(self.nc.psum_tensor('pp_' + name, list(shape), dt))
        return T(t[tuple(slice(None) for _ in shape)], self._res(name))

    def dram(self, name, shape, dt, kind):
        t = self.nc.dram_tensor(name, list(shape), dt, kind=kind).ap()
        return T(t, self._res(name))

    def sub(self, t, name=None):
        return T(t.ap, self._res(name or t.res.name + '_sub'))

    def _need(self, eng, tok, waits):
        if tok is None:
            return
        if tok[0] == 'eng':
            _, f, k = tok
            if f == eng and not (SAME_ENGINE_SYNC and eng != 'pe'):
                return
            key = ('eng', f)
            if self.seen[eng].get(key, 0) >= k:
                return
            self.seen[eng][key] = k
            self.signal[f].add(k)
            waits.append(tok)
        else:
            _, res, cnt = tok
            key = ('dma', id(res))
            if self.seen[eng].get(key, 0) >= cnt:
                return
            self.seen[eng][key] = cnt
            waits.append(tok)

    def _deps(self, eng, reads, writes):
        waits = []
        for t in reads:
            self._need(eng, t.res.w, waits)
        for t in writes:
            self._need(eng, t.res.w, waits)
            for tok in t.res.r:
                self._need(eng, tok, waits)
        return waits

    def op(self, eng, fn, reads=(), writes=()):
        waits = self._deps(eng, reads, writes)
        self.nops[eng] += 1
        k = self.nops[eng]
        tok = ('eng', eng, k)
        for t in reads:
            t.res.r.append(tok)
        for t in writes:
            t.res.w = tok
            t.res.r = []
        self.ops[eng].append((waits, fn, k, None))

    def dma(self, q, out, in_, **kw):
        waits = self._deps(q, [in_], [out])
        res = out.res
        if res.dsem is None:
            res.dsem = self.stack.enter_context(self.nc.semaphore('d_' + res.name))
            self.dma_res.append(res)
        res.dcount += 1
        tok = ('dma', res, res.dcount)
        in_.res.r.append(tok)
        res.w = tok
        res.r = []
        oap, iap = out.ap, in_.ap
        self.ops[q].append((waits, lambda e: e.dma_start(out=oap, in_=iap, **kw), None, res))

    def barrier(self):
        for e in ENGS:
            waits = []
            for f in ENGS:
                if f != e and self.nops[f] > 0:
                    self._need(e, ('eng', f, self.nops[f]), waits)
            for res in self.dma_res:
                self._need(e, ('dma', res, res.dcount), waits)
            if waits:
                self.ops[e].append((waits, None, None, None))

    @contextlib.contextmanager
    def scope(self):
        st = contextlib.ExitStack()
        try:
            yield st
        finally:
            self.barrier()
            st.close()

    def finalize(self):
        nc = self.nc
        sems = {e: self.stack.enter_context(nc.semaphore('s_' + e)) for e in ENGS}
        self.barrier()
        rank = {}
        for e in ENGS:
            for i, k in enumerate(sorted(self.signal[e])):
                rank[(e, k)] = i + 1

        def run(ename, eng):
            for waits, fn, k, dres in self.ops[ename]:
                for tok in waits:
                    if tok[0] == 'eng':
                        eng.wait_ge(sems[tok[1]], rank[(tok[1], tok[2])])
                    else:
                        eng.wait_ge(tok[1].dsem, 16 * tok[2])
                if fn is None:
                    continue
                ins = fn(eng)
                if dres is not None:
                    ins.then_inc(dres.dsem, 16)
                elif (ename, k) in rank:
                    ins.then_inc(sems[ename], 1)

        with nc.Block() as block:
            @block.tensor
            def _(e):
                run('pe', e)

            @block.scalar
            def _(e):
                run('act', e)

            @block.vector
            def _(e):
                run('dve', e)

            @block.gpsimd
            def _(e):
                run('pool', e)

            @block.sync
            def _(e):
                run('sp', e)
        self.stack.close()

    def mm(self, out, lhsT, rhs, start=True, stop=True):
        o, l, r = out.ap, lhsT.ap, rhs.ap
        self.op('pe', lambda e: e.matmul(o, l, r, start=start, stop=stop), [lhsT, rhs], [out])

    def tr(self, out, in_, ident):
        o, i, d = out.ap, in_.ap, ident.ap
        self.op('pe', lambda e: e.transpose(o, i, d), [in_, ident], [out])

    def act(self, out, in_, func, bias=None, scale=1.0, accum=None, eng='act'):
        o, i = out.ap, in_.ap
        reads = [in_]
        kw = {}
        if bias is not None:
            if isinstance(bias, T):
                reads.append(bias)
                kw['bias'] = bias.ap
            else:
                kw['bias'] = bias
        if isinstance(scale, T):
            reads.append(scale)
            kw['scale'] = scale.ap
        else:
            kw['scale'] = scale
        writes = [out]
        if accum is not None:
            writes.append(accum)
            kw['accum_out'] = accum.ap
        self.op(eng, lambda e: e.activation(o, i, func, **kw), reads, writes)

    def tt(self, eng, out, in0, in1, op):
        o, a, b = out.ap, in0.ap, in1.ap
        self.op(eng, lambda e: e.tensor_tensor(o, a, b, op), [in0, in1], [out])

    def ts(self, eng, out, in0, s1, op0, s2=None, op1=None, accum=None):
        o, a = out.ap, in0.ap
        reads = [in0]
        v1 = s1
        if isinstance(s1, T):
            reads.append(s1)
            v1 = s1.ap
        v2 = s2
        if isinstance(s2, T):
            reads.append(s2)
            v2 = s2.ap
        kw = {}
        if op1 is not None:
            kw['op1'] = op1
        writes = [out]
        if accum is not None:
            kw['accum_out'] = accum.ap
            writes.append(accum)
        self.op(eng, lambda e: e.tensor_scalar(o, a, v1, v2, op0, **kw), reads, writes)

    def stt(self, eng, out, in0, scalar, in1, op0, op1):
        o, a, b = out.ap, in0.ap, in1.ap
        reads = [in0, in1]
        v = scalar
        if isinstance(scalar, T):
            reads.append(scalar)
            v = scalar.ap
        self.op(eng, lambda e: e.scalar_tensor_tensor(o, a, v, b, op0, op1), reads, [out])

    def copy(self, eng, out, in_):
        o, i = out.ap, in_.ap
        if eng == 'act':
            self.op(eng, lambda e: e.activation(o, i, AF.Copy), [in_], [out])
        else:
            self.op(eng, lambda e: e.tensor_copy(o, i), [in_], [out])

    def memset(self, eng, out, val):
        o = out.ap
        self.op(eng, lambda e: e.memset(o, val), [], [out])

    def reduce(self, eng, out, in_, op, axis=AX.X):
        o, i = out.ap, in_.ap
        self.op(eng, lambda e: e.tensor_reduce(o, i, axis, op), [in_], [out])

    def recip(self, out, in_):
        o, i = out.ap, in_.ap
        self.op('dve', lambda e: e.reciprocal(o, i), [in_], [out])


NORM_EPS = 1e-6
D = 1024
DBG = None
TRACE = False


def build_post(NT, E, H, BLK, has_ctx):
    nc = bass.Bass("TRN2", target_bir_lowering=False)
    p = Prog(nc)
    NTOK = NT * 128
    HC = H // 128
    NB = HC // BLK
    assert NB * BLK == HC
    x_d = p.dram("x", [NTOK, D], F32, "ExternalInput")
    mixT_d = p.dram("mixT", [D, NTOK], F32, "ExternalInput")
    cT_d = p.dram("cT", [128, 8, 2], F32, "ExternalInput")
    modw_d = p.dram("mod_w", [D, 6 * D], F32, "ExternalInput")
    modb_d = p.dram("mod_b", [2, 6 * D], F32, "ExternalInput")
    ng_d = p.dram("norm_g", [4, D], F32, "ExternalInput")
    wout_d = p.dram("w_out", [D, D], F32, "ExternalInput")
    wg_d = p.dram("wg", [E, D, H], F32, "ExternalInput")
    wu_d = p.dram("wu", [E, D, H], F32, "ExternalInput")
    wd_d = p.dram("wd", [E, H, D], F32, "ExternalInput")
    rt_d = p.dram("router", [D, 128], F32, "ExternalInput")
    id_d = p.dram("ident", [128, 128], F32, "ExternalInput")
    out_d = p.dram("out", [NTOK, D], F32, "ExternalOutput")
    mod_s = p.dram("mod_s", [2, 6 * D], F32, "Internal")
    x1_s = p.dram("x1_s", [NTOK, D], F32, "Internal")

    ps = [p.ps(f"ps{i}", [128, 512]) for i in range(8)]
    ident = p.sb("ident", [128, 128], F32)
    p.dma('sp', ident, id_d)
    epsc = p.sb("epsc", [128, 1], F32)
    p.memset('dve', epsc, NORM_EPS)
    h2T = p.sb("h2T", [128, 8, NTOK], BF16)
    gates = p.sb("gates", [128, NT, 8], F32)
    nvar = 2 if has_ctx else 1

    with p.scope() as st:
        cT = p.sb("cT", [128, 8, 2], F32, st)
        sc = p.sb("silu_c", [128, 8, 2], F32, st)
        p.dma('sp', cT, cT_d)
        p.act(sc, cT, AF.Silu)
        modrow = p.sb("modrow", [2, 6 * D], F32, st)
        modb = p.sb("modb", [2, 6 * D], F32, st)
        p.dma('sp', modb, modb_d)
        wblk = [p.sb(f"modw{i}", [128, 8, 512], F32, st) for i in range(2)]
        for cb in range(2, 12):
            wb = wblk[cb % 2]
            p.dma('sp', wb, modw_d.with_ap(modw_d.ap.rearrange("(k p) n -> p k n", p=128)[:, :, cb * 512:(cb + 1) * 512]))
            pp = ps[cb % 2]
            for k in range(8):
                p.mm(pp[0:2, :], sc[:, k, :], wb[:, k, :], start=(k == 0), stop=(k == 7))
            p.tt('dve', modrow[:, cb * 512:(cb + 1) * 512], pp[0:2, :], modb[:, cb * 512:(cb + 1) * 512], ALU.add)
        p.dma('sp', mod_s[:, 2 * D:6 * D], modrow[:, 2 * D:6 * D])

    def bcast_row(dst, src_row_ap):
        p.dma('sp', dst, src_row_ap)

    def rms_rstd(dst_col, src, sq_tmp):
        p.act(sq_tmp, src, AF.Square)
        p.reduce('dve', dst_col, sq_tmp, ALU.add)
        p.act(dst_col, dst_col, AF.Sqrt, bias=epsc, scale=1.0 / D)
        p.recip(dst_col, dst_col)

    with p.scope() as st:
        G1 = [p.sb(f"G1_{v}", [128, D], F32, st) for v in range(nvar)]
        A2 = [p.sb(f"A2_{v}", [128, D], F32, st) for v in range(nvar)]
        B2 = [p.sb(f"B2_{v}", [128, D], F32, st) for v in range(nvar)]
        ngb = p.sb("ngb", [128, 2, D], F32, st)
        bcast_row(ngb[:, 0, :], ng_d.with_ap(ng_d.ap[1:2, :].partition_broadcast(128)))
        bcast_row(ngb[:, 1, :], ng_d.with_ap(ng_d.ap[2:3, :].partition_broadcast(128)))
        for v in range(nvar):
            bcast_row(G1[v], mod_s.with_ap(mod_s.ap[v:v + 1, 2 * D:3 * D].partition_broadcast(128)))
            p.tt('dve', G1[v], G1[v], ngb[:, 0, :], ALU.mult)
            bcast_row(B2[v], mod_s.with_ap(mod_s.ap[v:v + 1, 3 * D:4 * D].partition_broadcast(128)))
            bcast_row(A2[v], mod_s.with_ap(mod_s.ap[v:v + 1, 4 * D:5 * D].partition_broadcast(128)))
            p.ts('dve', A2[v], A2[v], 1.0, ALU.add)
            p.tt('dve', A2[v], A2[v], ngb[:, 1, :], ALU.mult)
        mixT = p.sb("mixT", [128, 8, NTOK], BF16, st)
        mview = mixT_d.ap.rearrange("(k p) n -> p k n", p=128)
        for k in range(8):
            p.dma('pool', mixT[:, k, :], mixT_d.with_ap(mview[:, k, :]))
        wout = p.sb("wout", [128, 8, D], BF16, st)
        wv = wout_d.ap.rearrange("(k p) n -> p k n", p=128)
        for k in range(8):
            p.dma('pool', wout[:, k, :], wout_d.with_ap(wv[:, k, :]))
        if E > 1:
            rt = p.sb("rt", [128, 8, 128], F32, st)
            p.dma('sp', rt, rt_d.with_ap(rt_d.ap.rearrange("(k p) e -> p k e", p=128)))
            h2Tf = p.sb("h2Tf", [128, 8, 128], F32, st)
        xt = [p.sb(f"xt{i}", [128, D], F32, st) for i in range(2)]
        tmp = [p.sb(f"tmp{i}", [128, D], F32, st) for i in range(2)]
        sq = p.sb("sq", [128, D], F32, st)
        cols = p.sb("cols", [128, NT, 8], F32, st)
        lg = p.sb("lg", [128, 4, 8], F32, st)
        for t in range(NT):
            v = 1 if (has_ctx and t == NT - 1) else 0
            x_t, tm = xt[t % 2], tmp[t % 2]
            p.dma('sp', x_t, x_d[t * 128:(t + 1) * 128, :])
            py = [ps[0], ps[1]]
            for fh in range(2):
                for k in range(8):
                    p.mm(py[fh], mixT[:, k, t * 128:(t + 1) * 128], wout[:, k, fh * 512:(fh + 1) * 512], start=(k == 0), stop=(k == 7))
            for fh in range(2):
                p.copy('act', tm[:, fh * 512:(fh + 1) * 512], py[fh])
            rc = cols[:, t, 0:1]
            rms_rstd(rc, tm, sq)
            p.stt('dve', tm, tm, rc, G1[v], ALU.mult, ALU.mult)
            p.tt('pool', x_t, x_t, tm, ALU.add)
            p.dma('sp', x1_s[t * 128:(t + 1) * 128, :], x_t)
            rc2 = cols[:, t, 1:2]
            rms_rstd(rc2, x_t, sq)
            p.stt('dve', tm, x_t, rc2, A2[v], ALU.mult, ALU.mult)
            p.tt('pool', tm, tm, B2[v], ALU.add)
            for k in range(8):
                pt = ps[2 + (k % 4)]
                p.tr(pt[:, 0:128], tm[:, k * 128:(k + 1) * 128], ident)
                p.copy('act' if k % 2 else 'dve', h2T[:, k, t * 128:(t + 1) * 128], pt[:, 0:128])
                if E > 1:
                    p.copy('dve' if k % 2 else 'act', h2Tf[:, k, :], pt[:, 0:128])
            if E > 1 and DBG not in ('norouter', 'e1only', 'e0only'):
                pl = ps[6]
                for k in range(8):
                    p.mm(pl[:, 0:128], h2Tf[:, k, :], rt[:, k, :], start=(k == 0), stop=(k == 7))
                L = lg[:, 0, :]
                p.copy('dve', L, pl[:, 0:8])
                m1 = cols[:, t, 2:3]
                m2 = cols[:, t, 3:4]
                p.reduce('dve', m1, L, ALU.max)
                mk1 = lg[:, 1, :]
                p.ts('dve', mk1, L, m1, ALU.is_equal)
                L2 = lg[:, 2, :]
                p.stt('dve', L2, mk1, -1e30, L, ALU.mult, ALU.add)
                p.reduce('dve', m2, L2, ALU.max)
                mk2 = lg[:, 3, :]
                p.ts('dve', mk2, L2, m2, ALU.is_equal)
                w1 = cols[:, t, 4:5]
                w2 = cols[:, t, 5:6]
                p.tt('dve', w1, m1, m2, ALU.subtract)
                p.act(w1, w1, AF.Sigmoid)
                p.ts('dve', w2, w1, -1.0, ALU.mult, 1.0, ALU.add)
                p.ts('dve', gates[:, t, :], mk1, w1, ALU.mult)
                p.stt('dve', gates[:, t, :], mk2, w2, gates[:, t, :], ALU.mult, ALU.add)

    with p.scope() as st:
        acc = p.sb("acc", [128, NT, D], F32, st)
        wgb = [p.sb(f"wg{i}", [128, 8, BLK * 128], BF16, st) for i in range(2)]
        wub = [p.sb(f"wu{i}", [128, 8, BLK * 128], BF16, st) for i in range(2)]
        wdb = [p.sb(f"wd{i}", [128, BLK, D], BF16, st) for i in range(2)]
        actT = [p.sb(f"actT{i}", [128, BLK, 512], BF16, st) for i in range(2)]
        sg = [p.sb(f"sg{i}", [128, 512], F32, st) for i in range(2)]
        groups = []
        t0 = 0
        while t0 < NT:
            n = min(4, NT - t0)
            groups.append((t0, n))
            t0 += n
        it = 0
        gi = 0
        first = True
        for e in ([1] if DBG == 'e1only' else [0] if DBG == 'e0only' else range(E)):
            wgv = wg_d.ap.rearrange("e (k p) h -> e p k h", p=128)[e]
            wuv = wu_d.ap.rearrange("e (k p) h -> e p k h", p=128)[e]
            wdv = wd_d.ap.rearrange("e (c p) f -> e p c f", p=128)[e]
            for b in range(NB):
                s = it % 2
                it += 1
                hs = slice(b * BLK * 128, (b + 1) * BLK * 128)
                for k in range(8):
                    p.dma('pool', wgb[s][:, k, :], wg_d.with_ap(wgv[:, k, hs]))
                    p.dma('pool', wub[s][:, k, :], wu_d.with_ap(wuv[:, k, hs]))
                for c in range(BLK):
                    p.dma('pool', wdb[s][:, c, :], wd_d.with_ap(wdv[:, b * BLK + c, :]))
                for (t0, n) in groups:
                    ts_ = slice(t0 * 128, (t0 + n) * 128)
                    W = n * 128
                    a = actT[gi % 2]
                    gi += 1
                    for c in range(BLK):
                        pg, pu = ps[(c % 2) * 2], ps[(c % 2) * 2 + 1]
                        for k in range(8):
                            p.mm(pg[:, 0:W], wgb[s][:, k, c * 128:(c + 1) * 128], h2T[:, k, ts_], start=(k == 0), stop=(k == 7))
                        for k in range(8):
                            p.mm(pu[:, 0:W], wub[s][:, k, c * 128:(c + 1) * 128], h2T[:, k, ts_], start=(k == 0), stop=(k == 7))
                        sgt = sg[c % 2]
                        p.act(sgt[:, 0:W], pg[:, 0:W], AF.Silu)
                        p.tt('dve', a[:, c, 0:W], sgt[:, 0:W], pu[:, 0:W], ALU.mult)
                    for j in range(n):
                        t = t0 + j
                        for fh in range(2):
                            pd = ps[4 + ((j * 2 + fh) % 4)]
                            for c in range(BLK):
                                p.mm(pd, a[:, c, j * 128:(j + 1) * 128], wdb[s][:, c, fh * 512:(fh + 1) * 512], start=(c == 0), stop=(c == BLK - 1))
                            dst = acc[:, t, fh * 512:(fh + 1) * 512]
                            gsc = gates[:, t, e:e + 1] if (E > 1 and DBG not in ('norouter', 'nogate', 'e1only', 'e0only')) else 1.0
                            if first:
                                if E > 1 and DBG not in ('norouter', 'nogate', 'e1only', 'e0only'):
                                    p.ts('dve', dst, pd, gsc, ALU.mult)
                                else:
                                    p.copy('dve', dst, pd)
                            else:
                                p.stt('dve', dst, pd, gsc, dst, ALU.mult, ALU.add)
                first = False
        G2 = [p.sb(f"G2_{v}", [128, D], F32, st) for v in range(nvar)]
        ng3 = p.sb("ng3", [128, D], F32, st)
        bcast_row(ng3, ng_d.with_ap(ng_d.ap[3:4, :].partition_broadcast(128)))
        for v in range(nvar):
            bcast_row(G2[v], mod_s.with_ap(mod_s.ap[v:v + 1, 5 * D:6 * D].partition_broadcast(128)))
            p.tt('dve', G2[v], G2[v], ng3, ALU.mult)
        x1t = [p.sb(f"x1t{i}", [128, D], F32, st) for i in range(2)]
        sq2 = p.sb("sq2", [128, D], F32, st)
        cols2 = p.sb("cols2", [128, NT], F32, st)
        for t in range(NT):
            v = 1 if (has_ctx and t == NT - 1) else 0
            x1 = x1t[t % 2]
            p.dma('sp', x1, x1_s[t * 128:(t + 1) * 128, :])
            rc = cols2[:, t:t + 1]
            rms_rstd(rc, acc[:, t, :], sq2)
            p.stt('dve', acc[:, t, :], acc[:, t, :], rc, G2[v], ALU.mult, ALU.mult)
            if DBG == 'x1':
                pass
            elif DBG == 'f':
                p.copy('pool', x1, acc[:, t, :])
            else:
                p.tt('pool', x1, x1, acc[:, t, :], ALU.add)
            p.dma('sp', out_d[t * 128:(t + 1) * 128, :], x1)
    p.finalize()
    return nc


def _cT(c_b, c_ctx):
    a = np.stack([c_b, c_ctx], axis=-1).astype(np.float32)
    return np.ascontiguousarray(a.reshape(8, 128, 2).transpose(1, 0, 2))


_IDENT = np.eye(128, dtype=np.float32)


def run_post(layer, x_lat, ctx, mix_lat, mix_ctx, c, c_ctx, mod_w, mod_b, norm_g, w_out, wg, wu, wd, router, has_ctx, E, H, BLK):
    NT = 17 if has_ctx else 16
    nc = build_post(NT, E, H, BLK, has_ctx)
    in_maps = []
    for core in range(8):
        b, q = core // 4, core % 4
        xs = [x_lat[b, q * 2048:(q + 1) * 2048]]
        ms = [mix_lat[b, q * 2048:(q + 1) * 2048]]
        if has_ctx:
            cs = (q % 2) * 128
            xs.append(ctx[b, cs:cs + 128])
            ms.append(mix_ctx[b, cs:cs + 128])
        xo = np.ascontiguousarray(np.concatenate(xs, 0), dtype=np.float32)
        mo = np.ascontiguousarray(np.concatenate(ms, 0).T, dtype=np.float32)
        in_maps.append({
            "x": xo, "mixT": mo, "cT": _cT(c[b], c_ctx),
            "mod_w": np.ascontiguousarray(mod_w[layer]), "mod_b": np.ascontiguousarray(np.stack([mod_b[layer]] * 2)),
            "norm_g": np.ascontiguousarray(norm_g[layer]), "w_out": np.ascontiguousarray(w_out),
            "wg": np.ascontiguousarray(wg), "wu": np.ascontiguousarray(wu), "wd": np.ascontiguousarray(wd),
            "router": np.ascontiguousarray(np.concatenate([router, np.zeros((1024, 120), np.float32)], 1)), "ident": _IDENT,
        })
    res = run_bass_kernel_spmd(nc, in_maps, core_ids=list(range(8)), trace=TRACE)
    if TRACE:
        print("DEV_NS", res.exec_time_ns)
    x2 = np.empty_like(x_lat)
    ctx2 = np.empty_like(ctx) if has_ctx else None
    for core in range(8):
        b, q = core // 4, core % 4
        o = res.results[core]["out"]
        x2[b, q * 2048:(q + 1) * 2048] = o[0:2048]
        if has_ctx and q < 2:
            ctx2[b, q * 128:(q + 1) * 128] = o[2048:2176]
    return x2, ctx2


def mod_rows(p, ps, st, cT_d, modw_d, modb_d, mod_s, blocks):
    cT = p.sb("cT", [128, 8, 2], F32, st)
    sc = p.sb("silu_c", [128, 8, 2], F32, st)
    p.dma('sp', cT, cT_d)
    p.act(sc, cT, AF.Silu)
    lo, hi = blocks[0] * 512, (blocks[-1] + 1) * 512
    modrow = p.sb("modrow", [2, 6 * D], F32, st)
    modb = p.sb("modb", [2, 6 * D], F32, st)
    p.dma('sp', modb, modb_d)
    wblk = [p.sb(f"modw{i}", [128, 8, 512], F32, st) for i in range(2)]
    for cb in blocks:
        wb = wblk[cb % 2]
        p.dma('sp', wb, modw_d.with_ap(modw_d.ap.rearrange("(k p) n -> p k n", p=128)[:, :, cb * 512:(cb + 1) * 512]))
        pp = ps[cb % 2]
        for k in range(8):
            p.mm(pp[0:2, :], sc[:, k, :], wb[:, k, :], start=(k == 0), stop=(k == 7))
        p.tt('dve', modrow[:, cb * 512:(cb + 1) * 512], pp[0:2, :], modb[:, cb * 512:(cb + 1) * 512], ALU.add)
    p.dma('sp', mod_s[:, lo:hi], modrow[:, lo:hi])


def rope_tm(p, dst1, dst2, x1, x2, cos, sin, t1, t2, e1='dve', e2='pool'):
    p.tt(e1, t1, x1, cos, ALU.mult)
    p.tt(e2, t2, x2, sin, ALU.mult)
    p.tt(e1, t1, t1, t2, ALU.subtract)
    p.tt(e2, t2, x1, sin, ALU.mult)
    p.tt(e1, dst2, x2, cos, ALU.mult)
    p.tt(e1, dst2, dst2, t2, ALU.add)
    p.copy(e2, dst1, t1)


NTA = 66


def build_mix0():
    nc = bass.Bass("TRN2", target_bir_lowering=False)
    p = Prog(nc)
    NTOK = NTA * 128
    x_d = p.dram("x", [NTOK, D], F32, "ExternalInput")
    cT_d = p.dram("cT", [128, 8, 2], F32, "ExternalInput")
    modw_d = p.dram("mod_w", [D, 6 * D], F32, "ExternalInput")
    modb_d = p.dram("mod_b", [2, 6 * D], F32, "ExternalInput")
    ng_d = p.dram("norm_g", [4, D], F32, "ExternalInput")
    win_d = p.dram("w_in", [D, 896], F32, "ExternalInput")
    cos_d = p.dram("cos", [NTOK, 64], F32, "ExternalInput")
    sin_d = p.dram("sin", [NTOK, 64], F32, "ExternalInput")
    dec_d = p.dram("dec", [128, 2], F32, "ExternalInput")
    gb_d = p.dram("gb", [3, 128, 128], F32, "ExternalInput")
    cst_d = p.dram("cst", [6, 128, 128], F32, "ExternalInput")
    out_d = p.dram("mixT", [256, NTOK], F32, "ExternalOutput")
    mod_s = p.dram("mod_s", [2, 6 * D], F32, "Internal")
    qa_s = p.dram("qa_s", [128, NTOK], BF16, "Internal")
    ka_s = p.dram("ka_s", [128, NTOK], BF16, "Internal")
    va_s = p.dram("va_s", [128, NTA, 128], BF16, "Internal")

    ps = [p.ps(f"ps{i}", [128, 512]) for i in range(8)]
    cst = p.sb("cst", [128, 6, 128], F32)
    p.dma('sp', cst, cst_d.with_ap(cst_d.ap.rearrange("c p n -> p c n")))
    ident = cst[:, 0, :]
    gb = p.sb("gb", [128, 3, 128], F32)
    p.dma('sp', gb, gb_d.with_ap(gb_d.ap.rearrange("c p n -> p c n")))
    epsc = p.sb("epsc", [128, 1], F32)
    p.memset('dve', epsc, NORM_EPS)
    ones_bf = p.sb("ones_bf", [128, 128], BF16)
    p.memset('dve', ones_bf, 1.0)
    dec = p.sb("dec", [128, 2], F32)
    p.dma('sp', dec, dec_d)
    lg = p.sb("lg", [128, 2], F32)
    p.act(lg, dec, AF.Exp, scale=-math.log(2.0))
    p.act(lg, lg, AF.Ln, scale=-1.0, bias=1.0)
    MT = p.sb("MT", [128, 2, 128], F32)
    dcol = p.sb("dcol", [128, 8], F32)
    p.act(MT[:, 0, :], cst[:, 1, :], AF.Exp, scale=lg[:, 0:1])
    p.tt('dve', MT[:, 0, :], MT[:, 0, :], cst[:, 3, :], ALU.mult)
    p.act(MT[:, 1, :], cst[:, 2, :], AF.Exp, scale=lg[:, 1:2])
    p.tt('dve', MT[:, 1, :], MT[:, 1, :], cst[:, 4, :], ALU.mult)
    colc = cst[:, 5, :]
    p.act(dcol[:, 0:1], colc[:, 0:1], AF.Exp, scale=lg[:, 0:1])
    p.act(dcol[:, 1:2], colc[:, 1:2], AF.Exp, scale=lg[:, 1:2])
    p.act(dcol[:, 2:3], colc[:, 2:3], AF.Exp, scale=lg[:, 0:1])
    p.act(dcol[:, 3:4], colc[:, 3:4], AF.Exp, scale=lg[:, 1:2])
    p.act(dcol[:, 4:5], colc[:, 4:5], AF.Exp, scale=lg[:, 0:1])
    p.act(dcol[:, 5:6], colc[:, 4:5], AF.Exp, scale=lg[:, 1:2])

    QrT = p.sb("QrT", [128, NTOK], BF16)
    KrT = p.sb("KrT", [128, NTOK], BF16)
    Vr = p.sb("Vr", [128, NTA, 128], BF16)
    Kd = [p.sb(f"Kd{d}", [128, NTA, 128], BF16) for d in range(2)]
    Gs = p.sb("Gs", [128, NTA, 128], BF16)

    with p.scope() as st0:
        with p.scope() as st:
            mod_rows(p, ps, st, cT_d, modw_d, modb_d, mod_s, [0, 1, 2, 3])
        st = st0
        A1 = [p.sb(f"A1_{v}", [128, D], F32, st) for v in range(2)]
        B1 = [p.sb(f"B1_{v}", [128, D], F32, st) for v in range(2)]
        ngb = p.sb("ngb", [128, D], F32, st)
        p.dma('sp', ngb, ng_d.with_ap(ng_d.ap[0:1, :].partition_broadcast(128)))
        for v in range(2):
            p.dma('sp', B1[v], mod_s.with_ap(mod_s.ap[v:v + 1, 0:D].partition_broadcast(128)))
            p.dma('sp', A1[v], mod_s.with_ap(mod_s.ap[v:v + 1, D:2 * D].partition_broadcast(128)))
            p.ts('dve', A1[v], A1[v], 1.0, ALU.add)
            p.tt('dve', A1[v], A1[v], ngb, ALU.mult)
        W = p.sb("W", [128, 8, 896], BF16, st)
        wv = win_d.ap.rearrange("(k p) n -> p k n", p=128)
        for k in range(8):
            p.dma('pool', W[:, k, :], win_d.with_ap(wv[:, k, :]))
        xt = [p.sb(f"xt{i}", [128, D], F32, st) for i in range(2)]
        tm = p.sb("tm", [128, D], F32, st)
        sq = p.sb("sq", [128, D], F32, st)
        hT = [p.sb(f"hT{i}", [128, 8, 128], BF16, st) for i in range(2)]
        cs = [p.sb(f"cs{i}", [128, 2, 64], F32, st) for i in range(2)]
        pr = [p.sb(f"pr{i}", [128, 896], F32, st) for i in range(2)]
        ro = [p.sb(f"ro{i}", [128, 4, 128], F32, st) for i in range(2)]
        rt = p.sb("rt", [128, 4, 64], F32, st)
        cols = p.sb("cols", [128, NTA, 4], F32, st)
        stg = [p.sb(f"stg{i}", [128, 3, 128], BF16, st) for i in range(2)]
        for t in range(NTA):
            v = 1 if t < 2 else 0
            x_t = xt[t % 2]
            p.dma('sp', x_t, x_d[t * 128:(t + 1) * 128, :])
            c_t = cs[t % 2]
            p.dma('sp', c_t[:, 0, :], cos_d[t * 128:(t + 1) * 128, :])
            p.dma('sp', c_t[:, 1, :], sin_d[t * 128:(t + 1) * 128, :])
            rc = cols[:, t, 0:1]
            p.act(sq, x_t, AF.Square)
            p.reduce('dve', rc, sq, ALU.add)
            p.act(rc, rc, AF.Sqrt, bias=epsc, scale=1.0 / D)
            p.recip(rc, rc)
            p.stt('dve', tm, x_t, rc, A1[v], ALU.mult, ALU.mult)
            p.tt('pool', tm, tm, B1[v], ALU.add)
            h = hT[t % 2]
            for k in range(8):
                pt = ps[2 + (k % 4)]
                p.tr(pt[:, 0:128], tm[:, k * 128:(k + 1) * 128], ident)
                p.copy('act' if k % 2 else 'dve', h[:, k, :], pt[:, 0:128])
            for k in range(8):
                p.mm(ps[0], h[:, k, :], W[:, k, 0:512], start=(k == 0), stop=(k == 7))
            for k in range(8):
                p.mm(ps[1][:, 0:384], h[:, k, :], W[:, k, 512:896], start=(k == 0), stop=(k == 7))
            P = pr[t % 2]
            p.copy('act', P[:, 0:512], ps[0])
            p.copy('dve', P[:, 512:896], ps[1][:, 0:384])
            p.ts('pool', P[:, 128:256], P[:, 128:256], 128.0 ** -0.5, ALU.mult)
            p.copy('pool', Vr[:, t, :], P[:, 256:384])
            p.act(Gs[:, t, :], P[:, 384:512], AF.Silu)
            sg = stg[t % 2]
            p.copy('pool', sg[:, 2, :], P[:, 768:896])
            p.dma('sp', va_s[:, t, :], sg[:, 2, :])
            for i, (c0, gi) in enumerate(((512, 1), (640, 2))):
                rcq = cols[:, t, 1 + i:2 + i]
                p.act(sq[:, 0:128], P[:, c0:c0 + 128], AF.Square)
                p.reduce('dve', rcq, sq[:, 0:128], ALU.add)
                p.act(rcq, rcq, AF.Sqrt, bias=epsc, scale=1.0 / 128)
                p.recip(rcq, rcq)
                p.stt('dve', P[:, c0:c0 + 128], P[:, c0:c0 + 128], rcq, gb[:, gi, :], ALU.mult, ALU.mult)
            R = ro[t % 2]
            for i, c0 in enumerate((0, 128, 512, 640)):
                rope_tm(p, R[:, i, 0:64], R[:, i, 64:128], P[:, c0:c0 + 64], P[:, c0 + 64:c0 + 128],
                        c_t[:, 0, :], c_t[:, 1, :], rt[:, i, :], sq[:, 128 + 64 * i:192 + 64 * i],
                        e1='dve' if i % 2 == 0 else 'pool', e2='pool' if i % 2 == 0 else 'dve')
            p.ts('dve', Kd[0][:, t, :], R[:, 1, :], dcol[:, 2:3], ALU.mult)
            p.ts('pool', Kd[1][:, t, :], R[:, 1, :], dcol[:, 3:4], ALU.mult)
            ts_ = slice(t * 128, (t + 1) * 128)
            for i in range(4):
                pt = ps[2 + i]
                p.tr(pt[:, 0:128], R[:, i, :], ident)
                if i == 0:
                    p.copy('act', QrT[:, ts_], pt[:, 0:128])
                elif i == 1:
                    p.copy('dve', KrT[:, ts_], pt[:, 0:128])
                else:
                    p.copy('act' if i == 2 else 'dve', sg[:, i - 2, :], pt[:, 0:128])
            p.dma('sp', qa_s[:, ts_], sg[:, 0, :])
            p.dma('sp', ka_s[:, ts_], sg[:, 1, :])

    with p.scope() as st:
        Y = p.sb("Y", [128, NTA, 128], F32, st)
        S = p.sb("S", [128, 128], F32, st)
        Sb = p.sb("Sb", [128, 128], BF16, st)
        AT = [p.sb(f"AT{i}", [128, 128], BF16, st) for i in range(2)]
        for d in range(2):
            order = list(range(NTA)) if d == 0 else [1, 0] + list(range(NTA - 1, 1, -1))
            p.memset('dve', S, 0.0)
            p.memset('pool', Sb, 0.0)
            for n, c in enumerate(order):
                cs_ = slice(c * 128, (c + 1) * 128)
                pS, pP1, pP2, pKV = ps[n % 2], ps[2 + n % 2], ps[4 + n % 2], ps[6 + n % 2]
                p.mm(pS[:, 0:128], KrT[:, cs_], QrT[:, cs_])
                a = AT[n % 2]
                p.tt('dve', a, pS[:, 0:128], MT[:, d, :], ALU.mult)
                p.mm(pP1[:, 0:128], a, Vr[:, c, :])
                p.mm(pP2[:, 0:128], QrT[:, cs_], Sb)
                if d == 0:
                    p.copy('act', Y[:, c, :], pP1[:, 0:128])
                else:
                    p.tt('pool' if False else 'dve', Y[:, c, :], pP1[:, 0:128], Y[:, c, :], ALU.add)
                p.stt('dve', Y[:, c, :], pP2[:, 0:128], dcol[:, d:d + 1], Y[:, c, :], ALU.mult, ALU.add)
                p.mm(pKV[:, 0:128], Kd[d][:, c, :], Vr[:, c, :])
                p.stt('dve', S, S, dcol[:, 4 + d:5 + d], pKV[:, 0:128], ALU.mult, ALU.add)
                p.copy('act', Sb, S)
        OUT = p.sb("OUT", [128, NTOK], F32, st)
        sq2 = p.sb("sq2", [128, 128], F32, st)
        cols2 = p.sb("cols2", [128, NTA], F32, st)
        for c in range(NTA):
            rc = cols2[:, c:c + 1]
            p.act(sq2, Y[:, c, :], AF.Square)
            p.reduce('dve', rc, sq2, ALU.add)
            p.act(rc, rc, AF.Sqrt, bias=epsc, scale=1.0 / 128)
            p.recip(rc, rc)
            p.stt('dve', Y[:, c, :], Y[:, c, :], rc, gb[:, 0, :], ALU.mult, ALU.mult)
            p.tt('pool', Y[:, c, :], Y[:, c, :], Gs[:, c, :], ALU.mult)
            pt = ps[c % 4]
            p.tr(pt[:, 0:128], Y[:, c, :], ident)
            p.copy('act', OUT[:, c * 128:(c + 1) * 128], pt[:, 0:128])
        p.dma('sp', out_d[0:128, :], OUT)

    with p.scope() as st:
        QaT = p.sb("QaT", [128, NTOK], BF16, st)
        KaT = p.sb("KaT", [128, NTOK], BF16, st)
        Va = p.sb("Va", [128, NTA, 128], BF16, st)
        p.dma('sp', QaT, qa_s)
        p.dma('sp', KaT, ka_s)
        p.dma('sp', Va, va_s)
        g2 = p.sb("g2", [128, 2, 128], F32, st)
        mx = p.sb("mx", [128, 4], F32, st)
        p.act(g2[:, 0, :], gb[:, 1, :], AF.Square)
        p.act(g2[:, 1, :], gb[:, 2, :], AF.Square)
        p.reduce('dve', mx[:, 0:1], g2[:, 0, :], ALU.max)
        p.reduce('dve', mx[:, 1:2], g2[:, 1, :], ALU.max)
        p.tt('dve', mx[:, 2:3], mx[:, 0:1], mx[:, 1:2], ALU.mult)
        p.act(mx[:, 3:4], mx[:, 2:3], AF.Sqrt, scale=128.0)
        p.ts('dve', mx[:, 3:4], mx[:, 3:4], -1.0, ALU.mult)
        negC = mx[:, 3:4]
        PT = [p.sb(f"PT{i}", [128, 512], BF16, st) for i in range(3)]
        rec = [p.sb(f"rec{i}", [128, 512], F32, st) for i in range(2)]
        og = [p.sb(f"og{i}", [128, 512], F32, st) for i in range(2)]
        groups = [(0, 256, [0, 1])] + [(256 + g * 512, 512, list(range(NTA))) for g in range(16)]
        it = 0
        for gi, (q0, W_, keys) in enumerate(groups):
            pO, pD = ps[4 + (gi % 2) * 2], ps[5 + (gi % 2) * 2]
            for n, kt in enumerate(keys):
                pS = ps[it % 3]
                pt_ = PT[it % 3]
                it += 1
                p.mm(pS[:, 0:W_], KaT[:, kt * 128:(kt + 1) * 128], QaT[:, q0:q0 + W_])
                p.act(pt_[:, 0:W_], pS[:, 0:W_], AF.Exp, bias=negC, scale=128.0 ** -0.5)
                p.mm(pO[:, 0:W_], Va[:, kt, :], pt_[:, 0:W_], start=(n == 0), stop=(n == len(keys) - 1))
                p.mm(pD[:, 0:W_], ones_bf, pt_[:, 0:W_], start=(n == 0), stop=(n == len(keys) - 1))
            r, o = rec[gi % 2], og[gi % 2]
            p.recip(r[:, 0:W_], pD[:, 0:W_])
            p.tt('dve', o[:, 0:W_], pO[:, 0:W_], r[:, 0:W_], ALU.mult)
            p.dma('sp', out_d[128:256, q0:q0 + W_], o[:, 0:W_])
    p.finalize()
    return nc


def _rope_tables(dim, rows=128, grid_w=64, theta=10000.0):
    row = np.repeat(np.arange(rows, dtype=np.float32), grid_w)
    col = np.tile(np.arange(grid_w, dtype=np.float32), rows)
    n_freq = dim // 4
    freqs = (np.float32(theta) ** (-np.arange(n_freq, dtype=np.float32) / np.float32(n_freq))).astype(np.float32)
    ang = np.concatenate([row[:, None] * freqs, col[:, None] * freqs], axis=-1).astype(np.float32)
    return np.cos(ang).astype(np.float32), np.sin(ang).astype(np.float32)


def _with_ctx_rope(cos, sin):
    n = cos.shape[1]
    c = np.concatenate([np.ones((256, n), np.float32), cos], 0)
    s = np.concatenate([np.zeros((256, n), np.float32), sin], 0)
    return np.ascontiguousarray(c), np.ascontiguousarray(s)


def _mix0_consts():
    ip = np.arange(128, dtype=np.float32)[:, None]
    i = np.arange(128, dtype=np.float32)[None, :]
    cst = np.zeros((6, 128, 128), np.float32)
    cst[0] = np.eye(128, dtype=np.float32)
    cst[1] = np.maximum(i - ip, 0)
    cst[2] = np.maximum(ip - i, 0)
    cst[3] = (i >= ip)
    cst[4] = (ip > i)
    cst[5, :, 0] = ip[:, 0] + 1
    cst[5, :, 1] = 128 - ip[:, 0]
    cst[5, :, 2] = 127 - ip[:, 0]
    cst[5, :, 3] = ip[:, 0]
    cst[5, :, 4] = 128
    return cst


def run_mix0(x, ctx, c, c_ctx, mod_w, mod_b, norm_g, w_in, decay_exp, ret_g, q_g, k_g):
    nc = build_mix0()
    cos, sin = _with_ctx_rope(*_rope_tables(128))
    cst = _mix0_consts()
    in_maps = []
    for core in range(8):
        b, j = core // 4, core % 4
        kv = j // 2
        colsel = np.concatenate([np.arange(j * 128, (j + 1) * 128) + off for off in (0, 512, 1024, 1536, 2048)] +
                                [np.arange(kv * 128, (kv + 1) * 128) + off for off in (2560, 2816)])
        gb = np.stack([np.broadcast_to(ret_g[j * 128:(j + 1) * 128], (128, 128)),
                       np.broadcast_to(q_g, (128, 128)), np.broadcast_to(k_g, (128, 128))]).astype(np.float32)
        in_maps.append({
            "x": np.ascontiguousarray(np.concatenate([ctx[b], x[b]], 0), dtype=np.float32),
            "cT": _cT(c[b], c_ctx), "mod_w": np.ascontiguousarray(mod_w), "mod_b": np.ascontiguousarray(np.stack([mod_b] * 2)),
            "norm_g": np.ascontiguousarray(norm_g), "w_in": np.ascontiguousarray(w_in[:, colsel]),
            "cos": cos, "sin": sin,
            "dec": np.ascontiguousarray(np.broadcast_to(decay_exp[:, j], (128, 2)), dtype=np.float32),
            "gb": np.ascontiguousarray(gb), "cst": cst,
        })
    res = run_bass_kernel_spmd(nc, in_maps, core_ids=list(range(8)), trace=TRACE)
    if TRACE:
        print("DEV_NS", res.exec_time_ns)
    mix = np.empty((2, NTA * 128, 1024), np.float32)
    for core in range(8):
        b, j = core // 4, core % 4
        o = res.results[core]["mixT"]
        mix[b, :, j * 128:(j + 1) * 128] = o[0:128].T
        mix[b, :, 512 + j * 128:512 + (j + 1) * 128] = o[128:256].T
    return mix[:, 256:], mix[:, :256]


LAM_INIT1 = 0.8 - 0.6 * math.exp(-0.3 * 1)
RWKV_LN_EPS = 64e-5
C_ID, C_TRI0, C_TRI1, C_MS0, C_MI0, C_MS1, C_SH, C_SHP, C_SHN, C_ONE = range(10)
B_W00, B_W01, B_A00, B_A01, B_KK, B_KA, B_RK, B_LNG, B_LNB = range(9)


def build_mix1():
    nc = bass.Bass("TRN2", target_bir_lowering=False)
    p = Prog(nc)
    NTOK = NTA * 128
    NLAT = 64 * 128
    x_d = p.dram("x", [NTOK, D], F32, "ExternalInput")
    cT_d = p.dram("cT", [128, 8, 2], F32, "ExternalInput")
    modw_d = p.dram("mod_w", [D, 6 * D], F32, "ExternalInput")
    modb_d = p.dram("mod_b", [2, 6 * D], F32, "ExternalInput")
    ng_d = p.dram("norm_g", [4, D], F32, "ExternalInput")
    win_d = p.dram("w_in", [D, 1152], F32, "ExternalInput")
    mu_d = p.dram("mu", [128, 768], F32, "ExternalInput")
    cos_d = p.dram("cos", [NTOK, 32], F32, "ExternalInput")
    sin_d = p.dram("sin", [NTOK, 32], F32, "ExternalInput")
    lam_d = p.dram("lamv", [128, 4, 64], F32, "ExternalInput")
    sg_d = p.dram("subg", [128, 1], F32, "ExternalInput")
    bc_d = p.dram("bc", [9, 128, 128], F32, "ExternalInput")
    mat_d = p.dram("mats", [3, 128, 128], F32, "ExternalInput")
    cst_d = p.dram("cst", [10, 128, 128], F32, "ExternalInput")
    out_d = p.dram("mixT", [256, NLAT], F32, "ExternalOutput")
    mod_s = p.dram("mod_s", [2, 6 * D], F32, "Internal")
    qd_s = p.dram("qd_s", [128, NTOK], BF16, "Internal")
    kd_s = p.dram("kd_s", [128, NTOK], BF16, "Internal")
    vd_s = p.dram("vd_s", [128, NTA, 128], BF16, "Internal")
    st_s = p.dram("st_s", [NTA, 128, 768], F32, "Internal")

    ps = [p.ps(f"ps{i}", [128, 512]) for i in range(8)]
    cst = p.sb("cst", [128, 10, 128], F32)
    p.dma('sp', cst, cst_d.with_ap(cst_d.ap.rearrange("c p n -> p c n")))
    cstb = p.sb("cstb", [128, 10, 128], BF16)
    p.copy('dve', cstb, cst)
    ident = cst[:, C_ID, :]
    bc = p.sb("bc", [128, 9, 128], F32)
    p.dma('sp', bc, bc_d.with_ap(bc_d.ap.rearrange("c p n -> p c n")))
    mats = p.sb("mats", [128, 3, 128], F32)
    p.dma('sp', mats, mat_d.with_ap(mat_d.ap.rearrange("c p n -> p c n")))
    epsc = p.sb("epsc", [128, 2], F32)
    p.memset('dve', epsc[:, 0:1], NORM_EPS)
    p.memset('dve', epsc[:, 1:2], RWKV_LN_EPS)
    ones_bf = cstb[:, C_ONE, :]
    Yacc = p.sb("Yacc", [128, NTA, 128], F32)
    Vst = p.sb("Vst", [128, NTA, 128], BF16)
    Gst = p.sb("Gst", [128, NTA, 128], BF16)
    BCf = p.sb("BCf", [128, NTA, 2], F32)
    ST = [[p.sb(f"ST{d}{h}", [64, 64], F32) for h in range(2)] for d in range(2)]

    _tmpn = [0]

    def scan_step(st, d, t, r, kd, v, kk, b, logw, first):
        T_ = TMP
        TRI = cst[:, C_TRI0 + d, :]
        MS = cst[:, C_MS0 if d == 0 else C_MS1, :]
        MST = cst[:, C_MS1 if d == 0 else C_MS0, :]
        MA = cst[:, C_MI0 if d == 0 else C_MS1, :]
        pc, pt = ps[0], ps[1]
        p.mm(pc[:, 0:128], TRI, logw)
        p.mm(pt[:, 0:128], cst[:, C_ONE, :], logw)
        cum = T_['cum']
        p.copy('act', cum, pc[:, 0:128])
        e_neg, e_x, e_in, e_end = T_['e_neg'], T_['e_x'], T_['e_in'], T_['e_end']
        p.act(e_neg, cum, AF.Exp, scale=-1.0)
        p.tt('dve', e_x, cum, logw, ALU.subtract)
        p.act(e_x, e_x, AF.Exp)
        if d == 0:
            p.act(e_in, cum, AF.Exp)
        p.tt('dve', e_end, pt[:, 0:128], cum, ALU.subtract)
        p.act(e_end, e_end, AF.Exp)
        at, bt, kt, rt, Bp, Kp = T_['at'], T_['bt'], T_['kt'], T_['rt'], T_['Bp'], T_['Kp']
        p.stt('dve', at, kk, -1.0, e_x, ALU.mult, ALU.mult)
        p.tt('pool', bt, b, e_neg, ALU.mult)
        p.tt('pool', kt, kd, e_neg, ALU.mult)
        p.tt('dve', rt, r, e_in if d == 0 else e_x, ALU.mult)
        p.tt('pool', Bp, b, e_end, ALU.mult)
        p.tt('pool', Kp, kd, e_end, ALU.mult)
        for hh in range(2):
            hs_ = slice(hh * 64, (hh + 1) * 64)
            fm = {}
            for i, (nm, src) in enumerate((('at', at), ('bt', bt), ('kt', kt), ('rt', rt))):
                pp = ps[2 + i % 2]
                p.tr(pp[0:64, 0:128], src[:, hs_], ident)
                dst = T_[f'{nm}T{hh}']
                p.copy('act' if i % 2 else 'dve', dst, pp[0:64, 0:128])
                fm[nm] = dst
            wc = T_[f'wc{hh}']
            p.mm(ps[1][0:64, 256:257], logw[:, hs_], cst[:, C_ONE, 0:1])
            p.act(wc, ps[1][0:64, 256:257], AF.Exp)
            LT, L, LakT, ArbT, ArkT = T_['LT'], T_['L'], T_['LakT'], T_['ArbT'], T_['ArkT']
            for i, (dst, lhs, rhs, msk) in enumerate(((LT, fm['bt'], fm['at'], MS), (L, fm['at'], fm['bt'], MST),
                                                       (LakT, fm['kt'], fm['at'], MS), (ArbT, fm['bt'], fm['rt'], MA),
                                                       (ArkT, fm['kt'], fm['rt'], MA))):
                pp = ps[4 + i % 2]
                p.mm(pp[:, 0:128], lhs, rhs)
                p.tt('dve', dst, pp[:, 0:128], msk, ALU.mult)
            G, Pa, PaT, Pb, PbT = T_['G'], T_['Pa'], T_['PaT'], T_['Pb'], T_['PbT']
            p.tt('pool', G, LT, ident, ALU.add)
            cur, curT = L, LT
            nxt = [(Pa, PaT), (Pb, PbT)]
            for lev in range(1, 7):
                Pn, PnT = nxt[lev % 2]
                pp = ps[4 + lev % 2]
                p.mm(pp[:, 0:128], curT, cur)
                p.copy('act', Pn, pp[:, 0:128])
                pq = ps[6]
                p.mm(pq[:, 0:128], Pn, G)
                p.tt('dve', G, pq[:, 0:128], G, ALU.add)
                if lev < 6:
                    pp2 = ps[7]
                    p.mm(pp2[:, 0:128], cur, curT)
                    p.copy('act', PnT, pp2[:, 0:128])
                cur, curT = Pn, PnT
            S = ST[d][hh]
            X, U = T_['X'], T_['U']
            px = ps[2]
            vh = v[:, hs_]
            p.mm(px[:, 0:64], fm['at'], S, start=True, stop=False)
            p.mm(px[:, 0:64], LakT, vh, start=False, stop=True)
            p.copy('act', X, px[:, 0:64])
            pu = ps[3]
            p.mm(pu[:, 0:64], G, X)
            p.copy('dve', U, pu[:, 0:64])
            py = ps[4 + hh]
            p.mm(py[:, 256:320], fm['rt'], S, start=True, stop=False)
            p.mm(py[:, 256:320], ArbT, U, start=False, stop=False)
            p.mm(py[:, 256:320], ArkT, vh, start=False, stop=True)
            ydst = Yacc[:, t, hs_]
            if first:
                p.copy('act', ydst, py[:, 256:320])
            else:
                p.tt('dve', ydst, py[:, 256:320], ydst, ALU.add)
            pss = ps[6 + hh]
            p.mm(pss[0:64, 256:320], Bp[:, hs_], U, start=True, stop=False)
            p.mm(pss[0:64, 256:320], Kp[:, hs_], vh, start=False, stop=True)
            p.stt('dve', S, S, wc, pss[0:64, 256:320], ALU.mult, ALU.add)

    with p.scope() as st0:
        with p.scope() as st:
            mod_rows(p, ps, st, cT_d, modw_d, modb_d, mod_s, [0, 1, 2, 3])
        st = st0
        TMP = {}
        for nm in ('cum', 'e_neg', 'e_x', 'e_in', 'e_end', 'at', 'bt', 'kt', 'rt', 'Bp', 'Kp', 'LT', 'L', 'LakT', 'ArbT',
                   'ArkT', 'G', 'Pa', 'PaT', 'Pb', 'PbT'):
            TMP[nm] = p.sb("t_" + nm, [128, 128], F32, st)
        for nm in ('X', 'U'):
            TMP[nm] = p.sb("t_" + nm, [128, 64], F32, st)
        for hh in range(2):
            for nm in ('at', 'bt', 'kt', 'rt'):
                TMP[f'{nm}T{hh}'] = p.sb(f"t_{nm}T{hh}", [64, 128], F32, st)
            TMP[f'wc{hh}'] = p.sb(f"t_wc{hh}", [64, 1], F32, st)
        for d in range(2):
            for hh in range(2):
                p.memset('dve', ST[d][hh], 0.0)
        with p.scope() as st:
            A1 = [p.sb(f"A1_{v}", [128, D], F32, st) for v in range(2)]
            B1 = [p.sb(f"B1_{v}", [128, D], F32, st) for v in range(2)]
            ngb = p.sb("ngb", [128, D], F32, st)
            p.dma('sp', ngb, ng_d.with_ap(ng_d.ap[0:1, :].partition_broadcast(128)))
            for v in range(2):
                p.dma('sp', B1[v], mod_s.with_ap(mod_s.ap[v:v + 1, 0:D].partition_broadcast(128)))
                p.dma('sp', A1[v], mod_s.with_ap(mod_s.ap[v:v + 1, D:2 * D].partition_broadcast(128)))
                p.ts('dve', A1[v], A1[v], 1.0, ALU.add)
                p.tt('dve', A1[v], A1[v], ngb, ALU.mult)
            Wd = p.sb("Wd", [128, 8, 384], BF16, st)
            W1 = p.sb("W1", [128, 8, 768], BF16, st)
            W2 = p.sb("W2", [128, 8, 768], BF16, st)
            mu = p.sb("mu", [128, 2, 768], F32, st)
            p.dma('sp', mu[:, 0, :], mu_d)
            p.ts('dve', mu[:, 1, :], mu[:, 0, :], 0.5, ALU.mult)
            p.ts('dve', mu[:, 0, :], mu[:, 0, :], -1.0, ALU.mult, 1.0, ALU.add)
            wv = win_d.ap.rearrange("(k p) n -> p k n", p=128)
            wst = [p.sb(f"wst{i}", [128, 768], F32, st) for i in range(2)]
            for k in range(8):
                p.dma('pool', Wd[:, k, :], win_d.with_ap(wv[:, k, 0:384]))
                w_ = wst[k % 2]
                p.dma('sp', w_, win_d.with_ap(wv[:, k, 384:1152]))
                p.tt('dve', W1[:, k, :], w_, mu[:, 0, :], ALU.mult)
                p.tt('pool', W2[:, k, :], w_, mu[:, 1, :], ALU.mult)
            xt = [p.sb(f"xt{i}", [128, D], F32, st) for i in range(2)]
            tm = p.sb("tm", [128, D], F32, st)
            sq = p.sb("sq", [128, D], F32, st)
            hb = [p.sb(f"hb{i}", [128, D], BF16, st) for i in range(3)]
            hT = p.sb("hT", [128, 8, 128], BF16, st)
            hsT = p.sb("hsT", [128, 8, 128], BF16, st)
            cs = [p.sb(f"cs{i}", [128, 2, 32], F32, st) for i in range(2)]
            Pd = p.sb("Pd", [128, 384], F32, st)
            RKV = p.sb("RKV", [128, 384], F32, st)
            WAG = p.sb("WAG", [128, 384], F32, st)
            Rr = p.sb("Rr", [128, 2, 128], F32, st)
            rtmp = p.sb("rtmp", [128, 8, 32], F32, st)
            cols = p.sb("cols", [128, NTA, 8], F32, st)
            stg = [p.sb(f"stg{i}", [128, 3, 128], BF16, st) for i in range(2)]
            thT = p.sb("thT", [128, 3, 128], F32, st)
            Q = {}
            for nm in ('th', 'kk', 'tq', 'a0', 'a1', 'lw0', 'kd0', 'b0'):
                Q[nm] = p.sb("q_" + nm, [128, 128], F32, st)
            stash = [p.sb(f"stash{i}", [128, 768], F32, st) for i in range(2)]

            def stage1(t):
                v = 1 if t < 2 else 0
                x_t = xt[t % 2]
                p.dma('sp', x_t, x_d[t * 128:(t + 1) * 128, :])
                rc = cols[:, t, 0:1]
                p.act(sq, x_t, AF.Square)
                p.reduce('dve', rc, sq, ALU.add)
                p.act(rc, rc, AF.Sqrt, bias=epsc[:, 0:1], scale=1.0 / D)
                p.recip(rc, rc)
                p.stt('dve', tm, x_t, rc, A1[v], ALU.mult, ALU.mult)
                p.tt('pool', hb[t % 3], tm, B1[v], ALU.add)

            stage1(0)
            for t in range(NTA):
                if t + 1 < NTA:
                    stage1(t + 1)
                has_prev = t not in (0, 2)
                has_next = t not in (1, NTA - 1)
                h = hb[t % 3]
                for k in range(8):
                    ks = slice(k * 128, (k + 1) * 128)
                    pa = ps[k // 4][:, (k % 4) * 128:(k % 4 + 1) * 128]
                    p.mm(pa, h[:, ks], cstb[:, C_ID, :])
                    pb = ps[2 + k // 4][:, (k % 4) * 128:(k % 4 + 1) * 128]
                    p.mm(pb, h[:, ks], cstb[:, C_SH, :], start=True, stop=not (has_prev or has_next))
                    if has_prev:
                        p.mm(pb, hb[(t - 1) % 3][:, ks], cstb[:, C_SHP, :], start=False, stop=not has_next)
                    if has_next:
                        p.mm(pb, hb[(t + 1) % 3][:, ks], cstb[:, C_SHN, :], start=False, stop=True)
                for half in range(2):
                    p.copy('act', hT[:, half * 4:(half + 1) * 4, :], ps[half][:, :].with_ap(ps[half].ap.rearrange("p (k n) -> p k n", k=4)))
                    p.copy('dve', hsT[:, half * 4:(half + 1) * 4, :], ps[2 + half].with_ap(ps[2 + half].ap.rearrange("p (k n) -> p k n", k=4)))
                for k in range(8):
                    p.mm(ps[4][:, 0:384], hT[:, k, :], Wd[:, k, :], start=(k == 0), stop=(k == 7))
                for c0, pp in ((0, ps[5]), (384, ps[6])):
                    for k in range(8):
                        p.mm(pp[:, 0:384], hT[:, k, :], W1[:, k, c0:c0 + 384], start=(k == 0), stop=False)
                    for k in range(8):
                        p.mm(pp[:, 0:384], hsT[:, k, :], W2[:, k, c0:c0 + 384], start=False, stop=(k == 7))
                p.copy('act', Pd, ps[4][:, 0:384])
                p.copy('dve', RKV, ps[5][:, 0:384])
                p.copy('act', WAG, ps[6][:, 0:384])
                c_t = cs[t % 2]
                p.dma('sp', c_t[:, 0, :], cos_d[t * 128:(t + 1) * 128, :])
                p.dma('sp', c_t[:, 1, :], sin_d[t * 128:(t + 1) * 128, :])
                for i in range(4):
                    c0 = i * 64
                    rope_tm(p, Rr[:, i // 2, (i % 2) * 64:(i % 2) * 64 + 32], Rr[:, i // 2, (i % 2) * 64 + 32:(i % 2) * 64 + 64],
                            Pd[:, c0:c0 + 32], Pd[:, c0 + 32:c0 + 64], c_t[:, 0, :], c_t[:, 1, :],
                            rtmp[:, 2 * i, :], rtmp[:, 2 * i + 1, :],
                            e1='dve' if i % 2 == 0 else 'pool', e2='pool' if i % 2 == 0 else 'dve')
                sg = stg[t % 2]
                ts_ = slice(t * 128, (t + 1) * 128)
                for i in range(2):
                    pp = ps[i]
                    p.tr(pp[:, 0:128], Rr[:, i, :], ident)
                    p.copy('act' if i else 'dve', sg[:, i, :], pp[:, 0:128])
                p.copy('pool', sg[:, 2, :], Pd[:, 256:384])
                p.dma('sp', qd_s[:, ts_], sg[:, 0, :])
                p.dma('sp', kd_s[:, ts_], sg[:, 1, :])
                p.dma('sp', vd_s[:, t, :], sg[:, 2, :])
                r_, k_, v_ = RKV[:, 0:128], RKV[:, 128:256], RKV[:, 256:384]
                p.copy('pool', Vst[:, t, :], v_)
                th = Q['th']
                p.act(th, WAG[:, 0:128], AF.Tanh)
                p.tr(ps[0][:, 0:128], th, ident)
                p.copy('dve', thT[:, 0, :], ps[0][:, 0:128])
                p.tr(ps[1][:, 0:128], WAG[:, 128:256], ident)
                p.copy('act', thT[:, 1, :], ps[1][:, 0:128])
                p.act(th, WAG[:, 256:384], AF.Sigmoid)
                p.tr(ps[2][:, 0:128], th, ident)
                p.copy('dve', thT[:, 2, :], ps[2][:, 0:128])
                p.mm(ps[3][:, 0:128], thT[:, 2, :], mats[:, 2, :])
                p.copy('act', Gst[:, t, :], ps[3][:, 0:128])
                kk = Q['kk']
                tq = Q['tq']
                p.tt('dve', kk, k_, bc[:, B_KK, :], ALU.mult)
                p.act(tq, kk, AF.Square)
                nrm = cols[:, t, 2:4]
                p.reduce('dve', nrm, tq.with_ap(tq.ap.rearrange("p (h c) -> p h c", h=2)), ALU.add)
                p.act(nrm, nrm, AF.Sqrt)
                p.ts('dve', nrm, nrm, 1e-12, ALU.max)
                p.recip(nrm, nrm)
                for hh in range(2):
                    p.ts('dve', kk[:, hh * 64:(hh + 1) * 64], kk[:, hh * 64:(hh + 1) * 64], cols[:, t, 2 + hh:3 + hh], ALU.mult)
                p.tt('pool', tq, r_, k_, ALU.mult)
                p.tt('pool', tq, tq, bc[:, B_RK, :], ALU.mult)
                p.reduce('dve', BCf[:, t, :], tq.with_ap(tq.ap.rearrange("p (h c) -> p h c", h=2)), ALU.add)
                sth = stash[t % 2]
                res = []
                for d in range(2):
                    ds_ = slice(d * 64, (d + 1) * 64)
                    a_d = Q['a0'] if d == 0 else sth[:, 512:640]
                    lw = Q['lw0'] if d == 0 else sth[:, 640:768]
                    kd = Q['kd0'] if d == 0 else sth[:, 128:256]
                    pw, pa_ = ps[4 + d], ps[6 + d]
                    p.mm(pw[:, 0:128], thT[ds_, 0, :], mats[ds_, 0, :])
                    p.mm(pa_[:, 0:128], thT[ds_, 1, :], mats[ds_, 1, :])
                    p.tt('dve', lw, pw[:, 0:128], bc[:, B_W00 + d, :], ALU.add)
                    p.act(lw, lw, AF.Sigmoid)
                    p.ts('pool', lw, lw, -math.exp(-0.5), ALU.mult)
                    p.tt('dve', a_d, pa_[:, 0:128], bc[:, B_A00 + d, :], ALU.add)
                    p.act(a_d, a_d, AF.Sigmoid)
                    p.stt('dve', kd, a_d, -1.0, bc[:, B_KA, :], ALU.add, ALU.mult)
                    p.ts('pool', kd, kd, 1.0, ALU.add)
                    p.tt('pool', kd, kd, k_, ALU.mult)
                    b_d = Q['b0'] if d == 0 else sth[:, 512:640]
                    p.tt('dve', b_d, kk, a_d, ALU.mult)
                    res.append((kd, b_d, lw))
                p.copy('pool', sth[:, 0:128], r_)
                p.copy('pool', sth[:, 256:384], v_)
                p.copy('pool', sth[:, 384:512], kk)
                p.dma('sp', st_s[t], sth)
                scan_step(st, 0, t, r_, res[0][0], v_, kk, res[0][1], res[0][2], True)
        with p.scope() as st:
            stash = [p.sb(f"stashb{i}", [128, 768], F32, st) for i in range(2)]
            order = [1, 0] + list(range(NTA - 1, 1, -1))
            for n, t in enumerate(order):
                sth = stash[n % 2]
                p.dma('sp', sth, st_s[t])
                scan_step(st, 1, t, sth[:, 0:128], sth[:, 128:256], sth[:, 256:384], sth[:, 384:512], sth[:, 512:640], sth[:, 640:768], False)
            OUT = p.sb("OUTr", [128, NLAT], F32, st)
            fq = [p.sb(f"fq{i}", [128, 128], F32, st) for i in range(3)]
            fcol = p.sb("fcol", [128, NTA, 8], F32, st)
            for t in range(2, NTA):
                y = Yacc[:, t, :]
                y3 = y.with_ap(y.ap.rearrange("p (h c) -> p h c", h=2))
                mean = fcol[:, t, 0:2]
                p.reduce('dve', mean, y3, ALU.add)
                p.ts('dve', mean, mean, 1.0 / 64, ALU.mult)
                yc = fq[0]
                for hh in range(2):
                    p.ts('dve', yc[:, hh * 64:(hh + 1) * 64], y[:, hh * 64:(hh + 1) * 64], fcol[:, t, hh:hh + 1], ALU.subtract)
                p.act(fq[1], yc, AF.Square)
                var = fcol[:, t, 2:4]
                p.reduce('dve', var, fq[1].with_ap(fq[1].ap.rearrange("p (h c) -> p h c", h=2)), ALU.add)
                p.act(var, var, AF.Sqrt, bias=epsc[:, 1:2], scale=1.0 / 64)
                p.recip(var, var)
                for hh in range(2):
                    hs_ = slice(hh * 64, (hh + 1) * 64)
                    p.stt('dve', yc[:, hs_], yc[:, hs_], fcol[:, t, 2 + hh:3 + hh], bc[:, B_LNG, hs_], ALU.mult, ALU.mult)
                p.tt('pool', yc, yc, bc[:, B_LNB, :], ALU.add)
                for hh in range(2):
                    hs_ = slice(hh * 64, (hh + 1) * 64)
                    p.stt('dve', yc[:, hs_], Vst[:, t, hs_], BCf[:, t, hh:hh + 1], yc[:, hs_], ALU.mult, ALU.add)
                p.tt('pool', fq[2], yc, Gst[:, t, :], ALU.mult)
                pp = ps[t % 4]
                p.tr(pp[:, 0:128], fq[2], ident)
                p.copy('act', OUT[:, (t - 2) * 128:(t - 1) * 128], pp[:, 0:128])
            p.dma('sp', out_d[128:256, :], OUT)

    with p.scope() as st:
        QT = p.sb("QdT", [128, NTOK], BF16, st)
        KT = p.sb("KdT", [128, NTOK], BF16, st)
        Vd = p.sb("Vd", [128, NTA, 128], BF16, st)
        p.dma('sp', QT, qd_s)
        p.dma('sp', KT, kd_s)
        p.dma('sp', Vd, vd_s)
        lamv = p.sb("lamv", [128, 4, 64], F32, st)
        p.dma('sp', lamv, lam_d)
        lc = p.sb("lc", [128, 8], F32, st)
        lt = p.sb("lt", [128, 2, 64], F32, st)
        p.tt('dve', lt[:, 0, :], lamv[:, 0, :], lamv[:, 1, :], ALU.mult)
        p.tt('dve', lt[:, 1, :], lamv[:, 2, :], lamv[:, 3, :], ALU.mult)
        p.reduce('dve', lc[:, 0:2], lt, ALU.add)
        p.act(lc[:, 2:4], lc[:, 0:2], AF.Exp)
        p.tt('dve', lc[:, 4:5], lc[:, 2:3], lc[:, 3:4], ALU.subtract)
        p.ts('dve', lc[:, 5:6], lc[:, 4:5], LAM_INIT1, ALU.add, -1.0, ALU.mult)
        neglam = lc[:, 5:6]
        subg = p.sb("subg", [128, 1], F32, st)
        p.dma('sp', subg, sg_d)
        p.ts('dve', subg, subg, 1.0 - LAM_INIT1, ALU.mult)
        PT = [p.sb(f"PTd{i}", [128, 512], BF16, st) for i in range(3)]
        rec = [p.sb(f"recd{i}", [128, 512], F32, st) for i in range(2)]
        o0 = p.sb("o0", [128, 512], F32, st)
        o1 = p.sb("o1", [128, 512], F32, st)
        osq = p.sb("osq", [128, 512], F32, st)
        it = 0
        for g in range(16):
            q0 = 256 + g * 512
            for m in range(2):
                ms = slice(m * 64, (m + 1) * 64)
                pO, pD = ps[4 + m * 2], ps[5 + m * 2]
                for kt in range(NTA):
                    pS = ps[it % 3]
                    pt_ = PT[it % 3]
                    it += 1
                    p.mm(pS, KT[ms, kt * 128:(kt + 1) * 128], QT[ms, q0:q0 + 512])
                    p.act(pt_, pS, AF.Exp, scale=64.0 ** -0.5)
                    p.mm(pO, Vd[:, kt, :], pt_, start=(kt == 0), stop=(kt == NTA - 1))
                    p.mm(pD, ones_bf, pt_, start=(kt == 0), stop=(kt == NTA - 1))
                p.recip(rec[m], pD)
                p.tt('dve', o0 if m == 0 else o1, pO, rec[m], ALU.mult)
            p.stt('dve', o0, o1, neglam, o0, ALU.mult, ALU.add)
            p.act(osq, o0, AF.Square)
            pn = ps[3]
            p.mm(pn, cst[:, C_ONE, :], osq)
            p.act(osq, pn, AF.Sqrt, bias=epsc[:, 0:1], scale=1.0 / 128)
            p.recip(osq, osq)
            p.stt('dve', o0, o0, subg, osq, ALU.mult, ALU.mult)
            p.dma('sp', out_d[0:128, g * 512:(g + 1) * 512], o0)
    p.finalize()
    return nc


def _mix1_consts():
    s = np.arange(128, dtype=np.float32)[:, None]
    t = np.arange(128, dtype=np.float32)[None, :]
    c = np.zeros((10, 128, 128), np.float32)
    c[C_ID] = np.eye(128, dtype=np.float32)
    c[C_TRI0] = (s <= t)
    c[C_TRI1] = (s >= t)
    c[C_MS0] = (s < t)
    c[C_MI0] = (s <= t)
    c[C_MS1] = (s > t)
    c[C_SH] = (np.abs(s - t) == 1)
    c[C_SHP] = (s == 127) & (t == 0)
    c[C_SHN] = (s == 0) & (t == 127)
    c[C_ONE] = 1.0
    return c


def run_mix1(x, ctx, c, c_ctx, mod_w, mod_b, norm_g, w_in, lamv, subg, mu, w0, w2, a0, a2, g2, k_k, k_a, r_k, ln_g, ln_b):
    nc = build_mix1()
    cos, sin = _with_ctx_rope(*_rope_tables(64))
    cst = _mix1_consts()
    in_maps = []
    for core in range(8):
        b, j = core // 4, core % 4
        hc = np.arange(j * 128, (j + 1) * 128)
        a128 = np.arange(128)
        colsel = np.concatenate([hc, 512 + hc, 1024 + hc, 1536 + hc, 2048 + hc, 2560 + hc, 3072 + a128, 3200 + a128, 3328 + a128])
        musel = np.concatenate([hc, 512 + hc, 1024 + hc, 1536 + a128, 1664 + a128, 1792 + a128])
        rows = [w0[0][hc], w0[1][hc], a0[0][hc], a0[1][hc], k_k[hc], k_a[hc], r_k.reshape(-1)[hc], ln_g[hc], ln_b[hc]]
        bc = np.stack([np.broadcast_to(r, (128, 128)) for r in rows]).astype(np.float32)
        mats = np.stack([np.concatenate([w2[0][:, hc], w2[1][:, hc]], 0), np.concatenate([a2[0][:, hc], a2[1][:, hc]], 0), g2[:, hc]]).astype(np.float32)
        in_maps.append({
            "x": np.ascontiguousarray(np.concatenate([ctx[b], x[b]], 0), dtype=np.float32),
            "cT": _cT(c[b], c_ctx), "mod_w": np.ascontiguousarray(mod_w), "mod_b": np.ascontiguousarray(np.stack([mod_b] * 2)),
            "norm_g": np.ascontiguousarray(norm_g), "w_in": np.ascontiguousarray(w_in[:, colsel]),
            "mu": np.ascontiguousarray(np.broadcast_to(mu[musel], (128, 768)), dtype=np.float32),
            "cos": cos, "sin": sin,
            "lamv": np.ascontiguousarray(np.broadcast_to(lamv, (128, 4, 64)), dtype=np.float32),
            "subg": np.ascontiguousarray(subg.reshape(128, 1), dtype=np.float32),
            "bc": np.ascontiguousarray(bc), "mats": np.ascontiguousarray(mats), "cst": cst,
        })
    res = run_bass_kernel_spmd(nc, in_maps, core_ids=list(range(8)), trace=TRACE)
    if TRACE:
        print("DEV_NS", res.exec_time_ns)
    mix = np.empty((2, 8192, 1024), np.float32)
    for core in range(8):
        b, j = core // 4, core % 4
        o = res.results[core]["mixT"]
        mix[b, :, j * 128:(j + 1) * 128] = o[0:128].T
        mix[b, :, 512 + j * 128:512 + (j + 1) * 128] = o[128:256].T
    return mix


def kernel(x, c, ctx, c_ctx, mod_w, mod_b, norm_g, even_w_in, even_w_out, ret_decay_exp, ret_norm_g,
           gqa_q_norm, gqa_k_norm, ffn_w_gate, ffn_w_up, ffn_w_down, odd_w_in, odd_w_out, diff_lambda,
           diff_subln_g, rwkv_mu, rwkv_w0, rwkv_w2, rwkv_a0, rwkv_a2, rwkv_g2, rwkv_k_k, rwkv_k_a, rwkv_r_k,
           rwkv_ln_g, rwkv_ln_b, moe_router, moe_w_gate, moe_w_up, moe_w_down):
    f = lambda a: np.asarray(a, dtype=np.float32)
    x, c, ctx, c_ctx, mod_w, mod_b, norm_g = map(f, (x, c, ctx, c_ctx, mod_w, mod_b, norm_g))
    mix_lat, mix_ctx = run_mix0(x, ctx, c, c_ctx, mod_w[0], mod_b[0], norm_g[0], f(even_w_in)[0], f(ret_decay_exp)[0],
                                f(ret_norm_g)[0], f(gqa_q_norm)[0], f(gqa_k_norm)[0])
    x1, ctx1 = run_post(0, x, ctx, mix_lat, mix_ctx, c, c_ctx, mod_w, mod_b, norm_g, f(even_w_out)[0],
                        f(ffn_w_gate), f(ffn_w_up), f(ffn_w_down), np.zeros((1024, 8), np.float32), True, 1, 2816, 2)
    mix1 = run_mix1(x1, ctx1, c, c_ctx, mod_w[1], mod_b[1], norm_g[1], f(odd_w_in)[0], f(diff_lambda)[0], f(diff_subln_g)[0],
                    f(rwkv_mu)[0], f(rwkv_w0)[0], f(rwkv_w2)[0], f(rwkv_a0)[0], f(rwkv_a2)[0], f(rwkv_g2)[0], f(rwkv_k_k)[0],
                    f(rwkv_k_a)[0], f(rwkv_r_k)[0], f(rwkv_ln_g)[0], f(rwkv_ln_b)[0])
    x2 = run_post1(x1, mix1, c, c_ctx, mod_w[1], mod_b[1], norm_g[1], f(odd_w_out)[0],
                   f(moe_w_gate)[0], f(moe_w_up)[0], f(moe_w_down)[0], f(moe_router)[0])
    return x2


def build_d1(NT=16):
    nc = bass.Bass("TRN2", target_bir_lowering=False)
    p = Prog(nc)
    NTOK = NT * 128
    x_d = p.dram("x", [NTOK, D], F32, "ExternalInput")
    mixT_d = p.dram("mixT", [D, NTOK], F32, "ExternalInput")
    cT_d = p.dram("cT", [128, 8, 2], F32, "ExternalInput")
    modw_d = p.dram("mod_w", [D, 6 * D], F32, "ExternalInput")
    modb_d = p.dram("mod_b", [2, 6 * D], F32, "ExternalInput")
    ng_d = p.dram("norm_g", [4, D], F32, "ExternalInput")
    wout_d = p.dram("w_out", [D, D], F32, "ExternalInput")
    rt_d = p.dram("router", [D, 128], F32, "ExternalInput")
    id_d = p.dram("ident", [128, 128], F32, "ExternalInput")
    x1_d = p.dram("x1", [NTOK, D], F32, "ExternalOutput")
    h2T_d = p.dram("h2T", [D, NTOK], BF16, "ExternalOutput")
    gates_d = p.dram("gates", [NTOK, 8], F32, "ExternalOutput")
    mod_s = p.dram("mod_s", [2, 6 * D], F32, "Internal")
    ps = [p.ps(f"ps{i}", [128, 512]) for i in range(8)]
    ident = p.sb("ident", [128, 128], F32)
    p.dma('sp', ident, id_d)
    epsc = p.sb("epsc", [128, 1], F32)
    p.memset('dve', epsc, NORM_EPS)
    h2T = p.sb("h2T", [128, 8, NTOK], BF16)
    gates = p.sb("gates", [128, NT, 8], F32)
    with p.scope() as st:
        mod_rows(p, ps, st, cT_d, modw_d, modb_d, mod_s, [4, 5, 6, 7, 8, 9])

    def rms_rstd(dst_col, src, sq_tmp):
        p.act(sq_tmp, src, AF.Square)
        p.reduce('dve', dst_col, sq_tmp, ALU.add)
        p.act(dst_col, dst_col, AF.Sqrt, bias=epsc, scale=1.0 / D)
        p.recip(dst_col, dst_col)

    with p.scope() as st:
        G1 = p.sb("G1", [128, D], F32, st)
        A2 = p.sb("A2", [128, D], F32, st)
        B2 = p.sb("B2", [128, D], F32, st)
        ngb = p.sb("ngb", [128, 2, D], F32, st)
        p.dma('sp', ngb[:, 0, :], ng_d.with_ap(ng_d.ap[1:2, :].partition_broadcast(128)))
        p.dma('sp', ngb[:, 1, :], ng_d.with_ap(ng_d.ap[2:3, :].partition_broadcast(128)))
        p.dma('sp', G1, mod_s.with_ap(mod_s.ap[0:1, 2 * D:3 * D].partition_broadcast(128)))
        p.tt('dve', G1, G1, ngb[:, 0, :], ALU.mult)
        p.dma('sp', B2, mod_s.with_ap(mod_s.ap[0:1, 3 * D:4 * D].partition_broadcast(128)))
        p.dma('sp', A2, mod_s.with_ap(mod_s.ap[0:1, 4 * D:5 * D].partition_broadcast(128)))
        p.ts('dve', A2, A2, 1.0, ALU.add)
        p.tt('dve', A2, A2, ngb[:, 1, :], ALU.mult)
        mixT = p.sb("mixT", [128, 8, NTOK], BF16, st)
        mview = mixT_d.ap.rearrange("(k p) n -> p k n", p=128)
        for k in range(8):
            p.dma('pool', mixT[:, k, :], mixT_d.with_ap(mview[:, k, :]))
        wout = p.sb("wout", [128, 8, D], BF16, st)
        wv = wout_d.ap.rearrange("(k p) n -> p k n", p=128)
        for k in range(8):
            p.dma('pool', wout[:, k, :], wout_d.with_ap(wv[:, k, :]))
        rt = p.sb("rt", [128, 8, 128], F32, st)
        if DBG != 'd1c':
            p.dma('sp', rt, rt_d.with_ap(rt_d.ap.rearrange("(k p) e -> p k e", p=128)))
        h2Tf = p.sb("h2Tf", [128, 8, 128], F32, st)
        xt = [p.sb(f"xt{i}", [128, D], F32, st) for i in range(2)]
        tmp = [p.sb(f"tmp{i}", [128, D], F32, st) for i in range(2)]
        sq = p.sb("sq", [128, D], F32, st)
        cols = p.sb("cols", [128, NT, 8], F32, st)
        lg = p.sb("lg", [128, 4, 8], F32, st)
        for t in range(NT):
            x_t, tm = xt[t % 2], tmp[t % 2]
            p.dma('sp', x_t, x_d[t * 128:(t + 1) * 128, :])
            py = [ps[0], ps[1]]
            for fh in range(2):
                for k in range(8):
                    p.mm(py[fh], mixT[:, k, t * 128:(t + 1) * 128], wout[:, k, fh * 512:(fh + 1) * 512], start=(k == 0), stop=(k == 7))
            for fh in range(2):
                p.copy('act', tm[:, fh * 512:(fh + 1) * 512], py[fh])
            rc = cols[:, t, 0:1]
            rms_rstd(rc, tm, sq)
            p.stt('dve', tm, tm, rc, G1, ALU.mult, ALU.mult)
            p.tt('pool', x_t, x_t, tm, ALU.add)
            p.dma('sp', x1_d[t * 128:(t + 1) * 128, :], x_t)
            rc2 = cols[:, t, 1:2]
            rms_rstd(rc2, x_t, sq)
            p.stt('dve', tm, x_t, rc2, A2, ALU.mult, ALU.mult)
            p.tt('pool', tm, tm, B2, ALU.add)
            for k in range(8):
                pt = ps[2 + (k % 4)]
                p.tr(pt[:, 0:128], tm[:, k * 128:(k + 1) * 128], ident)
                p.copy('act' if k % 2 else 'dve', h2Tf[:, k, :], pt[:, 0:128])
                p.copy('pool', h2T[:, k, t * 128:(t + 1) * 128], h2Tf[:, k, :])
            if DBG in ('d1a', 'd1b', 'd1c'):
                p.memset('dve', gates[:, t, :], 0.125)
                continue
            pl = ps[6]
            for k in range(8):
                p.mm(pl[:, 0:128], h2Tf[:, k, :], rt[:, k, :], start=(k == 0), stop=(k == 7))
            L = lg[:, 0, :]
            p.copy('dve', L, pl[:, 0:8])
            m1 = cols[:, t, 2:3]
            m2 = cols[:, t, 3:4]
            p.reduce('dve', m1, L, ALU.max)
            mk1 = lg[:, 1, :]
            p.ts('dve', mk1, L, m1, ALU.is_equal)
            L2 = lg[:, 2, :]
            p.stt('dve', L2, mk1, -1e30, L, ALU.mult, ALU.add)
            p.reduce('dve', m2, L2, ALU.max)
            mk2 = lg[:, 3, :]
            p.ts('dve', mk2, L2, m2, ALU.is_equal)
            w1 = cols[:, t, 4:5]
            w2 = cols[:, t, 5:6]
            p.tt('dve', w1, m1, m2, ALU.subtract)
            p.act(w1, w1, AF.Sigmoid)
            p.ts('dve', w2, w1, -1.0, ALU.mult, 1.0, ALU.add)
            p.ts('dve', gates[:, t, :], mk1, w1, ALU.mult)
            p.stt('dve', gates[:, t, :], mk2, w2, gates[:, t, :], ALU.mult, ALU.add)
        hv = h2T_d.ap.rearrange("(k p) n -> p k n", p=128)
        for k in range(8):
            p.dma('sp', h2T_d.with_ap(hv[:, k, :]), h2T[:, k, :])
        p.dma('sp', gates_d.with_ap(gates_d.ap.rearrange("(t p) e -> p t e", p=128)), gates)
    p.finalize()
    return nc


def build_d2(NCH=8, H=3584, BLK=4):
    nc = bass.Bass("TRN2", target_bir_lowering=False)
    p = Prog(nc)
    CT = 16
    NTOK = NCH * CT * 128
    HC = H // 128
    NB = HC // BLK
    h2T_d = p.dram("h2T", [D, NTOK], BF16, "ExternalInput")
    g_d = p.dram("gate", [128, NCH * CT], F32, "ExternalInput")
    wg_d = p.dram("wg", [D, H], F32, "ExternalInput")
    wu_d = p.dram("wu", [D, H], F32, "ExternalInput")
    wd_d = p.dram("wd", [H, D], F32, "ExternalInput")
    out_d = p.dram("y", [NTOK, D], F32, "ExternalOutput")
    ps = [p.ps(f"ps{i}", [128, 512]) for i in range(8)]
    gt = p.sb("gt", [128, NCH * CT], F32)
    p.dma('sp', gt, g_d)
    h2T = [p.sb(f"h2T{i}", [128, 8, CT * 128], BF16) for i in range(2)]
    acc = p.sb("acc", [128, CT, D], F32)
    wgb = [p.sb(f"wg{i}", [128, 8, BLK * 128], BF16) for i in range(2)]
    wub = [p.sb(f"wu{i}", [128, 8, BLK * 128], BF16) for i in range(2)]
    wdb = [p.sb(f"wd{i}", [128, BLK, D], BF16) for i in range(2)]
    actT = [p.sb(f"actT{i}", [128, BLK, 512], BF16) for i in range(2)]
    sg = [p.sb(f"sg{i}", [128, 512], F32) for i in range(2)]
    yo = [p.sb(f"yo{i}", [128, D], F32) for i in range(2)]
    wgv = wg_d.ap.rearrange("(k p) h -> p k h", p=128)
    wuv = wu_d.ap.rearrange("(k p) h -> p k h", p=128)
    wdv = wd_d.ap.rearrange("(c p) f -> p c f", p=128)
    hv = h2T_d.ap.rearrange("(k p) n -> p k n", p=128)
    it = 0
    gi = 0
    for ch in range(NCH):
        hb = h2T[ch % 2]
        for k in range(8):
            p.dma('sp', hb[:, k, :], h2T_d.with_ap(hv[:, k, ch * CT * 128:(ch + 1) * CT * 128]))
        for b in range(NB):
            s = it % 2
            it += 1
            hs = slice(b * BLK * 128, (b + 1) * BLK * 128)
            for k in range(8):
                p.dma('pool', wgb[s][:, k, :], wg_d.with_ap(wgv[:, k, hs]))
                p.dma('pool', wub[s][:, k, :], wu_d.with_ap(wuv[:, k, hs]))
            for c in range(BLK):
                p.dma('pool', wdb[s][:, c, :], wd_d.with_ap(wdv[:, b * BLK + c, :]))
            for t0 in range(0, CT, 4):
                ts_ = slice(t0 * 128, (t0 + 4) * 128)
                a = actT[gi % 2]
                gi += 1
                for c in range(BLK):
                    pg, pu = ps[(c % 2) * 2], ps[(c % 2) * 2 + 1]
                    for k in range(8):
                        p.mm(pg, wgb[s][:, k, c * 128:(c + 1) * 128], hb[:, k, ts_], start=(k == 0), stop=(k == 7))
                    for k in range(8):
                        p.mm(pu, wub[s][:, k, c * 128:(c + 1) * 128], hb[:, k, ts_], start=(k == 0), stop=(k == 7))
                    sgt = sg[c % 2]
                    p.act(sgt, pg, AF.Silu)
                    p.tt('dve', a[:, c, :], sgt, pu, ALU.mult)
                for j in range(4):
                    t = t0 + j
                    for fh in range(2):
                        pd = ps[4 + ((j * 2 + fh) % 4)]
                        for c in range(BLK):
                            p.mm(pd, a[:, c, j * 128:(j + 1) * 128], wdb[s][:, c, fh * 512:(fh + 1) * 512], start=(c == 0), stop=(c == BLK - 1))
                        dst = acc[:, t, fh * 512:(fh + 1) * 512]
                        if b == 0:
                            p.copy('dve', dst, pd)
                        else:
                            p.tt('dve', dst, pd, dst, ALU.add)
        for t in range(CT):
            gt_ = ch * CT + t
            y = yo[t % 2]
            p.ts('pool', y, acc[:, t, :], gt[:, gt_:gt_ + 1], ALU.mult)
            p.dma('sp', out_d[gt_ * 128:(gt_ + 1) * 128, :], y)
    p.finalize()
    return nc


def build_d3(NT=16, NE=8):
    nc = bass.Bass("TRN2", target_bir_lowering=False)
    p = Prog(nc)
    NTOK = NT * 128
    ys_d = p.dram("ys", [NE, NTOK, D], F32, "ExternalInput")
    x1_d = p.dram("x1", [NTOK, D], F32, "ExternalInput")
    cT_d = p.dram("cT", [128, 8, 2], F32, "ExternalInput")
    modw_d = p.dram("mod_w", [D, 6 * D], F32, "ExternalInput")
    modb_d = p.dram("mod_b", [2, 6 * D], F32, "ExternalInput")
    ng_d = p.dram("norm_g", [4, D], F32, "ExternalInput")
    out_d = p.dram("out", [NTOK, D], F32, "ExternalOutput")
    mod_s = p.dram("mod_s", [2, 6 * D], F32, "Internal")
    ps = [p.ps(f"ps{i}", [128, 512]) for i in range(8)]
    epsc = p.sb("epsc", [128, 1], F32)
    p.memset('dve', epsc, NORM_EPS)
    with p.scope() as st:
        mod_rows(p, ps, st, cT_d, modw_d, modb_d, mod_s, [10, 11])
    with p.scope() as st:
        G2 = p.sb("G2", [128, D], F32, st)
        ng3 = p.sb("ng3", [128, D], F32, st)
        p.dma('sp', ng3, ng_d.with_ap(ng_d.ap[3:4, :].partition_broadcast(128)))
        p.dma('sp', G2, mod_s.with_ap(mod_s.ap[0:1, 5 * D:6 * D].partition_broadcast(128)))
        p.tt('dve', G2, G2, ng3, ALU.mult)
        yt = [p.sb(f"yt{i}", [128, NE, D], F32, st) for i in range(2)]
        x1t = [p.sb(f"x1t{i}", [128, D], F32, st) for i in range(2)]
        sq = p.sb("sq", [128, D], F32, st)
        cols = p.sb("cols", [128, NT], F32, st)
        for t in range(NT):
            y = yt[t % 2]
            x1 = x1t[t % 2]
            for e in range(NE):
                p.dma('sp', y[:, e, :], ys_d[e, t * 128:(t + 1) * 128, :])
            p.dma('sp', x1, x1_d[t * 128:(t + 1) * 128, :])
            f = y[:, 0, :]
            for e in range(1, NE):
                p.tt('dve' if e % 2 else 'pool', f, f, y[:, e, :], ALU.add)
            rc = cols[:, t:t + 1]
            p.act(sq, f, AF.Square)
            p.reduce('dve', rc, sq, ALU.add)
            p.act(rc, rc, AF.Sqrt, bias=epsc, scale=1.0 / D)
            p.recip(rc, rc)
            p.stt('dve', f, f, rc, G2, ALU.mult, ALU.mult)
            p.tt('pool', x1, x1, f, ALU.add)
            p.dma('sp', out_d[t * 128:(t + 1) * 128, :], x1)
    p.finalize()
    return nc


def run_post1(x_lat, mix_lat, c, c_ctx, mod_w, mod_b, norm_g, w_out, wg, wu, wd, router):
    import ml_dtypes
    cores = list(range(8))
    nc = build_d1()
    rpad = np.ascontiguousarray(np.concatenate([router, np.zeros((1024, 120), np.float32)], 1))
    in_maps = []
    for core in cores:
        b, q = core // 4, core % 4
        in_maps.append({
            "x": np.ascontiguousarray(x_lat[b, q * 2048:(q + 1) * 2048]),
            "mixT": np.ascontiguousarray(mix_lat[b, q * 2048:(q + 1) * 2048].T),
            "cT": _cT(c[b], c_ctx), "mod_w": np.ascontiguousarray(mod_w), "mod_b": np.ascontiguousarray(np.stack([mod_b] * 2)),
            "norm_g": np.ascontiguousarray(norm_g), "w_out": np.ascontiguousarray(w_out), "router": rpad, "ident": _IDENT,
        })
    r1 = run_bass_kernel_spmd(nc, in_maps, core_ids=cores, trace=TRACE).results
    if TRACE:
        pass
    h2T_all = np.ascontiguousarray(np.concatenate([np.asarray(r1[k]["h2T"]) for k in cores], axis=1))
    gates_all = np.concatenate([np.asarray(r1[k]["gates"]) for k in cores], axis=0)
    nc = build_d2()
    in_maps = []
    for e in cores:
        in_maps.append({
            "h2T": h2T_all,
            "gate": np.ascontiguousarray(gates_all[:, e].reshape(128, 128).T),
            "wg": np.ascontiguousarray(wg[e]), "wu": np.ascontiguousarray(wu[e]), "wd": np.ascontiguousarray(wd[e]),
        })
    r2 = run_bass_kernel_spmd(nc, in_maps, core_ids=cores, trace=TRACE).results
    nc = build_d3()
    in_maps = []
    for core in cores:
        b, q = core // 4, core % 4
        ys = np.ascontiguousarray(np.stack([np.asarray(r2[e]["y"])[core * 2048:(core + 1) * 2048] for e in cores]))
        in_maps.append({
            "ys": ys, "x1": np.asarray(r1[core]["x1"]),
            "cT": _cT(c[b], c_ctx), "mod_w": np.ascontiguousarray(mod_w), "mod_b": np.ascontiguousarray(np.stack([mod_b] * 2)),
            "norm_g": np.ascontiguousarray(norm_g),
        })
    r3 = run_bass_kernel_spmd(nc, in_maps, core_ids=cores, trace=TRACE).results
    x2 = np.empty_like(x_lat)
    for core in cores:
        b, q = core // 4, core % 4
        x2[b, q * 2048:(q + 1) * 2048] = r3[core]["out"]
    return x2
```

```python
import contextlib
import math
import numpy as np
import concourse.bass as bass
import concourse.mybir as mybir
from concourse.bass_utils import run_bass_kernel_spmd

ALU = mybir.AluOpType
AF = mybir.ActivationFunctionType
AX = mybir.AxisListType
F32 = mybir.dt.float32
BF16 = mybir.dt.bfloat16

SAME_ENGINE_SYNC = True
ENGS = ('pe', 'act', 'dve', 'pool', 'sp')


class Res:
    __slots__ = ('name', 'w', 'r', 'dsem', 'dcount')

    def __init__(self, name):
        self.name = name
        self.w = None
        self.r = []
        self.dsem = None
        self.dcount = 0


class T:
    __slots__ = ('ap', 'res')

    def __init__(self, ap, res):
        self.ap = ap
        self.res = res

    def __getitem__(self, idx):
        return T(self.ap[idx], self.res)

    def with_ap(self, ap):
        return T(ap, self.res)


class Prog:
    def __init__(self, nc):
        self.nc = nc
        self.stack = contextlib.ExitStack()
        self.ops = {e: [] for e in ENGS}
        self.nops = {e: 0 for e in ENGS}
        self.seen = {e: {} for e in ENGS}
        self.signal = {e: set() for e in ENGS}
        self.dma_res = []
        self.all_res = []
        self.nsb = 0

    def _res(self, name):
        r = Res(name)
        self.all_res.append(r)
        return r

    def sb(self, name, shape, dt, stack=None):
        t = (stack or self.stack).enter_context(self.nc.sbuf_tensor('sb_' + name, list(shape), dt))
        return T(t[tuple(slice(None) for _ in shape)], self._res(name))

    def ps(self, name, shape, dt=F32, stack=None):
        t = (stack or self.stack).enter_context(self.nc.psum_tensor('pp_' + name, list(shape), dt))
        return T(t[tuple(slice(None) for _ in shape)], self._res(name))

    def dram(self, name, shape, dt, kind):
        t = self.nc.dram_tensor(name, list(shape), dt, kind=kind).ap()
        return T(t, self._res(name))

    def sub(self, t, name=None):
        return T(t.ap, self._res(name or t.res.name + '_sub'))

    def _need(self, eng, tok, waits):
        if tok is None:
            return
        if tok[0] == 'eng':
            _, f, k = tok
            if f == eng and not (SAME_ENGINE_SYNC and eng != 'pe'):
                return
            key = ('eng', f)
            if self.seen[eng].get(key, 0) >= k:
                return
            self.seen[eng][key] = k
            self.signal[f].add(k)
            waits.append(tok)
        else:
            _, res, cnt = tok
            key = ('dma', id(res))
            if self.seen[eng].get(key, 0) >= cnt:
                return
            self.seen[eng][key] = cnt
            waits.append(tok)

    def _deps(self, eng, reads, writes):
        waits = []
        for t in reads:
            self._need(eng, t.res.w, waits)
        for t in writes:
            self._need(eng, t.res.w, waits)
            for tok in t.res.r:
                self._need(eng, tok, waits)
        return waits

    def op(self, eng, fn, reads=(), writes=()):
        waits = self._deps(eng, reads, writes)
        self.nops[eng] += 1
        k = self.nops[eng]
        tok = ('eng', eng, k)
        for t in reads:
            t.res.r.append(tok)
        for t in writes:
            t.res.w = tok
            t.res.r = []
        self.ops[eng].append((waits, fn, k, None))

    def dma(self, q, out, in_, **kw):
        waits = self._deps(q, [in_], [out])
        res = out.res
        if res.dsem is None:
            res.dsem = self.stack.enter_context(self.nc.semaphore('d_' + res.name))
            self.dma_res.append(res)
        res.dcount += 1
        tok = ('dma', res, res.dcount)
        in_.res.r.append(tok)
        res.w = tok
        res.r = []
        oap, iap = out.ap, in_.ap
        self.ops[q].append((waits, lambda e: e.dma_start(out=oap, in_=iap, **kw), None, res))

    def barrier(self):
        for e in ENGS:
            waits = []
            for f in ENGS:
                if f != e and self.nops[f] > 0:
                    self._need(e, ('eng', f, self.nops[f]), waits)
            for res in self.dma_res:
                self._need(e, ('dma', res, res.dcount), waits)
            if waits:
                self.ops[e].append((waits, None, None, None))

    @contextlib.contextmanager
    def scope(self):
        st = contextlib.ExitStack()
        try:
            yield st
        finally:
            self.barrier()
            st.close()

    def finalize(self):
        nc = self.nc
        sems = {e: self.stack.enter_context(nc.semaphore('s_' + e)) for e in ENGS}
        self.barrier()
        rank = {}
        for e in ENGS:
            for i, k in enumerate(sorted(self.signal[e])):
                rank[(e, k)] = i + 1

        def run(ename, eng):
            for waits, fn, k, dres in self.ops[ename]:
                for tok in waits:
                    if tok[0] == 'eng':
                        eng.wait_ge(sems[tok[1]], rank[(tok[1], tok[2])])
                    else:
                        eng.wait_ge(tok[1].dsem, 16 * tok[2])
                if fn is None:
                    continue
                ins = fn(eng)
                if dres is not None:
                    ins.then_inc(dres.dsem, 16)
                elif (ename, k) in rank:
                    ins.then_inc(sems[ename], 1)

        with nc.Block() as block:
            @block.tensor
            def _(e):
                run('pe', e)

            @block.scalar
            def _(e):
                run('act', e)

            @block.vector
            def _(e):
                run('dve', e)

            @block.gpsimd
            def _(e):
                run('pool', e)

            @block.sync
            def _(e):
                run('sp', e)
        self.stack.close()

    def mm(self, out, lhsT, rhs, start=True, stop=True):
        o, l, r = out.ap, lhsT.ap, rhs.ap
        self.op('pe', lambda e: e.matmul(o, l, r, start=start, stop=stop), [lhsT, rhs], [out])

    def tr(self, out, in_, ident):
        o, i, d = out.ap, in_.ap, ident.ap
        self.op('pe', lambda e: e.transpose(o, i, d), [in_, ident], [out])

    def act(self, out, in_, func, bias=None, scale=1.0, accum=None, eng='act'):
        o, i = out.ap, in_.ap
        reads = [in_]
        kw = {}
        if bias is not None:
            if isinstance(bias, T):
                reads.append(bias)
                kw['bias'] = bias.ap
            else:
                kw['bias'] = bias
        if isinstance(scale, T):
            reads.append(scale)
            kw['scale'] = scale.ap
        else:
            kw['scale'] = scale
        writes = [out]
        if accum is not None:
            writes.append(accum)
            kw['accum_out'] = accum.ap
        self.op(eng, lambda e: e.activation(o, i, func, **kw), reads, writes)

    def tt(self, eng, out, in0, in1, op):
        o, a, b = out.ap, in0.ap, in1.ap
        self.op(eng, lambda e: e.tensor_tensor(o, a, b, op), [in0, in1], [out])

    def ts(self, eng, out, in0, s1, op0, s2=None, op1=None, accum=None):
        o, a = out.ap, in0.ap
        reads = [in0]
        v1 = s1
        if isinstance(s1, T):
            reads.append(s1)
            v1 = s1.ap
        v2 = s2
        if isinstance(s2, T):
            reads.append(s2)
            v2 = s2.ap
        kw = {}
        if op1 is not None:
            kw['op1'] = op1
        writes = [out]
        if accum is not None:
            kw['accum_out'] = accum.ap
            writes.append(accum)
        self.op(eng, lambda e: e.tensor_scalar(o, a, v1, v2, op0, **kw), reads, writes)

    def stt(self, eng, out, in0, scalar, in1, op0, op1):
        o, a, b = out.ap, in0.ap, in1.ap
        reads = [in0, in1]
        v = scalar
        if isinstance(scalar, T):
            reads.append(scalar)
            v = scalar.ap
        self.op(eng, lambda e: e.scalar_tensor_tensor(o, a, v, b, op0, op1), reads, [out])

    def copy(self, eng, out, in_):
        o, i = out.ap, in_.ap
        if eng == 'act':
            self.op(eng, lambda e: e.activation(o, i, AF.Copy), [in_], [out])
        else:
            self.op(eng, lambda e: e.tensor_copy(o, i), [in_], [out])

    def memset(self, eng, out, val):
        o = out.ap
        self.op(eng, lambda e: e.memset(o, val), [], [out])

    def reduce(self, eng, out, in_, op, axis=AX.X):
        o, i = out.ap, in_.ap
        self.op(eng, lambda e: e.tensor_reduce(o, i, axis, op), [in_], [out])

    def recip(self, out, in_):
        o, i = out.ap, in_.ap
        self.op('dve', lambda e: e.reciprocal(o, i), [in_], [out])


NORM_EPS = 1e-6
D = 1024
DBG = None
TRACE = False


def build_post(NT, E, H, BLK, has_ctx):
    nc = bass.Bass("TRN2", target_bir_lowering=False)
    p = Prog(nc)
    NTOK = NT * 128
    HC = H // 128
    NB = HC // BLK
    assert NB * BLK == HC
    x_d = p.dram("x", [NTOK, D], F32, "ExternalInput")
    mixT_d = p.dram("mixT", [D, NTOK], F32, "ExternalInput")
    cT_d = p.dram("cT", [128, 8, 2], F32, "ExternalInput")
    modw_d = p.dram("mod_w", [D, 6 * D], F32, "ExternalInput")
    modb_d = p.dram("mod_b", [2, 6 * D], F32, "ExternalInput")
    ng_d = p.dram("norm_g", [4, D], F32, "ExternalInput")
    wout_d = p.dram("w_out", [D, D], F32, "ExternalInput")
    wg_d = p.dram("wg", [E, D, H], F32, "ExternalInput")
    wu_d = p.dram("wu", [E, D, H], F32, "ExternalInput")
    wd_d = p.dram("wd", [E, H, D], F32, "ExternalInput")
    rt_d = p.dram("router", [D, 128], F32, "ExternalInput")
    id_d = p.dram("ident", [128, 128], F32, "ExternalInput")
    out_d = p.dram("out", [NTOK, D], F32, "ExternalOutput")
    mod_s = p.dram("mod_s", [2, 6 * D], F32, "Internal")
    x1_s = p.dram("x1_s", [NTOK, D], F32, "Internal")

    ps = [p.ps(f"ps{i}", [128, 512]) for i in range(8)]
    ident = p.sb("ident", [128, 128], F32)
    p.dma('sp', ident, id_d)
    epsc = p.sb("epsc", [128, 1], F32)
    p.memset('dve', epsc, NORM_EPS)
    h2T = p.sb("h2T", [128, 8, NTOK], BF16)
    gates = p.sb("gates", [128, NT, 8], F32)
    nvar = 2 if has_ctx else 1

    with p.scope() as st:
        cT = p.sb("cT", [128, 8, 2], F32, st)
        sc = p.sb("silu_c", [128, 8, 2], F32, st)
        p.dma('sp', cT, cT_d)
        p.act(sc, cT, AF.Silu)
        modrow = p.sb("modrow", [2, 6 * D], F32, st)
        modb = p.sb("modb", [2, 6 * D], F32, st)
        p.dma('sp', modb, modb_d)
        wblk = [p.sb(f"modw{i}", [128, 8, 512], F32, st) for i in range(2)]
        for cb in range(2, 12):
            wb = wblk[cb % 2]
            p.dma('sp', wb, modw_d.with_ap(modw_d.ap.rearrange("(k p) n -> p k n", p=128)[:, :, cb * 512:(cb + 1) * 512]))
            pp = ps[cb % 2]
            for k in range(8):
                p.mm(pp[0:2, :], sc[:, k, :], wb[:, k, :], start=(k == 0), stop=(k == 7))
            p.tt('dve', modrow[:, cb * 512:(cb + 1) * 512], pp[0:2, :], modb[:, cb * 512:(cb + 1) * 512], ALU.add)
        p.dma('sp', mod_s[:, 2 * D:6 * D], modrow[:, 2 * D:6 * D])

    def bcast_row(dst, src_row_ap):
        p.dma('sp', dst, src_row_ap)

    def rms_rstd(dst_col, src, sq_tmp):
        p.act(sq_tmp, src, AF.Square)
        p.reduce('dve', dst_col, sq_tmp, ALU.add)
        p.act(dst_col, dst_col, AF.Sqrt, bias=epsc, scale=1.0 / D)
        p.recip(dst_col, dst_col)

    with p.scope() as st:
        G1 = [p.sb(f"G1_{v}", [128, D], F32, st) for v in range(nvar)]
        A2 = [p.sb(f"A2_{v}", [128, D], F32, st) for v in range(nvar)]
        B2 = [p.sb(f"B2_{v}", [128, D], F32, st) for v in range(nvar)]
        ngb = p.sb("ngb", [128, 2, D], F32, st)
        bcast_row(ngb[:, 0, :], ng_d.with_ap(ng_d.ap[1:2, :].partition_broadcast(128)))
        bcast_row(ngb[:, 1, :], ng_d.with_ap(ng_d.ap[2:3, :].partition_broadcast(128)))
        for v in range(nvar):
            bcast_row(G1[v], mod_s.with_ap(mod_s.ap[v:v + 1, 2 * D:3 * D].partition_broadcast(128)))
            p.tt('dve', G1[v], G1[v], ngb[:, 0, :], ALU.mult)
            bcast_row(B2[v], mod_s.with_ap(mod_s.ap[v:v + 1, 3 * D:4 * D].partition_broadcast(128)))
            bcast_row(A2[v], mod_s.with_ap(mod_s.ap[v:v + 1, 4 * D:5 * D].partition_broadcast(128)))
            p.ts('dve', A2[v], A2[v], 1.0, ALU.add)
            p.tt('dve', A2[v], A2[v], ngb[:, 1, :], ALU.mult)
        mixT = p.sb("mixT", [128, 8, NTOK], BF16, st)
        mview = mixT_d.ap.rearrange("(k p) n -> p k n", p=128)
        for k in range(8):
            p.dma('pool', mixT[:, k, :], mixT_d.with_ap(mview[:, k, :]))
        wout = p.sb("wout", [128, 8, D], BF16, st)
        wv = wout_d.ap.rearrange("(k p) n -> p k n", p=128)
        for k in range(8):
            p.dma('pool', wout[:, k, :], wout_d.with_ap(wv[:, k, :]))
        if E > 1:
            rt = p.sb("rt", [128, 8, 128], F32, st)
            p.dma('sp', rt, rt_d.with_ap(rt_d.ap.rearrange("(k p) e -> p k e", p=128)))
            h2Tf = p.sb("h2Tf", [128, 8, 128], F32, st)
        xt = [p.sb(f"xt{i}", [128, D], F32, st) for i in range(2)]
        tmp = [p.sb(f"tmp{i}", [128, D], F32, st) for i in range(2)]
        sq = p.sb("sq", [128, D], F32, st)
        cols = p.sb("cols", [128, NT, 8], F32, st)
        lg = p.sb("lg", [128, 4, 8], F32, st)
        for t in range(NT):
            v = 1 if (has_ctx and t == NT - 1) else 0
            x_t, tm = xt[t % 2], tmp[t % 2]
            p.dma('sp', x_t, x_d[t * 128:(t + 1) * 128, :])
            py = [ps[0], ps[1]]
            for fh in range(2):
                for k in range(8):
                    p.mm(py[fh], mixT[:, k, t * 128:(t + 1) * 128], wout[:, k, fh * 512:(fh + 1) * 512], start=(k == 0), stop=(k == 7))
            for fh in range(2):
                p.copy('act', tm[:, fh * 512:(fh + 1) * 512], py[fh])
            rc = cols[:, t, 0:1]
            rms_rstd(rc, tm, sq)
            p.stt('dve', tm, tm, rc, G1[v], ALU.mult, ALU.mult)
            p.tt('pool', x_t, x_t, tm, ALU.add)
            p.dma('sp', x1_s[t * 128:(t + 1) * 128, :], x_t)
            rc2 = cols[:, t, 1:2]
            rms_rstd(rc2, x_t, sq)
            p.stt('dve', tm, x_t, rc2, A2[v], ALU.mult, ALU.mult)
            p.tt('pool', tm, tm, B2[v], ALU.add)
            for k in range(8):
                pt = ps[2 + (k % 4)]
                p.tr(pt[:, 0:128], tm[:, k * 128:(k + 1) * 128], ident)
                p.copy('act' if k % 2 else 'dve', h2T[:, k, t * 128:(t + 1) * 128], pt[:, 0:128])
                if E > 1:
                    p.copy('dve' if k % 2 else 'act', h2Tf[:, k, :], pt[:, 0:128])
            if E > 1 and DBG not in ('norouter', 'e1only', 'e0only'):
                pl = ps[6]
                for k in range(8):
                    p.mm(pl[:, 0:128], h2Tf[:, k, :], rt[:, k, :], start=(k == 0), stop=(k == 7))
                L = lg[:, 0, :]
                p.copy('dve', L, pl[:, 0:8])
                m1 = cols[:, t, 2:3]
                m2 = cols[:, t, 3:4]
                p.reduce('dve', m1, L, ALU.max)
                mk1 = lg[:, 1, :]
                p.ts('dve', mk1, L, m1, ALU.is_equal)
                L2 = lg[:, 2, :]
                p.stt('dve', L2, mk1, -1e30, L, ALU.mult, ALU.add)
                p.reduce('dve', m2, L2, ALU.max)
                mk2 = lg[:, 3, :]
                p.ts('dve', mk2, L2, m2, ALU.is_equal)
                w1 = cols[:, t, 4:5]
                w2 = cols[:, t, 5:6]
                p.tt('dve', w1, m1, m2, ALU.subtract)
                p.act(w1, w1, AF.Sigmoid)
                p.ts('dve', w2, w1, -1.0, ALU.mult, 1.0, ALU.add)
                p.ts('dve', gates[:, t, :], mk1, w1, ALU.mult)
                p.stt('dve', gates[:, t, :], mk2, w2, gates[:, t, :], ALU.mult, ALU.add)

    with p.scope() as st:
        acc = p.sb("acc", [128, NT, D], F32, st)
        wgb = [p.sb(f"wg{i}", [128, 8, BLK * 128], BF16, st) for i in range(2)]
        wub = [p.sb(f"wu{i}", [128, 8, BLK * 128], BF16, st) for i in range(2)]
        wdb = [p.sb(f"wd{i}", [128, BLK, D], BF16, st) for i in range(2)]
        actT = [p.sb(f"actT{i}", [128, BLK, 512], BF16, st) for i in range(2)]
        sg = [p.sb(f"sg{i}", [128, 512], F32, st) for i in range(2)]
        groups = []
        t0 = 0
        while t0 < NT:
            n = min(4, NT - t0)
            groups.append((t0, n))
            t0 += n
        it = 0
        gi = 0
        first = True
        for e in ([1] if DBG == 'e1only' else [0] if DBG == 'e0only' else range(E)):
            wgv = wg_d.ap.rearrange("e (k p) h -> e p k h", p=128)[e]
            wuv = wu_d.ap.rearrange("e (k p) h -> e p k h", p=128)[e]
            wdv = wd_d.ap.rearrange("e (c p) f -> e p c f", p=128)[e]
            for b in range(NB):
                s = it % 2
                it += 1
                hs = slice(b * BLK * 128, (b + 1) * BLK * 128)
                for k in range(8):
                    p.dma('pool', wgb[s][:, k, :], wg_d.with_ap(wgv[:, k, hs]))
                    p.dma('pool', wub[s][:, k, :], wu_d.with_ap(wuv[:, k, hs]))
                for c in range(BLK):
                    p.dma('pool', wdb[s][:, c, :], wd_d.with_ap(wdv[:, b * BLK + c, :]))
                for (t0, n) in groups:
                    ts_ = slice(t0 * 128, (t0 + n) * 128)
                    W = n * 128
                    a = actT[gi % 2]
                    gi += 1
                    for c in range(BLK):
                        pg, pu = ps[(c % 2) * 2], ps[(c % 2) * 2 + 1]
                        for k in range(8):
                            p.mm(pg[:, 0:W], wgb[s][:, k, c * 128:(c + 1) * 128], h2T[:, k, ts_], start=(k == 0), stop=(k == 7))
                        for k in range(8):
                            p.mm(pu[:, 0:W], wub[s][:, k, c * 128:(c + 1) * 128], h2T[:, k, ts_], start=(k == 0), stop=(k == 7))
                        sgt = sg[c % 2]
                        p.act(sgt[:, 0:W], pg[:, 0:W], AF.Silu)
                        p.tt('dve', a[:, c, 0:W], sgt[:, 0:W], pu[:, 0:W], ALU.mult)
                    for j in range(n):
                        t = t0 + j
                        for fh in range(2):
                            pd = ps[4 + ((j * 2 + fh) % 4)]
                            for c in range(BLK):
                                p.mm(pd, a[:, c, j * 128:(j + 1) * 128], wdb[s][:, c, fh * 512:(fh + 1) * 512], start=(c == 0), stop=(c == BLK - 1))
                            dst = acc[:, t, fh * 512:(fh + 1) * 512]
                            gsc = gates[:, t, e:e + 1] if (E > 1 and DBG not in ('norouter', 'nogate', 'e1only', 'e0only')) else 1.0
                            if first:
                                if E > 1 and DBG not in ('norouter', 'nogate', 'e1only', 'e0only'):
                                    p.ts('dve', dst, pd, gsc, ALU.mult)
                                else:
                                    p.copy('dve', dst, pd)
                            else:
                                p.stt('dve', dst, pd, gsc, dst, ALU.mult, ALU.add)
                first = False
        G2 = [p.sb(f"G2_{v}", [128, D], F32, st) for v in range(nvar)]
        ng3 = p.sb("ng3", [128, D], F32, st)
        bcast_row(ng3, ng_d.with_ap(ng_d.ap[3:4, :].partition_broadcast(128)))
        for v in range(nvar):
            bcast_row(G2[v], mod_s.with_ap(mod_s.ap[v:v + 1, 5 * D:6 * D].partition_broadcast(128)))
            p.tt('dve', G2[v], G2[v], ng3, ALU.mult)
        x1t = [p.sb(f"x1t{i}", [128, D], F32, st) for i in range(2)]
        sq2 = p.sb("sq2", [128, D], F32, st)
        cols2 = p.sb("cols2", [128, NT], F32, st)
        for t in range(NT):
            v = 1 if (has_ctx and t == NT - 1) else 0
            x1 = x1t[t % 2]
            p.dma('sp', x1, x1_s[t * 128:(t + 1) * 128, :])
            rc = cols2[:, t:t + 1]
            rms_rstd(rc, acc[:, t, :], sq2)
            p.stt('dve', acc[:, t, :], acc[:, t, :], rc, G2[v], ALU.mult, ALU.mult)
            if DBG == 'x1':
                pass
            elif DBG == 'f':
                p.copy('pool', x1, acc[:, t, :])
            else:
                p.tt('pool', x1, x1, acc[:, t, :], ALU.add)
            p.dma('sp', out_d[t * 128:(t + 1) * 128, :], x1)
    p.finalize()
    return nc


def _cT(c_b, c_ctx):
    a = np.stack([c_b, c_ctx], axis=-1).astype(np.float32)
    return np.ascontiguousarray(a.reshape(8, 128, 2).transpose(1, 0, 2))


_IDENT = np.eye(128, dtype=np.float32)


def run_post(layer, x_lat, ctx, mix_lat, mix_ctx, c, c_ctx, mod_w, mod_b, norm_g, w_out, wg, wu, wd, router, has_ctx, E, H, BLK):
    NT = 17 if has_ctx else 16
    nc = build_post(NT, E, H, BLK, has_ctx)
    in_maps = []
    for core in range(8):
        b, q = core // 4, core % 4
        xs = [x_lat[b, q * 2048:(q + 1) * 2048]]
        ms = [mix_lat[b, q * 2048:(q + 1) * 2048]]
        if has_ctx:
            cs = (q % 2) * 128
            xs.append(ctx[b, cs:cs + 128])
            ms.append(mix_ctx[b, cs:cs + 128])
        xo = np.ascontiguousarray(np.concatenate(xs, 0), dtype=np.float32)
        mo = np.ascontiguousarray(np.concatenate(ms, 0).T, dtype=np.float32)
        in_maps.append({
            "x": xo, "mixT": mo, "cT": _cT(c[b], c_ctx),
            "mod_w": np.ascontiguousarray(mod_w[layer]), "mod_b": np.ascontiguousarray(np.stack([mod_b[layer]] * 2)),
            "norm_g": np.ascontiguousarray(norm_g[layer]), "w_out": np.ascontiguousarray(w_out),
            "wg": np.ascontiguousarray(wg), "wu": np.ascontiguousarray(wu), "wd": np.ascontiguousarray(wd),
            "router": np.ascontiguousarray(np.concatenate([router, np.zeros((1024, 120), np.float32)], 1)), "ident": _IDENT,
        })
    res = run_bass_kernel_spmd(nc, in_maps, core_ids=list(range(8)), trace=TRACE)
    if TRACE:
        print("DEV_NS", res.exec_time_ns)
    x2 = np.empty_like(x_lat)
    ctx2 = np.empty_like(ctx) if has_ctx else None
    for core in range(8):
        b, q = core // 4, core % 4
        o = res.results[core]["out"]
        x2[b, q * 2048:(q + 1) * 2048] = o[0:2048]
        if has_ctx and q < 2:
            ctx2[b, q * 128:(q + 1) * 128] = o[2048:2176]
    return x2, ctx2


def mod_rows(p, ps, st, cT_d, modw_d, modb_d, mod_s, blocks):
    cT = p.sb("cT", [128, 8, 2], F32, st)
    sc = p.sb("silu_c", [128, 8, 2], F32, st)
    p.dma('sp', cT, cT_d)
    p.act(sc, cT, AF.Silu)
    lo, hi = blocks[0] * 512, (blocks[-1] + 1) * 512
    modrow = p.sb("modrow", [2, 6 * D], F32, st)
    modb = p.sb("modb", [2, 6 * D], F32, st)
    p.dma('sp', modb, modb_d)
    wblk = [p.sb(f"modw{i}", [128, 8, 512], F32, st) for i in range(2)]
    for cb in blocks:
        wb = wblk[cb % 2]
        p.dma('sp', wb, modw_d.with_ap(modw_d.ap.rearrange("(k p) n -> p k n", p=128)[:, :, cb * 512:(cb + 1) * 512]))
        pp = ps[cb % 2]
        for k in range(8):
            p.mm(pp[0:2, :], sc[:, k, :], wb[:, k, :], start=(k == 0), stop=(k == 7))
        p.tt('dve', modrow[:, cb * 512:(cb + 1) * 512], pp[0:2, :], modb[:, cb * 512:(cb + 1) * 512], ALU.add)
    p.dma('sp', mod_s[:, lo:hi], modrow[:, lo:hi])


def rope_tm(p, dst1, dst2, x1, x2, cos, sin, t1, t2, e1='dve', e2='pool'):
    p.tt(e1, t1, x1, cos, ALU.mult)
    p.tt(e2, t2, x2, sin, ALU.mult)
    p.tt(e1, t1, t1, t2, ALU.subtract)
    p.tt(e2, t2, x1, sin, ALU.mult)
    p.tt(e1, dst2, x2, cos, ALU.mult)
    p.tt(e1, dst2, dst2, t2, ALU.add)
    p.copy(e2, dst1, t1)


NTA = 66


def build_mix0():
    nc = bass.Bass("TRN2", target_bir_lowering=False)
    p = Prog(nc)
    NTOK = NTA * 128
    x_d = p.dram("x", [NTOK, D], F32, "ExternalInput")
    cT_d = p.dram("cT", [128, 8, 2], F32, "ExternalInput")
    modw_d = p.dram("mod_w", [D, 6 * D], F32, "ExternalInput")
    modb_d = p.dram("mod_b", [2, 6 * D], F32, "ExternalInput")
    ng_d = p.dram("norm_g", [4, D], F32, "ExternalInput")
    win_d = p.dram("w_in", [D, 896], F32, "ExternalInput")
    cos_d = p.dram("cos", [NTOK, 64], F32, "ExternalInput")
    sin_d = p.dram("sin", [NTOK, 64], F32, "ExternalInput")
    dec_d = p.dram("dec", [128, 2], F32, "ExternalInput")
    gb_d = p.dram("gb", [3, 128, 128], F32, "ExternalInput")
    cst_d = p.dram("cst", [6, 128, 128], F32, "ExternalInput")
    out_d = p.dram("mixT", [256, NTOK], F32, "ExternalOutput")
    mod_s = p.dram("mod_s", [2, 6 * D], F32, "Internal")
    qa_s = p.dram("qa_s", [128, NTOK], BF16, "Internal")
    ka_s = p.dram("ka_s", [128, NTOK], BF16, "Internal")
    va_s = p.dram("va_s", [128, NTA, 128], BF16, "Internal")

    ps = [p.ps(f"ps{i}", [128, 512]) for i in range(8)]
    cst = p.sb("cst", [128, 6, 128], F32)
    p.dma('sp', cst, cst_d.with_ap(cst_d.ap.rearrange("c p n -> p c n")))
    ident = cst[:, 0, :]
    gb = p.sb("gb", [128, 3, 128], F32)
    p.dma('sp', gb, gb_d.with_ap(gb_d.ap.rearrange("c p n -> p c n")))
    epsc = p.sb("epsc", [128, 1], F32)
    p.memset('dve', epsc, NORM_EPS)
    ones_bf = p.sb("ones_bf", [128, 128], BF16)
    p.memset('dve', ones_bf, 1.0)
    dec = p.sb("dec", [128, 2], F32)
    p.dma('sp', dec, dec_d)
    lg = p.sb("lg", [128, 2], F32)
    p.act(lg, dec, AF.Exp, scale=-math.log(2.0))
    p.act(lg, lg, AF.Ln, scale=-1.0, bias=1.0)
    MT = p.sb("MT", [128, 2, 128], F32)
    dcol = p.sb("dcol", [128, 8], F32)
    p.act(MT[:, 0, :], cst[:, 1, :], AF.Exp, scale=lg[:, 0:1])
    p.tt('dve', MT[:, 0, :], MT[:, 0, :], cst[:, 3, :], ALU.mult)
    p.act(MT[:, 1, :], cst[:, 2, :], AF.Exp, scale=lg[:, 1:2])
    p.tt('dve', MT[:, 1, :], MT[:, 1, :], cst[:, 4, :], ALU.mult)
    colc = cst[:, 5, :]
    p.act(dcol[:, 0:1], colc[:, 0:1], AF.Exp, scale=lg[:, 0:1])
    p.act(dcol[:, 1:2], colc[:, 1:2], AF.Exp, scale=lg[:, 1:2])
    p.act(dcol[:, 2:3], colc[:, 2:3], AF.Exp, scale=lg[:, 0:1])
    p.act(dcol[:, 3:4], colc[:, 3:4], AF.Exp, scale=lg[:, 1:2])
    p.act(dcol[:, 4:5], colc[:, 4:5], AF.Exp, scale=lg[:, 0:1])
    p.act(dcol[:, 5:6], colc[:, 4:5], AF.Exp, scale=lg[:, 1:2])

    QrT = p.sb("QrT", [128, NTOK], BF16)
    KrT = p.sb("KrT", [128, NTOK], BF16)
    Vr = p.sb("Vr", [128, NTA, 128], BF16)
    Kd = [p.sb(f"Kd{d}", [128, NTA, 128], BF16) for d in range(2)]
    Gs = p.sb("Gs", [128, NTA, 128], BF16)

    with p.scope() as st0:
        with p.scope() as st:
            mod_rows(p, ps, st, cT_d, modw_d, modb_d, mod_s, [0, 1, 2, 3])
        st = st0
        A1 = [p.sb(f"A1_{v}", [128, D], F32, st) for v in range(2)]
        B1 = [p.sb(f"B1_{v}", [128, D], F32, st) for v in range(2)]
        ngb = p.sb("ngb", [128, D], F32, st)
        p.dma('sp', ngb, ng_d.with_ap(ng_d.ap[0:1, :].partition_broadcast(128)))
        for v in range(2):
            p.dma('sp', B1[v], mod_s.with_ap(mod_s.ap[v:v + 1, 0:D].partition_broadcast(128)))
            p.dma('sp', A1[v], mod_s.with_ap(mod_s.ap[v:v + 1, D:2 * D].partition_broadcast(128)))
            p.ts('dve', A1[v], A1[v], 1.0, ALU.add)
            p.tt('dve', A1[v], A1[v], ngb, ALU.mult)
        W = p.sb("W", [128, 8, 896], BF16, st)
        wv = win_d.ap.rearrange("(k p) n -> p k n", p=128)
        for k in range(8):
            p.dma('pool', W[:, k, :], win_d.with_ap(wv[:, k, :]))
        xt = [p.sb(f"xt{i}", [128, D], F32, st) for i in range(2)]
        tm = p.sb("tm", [128, D], F32, st)
        sq = p.sb("sq", [128, D], F32, st)
        hT = [p.sb(f"hT{i}", [128, 8, 128], BF16, st) for i in range(2)]
        cs = [p.sb(f"cs{i}", [128, 2, 64], F32, st) for i in range(2)]
        pr = [p.sb(f"pr{i}", [128, 896], F32, st) for i in range(2)]
        ro = [p.sb(f"ro{i}", [128, 4, 128], F32, st) for i in range(2)]
        rt = p.sb("rt", [128, 4, 64], F32, st)
        cols = p.sb("cols", [128, NTA, 4], F32, st)
        stg = [p.sb(f"stg{i}", [128, 3, 128], BF16, st) for i in range(2)]
        for t in range(NTA):
            v = 1 if t < 2 else 0
            x_t = xt[t % 2]
            p.dma('sp', x_t, x_d[t * 128:(t + 1) * 128, :])
            c_t = cs[t % 2]
            p.dma('sp', c_t[:, 0, :], cos_d[t * 128:(t + 1) * 128, :])
            p.dma('sp', c_t[:, 1, :], sin_d[t * 128:(t + 1) * 128, :])
            rc = cols[:, t, 0:1]
            p.act(sq, x_t, AF.Square)
            p.reduce('dve', rc, sq, ALU.add)
            p.act(rc, rc, AF.Sqrt, bias=epsc, scale=1.0 / D)
            p.recip(rc, rc)
            p.stt('dve', tm, x_t, rc, A1[v], ALU.mult, ALU.mult)
            p.tt('pool', tm, tm, B1[v], ALU.add)
            h = hT[t % 2]
            for k in range(8):
                pt = ps[2 + (k % 4)]
                p.tr(pt[:, 0:128], tm[:, k * 128:(k + 1) * 128], ident)
                p.copy('act' if k % 2 else 'dve', h[:, k, :], pt[:, 0:128])
            for k in range(8):
                p.mm(ps[0], h[:, k, :], W[:, k, 0:512], start=(k == 0), stop=(k == 7))
            for k in range(8):
                p.mm(ps[1][:, 0:384], h[:, k, :], W[:, k, 512:896], start=(k == 0), stop=(k == 7))
            P = pr[t % 2]
            p.copy('act', P[:, 0:512], ps[0])
            p.copy('dve', P[:, 512:896], ps[1][:, 0:384])
            p.ts('pool', P[:, 128:256], P[:, 128:256], 128.0 ** -0.5, ALU.mult)
            p.copy('pool', Vr[:, t, :], P[:, 256:384])
            p.act(Gs[:, t, :], P[:, 384:512], AF.Silu)
            sg = stg[t % 2]
            p.copy('pool', sg[:, 2, :], P[:, 768:896])
            p.dma('sp', va_s[:, t, :], sg[:, 2, :])
            for i, (c0, gi) in enumerate(((512, 1), (640, 2))):
                rcq = cols[:, t, 1 + i:2 + i]
                p.act(sq[:, 0:128], P[:, c0:c0 + 128], AF.Square)
                p.reduce('dve', rcq, sq[:, 0:128], ALU.add)
                p.act(rcq, rcq, AF.Sqrt, bias=epsc, scale=1.0 / 128)
                p.recip(rcq, rcq)
                p.stt('dve', P[:, c0:c0 + 128], P[:, c0:c0 + 128], rcq, gb[:, gi, :], ALU.mult, ALU.mult)
            R = ro[t % 2]
            for i, c0 in enumerate((0, 128, 512, 640)):
                rope_tm(p, R[:, i, 0:64], R[:, i, 64:128], P[:, c0:c0 + 64], P[:, c0 + 64:c0 + 128],
                        c_t[:, 0, :], c_t[:, 1, :], rt[:, i, :], sq[:, 128 + 64 * i:192 + 64 * i],
                        e1='dve' if i % 2 == 0 else 'pool', e2='pool' if i % 2 == 0 else 'dve')
            p.ts('dve', Kd[0][:, t, :], R[:, 1, :], dcol[:, 2:3], ALU.mult)
            p.ts('pool', Kd[1][:, t, :], R[:, 1, :], dcol[:, 3:4], ALU.mult)
            ts_ = slice(t * 128, (t + 1) * 128)
            for i in range(4):
                pt = ps[2 + i]
                p.tr(pt[:, 0:128], R[:, i, :], ident)
                if i == 0:
                    p.copy('act', QrT[:, ts_], pt[:, 0:128])
                elif i == 1:
                    p.copy('dve', KrT[:, ts_], pt[:, 0:128])
                else:
                    p.copy('act' if i == 2 else 'dve', sg[:, i - 2, :], pt[:, 0:128])
            p.dma('sp', qa_s[:, ts_], sg[:, 0, :])
            p.dma('sp', ka_s[:, ts_], sg[:, 1, :])

    with p.scope() as st:
        Y = p.sb("Y", [128, NTA, 128], F32, st)
        S = p.sb("S", [128, 128], F32, st)
        Sb = p.sb("Sb", [128, 128], BF16, st)
        AT = [p.sb(f"AT{i}", [128, 128], BF16, st) for i in range(2)]
        for d in range(2):
            order = list(range(NTA)) if d == 0 else [1, 0] + list(range(NTA - 1, 1, -1))
            p.memset('dve', S, 0.0)
            p.memset('pool', Sb, 0.0)
            for n, c in enumerate(order):
                cs_ = slice(c * 128, (c + 1) * 128)
                pS, pP1, pP2, pKV = ps[n % 2], ps[2 + n % 2], ps[4 + n % 2], ps[6 + n % 2]
                p.mm(pS[:, 0:128], KrT[:, cs_], QrT[:, cs_])
                a = AT[n % 2]
                p.tt('dve', a, pS[:, 0:128], MT[:, d, :], ALU.mult)
                p.mm(pP1[:, 0:128], a, Vr[:, c, :])
                p.mm(pP2[:, 0:128], QrT[:, cs_], Sb)
                if d == 0:
                    p.copy('act', Y[:, c, :], pP1[:, 0:128])
                else:
                    p.tt('pool' if False else 'dve', Y[:, c, :], pP1[:, 0:128], Y[:, c, :], ALU.add)
                p.stt('dve', Y[:, c, :], pP2[:, 0:128], dcol[:, d:d + 1], Y[:, c, :], ALU.mult, ALU.add)
                p.mm(pKV[:, 0:128], Kd[d][:, c, :], Vr[:, c, :])
                p.stt('dve', S, S, dcol[:, 4 + d:5 + d], pKV[:, 0:128], ALU.mult, ALU.add)
                p.copy('act', Sb, S)
        OUT = p.sb("OUT", [128, NTOK], F32, st)
        sq2 = p.sb("sq2", [128, 128], F32, st)
        cols2 = p.sb("cols2", [128, NTA], F32, st)
        for c in range(NTA):
            rc = cols2[:, c:c + 1]
            p.act(sq2, Y[:, c, :], AF.Square)
            p.reduce('dve', rc, sq2, ALU.add)
            p.act(rc, rc, AF.Sqrt, bias=epsc, scale=1.0 / 128)
            p.recip(rc, rc)
            p.stt('dve', Y[:, c, :], Y[:, c, :], rc, gb[:, 0, :], ALU.mult, ALU.mult)
            p.tt('pool', Y[:, c, :], Y[:, c, :], Gs[:, c, :], ALU.mult)
            pt = ps[c % 4]
            p.tr(pt[:, 0:128], Y[:, c, :], ident)
            p.copy('act', OUT[:, c * 128:(c + 1) * 128], pt[:, 0:128])
        p.dma('sp', out_d[0:128, :], OUT)

    with p.scope() as st:
        QaT = p.sb("QaT", [128, NTOK], BF16, st)
        KaT = p.sb("KaT", [128, NTOK], BF16, st)
        Va = p.sb("Va", [128, NTA, 128], BF16, st)
        p.dma('sp', QaT, qa_s)
        p.dma('sp', KaT, ka_s)
        p.dma('sp', Va, va_s)
        g2 = p.sb("g2", [128, 2, 128], F32, st)
        mx = p.sb("mx", [128, 4], F32, st)
        p.act(g2[:, 0, :], gb[:, 1, :], AF.Square)
        p.act(g2[:, 1, :], gb[:, 2, :], AF.Square)
        p.reduce('dve', mx[:, 0:1], g2[:, 0, :], ALU.max)
        p.reduce('dve', mx[:, 1:2], g2[:, 1, :], ALU.max)
        p.tt('dve', mx[:, 2:3], mx[:, 0:1], mx[:, 1:2], ALU.mult)
        p.act(mx[:, 3:4], mx[:, 2:3], AF.Sqrt, scale=128.0)
        p.ts('dve', mx[:, 3:4], mx[:, 3:4], -1.0, ALU.mult)
        negC = mx[:, 3:4]
        PT = [p.sb(f"PT{i}", [128, 512], BF16, st) for i in range(3)]
        rec = [p.sb(f"rec{i}", [128, 512], F32, st) for i in range(2)]
        og = [p.sb(f"og{i}", [128, 512], F32, st) for i in range(2)]
        groups = [(0, 256, [0, 1])] + [(256 + g * 512, 512, list(range(NTA))) for g in range(16)]
        it = 0
        for gi, (q0, W_, keys) in enumerate(groups):
            pO, pD = ps[4 + (gi % 2) * 2], ps[5 + (gi % 2) * 2]
            for n, kt in enumerate(keys):
                pS = ps[it % 3]
                pt_ = PT[it % 3]
                it += 1
                p.mm(pS[:, 0:W_], KaT[:, kt * 128:(kt + 1) * 128], QaT[:, q0:q0 + W_])
                p.act(pt_[:, 0:W_], pS[:, 0:W_], AF.Exp, bias=negC, scale=128.0 ** -0.5)
                p.mm(pO[:, 0:W_], Va[:, kt, :], pt_[:, 0:W_], start=(n == 0), stop=(n == len(keys) - 1))
                p.mm(pD[:, 0:W_], ones_bf, pt_[:, 0:W_], start=(n == 0), stop=(n == len(keys) - 1))
            r, o = rec[gi % 2], og[gi % 2]
            p.recip(r[:, 0:W_], pD[:, 0:W_])
            p.tt('dve', o[:, 0:W_], pO[:, 0:W_], r[:, 0:W_], ALU.mult)
            p.dma('sp', out_d[128:256, q0:q0 + W_], o[:, 0:W_])
    p.finalize()
    return nc


def _rope_tables(dim, rows=128, grid_w=64, theta=10000.0):
    row = np.repeat(np.arange(rows, dtype=np.float32), grid_w)
    col = np.tile(np.arange(grid_w, dtype=np.float32), rows)
    n_freq = dim // 4
    freqs = (np.float32(theta) ** (-np.arange(n_freq, dtype=np.float32) / np.float32(n_freq))).astype(np.float32)
    ang = np.concatenate([row[:, None] * freqs, col[:, None] * freqs], axis=-1).astype(np.float32)
    return np.cos(ang).astype(np.float32), np.sin(ang).astype(np.float32)


def _with_ctx_rope(cos, sin):
    n = cos.shape[1]
    c = np.concatenate([np.ones((256, n), np.float32), cos], 0)
    s = np.concatenate([np.zeros((256, n), np.float32), sin], 0)
    return np.ascontiguousarray(c), np.ascontiguousarray(s)


def _mix0_consts():
    ip = np.arange(128, dtype=np.float32)[:, None]
    i = np.arange(128, dtype=np.float32)[None, :]
    cst = np.zeros((6, 128, 128), np.float32)
    cst[0] = np.eye(128, dtype=np.float32)
    cst[1] = np.maximum(i - ip, 0)
    cst[2] = np.maximum(ip - i, 0)
    cst[3] = (i >= ip)
    cst[4] = (ip > i)
    cst[5, :, 0] = ip[:, 0] + 1
    cst[5, :, 1] = 128 - ip[:, 0]
    cst[5, :, 2] = 127 - ip[:, 0]
    cst[5, :, 3] = ip[:, 0]
    cst[5, :, 4] = 128
    return cst


def run_mix0(x, ctx, c, c_ctx, mod_w, mod_b, norm_g, w_in, decay_exp, ret_g, q_g, k_g):
    nc = build_mix0()
    cos, sin = _with_ctx_rope(*_rope_tables(128))
    cst = _mix0_consts()
    in_maps = []
    for core in range(8):
        b, j = core // 4, core % 4
        kv = j // 2
        colsel = np.concatenate([np.arange(j * 128, (j + 1) * 128) + off for off in (0, 512, 1024, 1536, 2048)] +
                                [np.arange(kv * 128, (kv + 1) * 128) + off for off in (2560, 2816)])
        gb = np.stack([np.broadcast_to(ret_g[j * 128:(j + 1) * 128], (128, 128)),
                       np.broadcast_to(q_g, (128, 128)), np.broadcast_to(k_g, (128, 128))]).astype(np.float32)
        in_maps.append({
            "x": np.ascontiguousarray(np.concatenate([ctx[b], x[b]], 0), dtype=np.float32),
            "cT": _cT(c[b], c_ctx), "mod_w": np.ascontiguousarray(mod_w), "mod_b": np.ascontiguousarray(np.stack([mod_b] * 2)),
            "norm_g": np.ascontiguousarray(norm_g), "w_in": np.ascontiguousarray(w_in[:, colsel]),
            "cos": cos, "sin": sin,
            "dec": np.ascontiguousarray(np.broadcast_to(decay_exp[:, j], (128, 2)), dtype=np.float32),
            "gb": np.ascontiguousarray(gb), "cst": cst,
        })
    res = run_bass_kernel_spmd(nc, in_maps, core_ids=list(range(8)), trace=TRACE)
    if TRACE:
        print("DEV_NS", res.exec_time_ns)
    mix = np.empty((2, NTA * 128, 1024), np.float32)
    for core in range(8):
        b, j = core // 4, core % 4
        o = res.results[core]["mixT"]
        mix[b, :, j * 128:(j + 1) * 128] = o[0:128].T
        mix[b, :, 512 + j * 128:512 + (j + 1) * 128] = o[128:256].T
    return mix[:, 256:], mix[:, :256]


LAM_INIT1 = 0.8 - 0.6 * math.exp(-0.3 * 1)
RWKV_LN_EPS = 64e-5
C_ID, C_TRI0, C_TRI1, C_MS0, C_MI0, C_MS1, C_SH, C_SHP, C_SHN, C_ONE = range(10)
B_W00, B_W01, B_A00, B_A01, B_KK, B_KA, B_RK, B_LNG, B_LNB = range(9)


def build_mix1():
    nc = bass.Bass("TRN2", target_bir_lowering=False)
    p = Prog(nc)
    NTOK = NTA * 128
    NLAT = 64 * 128
    x_d = p.dram("x", [NTOK, D], F32, "ExternalInput")
    cT_d = p.dram("cT", [128, 8, 2], F32, "ExternalInput")
    modw_d = p.dram("mod_w", [D, 6 * D], F32, "ExternalInput")
    modb_d = p.dram("mod_b", [2, 6 * D], F32, "ExternalInput")
    ng_d = p.dram("norm_g", [4, D], F32, "ExternalInput")
    win_d = p.dram("w_in", [D, 1152], F32, "ExternalInput")
    mu_d = p.dram("mu", [128, 768], F32, "ExternalInput")
    cos_d = p.dram("cos", [NTOK, 32], F32, "ExternalInput")
    sin_d = p.dram("sin", [NTOK, 32], F32, "ExternalInput")
    lam_d = p.dram("lamv", [128, 4, 64], F32, "ExternalInput")
    sg_d = p.dram("subg", [128, 1], F32, "ExternalInput")
    bc_d = p.dram("bc", [9, 128, 128], F32, "ExternalInput")
    mat_d = p.dram("mats", [3, 128, 128], F32, "ExternalInput")
    cst_d = p.dram("cst", [10, 128, 128], F32, "ExternalInput")
    out_d = p.dram("mixT", [256, NLAT], F32, "ExternalOutput")
    mod_s = p.dram("mod_s", [2, 6 * D], F32, "Internal")
    qd_s = p.dram("qd_s", [128, NTOK], BF16, "Internal")
    kd_s = p.dram("kd_s", [128, NTOK], BF16, "Internal")
    vd_s = p.dram("vd_s", [128, NTA, 128], BF16, "Internal")
    st_s = p.dram("st_s", [NTA, 128, 768], F32, "Internal")

    ps = [p.ps(f"ps{i}", [128, 512]) for i in range(8)]
    cst = p.sb("cst", [128, 10, 128], F32)
    p.dma('sp', cst, cst_d.with_ap(cst_d.ap.rearrange("c p n -> p c n")))
    cstb = p.sb("cstb", [128, 10, 128], BF16)
    p.copy('dve', cstb, cst)
    ident = cst[:, C_ID, :]
    bc = p.sb("bc", [128, 9, 128], F32)
    p.dma('sp', bc, bc_d.with_ap(bc_d.ap.rearrange("c p n -> p c n")))
    mats = p.sb("mats", [128, 3, 128], F32)
    p.dma('sp', mats, mat_d.with_ap(mat_d.ap.rearrange("c p n -> p c n")))
    epsc = p.sb("epsc", [128, 2], F32)
    p.memset('dve', epsc[:, 0:1], NORM_EPS)
    p.memset('dve', epsc[:, 1:2], RWKV_LN_EPS)
    ones_bf = cstb[:, C_ONE, :]
    Yacc = p.sb("Yacc", [128, NTA, 128], F32)
    Vst = p.sb("Vst", [128, NTA, 128], BF16)
    Gst = p.sb("Gst", [128, NTA, 128], BF16)
    BCf = p.sb("BCf", [128, NTA, 2], F32)
    ST = [[p.sb(f"ST{d}{h}", [64, 64], F32) for h in range(2)] for d in range(2)]
    YH = [Yacc, Yacc]

    _tmpn = [0]

    def scan_step(st, d, t, r, kd, v, kk, b, logw, first):
        T_ = TMP
        TRI = cst[:, C_TRI0 + d, :]
        MS = cst[:, C_MS0 if d == 0 else C_MS1, :]
        MST = cst[:, C_MS1 if d == 0 else C_MS0, :]
        MA = cst[:, C_MI0 if d == 0 else C_MS1, :]
        pc, pt = ps[0], ps[1]
        p.mm(pc[:, 0:128], TRI, logw)
        p.mm(pt[:, 0:128], cst[:, C_ONE, :], logw)
        cum = T_['cum']
        p.copy('act', cum, pc[:, 0:128])
        e_neg, e_x, e_in, e_end = T_['e_neg'], T_['e_x'], T_['e_in'], T_['e_end']
        p.act(e_neg, cum, AF.Exp, scale=-1.0)
        p.tt('dve', e_x, cum, logw, ALU.subtract)
        p.act(e_x, e_x, AF.Exp)
        if d == 0:
            p.act(e_in, cum, AF.Exp)
        p.tt('dve', e_end, pt[:, 0:128], cum, ALU.subtract)
        p.act(e_end, e_end, AF.Exp)
        at, bt, kt, rt, Bp, Kp = T_['at'], T_['bt'], T_['kt'], T_['rt'], T_['Bp'], T_['Kp']
        p.stt('dve', at, kk, -1.0, e_x, ALU.mult, ALU.mult)
        p.tt('pool', bt, b, e_neg, ALU.mult)
        p.tt('pool', kt, kd, e_neg, ALU.mult)
        p.tt('dve', rt, r, e_in if d == 0 else e_x, ALU.mult)
        p.tt('pool', Bp, b, e_end, ALU.mult)
        p.tt('pool', Kp, kd, e_end, ALU.mult)
        def chain(hh):
            banks = [ps[2 + hh], ps[4 + hh], ps[6 + hh]]
            bi = [0]

            def nb():
                bk = banks[bi[0] % 3]
                bi[0] += 1
                return bk

            hs_ = slice(hh * 64, (hh + 1) * 64)
            fm = {}
            for i, (nm, src) in enumerate((('at', at), ('bt', bt), ('kt', kt), ('rt', rt))):
                pp = nb()
                p.tr(pp[0:64, 0:128], src[:, hs_], ident)
                dst = T_[f'{nm}T{hh}']
                p.copy('act' if i % 2 else 'dve', dst, pp[0:64, 0:128])
                fm[nm] = dst
                if i % 2:
                    yield
            wc = T_[f'wc{hh}']
            pw_ = nb()
            p.mm(pw_[0:64, 0:1], logw[:, hs_], cst[:, C_ONE, 0:1])
            p.act(wc, pw_[0:64, 0:1], AF.Exp)
            LT, L, LakT, ArbT, ArkT = (T_[f'{n_}{hh}'] for n_ in ('LT', 'L', 'LakT', 'ArbT', 'ArkT'))
            for i, (dst, lhs, rhs, msk) in enumerate(((LT, fm['bt'], fm['at'], MS), (L, fm['at'], fm['bt'], MST),
                                                       (LakT, fm['kt'], fm['at'], MS), (ArbT, fm['bt'], fm['rt'], MA),
                                                       (ArkT, fm['kt'], fm['rt'], MA))):
                pp = nb()
                p.mm(pp[:, 0:128], lhs, rhs)
                p.tt('dve', dst, pp[:, 0:128], msk, ALU.mult)
                if i in (1, 4):
                    yield
            G, Pa, PaT, Pb, PbT = (T_[f'{n_}{hh}'] for n_ in ('G', 'Pa', 'PaT', 'Pb', 'PbT'))
            p.tt('pool', G, LT, ident, ALU.add)
            cur, curT = L, LT
            nxt = [(Pa, PaT), (Pb, PbT)]
            for lev in range(1, 7):
                Pn, PnT = nxt[lev % 2]
                pp = nb()
                p.mm(pp[:, 0:128], curT, cur)
                if lev < 6:
                    pp2 = nb()
                    p.mm(pp2[:, 0:128], cur, curT)
                p.copy('act', Pn, pp[:, 0:128])
                if lev < 6:
                    p.copy('dve', PnT, pp2[:, 0:128])
                yield
                pq = nb()
                p.mm(pq[:, 0:128], Pn, G)
                p.tt('dve', G, pq[:, 0:128], G, ALU.add)
                cur, curT = Pn, PnT
                yield
            S = ST[d][hh]
            X, U = T_[f'X{hh}'], T_[f'U{hh}']
            px = nb()
            vh = v[:, hs_]
            p.mm(px[:, 0:64], fm['at'], S, start=True, stop=False)
            p.mm(px[:, 0:64], LakT, vh, start=False, stop=True)
            p.copy('act', X, px[:, 0:64])
            yield
            pu = nb()
            p.mm(pu[:, 0:64], G, X)
            p.copy('dve', U, pu[:, 0:64])
            yield
            py = nb()
            p.mm(py[:, 0:64], fm['rt'], S, start=True, stop=False)
            p.mm(py[:, 0:64], ArbT, U, start=False, stop=False)
            p.mm(py[:, 0:64], ArkT, vh, start=False, stop=True)
            pss = nb()
            p.mm(pss[0:64, 0:64], Bp[:, hs_], U, start=True, stop=False)
            p.mm(pss[0:64, 0:64], Kp[:, hs_], vh, start=False, stop=True)
            ydst = YH[hh][:, t, hs_]
            if first:
                p.copy('act', ydst, py[:, 0:64])
            else:
                p.tt('dve', ydst, py[:, 0:64], ydst, ALU.add)
            p.stt('dve', S, S, wc, pss[0:64, 0:64], ALU.mult, ALU.add)
            yield

        live = [chain(0), chain(1)]
        while live:
            for g_ in list(live):
                try:
                    next(g_)
                except StopIteration:
                    live.remove(g_)

    with p.scope() as st0:
        with p.scope() as st:
            mod_rows(p, ps, st, cT_d, modw_d, modb_d, mod_s, [0, 1, 2, 3])
        st = st0
        TMP = {}
        for nm in ('cum', 'e_neg', 'e_x', 'e_in', 'e_end', 'at', 'bt', 'kt', 'rt', 'Bp', 'Kp'):
            TMP[nm] = p.sb("t_" + nm, [128, 128], F32, st)
        for hh in range(2):
            for nm in ('LT', 'L', 'LakT', 'ArbT', 'ArkT', 'G', 'Pa', 'PaT', 'Pb', 'PbT'):
                TMP[f'{nm}{hh}'] = p.sb(f"t_{nm}{hh}", [128, 128], F32, st)
            for nm in ('X', 'U'):
                TMP[f'{nm}{hh}'] = p.sb(f"t_{nm}{hh}", [128, 64], F32, st)
        for hh in range(2):
            for nm in ('at', 'bt', 'kt', 'rt'):
                TMP[f'{nm}T{hh}'] = p.sb(f"t_{nm}T{hh}", [64, 128], F32, st)
            TMP[f'wc{hh}'] = p.sb(f"t_wc{hh}", [64, 1], F32, st)
        for d in range(2):
            for hh in range(2):
                p.memset('dve', ST[d][hh], 0.0)
        with p.scope() as st:
            A1 = [p.sb(f"A1_{v}", [128, D], F32, st) for v in range(2)]
            B1 = [p.sb(f"B1_{v}", [128, D], F32, st) for v in range(2)]
            ngb = p.sb("ngb", [128, D], F32, st)
            p.dma('sp', ngb, ng_d.with_ap(ng_d.ap[0:1, :].partition_broadcast(128)))
            for v in range(2):
                p.dma('sp', B1[v], mod_s.with_ap(mod_s.ap[v:v + 1, 0:D].partition_broadcast(128)))
                p.dma('sp', A1[v], mod_s.with_ap(mod_s.ap[v:v + 1, D:2 * D].partition_broadcast(128)))
                p.ts('dve', A1[v], A1[v], 1.0, ALU.add)
                p.tt('dve', A1[v], A1[v], ngb, ALU.mult)
            Wd = p.sb("Wd", [128, 8, 384], BF16, st)
            W1 = p.sb("W1", [128, 8, 768], BF16, st)
            W2 = p.sb("W2", [128, 8, 768], BF16, st)
            mu = p.sb("mu", [128, 2, 768], F32, st)
            p.dma('sp', mu[:, 0, :], mu_d)
            p.ts('dve', mu[:, 1, :], mu[:, 0, :], 0.5, ALU.mult)
            p.ts('dve', mu[:, 0, :], mu[:, 0, :], -1.0, ALU.mult, 1.0, ALU.add)
            wv = win_d.ap.rearrange("(k p) n -> p k n", p=128)
            wst = [p.sb(f"wst{i}", [128, 768], F32, st) for i in range(2)]
            for k in range(8):
                p.dma('pool', Wd[:, k, :], win_d.with_ap(wv[:, k, 0:384]))
                w_ = wst[k % 2]
                p.dma('sp', w_, win_d.with_ap(wv[:, k, 384:1152]))
                p.tt('dve', W1[:, k, :], w_, mu[:, 0, :], ALU.mult)
                p.tt('pool', W2[:, k, :], w_, mu[:, 1, :], ALU.mult)
            xt = [p.sb(f"xt{i}", [128, D], F32, st) for i in range(2)]
            tm = p.sb("tm", [128, D], F32, st)
            sq = p.sb("sq", [128, D], F32, st)
            hb = [p.sb(f"hb{i}", [128, D], BF16, st) for i in range(3)]
            hT = p.sb("hT", [128, 8, 128], BF16, st)
            hsT = p.sb("hsT", [128, 8, 128], BF16, st)
            cs = [p.sb(f"cs{i}", [128, 2, 32], F32, st) for i in range(2)]
            Pd = p.sb("Pd", [128, 384], F32, st)
            RKV = p.sb("RKV", [128, 384], F32, st)
            WAG = p.sb("WAG", [128, 384], F32, st)
            Rr = p.sb("Rr", [128, 2, 128], F32, st)
            rtmp = p.sb("rtmp", [128, 8, 32], F32, st)
            cols = p.sb("cols", [128, NTA, 8], F32, st)
            stg = [p.sb(f"stg{i}", [128, 3, 128], BF16, st) for i in range(2)]
            thT = p.sb("thT", [128, 3, 128], F32, st)
            Q = {}
            for nm in ('th', 'kk', 'tq', 'a0', 'a1', 'lw0', 'kd0', 'b0'):
                Q[nm] = p.sb("q_" + nm, [128, 128], F32, st)
            stash = wst

            def stage1(t):
                v = 1 if t < 2 else 0
                x_t = xt[t % 2]
                p.dma('sp', x_t, x_d[t * 128:(t + 1) * 128, :])
                rc = cols[:, t, 0:1]
                p.act(sq, x_t, AF.Square)
                p.reduce('dve', rc, sq, ALU.add)
                p.act(rc, rc, AF.Sqrt, bias=epsc[:, 0:1], scale=1.0 / D)
                p.recip(rc, rc)
                p.stt('dve', tm, x_t, rc, A1[v], ALU.mult, ALU.mult)
                p.tt('pool', hb[t % 3], tm, B1[v], ALU.add)

            stage1(0)
            for t in range(NTA):
                if t + 1 < NTA:
                    stage1(t + 1)
                has_prev = t not in (0, 2)
                has_next = t not in (1, NTA - 1)
                h = hb[t % 3]
                for k in range(8):
                    ks = slice(k * 128, (k + 1) * 128)
                    pa = ps[k // 4][:, (k % 4) * 128:(k % 4 + 1) * 128]
                    p.mm(pa, h[:, ks], cstb[:, C_ID, :])
                    pb = ps[2 + k // 4][:, (k % 4) * 128:(k % 4 + 1) * 128]
                    p.mm(pb, h[:, ks], cstb[:, C_SH, :], start=True, stop=not (has_prev or has_next))
                    if has_prev:
                        p.mm(pb, hb[(t - 1) % 3][:, ks], cstb[:, C_SHP, :], start=False, stop=not has_next)
                    if has_next:
                        p.mm(pb, hb[(t + 1) % 3][:, ks], cstb[:, C_SHN, :], start=False, stop=True)
                for half in range(2):
                    p.copy('act', hT[:, half * 4:(half + 1) * 4, :], ps[half][:, :].with_ap(ps[half].ap.rearrange("p (k n) -> p k n", k=4)))
                    p.copy('dve', hsT[:, half * 4:(half + 1) * 4, :], ps[2 + half].with_ap(ps[2 + half].ap.rearrange("p (k n) -> p k n", k=4)))
                for k in range(8):
                    p.mm(ps[4][:, 0:384], hT[:, k, :], Wd[:, k, :], start=(k == 0), stop=(k == 7))
                for c0, pp in ((0, ps[5]), (384, ps[6])):
                    for k in range(8):
                        p.mm(pp[:, 0:384], hT[:, k, :], W1[:, k, c0:c0 + 384], start=(k == 0), stop=False)
                    for k in range(8):
                        p.mm(pp[:, 0:384], hsT[:, k, :], W2[:, k, c0:c0 + 384], start=False, stop=(k == 7))
                p.copy('act', Pd, ps[4][:, 0:384])
                p.copy('dve', RKV, ps[5][:, 0:384])
                p.copy('act', WAG, ps[6][:, 0:384])
                c_t = cs[t % 2]
                p.dma('sp', c_t[:, 0, :], cos_d[t * 128:(t + 1) * 128, :])
                p.dma('sp', c_t[:, 1, :], sin_d[t * 128:(t + 1) * 128, :])
                for i in range(4):
                    c0 = i * 64
                    rope_tm(p, Rr[:, i // 2, (i % 2) * 64:(i % 2) * 64 + 32], Rr[:, i // 2, (i % 2) * 64 + 32:(i % 2) * 64 + 64],
                            Pd[:, c0:c0 + 32], Pd[:, c0 + 32:c0 + 64], c_t[:, 0, :], c_t[:, 1, :],
                            rtmp[:, 2 * i, :], rtmp[:, 2 * i + 1, :],
                            e1='dve' if i % 2 == 0 else 'pool', e2='pool' if i % 2 == 0 else 'dve')
                sg = stg[t % 2]
                ts_ = slice(t * 128, (t + 1) * 128)
                for i in range(2):
                    pp = ps[i]
                    p.tr(pp[:, 0:128], Rr[:, i, :], ident)
                    p.copy('act' if i else 'dve', sg[:, i, :], pp[:, 0:128])
                p.copy('pool', sg[:, 2, :], Pd[:, 256:384])
                p.dma('sp', qd_s[:, ts_], sg[:, 0, :])
                p.dma('sp', kd_s[:, ts_], sg[:, 1, :])
                p.dma('sp', vd_s[:, t, :], sg[:, 2, :])
                r_, k_, v_ = RKV[:, 0:128], RKV[:, 128:256], RKV[:, 256:384]
                p.copy('pool', Vst[:, t, :], v_)
                th = Q['th']
                p.act(th, WAG[:, 0:128], AF.Tanh)
                p.tr(ps[0][:, 0:128], th, ident)
                p.copy('dve', thT[:, 0, :], ps[0][:, 0:128])
                p.tr(ps[1][:, 0:128], WAG[:, 128:256], ident)
                p.copy('act', thT[:, 1, :], ps[1][:, 0:128])
                p.act(th, WAG[:, 256:384], AF.Sigmoid)
                p.tr(ps[2][:, 0:128], th, ident)
                p.copy('dve', thT[:, 2, :], ps[2][:, 0:128])
                p.mm(ps[3][:, 0:128], thT[:, 2, :], mats[:, 2, :])
                p.copy('act', Gst[:, t, :], ps[3][:, 0:128])
                kk = Q['kk']
                tq = Q['tq']
                p.tt('dve', kk, k_, bc[:, B_KK, :], ALU.mult)
                p.act(tq, kk, AF.Square)
                nrm = cols[:, t, 2:4]
                p.reduce('dve', nrm, tq.with_ap(tq.ap.rearrange("p (h c) -> p h c", h=2)), ALU.add)
                p.act(nrm, nrm, AF.Sqrt)
                p.ts('dve', nrm, nrm, 1e-12, ALU.max)
                p.recip(nrm, nrm)
                for hh in range(2):
                    p.ts('dve', kk[:, hh * 64:(hh + 1) * 64], kk[:, hh * 64:(hh + 1) * 64], cols[:, t, 2 + hh:3 + hh], ALU.mult)
                p.tt('pool', tq, r_, k_, ALU.mult)
                p.tt('pool', tq, tq, bc[:, B_RK, :], ALU.mult)
                p.reduce('dve', BCf[:, t, :], tq.with_ap(tq.ap.rearrange("p (h c) -> p h c", h=2)), ALU.add)
                sth = stash[t % 2]
                res = []
                for d in range(2):
                    ds_ = slice(d * 64, (d + 1) * 64)
                    a_d = Q['a0'] if d == 0 else sth[:, 512:640]
                    lw = Q['lw0'] if d == 0 else sth[:, 640:768]
                    kd = Q['kd0'] if d == 0 else sth[:, 128:256]
                    pw, pa_ = ps[4 + d], ps[6 + d]
                    p.mm(pw[:, 0:128], thT[ds_, 0, :], mats[ds_, 0, :])
                    p.mm(pa_[:, 0:128], thT[ds_, 1, :], mats[ds_, 1, :])
                    p.tt('dve', lw, pw[:, 0:128], bc[:, B_W00 + d, :], ALU.add)
                    p.act(lw, lw, AF.Sigmoid)
                    p.ts('pool', lw, lw, -math.exp(-0.5), ALU.mult)
                    p.tt('dve', a_d, pa_[:, 0:128], bc[:, B_A00 + d, :], ALU.add)
                    p.act(a_d, a_d, AF.Sigmoid)
                    p.stt('dve', kd, a_d, -1.0, bc[:, B_KA, :], ALU.add, ALU.mult)
                    p.ts('pool', kd, kd, 1.0, ALU.add)
                    p.tt('pool', kd, kd, k_, ALU.mult)
                    b_d = Q['b0'] if d == 0 else sth[:, 512:640]
                    p.tt('dve', b_d, kk, a_d, ALU.mult)
                    res.append((kd, b_d, lw))
                p.copy('pool', sth[:, 0:128], r_)
                p.copy('pool', sth[:, 256:384], v_)
                p.copy('pool', sth[:, 384:512], kk)
                p.dma('sp', st_s[t], sth)
                scan_step(st, 0, t, r_, res[0][0], v_, kk, res[0][1], res[0][2], True)
        with p.scope() as st:
            stash = [p.sb(f"stashb{i}", [128, 768], F32, st) for i in range(2)]
            order = [1, 0] + list(range(NTA - 1, 1, -1))
            for n, t in enumerate(order):
                sth = stash[n % 2]
                p.dma('sp', sth, st_s[t])
                scan_step(st, 1, t, sth[:, 0:128], sth[:, 128:256], sth[:, 256:384], sth[:, 384:512], sth[:, 512:640], sth[:, 640:768], False)
            OUT = p.sb("OUTr", [128, NLAT], F32, st)
            fq = [p.sb(f"fq{i}", [128, 128], F32, st) for i in range(3)]
            fcol = p.sb("fcol", [128, NTA, 8], F32, st)
            for t in range(2, NTA):
                y = Yacc[:, t, :]
                y3 = y.with_ap(y.ap.rearrange("p (h c) -> p h c", h=2))
                mean = fcol[:, t, 0:2]
                p.reduce('dve', mean, y3, ALU.add)
                p.ts('dve', mean, mean, 1.0 / 64, ALU.mult)
                yc = fq[0]
                for hh in range(2):
                    p.ts('dve', yc[:, hh * 64:(hh + 1) * 64], y[:, hh * 64:(hh + 1) * 64], fcol[:, t, hh:hh + 1], ALU.subtract)
                p.act(fq[1], yc, AF.Square)
                var = fcol[:, t, 2:4]
                p.reduce('dve', var, fq[1].with_ap(fq[1].ap.rearrange("p (h c) -> p h c", h=2)), ALU.add)
                p.act(var, var, AF.Sqrt, bias=epsc[:, 1:2], scale=1.0 / 64)
                p.recip(var, var)
                for hh in range(2):
                    hs_ = slice(hh * 64, (hh + 1) * 64)
                    p.stt('dve', yc[:, hs_], yc[:, hs_], fcol[:, t, 2 + hh:3 + hh], bc[:, B_LNG, hs_], ALU.mult, ALU.mult)
                p.tt('pool', yc, yc, bc[:, B_LNB, :], ALU.add)
                for hh in range(2):
                    hs_ = slice(hh * 64, (hh + 1) * 64)
                    p.stt('dve', yc[:, hs_], Vst[:, t, hs_], BCf[:, t, hh:hh + 1], yc[:, hs_], ALU.mult, ALU.add)
                p.tt('pool', fq[2], yc, Gst[:, t, :], ALU.mult)
                pp = ps[t % 4]
                p.tr(pp[:, 0:128], fq[2], ident)
                p.copy('act', OUT[:, (t - 2) * 128:(t - 1) * 128], pp[:, 0:128])
            p.dma('sp', out_d[128:256, :], OUT)

    with p.scope() as st:
        QT = p.sb("QdT", [128, NTOK], BF16, st)
        KT = p.sb("KdT", [128, NTOK], BF16, st)
        Vd = p.sb("Vd", [128, NTA, 128], BF16, st)
        p.dma('sp', QT, qd_s)
        p.dma('sp', KT, kd_s)
        p.dma('sp', Vd, vd_s)
        lamv = p.sb("lamv", [128, 4, 64], F32, st)
        p.dma('sp', lamv, lam_d)
        lc = p.sb("lc", [128, 8], F32, st)
        lt = p.sb("lt", [128, 2, 64], F32, st)
        p.tt('dve', lt[:, 0, :], lamv[:, 0, :], lamv[:, 1, :], ALU.mult)
        p.tt('dve', lt[:, 1, :], lamv[:, 2, :], lamv[:, 3, :], ALU.mult)
        p.reduce('dve', lc[:, 0:2], lt, ALU.add)
        p.act(lc[:, 2:4], lc[:, 0:2], AF.Exp)
        p.tt('dve', lc[:, 4:5], lc[:, 2:3], lc[:, 3:4], ALU.subtract)
        p.ts('dve', lc[:, 5:6], lc[:, 4:5], LAM_INIT1, ALU.add, -1.0, ALU.mult)
        neglam = lc[:, 5:6]
        subg = p.sb("subg", [128, 1], F32, st)
        p.dma('sp', subg, sg_d)
        p.ts('dve', subg, subg, 1.0 - LAM_INIT1, ALU.mult)
        PT = [p.sb(f"PTd{i}", [128, 512], BF16, st) for i in range(3)]
        rec = [p.sb(f"recd{i}", [128, 512], F32, st) for i in range(2)]
        o0 = p.sb("o0", [128, 512], F32, st)
        o1 = p.sb("o1", [128, 512], F32, st)
        osq = p.sb("osq", [128, 512], F32, st)
        it = 0
        for g in range(16):
            q0 = 256 + g * 512
            for m in range(2):
                ms = slice(m * 64, (m + 1) * 64)
                pO, pD = ps[4 + m * 2], ps[5 + m * 2]
                for kt in range(NTA):
                    pS = ps[it % 3]
                    pt_ = PT[it % 3]
                    it += 1
                    p.mm(pS, KT[ms, kt * 128:(kt + 1) * 128], QT[ms, q0:q0 + 512])
                    p.act(pt_, pS, AF.Exp, scale=64.0 ** -0.5)
                    p.mm(pO, Vd[:, kt, :], pt_, start=(kt == 0), stop=(kt == NTA - 1))
                    p.mm(pD, ones_bf, pt_, start=(kt == 0), stop=(kt == NTA - 1))
                p.recip(rec[m], pD)
                p.tt('dve', o0 if m == 0 else o1, pO, rec[m], ALU.mult)
            p.stt('dve', o0, o1, neglam, o0, ALU.mult, ALU.add)
            p.act(osq, o0, AF.Square)
            pn = ps[3]
            p.mm(pn, cst[:, C_ONE, :], osq)
            p.act(osq, pn, AF.Sqrt, bias=epsc[:, 0:1], scale=1.0 / 128)
            p.recip(osq, osq)
            p.stt('dve', o0, o0, subg, osq, ALU.mult, ALU.mult)
            p.dma('sp', out_d[0:128, g * 512:(g + 1) * 512], o0)
    p.finalize()
    return nc


def _mix1_consts():
    s = np.arange(128, dtype=np.float32)[:, None]
    t = np.arange(128, dtype=np.float32)[None, :]
    c = np.zeros((10, 128, 128), np.float32)
    c[C_ID] = np.eye(128, dtype=np.float32)
    c[C_TRI0] = (s <= t)
    c[C_TRI1] = (s >= t)
    c[C_MS0] = (s < t)
    c[C_MI0] = (s <= t)
    c[C_MS1] = (s > t)
    c[C_SH] = (np.abs(s - t) == 1)
    c[C_SHP] = (s == 127) & (t == 0)
    c[C_SHN] = (s == 0) & (t == 127)
    c[C_ONE] = 1.0
    return c


def run_mix1(x, ctx, c, c_ctx, mod_w, mod_b, norm_g, w_in, lamv, subg, mu, w0, w2, a0, a2, g2, k_k, k_a, r_k, ln_g, ln_b):
    nc = build_mix1()
    cos, sin = _with_ctx_rope(*_rope_tables(64))
    cst = _mix1_consts()
    in_maps = []
    for core in range(8):
        b, j = core // 4, core % 4
        hc = np.arange(j * 128, (j + 1) * 128)
        a128 = np.arange(128)
        colsel = np.concatenate([hc, 512 + hc, 1024 + hc, 1536 + hc, 2048 + hc, 2560 + hc, 3072 + a128, 3200 + a128, 3328 + a128])
        musel = np.concatenate([hc, 512 + hc, 1024 + hc, 1536 + a128, 1664 + a128, 1792 + a128])
        rows = [w0[0][hc], w0[1][hc], a0[0][hc], a0[1][hc], k_k[hc], k_a[hc], r_k.reshape(-1)[hc], ln_g[hc], ln_b[hc]]
        bc = np.stack([np.broadcast_to(r, (128, 128)) for r in rows]).astype(np.float32)
        mats = np.stack([np.concatenate([w2[0][:, hc], w2[1][:, hc]], 0), np.concatenate([a2[0][:, hc], a2[1][:, hc]], 0), g2[:, hc]]).astype(np.float32)
        in_maps.append({
            "x": np.ascontiguousarray(np.concatenate([ctx[b], x[b]], 0), dtype=np.float32),
            "cT": _cT(c[b], c_ctx), "mod_w": np.ascontiguousarray(mod_w), "mod_b": np.ascontiguousarray(np.stack([mod_b] * 2)),
            "norm_g": np.ascontiguousarray(norm_g), "w_in": np.ascontiguousarray(w_in[:, colsel]),
            "mu": np.ascontiguousarray(np.broadcast_to(mu[musel], (128, 768)), dtype=np.float32),
            "cos": cos, "sin": sin,
            "lamv": np.ascontiguousarray(np.broadcast_to(lamv, (128, 4, 64)), dtype=np.float32),
            "subg": np.ascontiguousarray(subg.reshape(128, 1), dtype=np.float32),
            "bc": np.ascontiguousarray(bc), "mats": np.ascontiguousarray(mats), "cst": cst,
        })
    res = run_bass_kernel_spmd(nc, in_maps, core_ids=list(range(8)), trace=TRACE)
    if TRACE:
        print("DEV_NS", res.exec_time_ns)
    mix = np.empty((2, 8192, 1024), np.float32)
    for core in range(8):
        b, j = core // 4, core % 4
        o = res.results[core]["mixT"]
        mix[b, :, j * 128:(j + 1) * 128] = o[0:128].T
        mix[b, :, 512 + j * 128:512 + (j + 1) * 128] = o[128:256].T
    return mix


def kernel(x, c, ctx, c_ctx, mod_w, mod_b, norm_g, even_w_in, even_w_out, ret_decay_exp, ret_norm_g,
           gqa_q_norm, gqa_k_norm, ffn_w_gate, ffn_w_up, ffn_w_down, odd_w_in, odd_w_out, diff_lambda,
           diff_subln_g, rwkv_mu, rwkv_w0, rwkv_w2, rwkv_a0, rwkv_a2, rwkv_g2, rwkv_k_k, rwkv_k_a, rwkv_r_k,
           rwkv_ln_g, rwkv_ln_b, moe_router, moe_w_gate, moe_w_up, moe_w_down):
    f = lambda a: np.asarray(a, dtype=np.float32)
    x, c, ctx, c_ctx, mod_w, mod_b, norm_g = map(f, (x, c, ctx, c_ctx, mod_w, mod_b, norm_g))
    mix_lat, mix_ctx = run_mix0(x, ctx, c, c_ctx, mod_w[0], mod_b[0], norm_g[0], f(even_w_in)[0], f(ret_decay_exp)[0],
                                f(ret_norm_g)[0], f(gqa_q_norm)[0], f(gqa_k_norm)[0])
    x1, ctx1 = run_post(0, x, ctx, mix_lat, mix_ctx, c, c_ctx, mod_w, mod_b, norm_g, f(even_w_out)[0],
                        f(ffn_w_gate), f(ffn_w_up), f(ffn_w_down), np.zeros((1024, 8), np.float32), True, 1, 2816, 2)
    mix1 = run_mix1(x1, ctx1, c, c_ctx, mod_w[1], mod_b[1], norm_g[1], f(odd_w_in)[0], f(diff_lambda)[0], f(diff_subln_g)[0],
                    f(rwkv_mu)[0], f(rwkv_w0)[0], f(rwkv_w2)[0], f(rwkv_a0)[0], f(rwkv_a2)[0], f(rwkv_g2)[0], f(rwkv_k_k)[0],
                    f(rwkv_k_a)[0], f(rwkv_r_k)[0], f(rwkv_ln_g)[0], f(rwkv_ln_b)[0])
    x2 = run_post1(x1, mix1, c, c_ctx, mod_w[1], mod_b[1], norm_g[1], f(odd_w_out)[0],
                   f(moe_w_gate)[0], f(moe_w_up)[0], f(moe_w_down)[0], f(moe_router)[0])
    return x2


def build_d1(NT=16):
    nc = bass.Bass("TRN2", target_bir_lowering=False)
    p = Prog(nc)
    NTOK = NT * 128
    x_d = p.dram("x", [NTOK, D], F32, "ExternalInput")
    mixT_d = p.dram("mixT", [D, NTOK], F32, "ExternalInput")
    cT_d = p.dram("cT", [128, 8, 2], F32, "ExternalInput")
    modw_d = p.dram("mod_w", [D, 6 * D], F32, "ExternalInput")
    modb_d = p.dram("mod_b", [2, 6 * D], F32, "ExternalInput")
    ng_d = p.dram("norm_g", [4, D], F32, "ExternalInput")
    wout_d = p.dram("w_out", [D, D], F32, "ExternalInput")
    rt_d = p.dram("router", [D, 128], F32, "ExternalInput")
    id_d = p.dram("ident", [128, 128], F32, "ExternalInput")
    x1_d = p.dram("x1", [NTOK, D], F32, "ExternalOutput")
    h2T_d = p.dram("h2T", [D, NTOK], BF16, "ExternalOutput")
    gates_d = p.dram("gates", [NTOK, 8], F32, "ExternalOutput")
    mod_s = p.dram("mod_s", [2, 6 * D], F32, "Internal")
    ps = [p.ps(f"ps{i}", [128, 512]) for i in range(8)]
    ident = p.sb("ident", [128, 128], F32)
    p.dma('sp', ident, id_d)
    epsc = p.sb("epsc", [128, 1], F32)
    p.memset('dve', epsc, NORM_EPS)
    h2T = p.sb("h2T", [128, 8, NTOK], BF16)
    gates = p.sb("gates", [128, NT, 8], F32)
    with p.scope() as st:
        mod_rows(p, ps, st, cT_d, modw_d, modb_d, mod_s, [4, 5, 6, 7, 8, 9])

    def rms_rstd(dst_col, src, sq_tmp):
        p.act(sq_tmp, src, AF.Square)
        p.reduce('dve', dst_col, sq_tmp, ALU.add)
        p.act(dst_col, dst_col, AF.Sqrt, bias=epsc, scale=1.0 / D)
        p.recip(dst_col, dst_col)

    with p.scope() as st:
        G1 = p.sb("G1", [128, D], F32, st)
        A2 = p.sb("A2", [128, D], F32, st)
        B2 = p.sb("B2", [128, D], F32, st)
        ngb = p.sb("ngb", [128, 2, D], F32, st)
        p.dma('sp', ngb[:, 0, :], ng_d.with_ap(ng_d.ap[1:2, :].partition_broadcast(128)))
        p.dma('sp', ngb[:, 1, :], ng_d.with_ap(ng_d.ap[2:3, :].partition_broadcast(128)))
        p.dma('sp', G1, mod_s.with_ap(mod_s.ap[0:1, 2 * D:3 * D].partition_broadcast(128)))
        p.tt('dve', G1, G1, ngb[:, 0, :], ALU.mult)
        p.dma('sp', B2, mod_s.with_ap(mod_s.ap[0:1, 3 * D:4 * D].partition_broadcast(128)))
        p.dma('sp', A2, mod_s.with_ap(mod_s.ap[0:1, 4 * D:5 * D].partition_broadcast(128)))
        p.ts('dve', A2, A2, 1.0, ALU.add)
        p.tt('dve', A2, A2, ngb[:, 1, :], ALU.mult)
        mixT = p.sb("mixT", [128, 8, NTOK], BF16, st)
        mview = mixT_d.ap.rearrange("(k p) n -> p k n", p=128)
        for k in range(8):
            p.dma('pool', mixT[:, k, :], mixT_d.with_ap(mview[:, k, :]))
        wout = p.sb("wout", [128, 8, D], BF16, st)
        wv = wout_d.ap.rearrange("(k p) n -> p k n", p=128)
        for k in range(8):
            p.dma('pool', wout[:, k, :], wout_d.with_ap(wv[:, k, :]))
        rt = p.sb("rt", [128, 8, 128], F32, st)
        if DBG != 'd1c':
            p.dma('sp', rt, rt_d.with_ap(rt_d.ap.rearrange("(k p) e -> p k e", p=128)))
        h2Tf = p.sb("h2Tf", [128, 8, 128], F32, st)
        xt = [p.sb(f"xt{i}", [128, D], F32, st) for i in range(2)]
        tmp = [p.sb(f"tmp{i}", [128, D], F32, st) for i in range(2)]
        sq = p.sb("sq", [128, D], F32, st)
        cols = p.sb("cols", [128, NT, 8], F32, st)
        lg = p.sb("lg", [128, 4, 8], F32, st)
        for t in range(NT):
            x_t, tm = xt[t % 2], tmp[t % 2]
            p.dma('sp', x_t, x_d[t * 128:(t + 1) * 128, :])
            py = [ps[0], ps[1]]
            for fh in range(2):
                for k in range(8):
                    p.mm(py[fh], mixT[:, k, t * 128:(t + 1) * 128], wout[:, k, fh * 512:(fh + 1) * 512], start=(k == 0), stop=(k == 7))
            for fh in range(2):
                p.copy('act', tm[:, fh * 512:(fh + 1) * 512], py[fh])
            rc = cols[:, t, 0:1]
            rms_rstd(rc, tm, sq)
            p.stt('dve', tm, tm, rc, G1, ALU.mult, ALU.mult)
            p.tt('pool', x_t, x_t, tm, ALU.add)
            p.dma('sp', x1_d[t * 128:(t + 1) * 128, :], x_t)
            rc2 = cols[:, t, 1:2]
            rms_rstd(rc2, x_t, sq)
            p.stt('dve', tm, x_t, rc2, A2, ALU.mult, ALU.mult)
            p.tt('pool', tm, tm, B2, ALU.add)
            for k in range(8):
                pt = ps[2 + (k % 4)]
                p.tr(pt[:, 0:128], tm[:, k * 128:(k + 1) * 128], ident)
                p.copy('act' if k % 2 else 'dve', h2Tf[:, k, :], pt[:, 0:128])
                p.copy('pool', h2T[:, k, t * 128:(t + 1) * 128], h2Tf[:, k, :])
            if DBG in ('d1a', 'd1b', 'd1c'):
                p.memset('dve', gates[:, t, :], 0.125)
                continue
            pl = ps[6]
            for k in range(8):
                p.mm(pl[:, 0:128], h2Tf[:, k, :], rt[:, k, :], start=(k == 0), stop=(k == 7))
            L = lg[:, 0, :]
            p.copy('dve', L, pl[:, 0:8])
            m1 = cols[:, t, 2:3]
            m2 = cols[:, t, 3:4]
            p.reduce('dve', m1, L, ALU.max)
            mk1 = lg[:, 1, :]
            p.ts('dve', mk1, L, m1, ALU.is_equal)
            L2 = lg[:, 2, :]
            p.stt('dve', L2, mk1, -1e30, L, ALU.mult, ALU.add)
            p.reduce('dve', m2, L2, ALU.max)
            mk2 = lg[:, 3, :]
            p.ts('dve', mk2, L2, m2, ALU.is_equal)
            w1 = cols[:, t, 4:5]
            w2 = cols[:, t, 5:6]
            p.tt('dve', w1, m1, m2, ALU.subtract)
            p.act(w1, w1, AF.Sigmoid)
            p.ts('dve', w2, w1, -1.0, ALU.mult, 1.0, ALU.add)
            p.ts('dve', gates[:, t, :], mk1, w1, ALU.mult)
            p.stt('dve', gates[:, t, :], mk2, w2, gates[:, t, :], ALU.mult, ALU.add)
        hv = h2T_d.ap.rearrange("(k p) n -> p k n", p=128)
        for k in range(8):
            p.dma('sp', h2T_d.with_ap(hv[:, k, :]), h2T[:, k, :])
        p.dma('sp', gates_d.with_ap(gates_d.ap.rearrange("(t p) e -> p t e", p=128)), gates)
    p.finalize()
    return nc


def build_d2(NCH=8, H=3584, BLK=4):
    nc = bass.Bass("TRN2", target_bir_lowering=False)
    p = Prog(nc)
    CT = 16
    NTOK = NCH * CT * 128
    HC = H // 128
    NB = HC // BLK
    h2T_d = p.dram("h2T", [D, NTOK], BF16, "ExternalInput")
    g_d = p.dram("gate", [128, NCH * CT], F32, "ExternalInput")
    wg_d = p.dram("wg", [D, H], F32, "ExternalInput")
    wu_d = p.dram("wu", [D, H], F32, "ExternalInput")
    wd_d = p.dram("wd", [H, D], F32, "ExternalInput")
    out_d = p.dram("y", [NTOK, D], F32, "ExternalOutput")
    ps = [p.ps(f"ps{i}", [128, 512]) for i in range(8)]
    gt = p.sb("gt", [128, NCH * CT], F32)
    p.dma('sp', gt, g_d)
    h2T = [p.sb(f"h2T{i}", [128, 8, CT * 128], BF16) for i in range(2)]
    acc = p.sb("acc", [128, CT, D], F32)
    wgb = [p.sb(f"wg{i}", [128, 8, BLK * 128], BF16) for i in range(2)]
    wub = [p.sb(f"wu{i}", [128, 8, BLK * 128], BF16) for i in range(2)]
    wdb = [p.sb(f"wd{i}", [128, BLK, D], BF16) for i in range(2)]
    actT = [p.sb(f"actT{i}", [128, BLK, 512], BF16) for i in range(2)]
    sg = [p.sb(f"sg{i}", [128, 512], F32) for i in range(2)]
    yo = [p.sb(f"yo{i}", [128, D], F32) for i in range(2)]
    wgv = wg_d.ap.rearrange("(k p) h -> p k h", p=128)
    wuv = wu_d.ap.rearrange("(k p) h -> p k h", p=128)
    wdv = wd_d.ap.rearrange("(c p) f -> p c f", p=128)
    hv = h2T_d.ap.rearrange("(k p) n -> p k n", p=128)
    it = 0
    gi = 0
    for ch in range(NCH):
        hb = h2T[ch % 2]
        for k in range(8):
            p.dma('sp', hb[:, k, :], h2T_d.with_ap(hv[:, k, ch * CT * 128:(ch + 1) * CT * 128]))
        for b in range(NB):
            s = it % 2
            it += 1
            hs = slice(b * BLK * 128, (b + 1) * BLK * 128)
            for k in range(8):
                p.dma('pool', wgb[s][:, k, :], wg_d.with_ap(wgv[:, k, hs]))
                p.dma('pool', wub[s][:, k, :], wu_d.with_ap(wuv[:, k, hs]))
            for c in range(BLK):
                p.dma('pool', wdb[s][:, c, :], wd_d.with_ap(wdv[:, b * BLK + c, :]))
            for t0 in range(0, CT, 4):
                ts_ = slice(t0 * 128, (t0 + 4) * 128)
                a = actT[gi % 2]
                gi += 1
                for c in range(BLK):
                    pg, pu = ps[(c % 2) * 2], ps[(c % 2) * 2 + 1]
                    for k in range(8):
                        p.mm(pg, wgb[s][:, k, c * 128:(c + 1) * 128], hb[:, k, ts_], start=(k == 0), stop=(k == 7))
                    for k in range(8):
                        p.mm(pu, wub[s][:, k, c * 128:(c + 1) * 128], hb[:, k, ts_], start=(k == 0), stop=(k == 7))
                    sgt = sg[c % 2]
                    p.act(sgt, pg, AF.Silu)
                    p.tt('dve', a[:, c, :], sgt, pu, ALU.mult)
                for j in range(4):
                    t = t0 + j
                    for fh in range(2):
                        pd = ps[4 + ((j * 2 + fh) % 4)]
                        for c in range(BLK):
                            p.mm(pd, a[:, c, j * 128:(j + 1) * 128], wdb[s][:, c, fh * 512:(fh + 1) * 512], start=(c == 0), stop=(c == BLK - 1))
                        dst = acc[:, t, fh * 512:(fh + 1) * 512]
                        if b == 0:
                            p.copy('dve', dst, pd)
                        else:
                            p.tt('dve', dst, pd, dst, ALU.add)
        for t in range(CT):
            gt_ = ch * CT + t
            y = yo[t % 2]
            p.ts('pool', y, acc[:, t, :], gt[:, gt_:gt_ + 1], ALU.mult)
            p.dma('sp', out_d[gt_ * 128:(gt_ + 1) * 128, :], y)
    p.finalize()
    return nc


def build_d3(NT=16, NE=8):
    nc = bass.Bass("TRN2", target_bir_lowering=False)
    p = Prog(nc)
    NTOK = NT * 128
    ys_d = p.dram("ys", [NE, NTOK, D], F32, "ExternalInput")
    x1_d = p.dram("x1", [NTOK, D], F32, "ExternalInput")
    cT_d = p.dram("cT", [128, 8, 2], F32, "ExternalInput")
    modw_d = p.dram("mod_w", [D, 6 * D], F32, "ExternalInput")
    modb_d = p.dram("mod_b", [2, 6 * D], F32, "ExternalInput")
    ng_d = p.dram("norm_g", [4, D], F32, "ExternalInput")
    out_d = p.dram("out", [NTOK, D], F32, "ExternalOutput")
    mod_s = p.dram("mod_s", [2, 6 * D], F32, "Internal")
    ps = [p.ps(f"ps{i}", [128, 512]) for i in range(8)]
    epsc = p.sb("epsc", [128, 1], F32)
    p.memset('dve', epsc, NORM_EPS)
    with p.scope() as st:
        mod_rows(p, ps, st, cT_d, modw_d, modb_d, mod_s, [10, 11])
    with p.scope() as st:
        G2 = p.sb("G2", [128, D], F32, st)
        ng3 = p.sb("ng3", [128, D], F32, st)
        p.dma('sp', ng3, ng_d.with_ap(ng_d.ap[3:4, :].partition_broadcast(128)))
        p.dma('sp', G2, mod_s.with_ap(mod_s.ap[0:1, 5 * D:6 * D].partition_broadcast(128)))
        p.tt('dve', G2, G2, ng3, ALU.mult)
        yt = [p.sb(f"yt{i}", [128, NE, D], F32, st) for i in range(2)]
        x1t = [p.sb(f"x1t{i}", [128, D], F32, st) for i in range(2)]
        sq = p.sb("sq", [128, D], F32, st)
        cols = p.sb("cols", [128, NT], F32, st)
        for t in range(NT):
            y = yt[t % 2]
            x1 = x1t[t % 2]
            for e in range(NE):
                p.dma('sp', y[:, e, :], ys_d[e, t * 128:(t + 1) * 128, :])
            p.dma('sp', x1, x1_d[t * 128:(t + 1) * 128, :])
            f = y[:, 0, :]
            for e in range(1, NE):
                p.tt('dve' if e % 2 else 'pool', f, f, y[:, e, :], ALU.add)
            rc = cols[:, t:t + 1]
            p.act(sq, f, AF.Square)
            p.reduce('dve', rc, sq, ALU.add)
            p.act(rc, rc, AF.Sqrt, bias=epsc, scale=1.0 / D)
            p.recip(rc, rc)
            p.stt('dve', f, f, rc, G2, ALU.mult, ALU.mult)
            p.tt('pool', x1, x1, f, ALU.add)
            p.dma('sp', out_d[t * 128:(t + 1) * 128, :], x1)
    p.finalize()
    return nc


def run_post1(x_lat, mix_lat, c, c_ctx, mod_w, mod_b, norm_g, w_out, wg, wu, wd, router):
    import ml_dtypes
    cores = list(range(8))
    nc = build_d1()
    rpad = np.ascontiguousarray(np.concatenate([router, np.zeros((1024, 120), np.float32)], 1))
    in_maps = []
    for core in cores:
        b, q = core // 4, core % 4
        in_maps.append({
            "x": np.ascontiguousarray(x_lat[b, q * 2048:(q + 1) * 2048]),
            "mixT": np.ascontiguousarray(mix_lat[b, q * 2048:(q + 1) * 2048].T),
            "cT": _cT(c[b], c_ctx), "mod_w": np.ascontiguousarray(mod_w), "mod_b": np.ascontiguousarray(np.stack([mod_b] * 2)),
            "norm_g": np.ascontiguousarray(norm_g), "w_out": np.ascontiguousarray(w_out), "router": rpad, "ident": _IDENT,
        })
    r1 = run_bass_kernel_spmd(nc, in_maps, core_ids=cores, trace=TRACE).results
    if TRACE:
        pass
    h2T_all = np.ascontiguousarray(np.concatenate([np.asarray(r1[k]["h2T"]) for k in cores], axis=1))
    gates_all = np.concatenate([np.asarray(r1[k]["gates"]) for k in cores], axis=0)
    nc = build_d2()
    in_maps = []
    for e in cores:
        in_maps.append({
            "h2T": h2T_all,
            "gate": np.ascontiguousarray(gates_all[:, e].reshape(128, 128).T),
            "wg": np.ascontiguousarray(wg[e]), "wu": np.ascontiguousarray(wu[e]), "wd": np.ascontiguousarray(wd[e]),
        })
    r2 = run_bass_kernel_spmd(nc, in_maps, core_ids=cores, trace=TRACE).results
    nc = build_d3()
    in_maps = []
    for core in cores:
        b, q = core // 4, core % 4
        ys = np.ascontiguousarray(np.stack([np.asarray(r2[e]["y"])[core * 2048:(core + 1) * 2048] for e in cores]))
        in_maps.append({
            "ys": ys, "x1": np.asarray(r1[core]["x1"]),
            "cT": _cT(c[b], c_ctx), "mod_w": np.ascontiguousarray(mod_w), "mod_b": np.ascontiguousarray(np.stack([mod_b] * 2)),
            "norm_g": np.ascontiguousarray(norm_g),
        })
    r3 = run_bass_kernel_spmd(nc, in_maps, core_ids=cores, trace=TRACE).results
    x2 = np.empty_like(x_lat)
    for core in cores:
        b, q = core // 4, core % 4
        x2[b, q * 2048:(q + 1) * 2048] = r3[core]["out"]
    return x2
```

```python
import contextlib
import math
import numpy as np
import concourse.bass as bass
import concourse.mybir as mybir
from concourse.bass_utils import run_bass_kernel_spmd

ALU = mybir.AluOpType
AF = mybir.ActivationFunctionType
AX = mybir.AxisListType
F32 = mybir.dt.float32
BF16 = mybir.dt.bfloat16

SAME_ENGINE_SYNC = True
ENGS = ('pe', 'act', 'dve', 'pool', 'sp')


class Res:
    __slots__ = ('name', 'w', 'r', 'dsem', 'dcount')

    def __init__(self, name):
        self.name = name
        self.w = None
        self.r = []
        self.dsem = None
        self.dcount = 0


class T:
    __slots__ = ('ap', 'res')

    def __init__(self, ap, res):
        self.ap = ap
        self.res = res

    def __getitem__(self, idx):
        return T(self.ap[idx], self.res)

    def with_ap(self, ap):
        return T(ap, self.res)


class Prog:
    def __init__(self, nc):
        self.nc = nc
        self.stack = contextlib.ExitStack()
        self.ops = {e: [] for e in ENGS}
        self.nops = {e: 0 for e in ENGS}
        self.seen = {e: {} for e in ENGS}
        self.signal = {e: set() for e in ENGS}
        self.dma_res = []
        self.all_res = []
        self.nsb = 0

    def _res(self, name):
        r = Res(name)
        self.all_res.append(r)
        return r

    def sb(self, name, shape, dt, stack=None):
        t = (stack or self.stack).enter_context(self.nc.sbuf_tensor('sb_' + name, list(shape), dt))
        return T(t[tuple(slice(None) for _ in shape)], self._res(name))

    def ps(self, name, shape, dt=F32, stack=None):
        t = (stack or self.stack).enter_context(self.nc.psum_tensor('pp_' + name, list(shape), dt))
        return T(t[tuple(slice(None) for _ in shape)], self._res(name))

    def dram(self, name, shape, dt, kind):
        t = self.nc.dram_tensor(name, list(shape), dt, kind=kind).ap()
        return T(t, self._res(name))

    def sub(self, t, name=None):
        return T(t.ap, self._res(name or t.res.name + '_sub'))

    def _need(self, eng, tok, waits):
        if tok is None:
            return
        if tok[0] == 'eng':
            _, f, k = tok
            if f == eng and not (SAME_ENGINE_SYNC and eng != 'pe'):
                return
            key = ('eng', f)
            if self.seen[eng].get(key, 0) >= k:
                return
            self.seen[eng][key] = k
            self.signal[f].add(k)
            waits.append(tok)
        else:
            _, res, cnt = tok
            key = ('dma', id(res))
            if self.seen[eng].get(key, 0) >= cnt:
                return
            self.seen[eng][key] = cnt
            waits.append(tok)

    def _deps(self, eng, reads, writes):
        waits = []
        for t in reads:
            self._need(eng, t.res.w, waits)
        for t in writes:
            self._need(eng, t.res.w, waits)
            for tok in t.res.r:
                self._need(eng, tok, waits)
        return waits

    def op(self, eng, fn, reads=(), writes=()):
        waits = self._deps(eng, reads, writes)
        self.nops[eng] += 1
        k = self.nops[eng]
        tok = ('eng', eng, k)
        for t in reads:
            t.res.r.append(tok)
        for t in writes:
            t.res.w = tok
            t.res.r = []
        self.ops[eng].append((waits, fn, k, None))

    def dma(self, q, out, in_, **kw):
        waits = self._deps(q, [in_], [out])
        res = out.res
        if res.dsem is None:
            res.dsem = self.stack.enter_context(self.nc.semaphore('d_' + res.name))
            self.dma_res.append(res)
        res.dcount += 1
        tok = ('dma', res, res.dcount)
        in_.res.r.append(tok)
        res.w = tok
        res.r = []
        oap, iap = out.ap, in_.ap
        self.ops[q].append((waits, lambda e: e.dma_start(out=oap, in_=iap, **kw), None, res))

    def barrier(self):
        for e in ENGS:
            waits = []
            for f in ENGS:
                if f != e and self.nops[f] > 0:
                    self._need(e, ('eng', f, self.nops[f]), waits)
            for res in self.dma_res:
                self._need(e, ('dma', res, res.dcount), waits)
            if waits:
                self.ops[e].append((waits, None, None, None))

    @contextlib.contextmanager
    def scope(self):
        st = contextlib.ExitStack()
        try:
            yield st
        finally:
            self.barrier()
            st.close()

    def finalize(self):
        nc = self.nc
        sems = {e: self.stack.enter_context(nc.semaphore('s_' + e)) for e in ENGS}
        self.barrier()
        rank = {}
        for e in ENGS:
            for i, k in enumerate(sorted(self.signal[e])):
                rank[(e, k)] = i + 1

        def run(ename, eng):
            for waits, fn, k, dres in self.ops[ename]:
                for tok in waits:
                    if tok[0] == 'eng':
                        eng.wait_ge(sems[tok[1]], rank[(tok[1], tok[2])])
                    else:
                        eng.wait_ge(tok[1].dsem, 16 * tok[2])
                if fn is None:
                    continue
                ins = fn(eng)
                if dres is not None:
                    ins.then_inc(dres.dsem, 16)
                elif (ename, k) in rank:
                    ins.then_inc(sems[ename], 1)

        with nc.Block() as block:
            @block.tensor
            def _(e):
                run('pe', e)

            @block.scalar
            def _(e):
                run('act', e)

            @block.vector
            def _(e):
                run('dve', e)

            @block.gpsimd
            def _(e):
                run('pool', e)

            @block.sync
            def _(e):
                run('sp', e)
        self.stack.close()

    def mm(self, out, lhsT, rhs, start=True, stop=True):
        o, l, r = out.ap, lhsT.ap, rhs.ap
        self.op('pe', lambda e: e.matmul(o, l, r, start=start, stop=stop), [lhsT, rhs], [out])

    def tr(self, out, in_, ident):
        o, i, d = out.ap, in_.ap, ident.ap
        self.op('pe', lambda e: e.transpose(o, i, d), [in_, ident], [out])

    def act(self, out, in_, func, bias=None, scale=1.0, accum=None, eng='act'):
        o, i = out.ap, in_.ap
        reads = [in_]
        kw = {}
        if bias is not None:
            if isinstance(bias, T):
                reads.append(bias)
                kw['bias'] = bias.ap
            else:
                kw['bias'] = bias
        if isinstance(scale, T):
            reads.append(scale)
            kw['scale'] = scale.ap
        else:
            kw['scale'] = scale
        writes = [out]
        if accum is not None:
            writes.append(accum)
            kw['accum_out'] = accum.ap
        self.op(eng, lambda e: e.activation(o, i, func, **kw), reads, writes)

    def tt(self, eng, out, in0, in1, op):
        o, a, b = out.ap, in0.ap, in1.ap
        self.op(eng, lambda e: e.tensor_tensor(o, a, b, op), [in0, in1], [out])

    def ts(self, eng, out, in0, s1, op0, s2=None, op1=None, accum=None):
        o, a = out.ap, in0.ap
        reads = [in0]
        v1 = s1
        if isinstance(s1, T):
            reads.append(s1)
            v1 = s1.ap
        v2 = s2
        if isinstance(s2, T):
            reads.append(s2)
            v2 = s2.ap
        kw = {}
        if op1 is not None:
            kw['op1'] = op1
        writes = [out]
        if accum is not None:
            kw['accum_out'] = accum.ap
            writes.append(accum)
        self.op(eng, lambda e: e.tensor_scalar(o, a, v1, v2, op0, **kw), reads, writes)

    def stt(self, eng, out, in0, scalar, in1, op0, op1):
        o, a, b = out.ap, in0.ap, in1.ap
        reads = [in0, in1]
        v = scalar
        if isinstance(scalar, T):
            reads.append(scalar)
            v = scalar.ap
        self.op(eng, lambda e: e.scalar_tensor_tensor(o, a, v, b, op0, op1), reads, [out])

    def copy(self, eng, out, in_):
        o, i = out.ap, in_.ap
        if eng == 'act':
            self.op(eng, lambda e: e.activation(o, i, AF.Copy), [in_], [out])
        else:
            self.op(eng, lambda e: e.tensor_copy(o, i), [in_], [out])

    def memset(self, eng, out, val):
        o = out.ap
        self.op(eng, lambda e: e.memset(o, val), [], [out])

    def reduce(self, eng, out, in_, op, axis=AX.X):
        o, i = out.ap, in_.ap
        self.op(eng, lambda e: e.tensor_reduce(o, i, axis, op), [in_], [out])

    def recip(self, out, in_):
        o, i = out.ap, in_.ap
        self.op('dve', lambda e: e.reciprocal(o, i), [in_], [out])


NORM_EPS = 1e-6
D = 1024
DBG = None
TRACE = False


def build_post(NT, E, H, BLK, has_ctx):
    nc = bass.Bass("TRN2", target_bir_lowering=False)
    p = Prog(nc)
    NTOK = NT * 128
    HC = H // 128
    NB = HC // BLK
    assert NB * BLK == HC
    x_d = p.dram("x", [NTOK, D], F32, "ExternalInput")
    mixT_d = p.dram("mixT", [D, NTOK], F32, "ExternalInput")
    cT_d = p.dram("cT", [128, 8, 2], F32, "ExternalInput")
    modw_d = p.dram("mod_w", [D, 6 * D], F32, "ExternalInput")
    modb_d = p.dram("mod_b", [2, 6 * D], F32, "ExternalInput")
    ng_d = p.dram("norm_g", [4, D], F32, "ExternalInput")
    wout_d = p.dram("w_out", [D, D], F32, "ExternalInput")
    wg_d = p.dram("wg", [E, D, H], F32, "ExternalInput")
    wu_d = p.dram("wu", [E, D, H], F32, "ExternalInput")
    wd_d = p.dram("wd", [E, H, D], F32, "ExternalInput")
    rt_d = p.dram("router", [D, 128], F32, "ExternalInput")
    id_d = p.dram("ident", [128, 128], F32, "ExternalInput")
    out_d = p.dram("out", [NTOK, D], F32, "ExternalOutput")
    mod_s = p.dram("mod_s", [2, 6 * D], F32, "Internal")
    x1_s = p.dram("x1_s", [NTOK, D], F32, "Internal")

    ps = [p.ps(f"ps{i}", [128, 512]) for i in range(8)]
    ident = p.sb("ident", [128, 128], F32)
    p.dma('sp', ident, id_d)
    epsc = p.sb("epsc", [128, 1], F32)
    p.memset('dve', epsc, NORM_EPS)
    h2T = p.sb("h2T", [128, 8, NTOK], BF16)
    gates = p.sb("gates", [128, NT, 8], F32)
    nvar = 2 if has_ctx else 1

    with p.scope() as st:
        cT = p.sb("cT", [128, 8, 2], F32, st)
        sc = p.sb("silu_c", [128, 8, 2], F32, st)
        p.dma('sp', cT, cT_d)
        p.act(sc, cT, AF.Silu)
        modrow = p.sb("modrow", [2, 6 * D], F32, st)
        modb = p.sb("modb", [2, 6 * D], F32, st)
        p.dma('sp', modb, modb_d)
        wblk = [p.sb(f"modw{i}", [128, 8, 512], F32, st) for i in range(2)]
        for cb in range(2, 12):
            wb = wblk[cb % 2]
            p.dma('sp', wb, modw_d.with_ap(modw_d.ap.rearrange("(k p) n -> p k n", p=128)[:, :, cb * 512:(cb + 1) * 512]))
            pp = ps[cb % 2]
            for k in range(8):
                p.mm(pp[0:2, :], sc[:, k, :], wb[:, k, :], start=(k == 0), stop=(k == 7))
            p.tt('dve', modrow[:, cb * 512:(cb + 1) * 512], pp[0:2, :], modb[:, cb * 512:(cb + 1) * 512], ALU.add)
        p.dma('sp', mod_s[:, 2 * D:6 * D], modrow[:, 2 * D:6 * D])

    def bcast_row(dst, src_row_ap):
        p.dma('sp', dst, src_row_ap)

    def rms_rstd(dst_col, src, sq_tmp):
        p.act(sq_tmp, src, AF.Square)
        p.reduce('dve', dst_col, sq_tmp, ALU.add)
        p.act(dst_col, dst_col, AF.Sqrt, bias=epsc, scale=1.0 / D)
        p.recip(dst_col, dst_col)

    with p.scope() as st:
        G1 = [p.sb(f"G1_{v}", [128, D], F32, st) for v in range(nvar)]
        A2 = [p.sb(f"A2_{v}", [128, D], F32, st) for v in range(nvar)]
        B2 = [p.sb(f"B2_{v}", [128, D], F32, st) for v in range(nvar)]
        ngb = p.sb("ngb", [128, 2, D], F32, st)
        bcast_row(ngb[:, 0, :], ng_d.with_ap(ng_d.ap[1:2, :].partition_broadcast(128)))
        bcast_row(ngb[:, 1, :], ng_d.with_ap(ng_d.ap[2:3, :].partition_broadcast(128)))
        for v in range(nvar):
            bcast_row(G1[v], mod_s.with_ap(mod_s.ap[v:v + 1, 2 * D:3 * D].partition_broadcast(128)))
            p.tt('dve', G1[v], G1[v], ngb[:, 0, :], ALU.mult)
            bcast_row(B2[v], mod_s.with_ap(mod_s.ap[v:v + 1, 3 * D:4 * D].partition_broadcast(128)))
            bcast_row(A2[v], mod_s.with_ap(mod_s.ap[v:v + 1, 4 * D:5 * D].partition_broadcast(128)))
            p.ts('dve', A2[v], A2[v], 1.0, ALU.add)
            p.tt('dve', A2[v], A2[v], ngb[:, 1, :], ALU.mult)
        mixT = p.sb("mixT", [128, 8, NTOK], BF16, st)
        mview = mixT_d.ap.rearrange("(k p) n -> p k n", p=128)
        for k in range(8):
            p.dma('pool', mixT[:, k, :], mixT_d.with_ap(mview[:, k, :]))
        wout = p.sb("wout", [128, 8, D], BF16, st)
        wv = wout_d.ap.rearrange("(k p) n -> p k n", p=128)
        for k in range(8):
            p.dma('pool', wout[:, k, :], wout_d.with_ap(wv[:, k, :]))
        if E > 1:
            rt = p.sb("rt", [128, 8, 128], F32, st)
            p.dma('sp', rt, rt_d.with_ap(rt_d.ap.rearrange("(k p) e -> p k e", p=128)))
            h2Tf = p.sb("h2Tf", [128, 8, 128], F32, st)
        xt = [p.sb(f"xt{i}", [128, D], F32, st) for i in range(2)]
        tmp = [p.sb(f"tmp{i}", [128, D], F32, st) for i in range(2)]
        sq = p.sb("sq", [128, D], F32, st)
        cols = p.sb("cols", [128, NT, 8], F32, st)
        lg = p.sb("lg", [128, 4, 8], F32, st)
        for t in range(NT):
            v = 1 if (has_ctx and t == NT - 1) else 0
            x_t, tm = xt[t % 2], tmp[t % 2]
            p.dma('sp', x_t, x_d[t * 128:(t + 1) * 128, :])
            py = [ps[0], ps[1]]
            for fh in range(2):
                for k in range(8):
                    p.mm(py[fh], mixT[:, k, t * 128:(t + 1) * 128], wout[:, k, fh * 512:(fh + 1) * 512], start=(k == 0), stop=(k == 7))
            for fh in range(2):
                p.copy('act', tm[:, fh * 512:(fh + 1) * 512], py[fh])
            rc = cols[:, t, 0:1]
            rms_rstd(rc, tm, sq)
            p.stt('dve', tm, tm, rc, G1[v], ALU.mult, ALU.mult)
            p.tt('pool', x_t, x_t, tm, ALU.add)
            p.dma('sp', x1_s[t * 128:(t + 1) * 128, :], x_t)
            rc2 = cols[:, t, 1:2]
            rms_rstd(rc2, x_t, sq)
            p.stt('dve', tm, x_t, rc2, A2[v], ALU.mult, ALU.mult)
            p.tt('pool', tm, tm, B2[v], ALU.add)
            for k in range(8):
                pt = ps[2 + (k % 4)]
                p.tr(pt[:, 0:128], tm[:, k * 128:(k + 1) * 128], ident)
                p.copy('act' if k % 2 else 'dve', h2T[:, k, t * 128:(t + 1) * 128], pt[:, 0:128])
                if E > 1:
                    p.copy('dve' if k % 2 else 'act', h2Tf[:, k, :], pt[:, 0:128])
            if E > 1 and DBG not in ('norouter', 'e1only', 'e0only'):
                pl = ps[6]
                for k in range(8):
                    p.mm(pl[:, 0:128], h2Tf[:, k, :], rt[:, k, :], start=(k == 0), stop=(k == 7))
                L = lg[:, 0, :]
                p.copy('dve', L, pl[:, 0:8])
                m1 = cols[:, t, 2:3]
                m2 = cols[:, t, 3:4]
                p.reduce('dve', m1, L, ALU.max)
                mk1 = lg[:, 1, :]
                p.ts('dve', mk1, L, m1, ALU.is_equal)
                L2 = lg[:, 2, :]
                p.stt('dve', L2, mk1, -1e30, L, ALU.mult, ALU.add)
                p.reduce('dve', m2, L2, ALU.max)
                mk2 = lg[:, 3, :]
                p.ts('dve', mk2, L2, m2, ALU.is_equal)
                w1 = cols[:, t, 4:5]
                w2 = cols[:, t, 5:6]
                p.tt('dve', w1, m1, m2, ALU.subtract)
                p.act(w1, w1, AF.Sigmoid)
                p.ts('dve', w2, w1, -1.0, ALU.mult, 1.0, ALU.add)
                p.ts('dve', gates[:, t, :], mk1, w1, ALU.mult)
                p.stt('dve', gates[:, t, :], mk2, w2, gates[:, t, :], ALU.mult, ALU.add)

    with p.scope() as st:
        acc = p.sb("acc", [128, NT, D], F32, st)
        wgb = [p.sb(f"wg{i}", [128, 8, BLK * 128], BF16, st) for i in range(2)]
        wub = [p.sb(f"wu{i}", [128, 8, BLK * 128], BF16, st) for i in range(2)]
        wdb = [p.sb(f"wd{i}", [128, BLK, D], BF16, st) for i in range(2)]
        actT = [p.sb(f"actT{i}", [128, BLK, 512], BF16, st) for i in range(2)]
        sg = [p.sb(f"sg{i}", [128, 512], F32, st) for i in range(2)]
        groups = []
        t0 = 0
        while t0 < NT:
            n = min(4, NT - t0)
            groups.append((t0, n))
            t0 += n
        it = 0
        gi = 0
        first = True
        for e in ([1] if DBG == 'e1only' else [0] if DBG == 'e0only' else range(E)):
            wgv = wg_d.ap.rearrange("e (k p) h -> e p k h", p=128)[e]
            wuv = wu_d.ap.rearrange("e (k p) h -> e p k h", p=128)[e]
            wdv = wd_d.ap.rearrange("e (c p) f -> e p c f", p=128)[e]
            for b in range(NB):
                s = it % 2
                it += 1
                hs = slice(b * BLK * 128, (b + 1) * BLK * 128)
                for k in range(8):
                    p.dma('pool', wgb[s][:, k, :], wg_d.with_ap(wgv[:, k, hs]))
                    p.dma('pool', wub[s][:, k, :], wu_d.with_ap(wuv[:, k, hs]))
                for c in range(BLK):
                    p.dma('pool', wdb[s][:, c, :], wd_d.with_ap(wdv[:, b * BLK + c, :]))
                for (t0, n) in groups:
                    ts_ = slice(t0 * 128, (t0 + n) * 128)
                    W = n * 128
                    a = actT[gi % 2]
                    gi += 1
                    for c in range(BLK):
                        pg, pu = ps[(c % 2) * 2], ps[(c % 2) * 2 + 1]
                        for k in range(8):
                            p.mm(pg[:, 0:W], wgb[s][:, k, c * 128:(c + 1) * 128], h2T[:, k, ts_], start=(k == 0), stop=(k == 7))
                        for k in range(8):
                            p.mm(pu[:, 0:W], wub[s][:, k, c * 128:(c + 1) * 128], h2T[:, k, ts_], start=(k == 0), stop=(k == 7))
                        sgt = sg[c % 2]
                        p.act(sgt[:, 0:W], pg[:, 0:W], AF.Silu)
                        p.tt('dve', a[:, c, 0:W], sgt[:, 0:W], pu[:, 0:W], ALU.mult)
                    for j in range(n):
                        t = t0 + j
                        for fh in range(2):
                            pd = ps[4 + ((j * 2 + fh) % 4)]
                            for c in range(BLK):
                                p.mm(pd, a[:, c, j * 128:(j + 1) * 128], wdb[s][:, c, fh * 512:(fh + 1) * 512], start=(c == 0), stop=(c == BLK - 1))
                            dst = acc[:, t, fh * 512:(fh + 1) * 512]
                            gsc = gates[:, t, e:e + 1] if (E > 1 and DBG not in ('norouter', 'nogate', 'e1only', 'e0only')) else 1.0
                            if first:
                                if E > 1 and DBG not in ('norouter', 'nogate', 'e1only', 'e0only'):
                                    p.ts('dve', dst, pd, gsc, ALU.mult)
                                else:
                                    p.copy('dve', dst, pd)
                            else:
                                p.stt('dve', dst, pd, gsc, dst, ALU.mult, ALU.add)
                first = False
        G2 = [p.sb(f"G2_{v}", [128, D], F32, st) for v in range(nvar)]
        ng3 = p.sb("ng3", [128, D], F32, st)
        bcast_row(ng3, ng_d.with_ap(ng_d.ap[3:4, :].partition_broadcast(128)))
        for v in range(nvar):
            bcast_row(G2[v], mod_s.with_ap(mod_s.ap[v:v + 1, 5 * D:6 * D].partition_broadcast(128)))
            p.tt('dve', G2[v], G2[v], ng3, ALU.mult)
        x1t = [p.sb(f"x1t{i}", [128, D], F32, st) for i in range(2)]
        sq2 = p.sb("sq2", [128, D], F32, st)
        cols2 = p.sb("cols2", [128, NT], F32, st)
        for t in range(NT):
            v = 1 if (has_ctx and t == NT - 1) else 0
            x1 = x1t[t % 2]
            p.dma('sp', x1, x1_s[t * 128:(t + 1) * 128, :])
            rc = cols2[:, t:t + 1]
            rms_rstd(rc, acc[:, t, :], sq2)
            p.stt('dve', acc[:, t, :], acc[:, t, :], rc, G2[v], ALU.mult, ALU.mult)
            if DBG == 'x1':
                pass
            elif DBG == 'f':
                p.copy('pool', x1, acc[:, t, :])
            else:
                p.tt('pool', x1, x1, acc[:, t, :], ALU.add)
            p.dma('sp', out_d[t * 128:(t + 1) * 128, :], x1)
    p.finalize()
    return nc


def _cT(c_b, c_ctx):
    a = np.stack([c_b, c_ctx], axis=-1).astype(np.float32)
    return np.ascontiguousarray(a.reshape(8, 128, 2).transpose(1, 0, 2))


_IDENT = np.eye(128, dtype=np.float32)


def run_post(layer, x_lat, ctx, mix_lat, mix_ctx, c, c_ctx, mod_w, mod_b, norm_g, w_out, wg, wu, wd, router, has_ctx, E, H, BLK):
    NT = 17 if has_ctx else 16
    nc = build_post(NT, E, H, BLK, has_ctx)
    in_maps = []
    for core in range(8):
        b, q = core // 4, core % 4
        xs = [x_lat[b, q * 2048:(q + 1) * 2048]]
        ms = [mix_lat[b, q * 2048:(q + 1) * 2048]]
        if has_ctx:
            cs = (q % 2) * 128
            xs.append(ctx[b, cs:cs + 128])
            ms.append(mix_ctx[b, cs:cs + 128])
        xo = np.ascontiguousarray(np.concatenate(xs, 0), dtype=np.float32)
        mo = np.ascontiguousarray(np.concatenate(ms, 0).T, dtype=np.float32)
        in_maps.append({
            "x": xo, "mixT": mo, "cT": _cT(c[b], c_ctx),
            "mod_w": np.ascontiguousarray(mod_w[layer]), "mod_b": np.ascontiguousarray(np.stack([mod_b[layer]] * 2)),
            "norm_g": np.ascontiguousarray(norm_g[layer]), "w_out": np.ascontiguousarray(w_out),
            "wg": np.ascontiguousarray(wg), "wu": np.ascontiguousarray(wu), "wd": np.ascontiguousarray(wd),
            "router": np.ascontiguousarray(np.concatenate([router, np.zeros((1024, 120), np.float32)], 1)), "ident": _IDENT,
        })
    res = run_bass_kernel_spmd(nc, in_maps, core_ids=list(range(8)), trace=TRACE)
    if TRACE:
        print("DEV_NS", res.exec_time_ns)
    x2 = np.empty_like(x_lat)
    ctx2 = np.empty_like(ctx) if has_ctx else None
    for core in range(8):
        b, q = core // 4, core % 4
        o = res.results[core]["out"]
        x2[b, q * 2048:(q + 1) * 2048] = o[0:2048]
        if has_ctx and q < 2:
            ctx2[b, q * 128:(q + 1) * 128] = o[2048:2176]
    return x2, ctx2


def mod_rows(p, ps, st, cT_d, modw_d, modb_d, mod_s, blocks):
    cT = p.sb("cT", [128, 8, 2], F32, st)
    sc = p.sb("silu_c", [128, 8, 2], F32, st)
    p.dma('sp', cT, cT_d)
    p.act(sc, cT, AF.Silu)
    lo, hi = blocks[0] * 512, (blocks[-1] + 1) * 512
    modrow = p.sb("modrow", [2, 6 * D], F32, st)
    modb = p.sb("modb", [2, 6 * D], F32, st)
    p.dma('sp', modb, modb_d)
    wblk = [p.sb(f"modw{i}", [128, 8, 512], F32, st) for i in range(2)]
    for cb in blocks:
        wb = wblk[cb % 2]
        p.dma('sp', wb, modw_d.with_ap(modw_d.ap.rearrange("(k p) n -> p k n", p=128)[:, :, cb * 512:(cb + 1) * 512]))
        pp = ps[cb % 2]
        for k in range(8):
            p.mm(pp[0:2, :], sc[:, k, :], wb[:, k, :], start=(k == 0), stop=(k == 7))
        p.tt('dve', modrow[:, cb * 512:(cb + 1) * 512], pp[0:2, :], modb[:, cb * 512:(cb + 1) * 512], ALU.add)
    p.dma('sp', mod_s[:, lo:hi], modrow[:, lo:hi])


def rope_tm(p, dst1, dst2, x1, x2, cos, sin, t1, t2, e1='dve', e2='pool'):
    p.tt(e1, t1, x1, cos, ALU.mult)
    p.tt(e2, t2, x2, sin, ALU.mult)
    p.tt(e1, t1, t1, t2, ALU.subtract)
    p.tt(e2, t2, x1, sin, ALU.mult)
    p.tt(e1, dst2, x2, cos, ALU.mult)
    p.tt(e1, dst2, dst2, t2, ALU.add)
    p.copy(e2, dst1, t1)


NTA = 66


def build_mix0():
    nc = bass.Bass("TRN2", target_bir_lowering=False)
    p = Prog(nc)
    NTOK = NTA * 128
    x_d = p.dram("x", [NTOK, D], F32, "ExternalInput")
    cT_d = p.dram("cT", [128, 8, 2], F32, "ExternalInput")
    modw_d = p.dram("mod_w", [D, 6 * D], F32, "ExternalInput")
    modb_d = p.dram("mod_b", [2, 6 * D], F32, "ExternalInput")
    ng_d = p.dram("norm_g", [4, D], F32, "ExternalInput")
    win_d = p.dram("w_in", [D, 896], F32, "ExternalInput")
    cos_d = p.dram("cos", [NTOK, 64], F32, "ExternalInput")
    sin_d = p.dram("sin", [NTOK, 64], F32, "ExternalInput")
    dec_d = p.dram("dec", [128, 2], F32, "ExternalInput")
    gb_d = p.dram("gb", [3, 128, 128], F32, "ExternalInput")
    cst_d = p.dram("cst", [6, 128, 128], F32, "ExternalInput")
    out_d = p.dram("mixT", [256, NTOK], F32, "ExternalOutput")
    mod_s = p.dram("mod_s", [2, 6 * D], F32, "Internal")
    qa_s = p.dram("qa_s", [128, NTOK], BF16, "Internal")
    ka_s = p.dram("ka_s", [128, NTOK], BF16, "Internal")
    va_s = p.dram("va_s", [128, NTA, 128], BF16, "Internal")

    ps = [p.ps(f"ps{i}", [128, 512]) for i in range(8)]
    cst = p.sb("cst", [128, 6, 128], F32)
    p.dma('sp', cst, cst_d.with_ap(cst_d.ap.rearrange("c p n -> p c n")))
    ident = cst[:, 0, :]
    gb = p.sb("gb", [128, 3, 128], F32)
    p.dma('sp', gb, gb_d.with_ap(gb_d.ap.rearrange("c p n -> p c n")))
    epsc = p.sb("epsc", [128, 1], F32)
    p.memset('dve', epsc, NORM_EPS)
    ones_bf = p.sb("ones_bf", [128, 128], BF16)
    p.memset('dve', ones_bf, 1.0)
    dec = p.sb("dec", [128, 2], F32)
    p.dma('sp', dec, dec_d)
    lg = p.sb("lg", [128, 2], F32)
    p.act(lg, dec, AF.Exp, scale=-math.log(2.0))
    p.act(lg, lg, AF.Ln, scale=-1.0, bias=1.0)
    MT = p.sb("MT", [128, 2, 128], F32)
    dcol = p.sb("dcol", [128, 8], F32)
    p.act(MT[:, 0, :], cst[:, 1, :], AF.Exp, scale=lg[:, 0:1])
    p.tt('dve', MT[:, 0, :], MT[:, 0, :], cst[:, 3, :], ALU.mult)
    p.act(MT[:, 1, :], cst[:, 2, :], AF.Exp, scale=lg[:, 1:2])
    p.tt('dve', MT[:, 1, :], MT[:, 1, :], cst[:, 4, :], ALU.mult)
    colc = cst[:, 5, :]
    p.act(dcol[:, 0:1], colc[:, 0:1], AF.Exp, scale=lg[:, 0:1])
    p.act(dcol[:, 1:2], colc[:, 1:2], AF.Exp, scale=lg[:, 1:2])
    p.act(dcol[:, 2:3], colc[:, 2:3], AF.Exp, scale=lg[:, 0:1])
    p.act(dcol[:, 3:4], colc[:, 3:4], AF.Exp, scale=lg[:, 1:2])
    p.act(dcol[:, 4:5], colc[:, 4:5], AF.Exp, scale=lg[:, 0:1])
    p.act(dcol[:, 5:6], colc[:, 4:5], AF.Exp, scale=lg[:, 1:2])

    QrT = p.sb("QrT", [128, NTOK], BF16)
    KrT = p.sb("KrT", [128, NTOK], BF16)
    Vr = p.sb("Vr", [128, NTA, 128], BF16)
    Kd = [p.sb(f"Kd{d}", [128, NTA, 128], BF16) for d in range(2)]
    Gs = p.sb("Gs", [128, NTA, 128], BF16)

    with p.scope() as st0:
        with p.scope() as st:
            mod_rows(p, ps, st, cT_d, modw_d, modb_d, mod_s, [0, 1, 2, 3])
        st = st0
        A1 = [p.sb(f"A1_{v}", [128, D], F32, st) for v in range(2)]
        B1 = [p.sb(f"B1_{v}", [128, D], F32, st) for v in range(2)]
        ngb = p.sb("ngb", [128, D], F32, st)
        p.dma('sp', ngb, ng_d.with_ap(ng_d.ap[0:1, :].partition_broadcast(128)))
        for v in range(2):
            p.dma('sp', B1[v], mod_s.with_ap(mod_s.ap[v:v + 1, 0:D].partition_broadcast(128)))
            p.dma('sp', A1[v], mod_s.with_ap(mod_s.ap[v:v + 1, D:2 * D].partition_broadcast(128)))
            p.ts('dve', A1[v], A1[v], 1.0, ALU.add)
            p.tt('dve', A1[v], A1[v], ngb, ALU.mult)
        W = p.sb("W", [128, 8, 896], BF16, st)
        wv = win_d.ap.rearrange("(k p) n -> p k n", p=128)
        for k in range(8):
            p.dma('pool', W[:, k, :], win_d.with_ap(wv[:, k, :]))
        xt = [p.sb(f"xt{i}", [128, D], F32, st) for i in range(2)]
        tms = [p.sb(f"tm{i}", [128, D], F32, st) for i in range(2)]
        sqs = [p.sb(f"sq{i}", [128, D], F32, st) for i in range(2)]
        hT = [p.sb(f"hT{i}", [128, 8, 128], BF16, st) for i in range(2)]
        cs = [p.sb(f"cs{i}", [128, 2, 64], F32, st) for i in range(2)]
        pr = [p.sb(f"pr{i}", [128, 896], F32, st) for i in range(2)]
        ro = [p.sb(f"ro{i}", [128, 4, 128], F32, st) for i in range(2)]
        rts = [p.sb(f"rt{i}", [128, 4, 64], F32, st) for i in range(2)]
        colsP = [p.sb(f"cols{i}", [128, NTA, 4], F32, st) for i in range(2)]
        stg = [p.sb(f"stg{i}", [128, 3, 128], BF16, st) for i in range(2)]

        def tile(t):
            v = 1 if t < 2 else 0
            par = t % 2
            pb_ = ps[par * 4:par * 4 + 4]
            tm, sq, rt = tms[par], sqs[par], rts[par]
            cols = colsP[par]
            x_t = xt[par]
            p.dma('sp', x_t, x_d[t * 128:(t + 1) * 128, :])
            c_t = cs[par]
            p.dma('sp', c_t[:, 0, :], cos_d[t * 128:(t + 1) * 128, :])
            p.dma('sp', c_t[:, 1, :], sin_d[t * 128:(t + 1) * 128, :])
            rc = cols[:, t, 0:1]
            p.act(sq, x_t, AF.Square)
            p.reduce('dve', rc, sq, ALU.add)
            yield
            p.act(rc, rc, AF.Sqrt, bias=epsc, scale=1.0 / D)
            p.recip(rc, rc)
            p.stt('dve', tm, x_t, rc, A1[v], ALU.mult, ALU.mult)
            p.tt('pool', tm, tm, B1[v], ALU.add)
            yield
            h = hT[par]
            for k in range(8):
                pt = pb_[2 + (k % 2)]
                p.tr(pt[:, 0:128], tm[:, k * 128:(k + 1) * 128], ident)
                p.copy('act' if k % 2 else 'dve', h[:, k, :], pt[:, 0:128])
                if k % 4 == 3:
                    yield
            for k in range(8):
                p.mm(pb_[0], h[:, k, :], W[:, k, 0:512], start=(k == 0), stop=(k == 7))
            for k in range(8):
                p.mm(pb_[1][:, 0:384], h[:, k, :], W[:, k, 512:896], start=(k == 0), stop=(k == 7))
            P = pr[par]
            p.copy('act', P[:, 0:512], pb_[0])
            p.copy('dve', P[:, 512:896], pb_[1][:, 0:384])
            yield
            p.ts('pool', P[:, 128:256], P[:, 128:256], 128.0 ** -0.5, ALU.mult)
            p.copy('pool', Vr[:, t, :], P[:, 256:384])
            p.act(Gs[:, t, :], P[:, 384:512], AF.Silu)
            sg = stg[par]
            p.copy('pool', sg[:, 2, :], P[:, 768:896])
            p.dma('sp', va_s[:, t, :], sg[:, 2, :])
            for i, (c0, gi) in enumerate(((512, 1), (640, 2))):
                rcq = cols[:, t, 1 + i:2 + i]
                p.act(sq[:, 0:128], P[:, c0:c0 + 128], AF.Square)
                p.reduce('dve', rcq, sq[:, 0:128], ALU.add)
                p.act(rcq, rcq, AF.Sqrt, bias=epsc, scale=1.0 / 128)
                p.recip(rcq, rcq)
                p.stt('dve', P[:, c0:c0 + 128], P[:, c0:c0 + 128], rcq, gb[:, gi, :], ALU.mult, ALU.mult)
                yield
            R = ro[par]
            for i, c0 in enumerate((0, 128, 512, 640)):
                rope_tm(p, R[:, i, 0:64], R[:, i, 64:128], P[:, c0:c0 + 64], P[:, c0 + 64:c0 + 128],
                        c_t[:, 0, :], c_t[:, 1, :], rt[:, i, :], sq[:, 128 + 64 * i:192 + 64 * i],
                        e1='dve' if i % 2 == 0 else 'pool', e2='pool' if i % 2 == 0 else 'dve')
                if i % 2:
                    yield
            p.ts('dve', Kd[0][:, t, :], R[:, 1, :], dcol[:, 2:3], ALU.mult)
            p.ts('pool', Kd[1][:, t, :], R[:, 1, :], dcol[:, 3:4], ALU.mult)
            ts_ = slice(t * 128, (t + 1) * 128)
            for i in range(4):
                pt = pb_[i]
                p.tr(pt[:, 0:128], R[:, i, :], ident)
                if i == 0:
                    p.copy('act', QrT[:, ts_], pt[:, 0:128])
                elif i == 1:
                    p.copy('dve', KrT[:, ts_], pt[:, 0:128])
                else:
                    p.copy('act' if i == 2 else 'dve', sg[:, i - 2, :], pt[:, 0:128])
            p.dma('sp', qa_s[:, ts_], sg[:, 0, :])
            p.dma('sp', ka_s[:, ts_], sg[:, 1, :])
            yield

        live, nxt_t, tick = [], 0, 0
        while live or nxt_t < NTA:
            if nxt_t < NTA and (not live or (len(live) < 2 and tick >= 4)):
                live.append(tile(nxt_t))
                nxt_t += 1
                tick = 0
            for g_ in list(live):
                try:
                    next(g_)
                except StopIteration:
                    live.remove(g_)
            tick += 1

    with p.scope() as st:
        Y = p.sb("Y", [128, NTA, 128], F32, st)
        S = p.sb("S", [128, 128], F32, st)
        Sb = p.sb("Sb", [128, 128], BF16, st)
        AT = [p.sb(f"AT{i}", [128, 128], BF16, st) for i in range(2)]
        for d in range(2):
            order = list(range(NTA)) if d == 0 else [1, 0] + list(range(NTA - 1, 1, -1))
            p.memset('dve', S, 0.0)
            p.memset('pool', Sb, 0.0)
            for n, c in enumerate(order):
                cs_ = slice(c * 128, (c + 1) * 128)
                pS, pP1, pP2, pKV = ps[n % 2], ps[2 + n % 2], ps[4 + n % 2], ps[6 + n % 2]
                p.mm(pS[:, 0:128], KrT[:, cs_], QrT[:, cs_])
                a = AT[n % 2]
                p.tt('dve', a, pS[:, 0:128], MT[:, d, :], ALU.mult)
                p.mm(pP1[:, 0:128], a, Vr[:, c, :])
                p.mm(pP2[:, 0:128], QrT[:, cs_], Sb)
                if d == 0:
                    p.copy('act', Y[:, c, :], pP1[:, 0:128])
                else:
                    p.tt('pool' if False else 'dve', Y[:, c, :], pP1[:, 0:128], Y[:, c, :], ALU.add)
                p.stt('dve', Y[:, c, :], pP2[:, 0:128], dcol[:, d:d + 1], Y[:, c, :], ALU.mult, ALU.add)
                p.mm(pKV[:, 0:128], Kd[d][:, c, :], Vr[:, c, :])
                p.stt('dve', S, S, dcol[:, 4 + d:5 + d], pKV[:, 0:128], ALU.mult, ALU.add)
                p.copy('act', Sb, S)
        OUT = p.sb("OUT", [128, NTOK], F32, st)
        sq2 = p.sb("sq2", [128, 128], F32, st)
        cols2 = p.sb("cols2", [128, NTA], F32, st)
        for c in range(NTA):
            rc = cols2[:, c:c + 1]
            p.act(sq2, Y[:, c, :], AF.Square)
            p.reduce('dve', rc, sq2, ALU.add)
            p.act(rc, rc, AF.Sqrt, bias=epsc, scale=1.0 / 128)
            p.recip(rc, rc)
            p.stt('dve', Y[:, c, :], Y[:, c, :], rc, gb[:, 0, :], ALU.mult, ALU.mult)
            p.tt('pool', Y[:, c, :], Y[:, c, :], Gs[:, c, :], ALU.mult)
            pt = ps[c % 4]
            p.tr(pt[:, 0:128], Y[:, c, :], ident)
            p.copy('act', OUT[:, c * 128:(c + 1) * 128], pt[:, 0:128])
        p.dma('sp', out_d[0:128, :], OUT)

    with p.scope() as st:
        QaT = p.sb("QaT", [128, NTOK], BF16, st)
        KaT = p.sb("KaT", [128, NTOK], BF16, st)
        Va = p.sb("Va", [128, NTA, 128], BF16, st)
        p.dma('sp', QaT, qa_s)
        p.dma('sp', KaT, ka_s)
        p.dma('sp', Va, va_s)
        g2 = p.sb("g2", [128, 2, 128], F32, st)
        mx = p.sb("mx", [128, 4], F32, st)
        p.act(g2[:, 0, :], gb[:, 1, :], AF.Square)
        p.act(g2[:, 1, :], gb[:, 2, :], AF.Square)
        p.reduce('dve', mx[:, 0:1], g2[:, 0, :], ALU.max)
        p.reduce('dve', mx[:, 1:2], g2[:, 1, :], ALU.max)
        p.tt('dve', mx[:, 2:3], mx[:, 0:1], mx[:, 1:2], ALU.mult)
        p.act(mx[:, 3:4], mx[:, 2:3], AF.Sqrt, scale=128.0)
        p.ts('dve', mx[:, 3:4], mx[:, 3:4], -1.0, ALU.mult)
        negC = mx[:, 3:4]
        PT = [p.sb(f"PT{i}", [128, 512], BF16, st) for i in range(3)]
        rec = [p.sb(f"rec{i}", [128, 512], F32, st) for i in range(2)]
        og = [p.sb(f"og{i}", [128, 512], F32, st) for i in range(2)]
        groups = [(0, 256, [0, 1])] + [(256 + g * 512, 512, list(range(NTA))) for g in range(16)]
        it = 0
        for gi, (q0, W_, keys) in enumerate(groups):
            pO, pD = ps[4 + (gi % 2) * 2], ps[5 + (gi % 2) * 2]
            for n, kt in enumerate(keys):
                pS = ps[it % 3]
                pt_ = PT[it % 3]
                it += 1
                p.mm(pS[:, 0:W_], KaT[:, kt * 128:(kt + 1) * 128], QaT[:, q0:q0 + W_])
                p.act(pt_[:, 0:W_], pS[:, 0:W_], AF.Exp, bias=negC, scale=128.0 ** -0.5)
                p.mm(pO[:, 0:W_], Va[:, kt, :], pt_[:, 0:W_], start=(n == 0), stop=(n == len(keys) - 1))
                p.mm(pD[:, 0:W_], ones_bf, pt_[:, 0:W_], start=(n == 0), stop=(n == len(keys) - 1))
            r, o = rec[gi % 2], og[gi % 2]
            p.recip(r[:, 0:W_], pD[:, 0:W_])
            p.tt('dve', o[:, 0:W_], pO[:, 0:W_], r[:, 0:W_], ALU.mult)
            p.dma('sp', out_d[128:256, q0:q0 + W_], o[:, 0:W_])
    p.finalize()
    return nc


def _rope_tables(dim, rows=128, grid_w=64, theta=10000.0):
    row = np.repeat(np.arange(rows, dtype=np.float32), grid_w)
    col = np.tile(np.arange(grid_w, dtype=np.float32), rows)
    n_freq = dim // 4
    freqs = (np.float32(theta) ** (-np.arange(n_freq, dtype=np.float32) / np.float32(n_freq))).astype(np.float32)
    ang = np.concatenate([row[:, None] * freqs, col[:, None] * freqs], axis=-1).astype(np.float32)
    return np.cos(ang).astype(np.float32), np.sin(ang).astype(np.float32)


def _with_ctx_rope(cos, sin):
    n = cos.shape[1]
    c = np.concatenate([np.ones((256, n), np.float32), cos], 0)
    s = np.concatenate([np.zeros((256, n), np.float32), sin], 0)
    return np.ascontiguousarray(c), np.ascontiguousarray(s)


def _mix0_consts():
    ip = np.arange(128, dtype=np.float32)[:, None]
    i = np.arange(128, dtype=np.float32)[None, :]
    cst = np.zeros((6, 128, 128), np.float32)
    cst[0] = np.eye(128, dtype=np.float32)
    cst[1] = np.maximum(i - ip, 0)
    cst[2] = np.maximum(ip - i, 0)
    cst[3] = (i >= ip)
    cst[4] = (ip > i)
    cst[5, :, 0] = ip[:, 0] + 1
    cst[5, :, 1] = 128 - ip[:, 0]
    cst[5, :, 2] = 127 - ip[:, 0]
    cst[5, :, 3] = ip[:, 0]
    cst[5, :, 4] = 128
    return cst


def run_mix0(x, ctx, c, c_ctx, mod_w, mod_b, norm_g, w_in, decay_exp, ret_g, q_g, k_g):
    nc = build_mix0()
    cos, sin = _with_ctx_rope(*_rope_tables(128))
    cst = _mix0_consts()
    in_maps = []
    for core in range(8):
        b, j = core // 4, core % 4
        kv = j // 2
        colsel = np.concatenate([np.arange(j * 128, (j + 1) * 128) + off for off in (0, 512, 1024, 1536, 2048)] +
                                [np.arange(kv * 128, (kv + 1) * 128) + off for off in (2560, 2816)])
        gb = np.stack([np.broadcast_to(ret_g[j * 128:(j + 1) * 128], (128, 128)),
                       np.broadcast_to(q_g, (128, 128)), np.broadcast_to(k_g, (128, 128))]).astype(np.float32)
        in_maps.append({
            "x": np.ascontiguousarray(np.concatenate([ctx[b], x[b]], 0), dtype=np.float32),
            "cT": _cT(c[b], c_ctx), "mod_w": np.ascontiguousarray(mod_w), "mod_b": np.ascontiguousarray(np.stack([mod_b] * 2)),
            "norm_g": np.ascontiguousarray(norm_g), "w_in": np.ascontiguousarray(w_in[:, colsel]),
            "cos": cos, "sin": sin,
            "dec": np.ascontiguousarray(np.broadcast_to(decay_exp[:, j], (128, 2)), dtype=np.float32),
            "gb": np.ascontiguousarray(gb), "cst": cst,
        })
    res = run_bass_kernel_spmd(nc, in_maps, core_ids=list(range(8)), trace=TRACE)
    if TRACE:
        print("DEV_NS", res.exec_time_ns)
    mix = np.empty((2, NTA * 128, 1024), np.float32)
    for core in range(8):
        b, j = core // 4, core % 4
        o = res.results[core]["mixT"]
        mix[b, :, j * 128:(j + 1) * 128] = o[0:128].T
        mix[b, :, 512 + j * 128:512 + (j + 1) * 128] = o[128:256].T
    return mix[:, 256:], mix[:, :256]


LAM_INIT1 = 0.8 - 0.6 * math.exp(-0.3 * 1)
RWKV_LN_EPS = 64e-5
C_ID, C_TRI0, C_TRI1, C_MS0, C_MI0, C_MS1, C_SH, C_SHP, C_SHN, C_ONE = range(10)
B_W00, B_W01, B_A00, B_A01, B_KK, B_KA, B_RK, B_LNG, B_LNB = range(9)


def build_mix1():
    nc = bass.Bass("TRN2", target_bir_lowering=False)
    p = Prog(nc)
    NTOK = NTA * 128
    NLAT = 64 * 128
    x_d = p.dram("x", [NTOK, D], F32, "ExternalInput")
    cT_d = p.dram("cT", [128, 8, 2], F32, "ExternalInput")
    modw_d = p.dram("mod_w", [D, 6 * D], F32, "ExternalInput")
    modb_d = p.dram("mod_b", [2, 6 * D], F32, "ExternalInput")
    ng_d = p.dram("norm_g", [4, D], F32, "ExternalInput")
    win_d = p.dram("w_in", [D, 1152], F32, "ExternalInput")
    mu_d = p.dram("mu", [128, 768], F32, "ExternalInput")
    cos_d = p.dram("cos", [NTOK, 32], F32, "ExternalInput")
    sin_d = p.dram("sin", [NTOK, 32], F32, "ExternalInput")
    lam_d = p.dram("lamv", [128, 4, 64], F32, "ExternalInput")
    sg_d = p.dram("subg", [128, 1], F32, "ExternalInput")
    bc_d = p.dram("bc", [9, 128, 128], F32, "ExternalInput")
    mat_d = p.dram("mats", [3, 128, 128], F32, "ExternalInput")
    cst_d = p.dram("cst", [10, 128, 128], F32, "ExternalInput")
    out_d = p.dram("mixT", [256, NLAT], F32, "ExternalOutput")
    mod_s = p.dram("mod_s", [2, 6 * D], F32, "Internal")
    qd_s = p.dram("qd_s", [128, NTOK], BF16, "Internal")
    kd_s = p.dram("kd_s", [128, NTOK], BF16, "Internal")
    vd_s = p.dram("vd_s", [128, NTA, 128], BF16, "Internal")
    st_s = p.dram("st_s", [NTA, 128, 768], F32, "Internal")

    ps = [p.ps(f"ps{i}", [128, 512]) for i in range(8)]
    cst = p.sb("cst", [128, 10, 128], F32)
    p.dma('sp', cst, cst_d.with_ap(cst_d.ap.rearrange("c p n -> p c n")))
    cstb = p.sb("cstb", [128, 10, 128], BF16)
    p.copy('dve', cstb, cst)
    ident = cst[:, C_ID, :]
    bc = p.sb("bc", [128, 9, 128], F32)
    p.dma('sp', bc, bc_d.with_ap(bc_d.ap.rearrange("c p n -> p c n")))
    mats = p.sb("mats", [128, 3, 128], F32)
    p.dma('sp', mats, mat_d.with_ap(mat_d.ap.rearrange("c p n -> p c n")))
    epsc = p.sb("epsc", [128, 2], F32)
    p.memset('dve', epsc[:, 0:1], NORM_EPS)
    p.memset('dve', epsc[:, 1:2], RWKV_LN_EPS)
    ones_bf = cstb[:, C_ONE, :]
    Yacc = p.sb("Yacc", [128, NTA, 128], F32)
    Vst = p.sb("Vst", [128, NTA, 128], BF16)
    Gst = p.sb("Gst", [128, NTA, 128], BF16)
    BCf = p.sb("BCf", [128, NTA, 2], F32)
    ST = [[p.sb(f"ST{d}{h}", [64, 64], F32) for h in range(2)] for d in range(2)]
    YH = [Yacc, Yacc]

    _tmpn = [0]

    def scan_step(st, d, t, r, kd, v, kk, b, logw, first):
        T_ = TMP
        TRI = cst[:, C_TRI0 + d, :]
        MS = cst[:, C_MS0 if d == 0 else C_MS1, :]
        MST = cst[:, C_MS1 if d == 0 else C_MS0, :]
        MA = cst[:, C_MI0 if d == 0 else C_MS1, :]
        pc, pt = ps[0], ps[1]
        p.mm(pc[:, 0:128], TRI, logw)
        p.mm(pt[:, 0:128], cst[:, C_ONE, :], logw)
        cum = T_['cum']
        p.copy('act', cum, pc[:, 0:128])
        e_neg, e_x, e_in, e_end = T_['e_neg'], T_['e_x'], T_['e_in'], T_['e_end']
        p.act(e_neg, cum, AF.Exp, scale=-1.0)
        p.tt('dve', e_x, cum, logw, ALU.subtract)
        p.act(e_x, e_x, AF.Exp)
        if d == 0:
            p.act(e_in, cum, AF.Exp)
        p.tt('dve', e_end, pt[:, 0:128], cum, ALU.subtract)
        p.act(e_end, e_end, AF.Exp)
        at, bt, kt, rt, Bp, Kp = T_['at'], T_['bt'], T_['kt'], T_['rt'], T_['Bp'], T_['Kp']
        p.stt('dve', at, kk, -1.0, e_x, ALU.mult, ALU.mult)
        p.tt('pool', bt, b, e_neg, ALU.mult)
        p.tt('pool', kt, kd, e_neg, ALU.mult)
        p.tt('dve', rt, r, e_in if d == 0 else e_x, ALU.mult)
        p.tt('pool', Bp, b, e_end, ALU.mult)
        p.tt('pool', Kp, kd, e_end, ALU.mult)
        def chain(hh):
            banks = [ps[2 + hh], ps[4 + hh], ps[6 + hh]]
            bi = [0]

            def nb():
                bk = banks[bi[0] % 3]
                bi[0] += 1
                return bk

            hs_ = slice(hh * 64, (hh + 1) * 64)
            fm = {}
            for i, (nm, src) in enumerate((('at', at), ('bt', bt), ('kt', kt), ('rt', rt))):
                pp = nb()
                p.tr(pp[0:64, 0:128], src[:, hs_], ident)
                dst = T_[f'{nm}T{hh}']
                p.copy('act' if i % 2 else 'dve', dst, pp[0:64, 0:128])
                fm[nm] = dst
                if i % 2:
                    yield
            wc = T_[f'wc{hh}']
            pw_ = nb()
            p.mm(pw_[0:64, 0:1], logw[:, hs_], cst[:, C_ONE, 0:1])
            p.act(wc, pw_[0:64, 0:1], AF.Exp)
            LT, L, LakT, ArbT, ArkT = (T_[f'{n_}{hh}'] for n_ in ('LT', 'L', 'LakT', 'ArbT', 'ArkT'))
            for i, (dst, lhs, rhs, msk) in enumerate(((LT, fm['bt'], fm['at'], MS), (L, fm['at'], fm['bt'], MST),
                                                       (LakT, fm['kt'], fm['at'], MS), (ArbT, fm['bt'], fm['rt'], MA),
                                                       (ArkT, fm['kt'], fm['rt'], MA))):
                pp = nb()
                p.mm(pp[:, 0:128], lhs, rhs)
                p.tt('dve', dst, pp[:, 0:128], msk, ALU.mult)
                if i in (1, 4):
                    yield
            G, Pa, PaT, Pb, PbT = (T_[f'{n_}{hh}'] for n_ in ('G', 'Pa', 'PaT', 'Pb', 'PbT'))
            p.tt('pool', G, LT, ident, ALU.add)
            cur, curT = L, LT
            nxt = [(Pa, PaT), (Pb, PbT)]
            for lev in range(1, 7):
                Pn, PnT = nxt[lev % 2]
                pp = nb()
                p.mm(pp[:, 0:128], curT, cur)
                if lev < 6:
                    pp2 = nb()
                    p.mm(pp2[:, 0:128], cur, curT)
                p.copy('act', Pn, pp[:, 0:128])
                if lev < 6:
                    p.copy('dve', PnT, pp2[:, 0:128])
                yield
                pq = nb()
                p.mm(pq[:, 0:128], Pn, G)
                p.tt('dve', G, pq[:, 0:128], G, ALU.add)
                cur, curT = Pn, PnT
                yield
            S = ST[d][hh]
            X, U = T_[f'X{hh}'], T_[f'U{hh}']
            px = nb()
            vh = v[:, hs_]
            p.mm(px[:, 0:64], fm['at'], S, start=True, stop=False)
            p.mm(px[:, 0:64], LakT, vh, start=False, stop=True)
            p.copy('act', X, px[:, 0:64])
            yield
            pu = nb()
            p.mm(pu[:, 0:64], G, X)
            p.copy('dve', U, pu[:, 0:64])
            yield
            py = nb()
            p.mm(py[:, 0:64], fm['rt'], S, start=True, stop=False)
            p.mm(py[:, 0:64], ArbT, U, start=False, stop=False)
            p.mm(py[:, 0:64], ArkT, vh, start=False, stop=True)
            pss = nb()
            p.mm(pss[0:64, 0:64], Bp[:, hs_], U, start=True, stop=False)
            p.mm(pss[0:64, 0:64], Kp[:, hs_], vh, start=False, stop=True)
            ydst = YH[hh][:, t, hs_]
            if first:
                p.copy('act', ydst, py[:, 0:64])
            else:
                p.tt('dve', ydst, py[:, 0:64], ydst, ALU.add)
            p.stt('dve', S, S, wc, pss[0:64, 0:64], ALU.mult, ALU.add)
            yield

        live = [chain(0), chain(1)]
        while live:
            for g_ in list(live):
                try:
                    next(g_)
                except StopIteration:
                    live.remove(g_)

    with p.scope() as st0:
        with p.scope() as st:
            mod_rows(p, ps, st, cT_d, modw_d, modb_d, mod_s, [0, 1, 2, 3])
        st = st0
        TMP = {}
        for nm in ('cum', 'e_neg', 'e_x', 'e_in', 'e_end', 'at', 'bt', 'kt', 'rt', 'Bp', 'Kp'):
            TMP[nm] = p.sb("t_" + nm, [128, 128], F32, st)
        for hh in range(2):
            for nm in ('LT', 'L', 'LakT', 'ArbT', 'ArkT', 'G', 'Pa', 'PaT', 'Pb', 'PbT'):
                TMP[f'{nm}{hh}'] = p.sb(f"t_{nm}{hh}", [128, 128], F32, st)
            for nm in ('X', 'U'):
                TMP[f'{nm}{hh}'] = p.sb(f"t_{nm}{hh}", [128, 64], F32, st)
        for hh in range(2):
            for nm in ('at', 'bt', 'kt', 'rt'):
                TMP[f'{nm}T{hh}'] = p.sb(f"t_{nm}T{hh}", [64, 128], F32, st)
            TMP[f'wc{hh}'] = p.sb(f"t_wc{hh}", [64, 1], F32, st)
        for d in range(2):
            for hh in range(2):
                p.memset('dve', ST[d][hh], 0.0)
        with p.scope() as st:
            A1 = [p.sb(f"A1_{v}", [128, D], F32, st) for v in range(2)]
            B1 = [p.sb(f"B1_{v}", [128, D], F32, st) for v in range(2)]
            ngb = p.sb("ngb", [128, D], F32, st)
            p.dma('sp', ngb, ng_d.with_ap(ng_d.ap[0:1, :].partition_broadcast(128)))
            for v in range(2):
                p.dma('sp', B1[v], mod_s.with_ap(mod_s.ap[v:v + 1, 0:D].partition_broadcast(128)))
                p.dma('sp', A1[v], mod_s.with_ap(mod_s.ap[v:v + 1, D:2 * D].partition_broadcast(128)))
                p.ts('dve', A1[v], A1[v], 1.0, ALU.add)
                p.tt('dve', A1[v], A1[v], ngb, ALU.mult)
            Wd = p.sb("Wd", [128, 8, 384], BF16, st)
            W1 = p.sb("W1", [128, 8, 768], BF16, st)
            W2 = p.sb("W2", [128, 8, 768], BF16, st)
            mu = p.sb("mu", [128, 2, 768], F32, st)
            p.dma('sp', mu[:, 0, :], mu_d)
            p.ts('dve', mu[:, 1, :], mu[:, 0, :], 0.5, ALU.mult)
            p.ts('dve', mu[:, 0, :], mu[:, 0, :], -1.0, ALU.mult, 1.0, ALU.add)
            wv = win_d.ap.rearrange("(k p) n -> p k n", p=128)
            wst = [p.sb(f"wst{i}", [128, 768], F32, st) for i in range(2)]
            for k in range(8):
                p.dma('pool', Wd[:, k, :], win_d.with_ap(wv[:, k, 0:384]))
                w_ = wst[k % 2]
                p.dma('sp', w_, win_d.with_ap(wv[:, k, 384:1152]))
                p.tt('dve', W1[:, k, :], w_, mu[:, 0, :], ALU.mult)
                p.tt('pool', W2[:, k, :], w_, mu[:, 1, :], ALU.mult)
            xt = [p.sb(f"xt{i}", [128, D], F32, st) for i in range(2)]
            tm = p.sb("tm", [128, D], F32, st)
            sq = p.sb("sq", [128, D], F32, st)
            hb = [p.sb(f"hb{i}", [128, D], BF16, st) for i in range(3)]
            hT = p.sb("hT", [128, 8, 128], BF16, st)
            hsT = p.sb("hsT", [128, 8, 128], BF16, st)
            cs = [p.sb(f"cs{i}", [128, 2, 32], F32, st) for i in range(2)]
            Pd = p.sb("Pd", [128, 384], F32, st)
            RKV = p.sb("RKV", [128, 384], F32, st)
            WAG = p.sb("WAG", [128, 384], F32, st)
            Rr = p.sb("Rr", [128, 2, 128], F32, st)
            rtmp = p.sb("rtmp", [128, 8, 32], F32, st)
            cols = p.sb("cols", [128, NTA, 8], F32, st)
            stg = [p.sb(f"stg{i}", [128, 3, 128], BF16, st) for i in range(2)]
            thT = p.sb("thT", [128, 3, 128], F32, st)
            Q = {}
            for nm in ('th', 'kk', 'tq', 'a0', 'a1', 'lw0', 'kd0', 'b0'):
                Q[nm] = p.sb("q_" + nm, [128, 128], F32, st)
            stash = wst

            def stage1(t):
                v = 1 if t < 2 else 0
                x_t = xt[t % 2]
                p.dma('sp', x_t, x_d[t * 128:(t + 1) * 128, :])
                rc = cols[:, t, 0:1]
                p.act(sq, x_t, AF.Square)
                p.reduce('dve', rc, sq, ALU.add)
                p.act(rc, rc, AF.Sqrt, bias=epsc[:, 0:1], scale=1.0 / D)
                p.recip(rc, rc)
                p.stt('dve', tm, x_t, rc, A1[v], ALU.mult, ALU.mult)
                p.tt('pool', hb[t % 3], tm, B1[v], ALU.add)

            stage1(0)
            for t in range(NTA):
                if t + 1 < NTA:
                    stage1(t + 1)
                has_prev = t not in (0, 2)
                has_next = t not in (1, NTA - 1)
                h = hb[t % 3]
                for k in range(8):
                    ks = slice(k * 128, (k + 1) * 128)
                    pa = ps[k // 4][:, (k % 4) * 128:(k % 4 + 1) * 128]
                    p.mm(pa, h[:, ks], cstb[:, C_ID, :])
                    pb = ps[2 + k // 4][:, (k % 4) * 128:(k % 4 + 1) * 128]
                    p.mm(pb, h[:, ks], cstb[:, C_SH, :], start=True, stop=not (has_prev or has_next))
                    if has_prev:
                        p.mm(pb, hb[(t - 1) % 3][:, ks], cstb[:, C_SHP, :], start=False, stop=not has_next)
                    if has_next:
                        p.mm(pb, hb[(t + 1) % 3][:, ks], cstb[:, C_SHN, :], start=False, stop=True)
                for half in range(2):
                    p.copy('act', hT[:, half * 4:(half + 1) * 4, :], ps[half][:, :].with_ap(ps[half].ap.rearrange("p (k n) -> p k n", k=4)))
                    p.copy('dve', hsT[:, half * 4:(half + 1) * 4, :], ps[2 + half].with_ap(ps[2 + half].ap.rearrange("p (k n) -> p k n", k=4)))
                for k in range(8):
                    p.mm(ps[4][:, 0:384], hT[:, k, :], Wd[:, k, :], start=(k == 0), stop=(k == 7))
                for c0, pp in ((0, ps[5]), (384, ps[6])):
                    for k in range(8):
                        p.mm(pp[:, 0:384], hT[:, k, :], W1[:, k, c0:c0 + 384], start=(k == 0), stop=False)
                    for k in range(8):
                        p.mm(pp[:, 0:384], hsT[:, k, :], W2[:, k, c0:c0 + 384], start=False, stop=(k == 7))
                p.copy('act', Pd, ps[4][:, 0:384])
                p.copy('dve', RKV, ps[5][:, 0:384])
                p.copy('act', WAG, ps[6][:, 0:384])
                c_t = cs[t % 2]
                p.dma('sp', c_t[:, 0, :], cos_d[t * 128:(t + 1) * 128, :])
                p.dma('sp', c_t[:, 1, :], sin_d[t * 128:(t + 1) * 128, :])
                for i in range(4):
                    c0 = i * 64
                    rope_tm(p, Rr[:, i // 2, (i % 2) * 64:(i % 2) * 64 + 32], Rr[:, i // 2, (i % 2) * 64 + 32:(i % 2) * 64 + 64],
                            Pd[:, c0:c0 + 32], Pd[:, c0 + 32:c0 + 64], c_t[:, 0, :], c_t[:, 1, :],
                            rtmp[:, 2 * i, :], rtmp[:, 2 * i + 1, :],
                            e1='dve' if i % 2 == 0 else 'pool', e2='pool' if i % 2 == 0 else 'dve')
                sg = stg[t % 2]
                ts_ = slice(t * 128, (t + 1) * 128)
                for i in range(2):
                    pp = ps[i]
                    p.tr(pp[:, 0:128], Rr[:, i, :], ident)
                    p.copy('act' if i else 'dve', sg[:, i, :], pp[:, 0:128])
                p.copy('pool', sg[:, 2, :], Pd[:, 256:384])
                p.dma('sp', qd_s[:, ts_], sg[:, 0, :])
                p.dma('sp', kd_s[:, ts_], sg[:, 1, :])
                p.dma('sp', vd_s[:, t, :], sg[:, 2, :])
                r_, k_, v_ = RKV[:, 0:128], RKV[:, 128:256], RKV[:, 256:384]
                p.copy('pool', Vst[:, t, :], v_)
                th = Q['th']
                p.act(th, WAG[:, 0:128], AF.Tanh)
                p.tr(ps[0][:, 0:128], th, ident)
                p.copy('dve', thT[:, 0, :], ps[0][:, 0:128])
                p.tr(ps[1][:, 0:128], WAG[:, 128:256], ident)
                p.copy('act', thT[:, 1, :], ps[1][:, 0:128])
                p.act(th, WAG[:, 256:384], AF.Sigmoid)
                p.tr(ps[2][:, 0:128], th, ident)
                p.copy('dve', thT[:, 2, :], ps[2][:, 0:128])
                p.mm(ps[3][:, 0:128], thT[:, 2, :], mats[:, 2, :])
                p.copy('act', Gst[:, t, :], ps[3][:, 0:128])
                kk = Q['kk']
                tq = Q['tq']
                p.tt('dve', kk, k_, bc[:, B_KK, :], ALU.mult)
                p.act(tq, kk, AF.Square)
                nrm = cols[:, t, 2:4]
                p.reduce('dve', nrm, tq.with_ap(tq.ap.rearrange("p (h c) -> p h c", h=2)), ALU.add)
                p.act(nrm, nrm, AF.Sqrt)
                p.ts('dve', nrm, nrm, 1e-12, ALU.max)
                p.recip(nrm, nrm)
                for hh in range(2):
                    p.ts('dve', kk[:, hh * 64:(hh + 1) * 64], kk[:, hh * 64:(hh + 1) * 64], cols[:, t, 2 + hh:3 + hh], ALU.mult)
                p.tt('pool', tq, r_, k_, ALU.mult)
                p.tt('pool', tq, tq, bc[:, B_RK, :], ALU.mult)
                p.reduce('dve', BCf[:, t, :], tq.with_ap(tq.ap.rearrange("p (h c) -> p h c", h=2)), ALU.add)
                sth = stash[t % 2]
                res = []
                for d in range(2):
                    ds_ = slice(d * 64, (d + 1) * 64)
                    a_d = Q['a0'] if d == 0 else sth[:, 512:640]
                    lw = Q['lw0'] if d == 0 else sth[:, 640:768]
                    kd = Q['kd0'] if d == 0 else sth[:, 128:256]
                    pw, pa_ = ps[4 + d], ps[6 + d]
                    p.mm(pw[:, 0:128], thT[ds_, 0, :], mats[ds_, 0, :])
                    p.mm(pa_[:, 0:128], thT[ds_, 1, :], mats[ds_, 1, :])
                    p.tt('dve', lw, pw[:, 0:128], bc[:, B_W00 + d, :], ALU.add)
                    p.act(lw, lw, AF.Sigmoid)
                    p.ts('pool', lw, lw, -math.exp(-0.5), ALU.mult)
                    p.tt('dve', a_d, pa_[:, 0:128], bc[:, B_A00 + d, :], ALU.add)
                    p.act(a_d, a_d, AF.Sigmoid)
                    p.stt('dve', kd, a_d, -1.0, bc[:, B_KA, :], ALU.add, ALU.mult)
                    p.ts('pool', kd, kd, 1.0, ALU.add)
                    p.tt('pool', kd, kd, k_, ALU.mult)
                    b_d = Q['b0'] if d == 0 else sth[:, 512:640]
                    p.tt('dve', b_d, kk, a_d, ALU.mult)
                    res.append((kd, b_d, lw))
                p.copy('pool', sth[:, 0:128], r_)
                p.copy('pool', sth[:, 256:384], v_)
                p.copy('pool', sth[:, 384:512], kk)
                p.dma('sp', st_s[t], sth)
                scan_step(st, 0, t, r_, res[0][0], v_, kk, res[0][1], res[0][2], True)
        with p.scope() as st:
            stash = [p.sb(f"stashb{i}", [128, 768], F32, st) for i in range(2)]
            order = [1, 0] + list(range(NTA - 1, 1, -1))
            for n, t in enumerate(order):
                sth = stash[n % 2]
                p.dma('sp', sth, st_s[t])
                scan_step(st, 1, t, sth[:, 0:128], sth[:, 128:256], sth[:, 256:384], sth[:, 384:512], sth[:, 512:640], sth[:, 640:768], False)
            OUT = p.sb("OUTr", [128, NLAT], F32, st)
            fq = [p.sb(f"fq{i}", [128, 128], F32, st) for i in range(3)]
            fcol = p.sb("fcol", [128, NTA, 8], F32, st)
            for t in range(2, NTA):
                y = Yacc[:, t, :]
                y3 = y.with_ap(y.ap.rearrange("p (h c) -> p h c", h=2))
                mean = fcol[:, t, 0:2]
                p.reduce('dve', mean, y3, ALU.add)
                p.ts('dve', mean, mean, 1.0 / 64, ALU.mult)
                yc = fq[0]
                for hh in range(2):
                    p.ts('dve', yc[:, hh * 64:(hh + 1) * 64], y[:, hh * 64:(hh + 1) * 64], fcol[:, t, hh:hh + 1], ALU.subtract)
                p.act(fq[1], yc, AF.Square)
                var = fcol[:, t, 2:4]
                p.reduce('dve', var, fq[1].with_ap(fq[1].ap.rearrange("p (h c) -> p h c", h=2)), ALU.add)
                p.act(var, var, AF.Sqrt, bias=epsc[:, 1:2], scale=1.0 / 64)
                p.recip(var, var)
                for hh in range(2):
                    hs_ = slice(hh * 64, (hh + 1) * 64)
                    p.stt('dve', yc[:, hs_], yc[:, hs_], fcol[:, t, 2 + hh:3 + hh], bc[:, B_LNG, hs_], ALU.mult, ALU.mult)
                p.tt('pool', yc, yc, bc[:, B_LNB, :], ALU.add)
                for hh in range(2):
                    hs_ = slice(hh * 64, (hh + 1) * 64)
                    p.stt('dve', yc[:, hs_], Vst[:, t, hs_], BCf[:, t, hh:hh + 1], yc[:, hs_], ALU.mult, ALU.add)
                p.tt('pool', fq[2], yc, Gst[:, t, :], ALU.mult)
                pp = ps[t % 4]
                p.tr(pp[:, 0:128], fq[2], ident)
                p.copy('act', OUT[:, (t - 2) * 128:(t - 1) * 128], pp[:, 0:128])
            p.dma('sp', out_d[128:256, :], OUT)

    with p.scope() as st:
        QT = p.sb("QdT", [128, NTOK], BF16, st)
        KT = p.sb("KdT", [128, NTOK], BF16, st)
        Vd = p.sb("Vd", [128, NTA, 128], BF16, st)
        p.dma('sp', QT, qd_s)
        p.dma('sp', KT, kd_s)
        p.dma('sp', Vd, vd_s)
        lamv = p.sb("lamv", [128, 4, 64], F32, st)
        p.dma('sp', lamv, lam_d)
        lc = p.sb("lc", [128, 8], F32, st)
        lt = p.sb("lt", [128, 2, 64], F32, st)
        p.tt('dve', lt[:, 0, :], lamv[:, 0, :], lamv[:, 1, :], ALU.mult)
        p.tt('dve', lt[:, 1, :], lamv[:, 2, :], lamv[:, 3, :], ALU.mult)
        p.reduce('dve', lc[:, 0:2], lt, ALU.add)
        p.act(lc[:, 2:4], lc[:, 0:2], AF.Exp)
        p.tt('dve', lc[:, 4:5], lc[:, 2:3], lc[:, 3:4], ALU.subtract)
        p.ts('dve', lc[:, 5:6], lc[:, 4:5], LAM_INIT1, ALU.add, -1.0, ALU.mult)
        neglam = lc[:, 5:6]
        subg = p.sb("subg", [128, 1], F32, st)
        p.dma('sp', subg, sg_d)
        p.ts('dve', subg, subg, 1.0 - LAM_INIT1, ALU.mult)
        PT = [p.sb(f"PTd{i}", [128, 512], BF16, st) for i in range(3)]
        rec = [p.sb(f"recd{i}", [128, 512], F32, st) for i in range(2)]
        o0 = p.sb("o0", [128, 512], F32, st)
        o1 = p.sb("o1", [128, 512], F32, st)
        osq = p.sb("osq", [128, 512], F32, st)
        it = 0
        for g in range(16):
            q0 = 256 + g * 512
            for m in range(2):
                ms = slice(m * 64, (m + 1) * 64)
                pO, pD = ps[4 + m * 2], ps[5 + m * 2]
                for kt in range(NTA):
                    pS = ps[it % 3]
                    pt_ = PT[it % 3]
                    it += 1
                    p.mm(pS, KT[ms, kt * 128:(kt + 1) * 128], QT[ms, q0:q0 + 512])
                    p.act(pt_, pS, AF.Exp, scale=64.0 ** -0.5)
                    p.mm(pO, Vd[:, kt, :], pt_, start=(kt == 0), stop=(kt == NTA - 1))
                    p.mm(pD, ones_bf, pt_, start=(kt == 0), stop=(kt == NTA - 1))
                p.recip(rec[m], pD)
                p.tt('dve', o0 if m == 0 else o1, pO, rec[m], ALU.mult)
            p.stt('dve', o0, o1, neglam, o0, ALU.mult, ALU.add)
            p.act(osq, o0, AF.Square)
            pn = ps[3]
            p.mm(pn, cst[:, C_ONE, :], osq)
            p.act(osq, pn, AF.Sqrt, bias=epsc[:, 0:1], scale=1.0 / 128)
            p.recip(osq, osq)
            p.stt('dve', o0, o0, subg, osq, ALU.mult, ALU.mult)
            p.dma('sp', out_d[0:128, g * 512:(g + 1) * 512], o0)
    p.finalize()
    return nc


def _mix1_consts():
    s = np.arange(128, dtype=np.float32)[:, None]
    t = np.arange(128, dtype=np.float32)[None, :]
    c = np.zeros((10, 128, 128), np.float32)
    c[C_ID] = np.eye(128, dtype=np.float32)
    c[C_TRI0] = (s <= t)
    c[C_TRI1] = (s >= t)
    c[C_MS0] = (s < t)
    c[C_MI0] = (s <= t)
    c[C_MS1] = (s > t)
    c[C_SH] = (np.abs(s - t) == 1)
    c[C_SHP] = (s == 127) & (t == 0)
    c[C_SHN] = (s == 0) & (t == 127)
    c[C_ONE] = 1.0
    return c


def run_mix1(x, ctx, c, c_ctx, mod_w, mod_b, norm_g, w_in, lamv, subg, mu, w0, w2, a0, a2, g2, k_k, k_a, r_k, ln_g, ln_b):
    nc = build_mix1()
    cos, sin = _with_ctx_rope(*_rope_tables(64))
    cst = _mix1_consts()
    in_maps = []
    for core in range(8):
        b, j = core // 4, core % 4
        hc = np.arange(j * 128, (j + 1) * 128)
        a128 = np.arange(128)
        colsel = np.concatenate([hc, 512 + hc, 1024 + hc, 1536 + hc, 2048 + hc, 2560 + hc, 3072 + a128, 3200 + a128, 3328 + a128])
        musel = np.concatenate([hc, 512 + hc, 1024 + hc, 1536 + a128, 1664 + a128, 1792 + a128])
        rows = [w0[0][hc], w0[1][hc], a0[0][hc], a0[1][hc], k_k[hc], k_a[hc], r_k.reshape(-1)[hc], ln_g[hc], ln_b[hc]]
        bc = np.stack([np.broadcast_to(r, (128, 128)) for r in rows]).astype(np.float32)
        mats = np.stack([np.concatenate([w2[0][:, hc], w2[1][:, hc]], 0), np.concatenate([a2[0][:, hc], a2[1][:, hc]], 0), g2[:, hc]]).astype(np.float32)
        in_maps.append({
            "x": np.ascontiguousarray(np.concatenate([ctx[b], x[b]], 0), dtype=np.float32),
            "cT": _cT(c[b], c_ctx), "mod_w": np.ascontiguousarray(mod_w), "mod_b": np.ascontiguousarray(np.stack([mod_b] * 2)),
            "norm_g": np.ascontiguousarray(norm_g), "w_in": np.ascontiguousarray(w_in[:, colsel]),
            "mu": np.ascontiguousarray(np.broadcast_to(mu[musel], (128, 768)), dtype=np.float32),
            "cos": cos, "sin": sin,
            "lamv": np.ascontiguousarray(np.broadcast_to(lamv, (128, 4, 64)), dtype=np.float32),
            "subg": np.ascontiguousarray(subg.reshape(128, 1), dtype=np.float32),
            "bc": np.ascontiguousarray(bc), "mats": np.ascontiguousarray(mats), "cst": cst,
        })
    res = run_bass_kernel_spmd(nc, in_maps, core_ids=list(range(8)), trace=TRACE)
    if TRACE:
        print("DEV_NS", res.exec_time_ns)
    mix = np.empty((2, 8192, 1024), np.float32)
    for core in range(8):
        b, j = core // 4, core % 4
        o = res.results[core]["mixT"]
        mix[b, :, j * 128:(j + 1) * 128] = o[0:128].T
        mix[b, :, 512 + j * 128:512 + (j + 1) * 128] = o[128:256].T
    return mix


def kernel(x, c, ctx, c_ctx, mod_w, mod_b, norm_g, even_w_in, even_w_out, ret_decay_exp, ret_norm_g,
           gqa_q_norm, gqa_k_norm, ffn_w_gate, ffn_w_up, ffn_w_down, odd_w_in, odd_w_out, diff_lambda,
           diff_subln_g, rwkv_mu, rwkv_w0, rwkv_w2, rwkv_a0, rwkv_a2, rwkv_g2, rwkv_k_k, rwkv_k_a, rwkv_r_k,
           rwkv_ln_g, rwkv_ln_b, moe_router, moe_w_gate, moe_w_up, moe_w_down):
    f = lambda a: np.asarray(a, dtype=np.float32)
    x, c, ctx, c_ctx, mod_w, mod_b, norm_g = map(f, (x, c, ctx, c_ctx, mod_w, mod_b, norm_g))
    mix_lat, mix_ctx = run_mix0(x, ctx, c, c_ctx, mod_w[0], mod_b[0], norm_g[0], f(even_w_in)[0], f(ret_decay_exp)[0],
                                f(ret_norm_g)[0], f(gqa_q_norm)[0], f(gqa_k_norm)[0])
    x1, ctx1 = run_post(0, x, ctx, mix_lat, mix_ctx, c, c_ctx, mod_w, mod_b, norm_g, f(even_w_out)[0],
                        f(ffn_w_gate), f(ffn_w_up), f(ffn_w_down), np.zeros((1024, 8), np.float32), True, 1, 2816, 2)
    mix1 = run_mix1(x1, ctx1, c, c_ctx, mod_w[1], mod_b[1], norm_g[1], f(odd_w_in)[0], f(diff_lambda)[0], f(diff_subln_g)[0],
                    f(rwkv_mu)[0], f(rwkv_w0)[0], f(rwkv_w2)[0], f(rwkv_a0)[0], f(rwkv_a2)[0], f(rwkv_g2)[0], f(rwkv_k_k)[0],
                    f(rwkv_k_a)[0], f(rwkv_r_k)[0], f(rwkv_ln_g)[0], f(rwkv_ln_b)[0])
    x2 = run_post1(x1, mix1, c, c_ctx, mod_w[1], mod_b[1], norm_g[1], f(odd_w_out)[0],
                   f(moe_w_gate)[0], f(moe_w_up)[0], f(moe_w_down)[0], f(moe_router)[0])
    return x2


def build_d1(NT=16):
    nc = bass.Bass("TRN2", target_bir_lowering=False)
    p = Prog(nc)
    NTOK = NT * 128
    x_d = p.dram("x", [NTOK, D], F32, "ExternalInput")
    mixT_d = p.dram("mixT", [D, NTOK], F32, "ExternalInput")
    cT_d = p.dram("cT", [128, 8, 2], F32, "ExternalInput")
    modw_d = p.dram("mod_w", [D, 6 * D], F32, "ExternalInput")
    modb_d = p.dram("mod_b", [2, 6 * D], F32, "ExternalInput")
    ng_d = p.dram("norm_g", [4, D], F32, "ExternalInput")
    wout_d = p.dram("w_out", [D, D], F32, "ExternalInput")
    rt_d = p.dram("router", [D, 128], F32, "ExternalInput")
    id_d = p.dram("ident", [128, 128], F32, "ExternalInput")
    x1_d = p.dram("x1", [NTOK, D], F32, "ExternalOutput")
    h2T_d = p.dram("h2T", [D, NTOK], BF16, "ExternalOutput")
    gates_d = p.dram("gates", [NTOK, 8], F32, "ExternalOutput")
    mod_s = p.dram("mod_s", [2, 6 * D], F32, "Internal")
    ps = [p.ps(f"ps{i}", [128, 512]) for i in range(8)]
    ident = p.sb("ident", [128, 128], F32)
    p.dma('sp', ident, id_d)
    epsc = p.sb("epsc", [128, 1], F32)
    p.memset('dve', epsc, NORM_EPS)
    h2T = p.sb("h2T", [128, 8, NTOK], BF16)
    gates = p.sb("gates", [128, NT, 8], F32)
    with p.scope() as st:
        mod_rows(p, ps, st, cT_d, modw_d, modb_d, mod_s, [4, 5, 6, 7, 8, 9])

    def rms_rstd(dst_col, src, sq_tmp):
        p.act(sq_tmp, src, AF.Square)
        p.reduce('dve', dst_col, sq_tmp, ALU.add)
        p.act(dst_col, dst_col, AF.Sqrt, bias=epsc, scale=1.0 / D)
        p.recip(dst_col, dst_col)

    with p.scope() as st:
        G1 = p.sb("G1", [128, D], F32, st)
        A2 = p.sb("A2", [128, D], F32, st)
        B2 = p.sb("B2", [128, D], F32, st)
        ngb = p.sb("ngb", [128, 2, D], F32, st)
        p.dma('sp', ngb[:, 0, :], ng_d.with_ap(ng_d.ap[1:2, :].partition_broadcast(128)))
        p.dma('sp', ngb[:, 1, :], ng_d.with_ap(ng_d.ap[2:3, :].partition_broadcast(128)))
        p.dma('sp', G1, mod_s.with_ap(mod_s.ap[0:1, 2 * D:3 * D].partition_broadcast(128)))
        p.tt('dve', G1, G1, ngb[:, 0, :], ALU.mult)
        p.dma('sp', B2, mod_s.with_ap(mod_s.ap[0:1, 3 * D:4 * D].partition_broadcast(128)))
        p.dma('sp', A2, mod_s.with_ap(mod_s.ap[0:1, 4 * D:5 * D].partition_broadcast(128)))
        p.ts('dve', A2, A2, 1.0, ALU.add)
        p.tt('dve', A2, A2, ngb[:, 1, :], ALU.mult)
        mixT = p.sb("mixT", [128, 8, NTOK], BF16, st)
        mview = mixT_d.ap.rearrange("(k p) n -> p k n", p=128)
        for k in range(8):
            p.dma('pool', mixT[:, k, :], mixT_d.with_ap(mview[:, k, :]))
        wout = p.sb("wout", [128, 8, D], BF16, st)
        wv = wout_d.ap.rearrange("(k p) n -> p k n", p=128)
        for k in range(8):
            p.dma('pool', wout[:, k, :], wout_d.with_ap(wv[:, k, :]))
        rt = p.sb("rt", [128, 8, 128], F32, st)
        if DBG != 'd1c':
            p.dma('sp', rt, rt_d.with_ap(rt_d.ap.rearrange("(k p) e -> p k e", p=128)))
        h2Tf = p.sb("h2Tf", [128, 8, 128], F32, st)
        xt = [p.sb(f"xt{i}", [128, D], F32, st) for i in range(2)]
        tmp = [p.sb(f"tmp{i}", [128, D], F32, st) for i in range(2)]
        sq = p.sb("sq", [128, D], F32, st)
        cols = p.sb("cols", [128, NT, 8], F32, st)
        lg = p.sb("lg", [128, 4, 8], F32, st)
        for t in range(NT):
            x_t, tm = xt[t % 2], tmp[t % 2]
            p.dma('sp', x_t, x_d[t * 128:(t + 1) * 128, :])
            py = [ps[0], ps[1]]
            for fh in range(2):
                for k in range(8):
                    p.mm(py[fh], mixT[:, k, t * 128:(t + 1) * 128], wout[:, k, fh * 512:(fh + 1) * 512], start=(k == 0), stop=(k == 7))
            for fh in range(2):
                p.copy('act', tm[:, fh * 512:(fh + 1) * 512], py[fh])
            rc = cols[:, t, 0:1]
            rms_rstd(rc, tm, sq)
            p.stt('dve', tm, tm, rc, G1, ALU.mult, ALU.mult)
            p.tt('pool', x_t, x_t, tm, ALU.add)
            p.dma('sp', x1_d[t * 128:(t + 1) * 128, :], x_t)
            rc2 = cols[:, t, 1:2]
            rms_rstd(rc2, x_t, sq)
            p.stt('dve', tm, x_t, rc2, A2, ALU.mult, ALU.mult)
            p.tt('pool', tm, tm, B2, ALU.add)
            for k in range(8):
                pt = ps[2 + (k % 4)]
                p.tr(pt[:, 0:128], tm[:, k * 128:(k + 1) * 128], ident)
                p.copy('act' if k % 2 else 'dve', h2Tf[:, k, :], pt[:, 0:128])
                p.copy('pool', h2T[:, k, t * 128:(t + 1) * 128], h2Tf[:, k, :])
            if DBG in ('d1a', 'd1b', 'd1c'):
                p.memset('dve', gates[:, t, :], 0.125)
                continue
            pl = ps[6]
            for k in range(8):
                p.mm(pl[:, 0:128], h2Tf[:, k, :], rt[:, k, :], start=(k == 0), stop=(k == 7))
            L = lg[:, 0, :]
            p.copy('dve', L, pl[:, 0:8])
            m1 = cols[:, t, 2:3]
            m2 = cols[:, t, 3:4]
            p.reduce('dve', m1, L, ALU.max)
            mk1 = lg[:, 1, :]
            p.ts('dve', mk1, L, m1, ALU.is_equal)
            L2 = lg[:, 2, :]
            p.stt('dve', L2, mk1, -1e30, L, ALU.mult, ALU.add)
            p.reduce('dve', m2, L2, ALU.max)
            mk2 = lg[:, 3, :]
            p.ts('dve', mk2, L2, m2, ALU.is_equal)
            w1 = cols[:, t, 4:5]
            w2 = cols[:, t, 5:6]
            p.tt('dve', w1, m1, m2, ALU.subtract)
            p.act(w1, w1, AF.Sigmoid)
            p.ts('dve', w2, w1, -1.0, ALU.mult, 1.0, ALU.add)
            p.ts('dve', gates[:, t, :], mk1, w1, ALU.mult)
            p.stt('dve', gates[:, t, :], mk2, w2, gates[:, t, :], ALU.mult, ALU.add)
        hv = h2T_d.ap.rearrange("(k p) n -> p k n", p=128)
        for k in range(8):
            p.dma('sp', h2T_d.with_ap(hv[:, k, :]), h2T[:, k, :])
        p.dma('sp', gates_d.with_ap(gates_d.ap.rearrange("(t p) e -> p t e", p=128)), gates)
    p.finalize()
    return nc


def build_d2(NCH=8, H=3584, BLK=4):
    nc = bass.Bass("TRN2", target_bir_lowering=False)
    p = Prog(nc)
    CT = 16
    NTOK = NCH * CT * 128
    HC = H // 128
    NB = HC // BLK
    h2T_d = p.dram("h2T", [D, NTOK], BF16, "ExternalInput")
    g_d = p.dram("gate", [128, NCH * CT], F32, "ExternalInput")
    wg_d = p.dram("wg", [D, H], F32, "ExternalInput")
    wu_d = p.dram("wu", [D, H], F32, "ExternalInput")
    wd_d = p.dram("wd", [H, D], F32, "ExternalInput")
    out_d = p.dram("y", [NTOK, D], F32, "ExternalOutput")
    ps = [p.ps(f"ps{i}", [128, 512]) for i in range(8)]
    gt = p.sb("gt", [128, NCH * CT], F32)
    p.dma('sp', gt, g_d)
    h2T = [p.sb(f"h2T{i}", [128, 8, CT * 128], BF16) for i in range(2)]
    acc = p.sb("acc", [128, CT, D], F32)
    wgb = [p.sb(f"wg{i}", [128, 8, BLK * 128], BF16) for i in range(2)]
    wub = [p.sb(f"wu{i}", [128, 8, BLK * 128], BF16) for i in range(2)]
    wdb = [p.sb(f"wd{i}", [128, BLK, D], BF16) for i in range(2)]
    actT = [p.sb(f"actT{i}", [128, BLK, 512], BF16) for i in range(2)]
    sg = [p.sb(f"sg{i}", [128, 512], F32) for i in range(2)]
    yo = [p.sb(f"yo{i}", [128, D], F32) for i in range(2)]
    wgv = wg_d.ap.rearrange("(k p) h -> p k h", p=128)
    wuv = wu_d.ap.rearrange("(k p) h -> p k h", p=128)
    wdv = wd_d.ap.rearrange("(c p) f -> p c f", p=128)
    hv = h2T_d.ap.rearrange("(k p) n -> p k n", p=128)
    it = 0
    gi = 0
    for ch in range(NCH):
        hb = h2T[ch % 2]
        for k in range(8):
            p.dma('sp', hb[:, k, :], h2T_d.with_ap(hv[:, k, ch * CT * 128:(ch + 1) * CT * 128]))
        for b in range(NB):
            s = it % 2
            it += 1
            hs = slice(b * BLK * 128, (b + 1) * BLK * 128)
            for k in range(8):
                p.dma('pool', wgb[s][:, k, :], wg_d.with_ap(wgv[:, k, hs]))
                p.dma('pool', wub[s][:, k, :], wu_d.with_ap(wuv[:, k, hs]))
            for c in range(BLK):
                p.dma('pool', wdb[s][:, c, :], wd_d.with_ap(wdv[:, b * BLK + c, :]))
            for t0 in range(0, CT, 4):
                ts_ = slice(t0 * 128, (t0 + 4) * 128)
                a = actT[gi % 2]
                gi += 1
                for c in range(BLK):
                    pg, pu = ps[(c % 2) * 2], ps[(c % 2) * 2 + 1]
                    for k in range(8):
                        p.mm(pg, wgb[s][:, k, c * 128:(c + 1) * 128], hb[:, k, ts_], start=(k == 0), stop=(k == 7))
                    for k in range(8):
                        p.mm(pu, wub[s][:, k, c * 128:(c + 1) * 128], hb[:, k, ts_], start=(k == 0), stop=(k == 7))
                    sgt = sg[c % 2]
                    p.act(sgt, pg, AF.Silu)
                    p.tt('dve', a[:, c, :], sgt, pu, ALU.mult)
                for j in range(4):
                    t = t0 + j
                    for fh in range(2):
                        pd = ps[4 + ((j * 2 + fh) % 4)]
                        for c in range(BLK):
                            p.mm(pd, a[:, c, j * 128:(j + 1) * 128], wdb[s][:, c, fh * 512:(fh + 1) * 512], start=(c == 0), stop=(c == BLK - 1))
                        dst = acc[:, t, fh * 512:(fh + 1) * 512]
                        if b == 0:
                            p.copy('dve', dst, pd)
                        else:
                            p.tt('dve', dst, pd, dst, ALU.add)
        for t in range(CT):
            gt_ = ch * CT + t
            y = yo[t % 2]
            p.ts('pool', y, acc[:, t, :], gt[:, gt_:gt_ + 1], ALU.mult)
            p.dma('sp', out_d[gt_ * 128:(gt_ + 1) * 128, :], y)
    p.finalize()
    return nc


def build_d3(NT=16, NE=8):
    nc = bass.Bass("TRN2", target_bir_lowering=False)
    p = Prog(nc)
    NTOK = NT * 128
    ys_d = p.dram("ys", [NE, NTOK, D], F32, "ExternalInput")
    x1_d = p.dram("x1", [NTOK, D], F32, "ExternalInput")
    cT_d = p.dram("cT", [128, 8, 2], F32, "ExternalInput")
    modw_d = p.dram("mod_w", [D, 6 * D], F32, "ExternalInput")
    modb_d = p.dram("mod_b", [2, 6 * D], F32, "ExternalInput")
    ng_d = p.dram("norm_g", [4, D], F32, "ExternalInput")
    out_d = p.dram("out", [NTOK, D], F32, "ExternalOutput")
    mod_s = p.dram("mod_s", [2, 6 * D], F32, "Internal")
    ps = [p.ps(f"ps{i}", [128, 512]) for i in range(8)]
    epsc = p.sb("epsc", [128, 1], F32)
    p.memset('dve', epsc, NORM_EPS)
    with p.scope() as st:
        mod_rows(p, ps, st, cT_d, modw_d, modb_d, mod_s, [10, 11])
    with p.scope() as st:
        G2 = p.sb("G2", [128, D], F32, st)
        ng3 = p.sb("ng3", [128, D], F32, st)
        p.dma('sp', ng3, ng_d.with_ap(ng_d.ap[3:4, :].partition_broadcast(128)))
        p.dma('sp', G2, mod_s.with_ap(mod_s.ap[0:1, 5 * D:6 * D].partition_broadcast(128)))
        p.tt('dve', G2, G2, ng3, ALU.mult)
        yt = [p.sb(f"yt{i}", [128, NE, D], F32, st) for i in range(2)]
        x1t = [p.sb(f"x1t{i}", [128, D], F32, st) for i in range(2)]
        sq = p.sb("sq", [128, D], F32, st)
        cols = p.sb("cols", [128, NT], F32, st)
        for t in range(NT):
            y = yt[t % 2]
            x1 = x1t[t % 2]
            for e in range(NE):
                p.dma('sp', y[:, e, :], ys_d[e, t * 128:(t + 1) * 128, :])
            p.dma('sp', x1, x1_d[t * 128:(t + 1) * 128, :])
            f = y[:, 0, :]
            for e in range(1, NE):
                p.tt('dve' if e % 2 else 'pool', f, f, y[:, e, :], ALU.add)
            rc = cols[:, t:t + 1]
            p.act(sq, f, AF.Square)
            p.reduce('dve', rc, sq, ALU.add)
            p.act(rc, rc, AF.Sqrt, bias=epsc, scale=1.0 / D)
            p.recip(rc, rc)
            p.stt('dve', f, f, rc, G2, ALU.mult, ALU.mult)
            p.tt('pool', x1, x1, f, ALU.add)
            p.dma('sp', out_d[t * 128:(t + 1) * 128, :], x1)
    p.finalize()
    return nc


def run_post1(x_lat, mix_lat, c, c_ctx, mod_w, mod_b, norm_g, w_out, wg, wu, wd, router):
    import ml_dtypes
    cores = list(range(8))
    nc = build_d1()
    rpad = np.ascontiguousarray(np.concatenate([router, np.zeros((1024, 120), np.float32)], 1))
    in_maps = []
    for core in cores:
        b, q = core // 4, core % 4
        in_maps.append({
            "x": np.ascontiguousarray(x_lat[b, q * 2048:(q + 1) * 2048]),
            "mixT": np.ascontiguousarray(mix_lat[b, q * 2048:(q + 1) * 2048].T),
            "cT": _cT(c[b], c_ctx), "mod_w": np.ascontiguousarray(mod_w), "mod_b": np.ascontiguousarray(np.stack([mod_b] * 2)),
            "norm_g": np.ascontiguousarray(norm_g), "w_out": np.ascontiguousarray(w_out), "router": rpad, "ident": _IDENT,
        })
    r1 = run_bass_kernel_spmd(nc, in_maps, core_ids=cores, trace=TRACE).results
    if TRACE:
        pass
    h2T_all = np.ascontiguousarray(np.concatenate([np.asarray(r1[k]["h2T"]) for k in cores], axis=1))
    gates_all = np.concatenate([np.asarray(r1[k]["gates"]) for k in cores], axis=0)
    nc = build_d2()
    in_maps = []
    for e in cores:
        in_maps.append({
            "h2T": h2T_all,
            "gate": np.ascontiguousarray(gates_all[:, e].reshape(128, 128).T),
            "wg": np.ascontiguousarray(wg[e]), "wu": np.ascontiguousarray(wu[e]), "wd": np.ascontiguousarray(wd[e]),
        })
    r2 = run_bass_kernel_spmd(nc, in_maps, core_ids=cores, trace=TRACE).results
    nc = build_d3()
    in_maps = []
    for core in cores:
        b, q = core // 4, core % 4
        ys = np.ascontiguousarray(np.stack([np.asarray(r2[e]["y"])[core * 2048:(core + 1) * 2048] for e in cores]))
        in_maps.append({
            "ys": ys, "x1": np.asarray(r1[core]["x1"]),
            "cT": _cT(c[b], c_ctx), "mod_w": np.ascontiguousarray(mod_w), "mod_b": np.ascontiguousarray(np.stack([mod_b] * 2)),
            "norm_g": np.ascontiguousarray(norm_g),
        })
    r3 = run_bass_kernel_spmd(nc, in_maps, core_ids=cores, trace=TRACE).results
    x2 = np.empty_like(x_lat)
    for core in cores:
        b, q = core // 4, core % 4
        x2[b, q * 2048:(q + 1) * 2048] = r3[core]["out"]
    return x2
```

```python
import contextlib
import math
import numpy as np
import concourse.bass as bass
import concourse.mybir as mybir
from concourse.bass_utils import run_bass_kernel_spmd

ALU = mybir.AluOpType
AF = mybir.ActivationFunctionType
AX = mybir.AxisListType
F32 = mybir.dt.float32
BF16 = mybir.dt.bfloat16

SAME_ENGINE_SYNC = True
ENGS = ('pe', 'act', 'dve', 'pool', 'sp')


class Res:
    __slots__ = ('name', 'w', 'r', 'dsem', 'dcount')

    def __init__(self, name):
        self.name = name
        self.w = None
        self.r = []
        self.dsem = None
        self.dcount = 0


class T:
    __slots__ = ('ap', 'res')

    def __init__(self, ap, res):
        self.ap = ap
        self.res = res

    def __getitem__(self, idx):
        return T(self.ap[idx], self.res)

    def with_ap(self, ap):
        return T(ap, self.res)


class Prog:
    def __init__(self, nc):
        self.nc = nc
        self.stack = contextlib.ExitStack()
        self.ops = {e: [] for e in ENGS}
        self.nops = {e: 0 for e in ENGS}
        self.seen = {e: {} for e in ENGS}
        self.signal = {e: set() for e in ENGS}
        self.dma_res = []
        self.all_res = []
        self.nsb = 0

    def _res(self, name):
        r = Res(name)
        self.all_res.append(r)
        return r

    def sb(self, name, shape, dt, stack=None):
        t = (stack or self.stack).enter_context(self.nc.sbuf_tensor('sb_' + name, list(shape), dt))
        return T(t[tuple(slice(None) for _ in shape)], self._res(name))

    def ps(self, name, shape, dt=F32, stack=None):
        t = (stack or self.stack).enter_context(self.nc.psum_tensor('pp_' + name, list(shape), dt))
        return T(t[tuple(slice(None) for _ in shape)], self._res(name))

    def dram(self, name, shape, dt, kind):
        t = self.nc.dram_tensor(name, list(shape), dt, kind=kind).ap()
        return T(t, self._res(name))

    def sub(self, t, name=None):
        return T(t.ap, self._res(name or t.res.name + '_sub'))

    def _need(self, eng, tok, waits):
        if tok is None:
            return
        if tok[0] == 'eng':
            _, f, k = tok
            if f == eng and not (SAME_ENGINE_SYNC and eng != 'pe'):
                return
            key = ('eng', f)
            if self.seen[eng].get(key, 0) >= k:
                return
            self.seen[eng][key] = k
            self.signal[f].add(k)
            waits.append(tok)
        else:
            _, res, cnt = tok
            key = ('dma', id(res))
            if self.seen[eng].get(key, 0) >= cnt:
                return
            self.seen[eng][key] = cnt
            waits.append(tok)

    def _deps(self, eng, reads, writes):
        waits = []
        for t in reads:
            self._need(eng, t.res.w, waits)
        for t in writes:
            self._need(eng, t.res.w, waits)
            for tok in t.res.r:
                self._need(eng, tok, waits)
        return waits

    def op(self, eng, fn, reads=(), writes=()):
        waits = self._deps(eng, reads, writes)
        self.nops[eng] += 1
        k = self.nops[eng]
        tok = ('eng', eng, k)
        for t in reads:
            t.res.r.append(tok)
        for t in writes:
            t.res.w = tok
            t.res.r = []
        self.ops[eng].append((waits, fn, k, None))

    def dma(self, q, out, in_, **kw):
        waits = self._deps(q, [in_], [out])
        res = out.res
        if res.dsem is None:
            res.dsem = self.stack.enter_context(self.nc.semaphore('d_' + res.name))
            self.dma_res.append(res)
        res.dcount += 1
        tok = ('dma', res, res.dcount)
        in_.res.r.append(tok)
        res.w = tok
        res.r = []
        oap, iap = out.ap, in_.ap
        self.ops[q].append((waits, lambda e: e.dma_start(out=oap, in_=iap, **kw), None, res))

    def barrier(self):
        for e in ENGS:
            waits = []
            for f in ENGS:
                if f != e and self.nops[f] > 0:
                    self._need(e, ('eng', f, self.nops[f]), waits)
            for res in self.dma_res:
                self._need(e, ('dma', res, res.dcount), waits)
            if waits:
                self.ops[e].append((waits, None, None, None))

    @contextlib.contextmanager
    def scope(self):
        st = contextlib.ExitStack()
        try:
            yield st
        finally:
            self.barrier()
            st.close()

    def finalize(self):
        nc = self.nc
        sems = {e: self.stack.enter_context(nc.semaphore('s_' + e)) for e in ENGS}
        self.barrier()
        rank = {}
        for e in ENGS:
            for i, k in enumerate(sorted(self.signal[e])):
                rank[(e, k)] = i + 1

        def run(ename, eng):
            for waits, fn, k, dres in self.ops[ename]:
                for tok in waits:
                    if tok[0] == 'eng':
                        eng.wait_ge(sems[tok[1]], rank[(tok[1], tok[2])])
                    else:
                        eng.wait_ge(tok[1].dsem, 16 * tok[2])
                if fn is None:
                    continue
                ins = fn(eng)
                if dres is not None:
                    ins.then_inc(dres.dsem, 16)
                elif (ename, k) in rank:
                    ins.then_inc(sems[ename], 1)

        with nc.Block() as block:
            @block.tensor
            def _(e):
                run('pe', e)

            @block.scalar
            def _(e):
                run('act', e)

            @block.vector
            def _(e):
                run('dve', e)

            @block.gpsimd
            def _(e):
                run('pool', e)

            @block.sync
            def _(e):
                run('sp', e)
        self.stack.close()

    def mm(self, out, lhsT, rhs, start=True, stop=True):
        o, l, r = out.ap, lhsT.ap, rhs.ap
        self.op('pe', lambda e: e.matmul(o, l, r, start=start, stop=stop), [lhsT, rhs], [out])

    def tr(self, out, in_, ident):
        o, i, d = out.ap, in_.ap, ident.ap
        self.op('pe', lambda e: e.transpose(o, i, d), [in_, ident], [out])

    def act(self, out, in_, func, bias=None, scale=1.0, accum=None, eng='act'):
        o, i = out.ap, in_.ap
        reads = [in_]
        kw = {}
        if bias is not None:
            if isinstance(bias, T):
                reads.append(bias)
                kw['bias'] = bias.ap
            else:
                kw['bias'] = bias
        if isinstance(scale, T):
            reads.append(scale)
            kw['scale'] = scale.ap
        else:
            kw['scale'] = scale
        writes = [out]
        if accum is not None:
            writes.append(accum)
            kw['accum_out'] = accum.ap
        self.op(eng, lambda e: e.activation(o, i, func, **kw), reads, writes)

    def tt(self, eng, out, in0, in1, op):
        o, a, b = out.ap, in0.ap, in1.ap
        self.op(eng, lambda e: e.tensor_tensor(o, a, b, op), [in0, in1], [out])

    def ts(self, eng, out, in0, s1, op0, s2=None, op1=None, accum=None):
        o, a = out.ap, in0.ap
        reads = [in0]
        v1 = s1
        if isinstance(s1, T):
            reads.append(s1)
            v1 = s1.ap
        v2 = s2
        if isinstance(s2, T):
            reads.append(s2)
            v2 = s2.ap
        kw = {}
        if op1 is not None:
            kw['op1'] = op1
        writes = [out]
        if accum is not None:
            kw['accum_out'] = accum.ap
            writes.append(accum)
        self.op(eng, lambda e: e.tensor_scalar(o, a, v1, v2, op0, **kw), reads, writes)

    def stt(self, eng, out, in0, scalar, in1, op0, op1):
        o, a, b = out.ap, in0.ap, in1.ap
        reads = [in0, in1]
        v = scalar
        if isinstance(scalar, T):
            reads.append(scalar)
            v = scalar.ap
        self.op(eng, lambda e: e.scalar_tensor_tensor(o, a, v, b, op0, op1), reads, [out])

    def copy(self, eng, out, in_):
        o, i = out.ap, in_.ap
        if eng == 'act':
            self.op(eng, lambda e: e.activation(o, i, AF.Copy), [in_], [out])
        else:
            self.op(eng, lambda e: e.tensor_copy(o, i), [in_], [out])

    def memset(self, eng, out, val):
        o = out.ap
        self.op(eng, lambda e: e.memset(o, val), [], [out])

    def reduce(self, eng, out, in_, op, axis=AX.X):
        o, i = out.ap, in_.ap
        self.op(eng, lambda e: e.tensor_reduce(o, i, axis, op), [in_], [out])

    def recip(self, out, in_):
        o, i = out.ap, in_.ap
        self.op('dve', lambda e: e.reciprocal(o, i), [in_], [out])


NORM_EPS = 1e-6
D = 1024
DBG = None
TRACE = False


def build_post(NT, E, H, BLK, has_ctx):
    nc = bass.Bass("TRN2", target_bir_lowering=False)
    p = Prog(nc)
    NTOK = NT * 128
    HC = H // 128
    NB = HC // BLK
    assert NB * BLK == HC
    x_d = p.dram("x", [NTOK, D], F32, "ExternalInput")
    mixT_d = p.dram("mixT", [D, NTOK], F32, "ExternalInput")
    cT_d = p.dram("cT", [128, 8, 2], F32, "ExternalInput")
    modw_d = p.dram("mod_w", [D, 6 * D], F32, "ExternalInput")
    modb_d = p.dram("mod_b", [2, 6 * D], F32, "ExternalInput")
    ng_d = p.dram("norm_g", [4, D], F32, "ExternalInput")
    wout_d = p.dram("w_out", [D, D], F32, "ExternalInput")
    wg_d = p.dram("wg", [E, D, H], F32, "ExternalInput")
    wu_d = p.dram("wu", [E, D, H], F32, "ExternalInput")
    wd_d = p.dram("wd", [E, H, D], F32, "ExternalInput")
    rt_d = p.dram("router", [D, 128], F32, "ExternalInput")
    id_d = p.dram("ident", [128, 128], F32, "ExternalInput")
    out_d = p.dram("out", [NTOK, D], F32, "ExternalOutput")
    mod_s = p.dram("mod_s", [2, 6 * D], F32, "Internal")
    x1_s = p.dram("x1_s", [NTOK, D], F32, "Internal")

    ps = [p.ps(f"ps{i}", [128, 512]) for i in range(8)]
    ident = p.sb("ident", [128, 128], F32)
    p.dma('sp', ident, id_d)
    epsc = p.sb("epsc", [128, 1], F32)
    p.memset('dve', epsc, NORM_EPS)
    h2T = p.sb("h2T", [128, 8, NTOK], BF16)
    gates = p.sb("gates", [128, NT, 8], F32)
    nvar = 2 if has_ctx else 1

    with p.scope() as st:
        cT = p.sb("cT", [128, 8, 2], F32, st)
        sc = p.sb("silu_c", [128, 8, 2], F32, st)
        p.dma('sp', cT, cT_d)
        p.act(sc, cT, AF.Silu)
        modrow = p.sb("modrow", [2, 6 * D], F32, st)
        modb = p.sb("modb", [2, 6 * D], F32, st)
        p.dma('sp', modb, modb_d)
        wblk = [p.sb(f"modw{i}", [128, 8, 512], F32, st) for i in range(2)]
        for cb in range(2, 12):
            wb = wblk[cb % 2]
            p.dma('sp', wb, modw_d.with_ap(modw_d.ap.rearrange("(k p) n -> p k n", p=128)[:, :, cb * 512:(cb + 1) * 512]))
            pp = ps[cb % 2]
            for k in range(8):
                p.mm(pp[0:2, :], sc[:, k, :], wb[:, k, :], start=(k == 0), stop=(k == 7))
            p.tt('dve', modrow[:, cb * 512:(cb + 1) * 512], pp[0:2, :], modb[:, cb * 512:(cb + 1) * 512], ALU.add)
        p.dma('sp', mod_s[:, 2 * D:6 * D], modrow[:, 2 * D:6 * D])

    def bcast_row(dst, src_row_ap):
        p.dma('sp', dst, src_row_ap)

    def rms_rstd(dst_col, src, sq_tmp):
        p.act(sq_tmp, src, AF.Square)
        p.reduce('dve', dst_col, sq_tmp, ALU.add)
        p.act(dst_col, dst_col, AF.Sqrt, bias=epsc, scale=1.0 / D)
        p.recip(dst_col, dst_col)

    with p.scope() as st:
        G1 = [p.sb(f"G1_{v}", [128, D], F32, st) for v in range(nvar)]
        A2 = [p.sb(f"A2_{v}", [128, D], F32, st) for v in range(nvar)]
        B2 = [p.sb(f"B2_{v}", [128, D], F32, st) for v in range(nvar)]
        ngb = p.sb("ngb", [128, 2, D], F32, st)
        bcast_row(ngb[:, 0, :], ng_d.with_ap(ng_d.ap[1:2, :].partition_broadcast(128)))
        bcast_row(ngb[:, 1, :], ng_d.with_ap(ng_d.ap[2:3, :].partition_broadcast(128)))
        for v in range(nvar):
            bcast_row(G1[v], mod_s.with_ap(mod_s.ap[v:v + 1, 2 * D:3 * D].partition_broadcast(128)))
            p.tt('dve', G1[v], G1[v], ngb[:, 0, :], ALU.mult)
            bcast_row(B2[v], mod_s.with_ap(mod_s.ap[v:v + 1, 3 * D:4 * D].partition_broadcast(128)))
            bcast_row(A2[v], mod_s.with_ap(mod_s.ap[v:v + 1, 4 * D:5 * D].partition_broadcast(128)))
            p.ts('dve', A2[v], A2[v], 1.0, ALU.add)
            p.tt('dve', A2[v], A2[v], ngb[:, 1, :], ALU.mult)
        mixT = p.sb("mixT", [128, 8, NTOK], BF16, st)
        mview = mixT_d.ap.rearrange("(k p) n -> p k n", p=128)
        for k in range(8):
            p.dma('pool', mixT[:, k, :], mixT_d.with_ap(mview[:, k, :]))
        wout = p.sb("wout", [128, 8, D], BF16, st)
        wv = wout_d.ap.rearrange("(k p) n -> p k n", p=128)
        for k in range(8):
            p.dma('pool', wout[:, k, :], wout_d.with_ap(wv[:, k, :]))
        if E > 1:
            rt = p.sb("rt", [128, 8, 128], F32, st)
            p.dma('sp', rt, rt_d.with_ap(rt_d.ap.rearrange("(k p) e -> p k e", p=128)))
            h2Tf = p.sb("h2Tf", [128, 8, 128], F32, st)
        xt = [p.sb(f"xt{i}", [128, D], F32, st) for i in range(2)]
        tmp = [p.sb(f"tmp{i}", [128, D], F32, st) for i in range(2)]
        sq = p.sb("sq", [128, D], F32, st)
        cols = p.sb("cols", [128, NT, 8], F32, st)
        lg = p.sb("lg", [128, 4, 8], F32, st)
        for t in range(NT):
            v = 1 if (has_ctx and t == NT - 1) else 0
            x_t, tm = xt[t % 2], tmp[t % 2]
            p.dma('sp', x_t, x_d[t * 128:(t + 1) * 128, :])
            py = [ps[0], ps[1]]
            for fh in range(2):
                for k in range(8):
                    p.mm(py[fh], mixT[:, k, t * 128:(t + 1) * 128], wout[:, k, fh * 512:(fh + 1) * 512], start=(k == 0), stop=(k == 7))
            for fh in range(2):
                p.copy('act', tm[:, fh * 512:(fh + 1) * 512], py[fh])
            rc = cols[:, t, 0:1]
            rms_rstd(rc, tm, sq)
            p.stt('dve', tm, tm, rc, G1[v], ALU.mult, ALU.mult)
            p.tt('pool', x_t, x_t, tm, ALU.add)
            p.dma('sp', x1_s[t * 128:(t + 1) * 128, :], x_t)
            rc2 = cols[:, t, 1:2]
            rms_rstd(rc2, x_t, sq)
            p.stt('dve', tm, x_t, rc2, A2[v], ALU.mult, ALU.mult)
            p.tt('pool', tm, tm, B2[v], ALU.add)
            for k in range(8):
                pt = ps[2 + (k % 4)]
                p.tr(pt[:, 0:128], tm[:, k * 128:(k + 1) * 128], ident)
                p.copy('act' if k % 2 else 'dve', h2T[:, k, t * 128:(t + 1) * 128], pt[:, 0:128])
                if E > 1:
                    p.copy('dve' if k % 2 else 'act', h2Tf[:, k, :], pt[:, 0:128])
            if E > 1 and DBG not in ('norouter', 'e1only', 'e0only'):
                pl = ps[6]
                for k in range(8):
                    p.mm(pl[:, 0:128], h2Tf[:, k, :], rt[:, k, :], start=(k == 0), stop=(k == 7))
                L = lg[:, 0, :]
                p.copy('dve', L, pl[:, 0:8])
                m1 = cols[:, t, 2:3]
                m2 = cols[:, t, 3:4]
                p.reduce('dve', m1, L, ALU.max)
                mk1 = lg[:, 1, :]
                p.ts('dve', mk1, L, m1, ALU.is_equal)
                L2 = lg[:, 2, :]
                p.stt('dve', L2, mk1, -1e30, L, ALU.mult, ALU.add)
                p.reduce('dve', m2, L2, ALU.max)
                mk2 = lg[:, 3, :]
                p.ts('dve', mk2, L2, m2, ALU.is_equal)
                w1 = cols[:, t, 4:5]
                w2 = cols[:, t, 5:6]
                p.tt('dve', w1, m1, m2, ALU.subtract)
                p.act(w1, w1, AF.Sigmoid)
                p.ts('dve', w2, w1, -1.0, ALU.mult, 1.0, ALU.add)
                p.ts('dve', gates[:, t, :], mk1, w1, ALU.mult)
                p.stt('dve', gates[:, t, :], mk2, w2, gates[:, t, :], ALU.mult, ALU.add)

    with p.scope() as st:
        acc = p.sb("acc", [128, NT, D], F32, st)
        wgb = [p.sb(f"wg{i}", [128, 8, BLK * 128], BF16, st) for i in range(2)]
        wub = [p.sb(f"wu{i}", [128, 8, BLK * 128], BF16, st) for i in range(2)]
        wdb = [p.sb(f"wd{i}", [128, BLK, D], BF16, st) for i in range(2)]
        actT = [p.sb(f"actT{i}", [128, BLK, 512], BF16, st) for i in range(2)]
        sg = [p.sb(f"sg{i}", [128, 512], F32, st) for i in range(2)]
        groups = []
        t0 = 0
        while t0 < NT:
            n = min(4, NT - t0)
            groups.append((t0, n))
            t0 += n
        it = 0
        gi = 0
        first = True
        for e in ([1] if DBG == 'e1only' else [0] if DBG == 'e0only' else range(E)):
            wgv = wg_d.ap.rearrange("e (k p) h -> e p k h", p=128)[e]
            wuv = wu_d.ap.rearrange("e (k p) h -> e p k h", p=128)[e]
            wdv = wd_d.ap.rearrange("e (c p) f -> e p c f", p=128)[e]
            for b in range(NB):
                s = it % 2
                it += 1
                hs = slice(b * BLK * 128, (b + 1) * BLK * 128)
                for k in range(8):
                    p.dma('pool', wgb[s][:, k, :], wg_d.with_ap(wgv[:, k, hs]))
                    p.dma('pool', wub[s][:, k, :], wu_d.with_ap(wuv[:, k, hs]))
                for c in range(BLK):
                    p.dma('pool', wdb[s][:, c, :], wd_d.with_ap(wdv[:, b * BLK + c, :]))
                for (t0, n) in groups:
                    ts_ = slice(t0 * 128, (t0 + n) * 128)
                    W = n * 128
                    a = actT[gi % 2]
                    gi += 1
                    for c in range(BLK):
                        pg, pu = ps[(c % 2) * 2], ps[(c % 2) * 2 + 1]
                        for k in range(8):
                            p.mm(pg[:, 0:W], wgb[s][:, k, c * 128:(c + 1) * 128], h2T[:, k, ts_], start=(k == 0), stop=(k == 7))
                        for k in range(8):
                            p.mm(pu[:, 0:W], wub[s][:, k, c * 128:(c + 1) * 128], h2T[:, k, ts_], start=(k == 0), stop=(k == 7))
                        sgt = sg[c % 2]
                        p.act(sgt[:, 0:W], pg[:, 0:W], AF.Silu)
                        p.tt('dve', a[:, c, 0:W], sgt[:, 0:W], pu[:, 0:W], ALU.mult)
                    for j in range(n):
                        t = t0 + j
                        for fh in range(2):
                            pd = ps[4 + ((j * 2 + fh) % 4)]
                            for c in range(BLK):
                                p.mm(pd, a[:, c, j * 128:(j + 1) * 128], wdb[s][:, c, fh * 512:(fh + 1) * 512], start=(c == 0), stop=(c == BLK - 1))
                            dst = acc[:, t, fh * 512:(fh + 1) * 512]
                            gsc = gates[:, t, e:e + 1] if (E > 1 and DBG not in ('norouter', 'nogate', 'e1only', 'e0only')) else 1.0
                            if first:
                                if E > 1 and DBG not in ('norouter', 'nogate', 'e1only', 'e0only'):
                                    p.ts('dve', dst, pd, gsc, ALU.mult)
                                else:
                                    p.copy('dve', dst, pd)
                            else:
                                p.stt('dve', dst, pd, gsc, dst, ALU.mult, ALU.add)
                first = False
        G2 = [p.sb(f"G2_{v}", [128, D], F32, st) for v in range(nvar)]
        ng3 = p.sb("ng3", [128, D], F32, st)
        bcast_row(ng3, ng_d.with_ap(ng_d.ap[3:4, :].partition_broadcast(128)))
        for v in range(nvar):
            bcast_row(G2[v], mod_s.with_ap(mod_s.ap[v:v + 1, 5 * D:6 * D].partition_broadcast(128)))
            p.tt('dve', G2[v], G2[v], ng3, ALU.mult)
        x1t = [p.sb(f"x1t{i}", [128, D], F32, st) for i in range(2)]
        sq2 = p.sb("sq2", [128, D], F32, st)
        cols2 = p.sb("cols2", [128, NT], F32, st)
        for t in range(NT):
            v = 1 if (has_ctx and t == NT - 1) else 0
            x1 = x1t[t % 2]
            p.dma('sp', x1, x1_s[t * 128:(t + 1) * 128, :])
            rc = cols2[:, t:t + 1]
            rms_rstd(rc, acc[:, t, :], sq2)
            p.stt('dve', acc[:, t, :], acc[:, t, :], rc, G2[v], ALU.mult, ALU.mult)
            if DBG == 'x1':
                pass
            elif DBG == 'f':
                p.copy('pool', x1, acc[:, t, :])
            else:
                p.tt('pool', x1, x1, acc[:, t, :], ALU.add)
            p.dma('sp', out_d[t * 128:(t + 1) * 128, :], x1)
    p.finalize()
    return nc


def _cT(c_b, c_ctx):
    a = np.stack([c_b, c_ctx], axis=-1).astype(np.float32)
    return np.ascontiguousarray(a.reshape(8, 128, 2).transpose(1, 0, 2))


_IDENT = np.eye(128, dtype=np.float32)


def run_post(layer, x_lat, ctx, mix_lat, mix_ctx, c, c_ctx, mod_w, mod_b, norm_g, w_out, wg, wu, wd, router, has_ctx, E, H, BLK):
    NT = 17 if has_ctx else 16
    nc = build_post(NT, E, H, BLK, has_ctx)
    in_maps = []
    for core in range(8):
        b, q = core // 4, core % 4
        xs = [x_lat[b, q * 2048:(q + 1) * 2048]]
        ms = [mix_lat[b, q * 2048:(q + 1) * 2048]]
        if has_ctx:
            cs = (q % 2) * 128
            xs.append(ctx[b, cs:cs + 128])
            ms.append(mix_ctx[b, cs:cs + 128])
        xo = np.ascontiguousarray(np.concatenate(xs, 0), dtype=np.float32)
        mo = np.ascontiguousarray(np.concatenate(ms, 0).T, dtype=np.float32)
        in_maps.append({
            "x": xo, "mixT": mo, "cT": _cT(c[b], c_ctx),
            "mod_w": np.ascontiguousarray(mod_w[layer]), "mod_b": np.ascontiguousarray(np.stack([mod_b[layer]] * 2)),
            "norm_g": np.ascontiguousarray(norm_g[layer]), "w_out": np.ascontiguousarray(w_out),
            "wg": np.ascontiguousarray(wg), "wu": np.ascontiguousarray(wu), "wd": np.ascontiguousarray(wd),
            "router": np.ascontiguousarray(np.concatenate([router, np.zeros((1024, 120), np.float32)], 1)), "ident": _IDENT,
        })
    res = run_bass_kernel_spmd(nc, in_maps, core_ids=list(range(8)), trace=TRACE)
    if TRACE:
        print("DEV_NS", res.exec_time_ns)
    x2 = np.empty_like(x_lat)
    ctx2 = np.empty_like(ctx) if has_ctx else None
    for core in range(8):
        b, q = core // 4, core % 4
        o = res.results[core]["out"]
        x2[b, q * 2048:(q + 1) * 2048] = o[0:2048]
        if has_ctx and q < 2:
            ctx2[b, q * 128:(q + 1) * 128] = o[2048:2176]
    return x2, ctx2


def mod_rows(p, ps, st, cT_d, modw_d, modb_d, mod_s, blocks):
    cT = p.sb("cT", [128, 8, 2], F32, st)
    sc = p.sb("silu_c", [128, 8, 2], F32, st)
    p.dma('sp', cT, cT_d)
    p.act(sc, cT, AF.Silu)
    lo, hi = blocks[0] * 512, (blocks[-1] + 1) * 512
    modrow = p.sb("modrow", [2, 6 * D], F32, st)
    modb = p.sb("modb", [2, 6 * D], F32, st)
    p.dma('sp', modb, modb_d)
    wblk = [p.sb(f"modw{i}", [128, 8, 512], F32, st) for i in range(2)]
    for cb in blocks:
        wb = wblk[cb % 2]
        p.dma('sp', wb, modw_d.with_ap(modw_d.ap.rearrange("(k p) n -> p k n", p=128)[:, :, cb * 512:(cb + 1) * 512]))
        pp = ps[cb % 2]
        for k in range(8):
            p.mm(pp[0:2, :], sc[:, k, :], wb[:, k, :], start=(k == 0), stop=(k == 7))
        p.tt('dve', modrow[:, cb * 512:(cb + 1) * 512], pp[0:2, :], modb[:, cb * 512:(cb + 1) * 512], ALU.add)
    p.dma('sp', mod_s[:, lo:hi], modrow[:, lo:hi])


def rope_tm(p, dst1, dst2, x1, x2, cos, sin, t1, t2, e1='dve', e2='pool'):
    p.tt(e1, t1, x1, cos, ALU.mult)
    p.tt(e2, t2, x2, sin, ALU.mult)
    p.tt(e1, t1, t1, t2, ALU.subtract)
    p.tt(e2, t2, x1, sin, ALU.mult)
    p.tt(e1, dst2, x2, cos, ALU.mult)
    p.tt(e1, dst2, dst2, t2, ALU.add)
    p.copy(e2, dst1, t1)


NTA = 66

def interleave(factories, window=2, skew=2):
    live, nxt, tick = [], 0, skew
    while live or nxt < len(factories):
        if nxt < len(factories) and len(live) < window and (not live or tick >= skew):
            live.append(factories[nxt]())
            nxt += 1
            tick = 0
        for g_ in list(live):
            try:
                next(g_)
            except StopIteration:
                live.remove(g_)
        tick += 1


def build_mix0():
    nc = bass.Bass("TRN2", target_bir_lowering=False)
    p = Prog(nc)
    NTOK = NTA * 128
    x_d = p.dram("x", [NTOK, D], F32, "ExternalInput")
    cT_d = p.dram("cT", [128, 8, 2], F32, "ExternalInput")
    modw_d = p.dram("mod_w", [D, 6 * D], F32, "ExternalInput")
    modb_d = p.dram("mod_b", [2, 6 * D], F32, "ExternalInput")
    ng_d = p.dram("norm_g", [4, D], F32, "ExternalInput")
    win_d = p.dram("w_in", [D, 896], F32, "ExternalInput")
    cos_d = p.dram("cos", [NTOK, 64], F32, "ExternalInput")
    sin_d = p.dram("sin", [NTOK, 64], F32, "ExternalInput")
    dec_d = p.dram("dec", [128, 2], F32, "ExternalInput")
    gb_d = p.dram("gb", [3, 128, 128], F32, "ExternalInput")
    cst_d = p.dram("cst", [6, 128, 128], F32, "ExternalInput")
    out_d = p.dram("mixT", [256, NTOK], F32, "ExternalOutput")
    mod_s = p.dram("mod_s", [2, 6 * D], F32, "Internal")
    qa_s = p.dram("qa_s", [128, NTOK], BF16, "Internal")
    ka_s = p.dram("ka_s", [128, NTOK], BF16, "Internal")
    va_s = p.dram("va_s", [128, NTA, 128], BF16, "Internal")

    ps = [p.ps(f"ps{i}", [128, 512]) for i in range(8)]
    cst = p.sb("cst", [128, 6, 128], F32)
    p.dma('sp', cst, cst_d.with_ap(cst_d.ap.rearrange("c p n -> p c n")))
    ident = cst[:, 0, :]
    gb = p.sb("gb", [128, 3, 128], F32)
    p.dma('sp', gb, gb_d.with_ap(gb_d.ap.rearrange("c p n -> p c n")))
    epsc = p.sb("epsc", [128, 1], F32)
    p.memset('dve', epsc, NORM_EPS)
    ones_bf = p.sb("ones_bf", [128, 128], BF16)
    p.memset('dve', ones_bf, 1.0)
    dec = p.sb("dec", [128, 2], F32)
    p.dma('sp', dec, dec_d)
    lg = p.sb("lg", [128, 2], F32)
    p.act(lg, dec, AF.Exp, scale=-math.log(2.0))
    p.act(lg, lg, AF.Ln, scale=-1.0, bias=1.0)
    MT = p.sb("MT", [128, 2, 128], F32)
    dcol = p.sb("dcol", [128, 8], F32)
    p.act(MT[:, 0, :], cst[:, 1, :], AF.Exp, scale=lg[:, 0:1])
    p.tt('dve', MT[:, 0, :], MT[:, 0, :], cst[:, 3, :], ALU.mult)
    p.act(MT[:, 1, :], cst[:, 2, :], AF.Exp, scale=lg[:, 1:2])
    p.tt('dve', MT[:, 1, :], MT[:, 1, :], cst[:, 4, :], ALU.mult)
    colc = cst[:, 5, :]
    p.act(dcol[:, 0:1], colc[:, 0:1], AF.Exp, scale=lg[:, 0:1])
    p.act(dcol[:, 1:2], colc[:, 1:2], AF.Exp, scale=lg[:, 1:2])
    p.act(dcol[:, 2:3], colc[:, 2:3], AF.Exp, scale=lg[:, 0:1])
    p.act(dcol[:, 3:4], colc[:, 3:4], AF.Exp, scale=lg[:, 1:2])
    p.act(dcol[:, 4:5], colc[:, 4:5], AF.Exp, scale=lg[:, 0:1])
    p.act(dcol[:, 5:6], colc[:, 4:5], AF.Exp, scale=lg[:, 1:2])

    QrT = p.sb("QrT", [128, NTOK], BF16)
    KrT = p.sb("KrT", [128, NTOK], BF16)
    Vr = p.sb("Vr", [128, NTA, 128], BF16)
    Kd = [p.sb(f"Kd{d}", [128, NTA, 128], BF16) for d in range(2)]
    Gs = p.sb("Gs", [128, NTA, 128], BF16)

    with p.scope() as st0:
        with p.scope() as st:
            mod_rows(p, ps, st, cT_d, modw_d, modb_d, mod_s, [0, 1, 2, 3])
        st = st0
        A1 = [p.sb(f"A1_{v}", [128, D], F32, st) for v in range(2)]
        B1 = [p.sb(f"B1_{v}", [128, D], F32, st) for v in range(2)]
        ngb = p.sb("ngb", [128, D], F32, st)
        p.dma('sp', ngb, ng_d.with_ap(ng_d.ap[0:1, :].partition_broadcast(128)))
        for v in range(2):
            p.dma('sp', B1[v], mod_s.with_ap(mod_s.ap[v:v + 1, 0:D].partition_broadcast(128)))
            p.dma('sp', A1[v], mod_s.with_ap(mod_s.ap[v:v + 1, D:2 * D].partition_broadcast(128)))
            p.ts('dve', A1[v], A1[v], 1.0, ALU.add)
            p.tt('dve', A1[v], A1[v], ngb, ALU.mult)
        W = p.sb("W", [128, 8, 896], BF16, st)
        wv = win_d.ap.rearrange("(k p) n -> p k n", p=128)
        for k in range(8):
            p.dma('pool', W[:, k, :], win_d.with_ap(wv[:, k, :]))
        xt = [p.sb(f"xt{i}", [128, D], F32, st) for i in range(2)]
        tms = [p.sb(f"tm{i}", [128, D], F32, st) for i in range(2)]
        sqs = [p.sb(f"sq{i}", [128, D], F32, st) for i in range(2)]
        hT = [p.sb(f"hT{i}", [128, 8, 128], BF16, st) for i in range(2)]
        cs = [p.sb(f"cs{i}", [128, 2, 64], F32, st) for i in range(2)]
        pr = [p.sb(f"pr{i}", [128, 896], F32, st) for i in range(2)]
        ro = [p.sb(f"ro{i}", [128, 4, 128], F32, st) for i in range(2)]
        rts = [p.sb(f"rt{i}", [128, 4, 64], F32, st) for i in range(2)]
        colsP = [p.sb(f"cols{i}", [128, NTA, 4], F32, st) for i in range(2)]
        stg = [p.sb(f"stg{i}", [128, 3, 128], BF16, st) for i in range(2)]

        def tile(t):
            v = 1 if t < 2 else 0
            par = t % 2
            pb_ = ps[par * 4:par * 4 + 4]
            tm, sq, rt = tms[par], sqs[par], rts[par]
            cols = colsP[par]
            x_t = xt[par]
            p.dma('sp', x_t, x_d[t * 128:(t + 1) * 128, :])
            c_t = cs[par]
            p.dma('sp', c_t[:, 0, :], cos_d[t * 128:(t + 1) * 128, :])
            p.dma('sp', c_t[:, 1, :], sin_d[t * 128:(t + 1) * 128, :])
            rc = cols[:, t, 0:1]
            p.act(sq, x_t, AF.Square)
            p.reduce('dve', rc, sq, ALU.add)
            yield
            p.act(rc, rc, AF.Sqrt, bias=epsc, scale=1.0 / D)
            p.recip(rc, rc)
            p.stt('dve', tm, x_t, rc, A1[v], ALU.mult, ALU.mult)
            p.tt('pool', tm, tm, B1[v], ALU.add)
            yield
            h = hT[par]
            for k in range(8):
                pt = pb_[2 + (k % 2)]
                p.tr(pt[:, 0:128], tm[:, k * 128:(k + 1) * 128], ident)
                p.copy('act' if k % 2 else 'dve', h[:, k, :], pt[:, 0:128])
                if k % 4 == 3:
                    yield
            for k in range(8):
                p.mm(pb_[0], h[:, k, :], W[:, k, 0:512], start=(k == 0), stop=(k == 7))
            for k in range(8):
                p.mm(pb_[1][:, 0:384], h[:, k, :], W[:, k, 512:896], start=(k == 0), stop=(k == 7))
            P = pr[par]
            p.copy('act', P[:, 0:512], pb_[0])
            p.copy('dve', P[:, 512:896], pb_[1][:, 0:384])
            yield
            p.ts('pool', P[:, 128:256], P[:, 128:256], 128.0 ** -0.5, ALU.mult)
            p.copy('pool', Vr[:, t, :], P[:, 256:384])
            p.act(Gs[:, t, :], P[:, 384:512], AF.Silu)
            sg = stg[par]
            p.copy('pool', sg[:, 2, :], P[:, 768:896])
            p.dma('sp', va_s[:, t, :], sg[:, 2, :])
            for i, (c0, gi) in enumerate(((512, 1), (640, 2))):
                rcq = cols[:, t, 1 + i:2 + i]
                p.act(sq[:, 0:128], P[:, c0:c0 + 128], AF.Square)
                p.reduce('dve', rcq, sq[:, 0:128], ALU.add)
                p.act(rcq, rcq, AF.Sqrt, bias=epsc, scale=1.0 / 128)
                p.recip(rcq, rcq)
                p.stt('dve', P[:, c0:c0 + 128], P[:, c0:c0 + 128], rcq, gb[:, gi, :], ALU.mult, ALU.mult)
                yield
            R = ro[par]
            for i, c0 in enumerate((0, 128, 512, 640)):
                rope_tm(p, R[:, i, 0:64], R[:, i, 64:128], P[:, c0:c0 + 64], P[:, c0 + 64:c0 + 128],
                        c_t[:, 0, :], c_t[:, 1, :], rt[:, i, :], sq[:, 128 + 64 * i:192 + 64 * i],
                        e1='dve' if i % 2 == 0 else 'pool', e2='pool' if i % 2 == 0 else 'dve')
                if i % 2:
                    yield
            p.ts('dve', Kd[0][:, t, :], R[:, 1, :], dcol[:, 2:3], ALU.mult)
            p.ts('pool', Kd[1][:, t, :], R[:, 1, :], dcol[:, 3:4], ALU.mult)
            ts_ = slice(t * 128, (t + 1) * 128)
            for i in range(4):
                pt = pb_[i]
                p.tr(pt[:, 0:128], R[:, i, :], ident)
                if i == 0:
                    p.copy('act', QrT[:, ts_], pt[:, 0:128])
                elif i == 1:
                    p.copy('dve', KrT[:, ts_], pt[:, 0:128])
                else:
                    p.copy('act' if i == 2 else 'dve', sg[:, i - 2, :], pt[:, 0:128])
            p.dma('sp', qa_s[:, ts_], sg[:, 0, :])
            p.dma('sp', ka_s[:, ts_], sg[:, 1, :])
            yield

        live, nxt_t, tick = [], 0, 0
        while live or nxt_t < NTA:
            if nxt_t < NTA and (not live or (len(live) < 2 and tick >= 4)):
                live.append(tile(nxt_t))
                nxt_t += 1
                tick = 0
            for g_ in list(live):
                try:
                    next(g_)
                except StopIteration:
                    live.remove(g_)
            tick += 1

    with p.scope() as st:
        Y = p.sb("Y", [128, NTA, 128], F32, st)
        S = p.sb("S", [128, 128], F32, st)
        Sb = p.sb("Sb", [128, 128], BF16, st)
        AT = [p.sb(f"AT{i}", [128, 128], BF16, st) for i in range(2)]
        for d in range(2):
            order = list(range(NTA)) if d == 0 else [1, 0] + list(range(NTA - 1, 1, -1))
            p.memset('dve', S, 0.0)
            p.memset('pool', Sb, 0.0)
            for n, c in enumerate(order):
                cs_ = slice(c * 128, (c + 1) * 128)
                pS, pP1, pP2, pKV = ps[n % 2], ps[2 + n % 2], ps[4 + n % 2], ps[6 + n % 2]
                p.mm(pS[:, 0:128], KrT[:, cs_], QrT[:, cs_])
                a = AT[n % 2]
                p.tt('dve', a, pS[:, 0:128], MT[:, d, :], ALU.mult)
                p.mm(pP1[:, 0:128], a, Vr[:, c, :])
                p.mm(pP2[:, 0:128], QrT[:, cs_], Sb)
                if d == 0:
                    p.copy('act', Y[:, c, :], pP1[:, 0:128])
                else:
                    p.tt('pool' if False else 'dve', Y[:, c, :], pP1[:, 0:128], Y[:, c, :], ALU.add)
                p.stt('dve', Y[:, c, :], pP2[:, 0:128], dcol[:, d:d + 1], Y[:, c, :], ALU.mult, ALU.add)
                p.mm(pKV[:, 0:128], Kd[d][:, c, :], Vr[:, c, :])
                p.stt('dve', S, S, dcol[:, 4 + d:5 + d], pKV[:, 0:128], ALU.mult, ALU.add)
                p.copy('act', Sb, S)
        OUT = p.sb("OUT", [128, NTOK], F32, st)
        sq2s = [p.sb(f"sq2_{i}", [128, 128], F32, st) for i in range(3)]
        cols2 = p.sb("cols2", [128, NTA], F32, st)
        p.barrier()

        def rtile(c):
            Yc = p.sub(Y[:, c, :], f"Y_{c}")
            rc = p.sub(cols2[:, c:c + 1], f"c2_{c}")
            sq2 = sq2s[c % 3]
            p.act(sq2, Yc, AF.Square)
            p.reduce('dve', rc, sq2, ALU.add)
            yield
            p.act(rc, rc, AF.Sqrt, bias=epsc, scale=1.0 / 128)
            p.recip(rc, rc)
            yield
            p.stt('dve', Yc, Yc, rc, gb[:, 0, :], ALU.mult, ALU.mult)
            p.tt('pool', Yc, Yc, Gs[:, c, :], ALU.mult)
            yield
            pt = ps[c % 4]
            p.tr(pt[:, 0:128], Yc, ident)
            p.copy('act', OUT[:, c * 128:(c + 1) * 128], pt[:, 0:128])
            yield

        interleave([(lambda c=c: rtile(c)) for c in range(NTA)], window=3, skew=1)
        p.dma('sp', out_d[0:128, :], OUT)

    with p.scope() as st:
        QaT = p.sb("QaT", [128, NTOK], BF16, st)
        KaT = p.sb("KaT", [128, NTOK], BF16, st)
        Va = p.sb("Va", [128, NTA, 128], BF16, st)
        p.dma('sp', QaT, qa_s)
        p.dma('sp', KaT, ka_s)
        p.dma('sp', Va, va_s)
        g2 = p.sb("g2", [128, 2, 128], F32, st)
        mx = p.sb("mx", [128, 4], F32, st)
        p.act(g2[:, 0, :], gb[:, 1, :], AF.Square)
        p.act(g2[:, 1, :], gb[:, 2, :], AF.Square)
        p.reduce('dve', mx[:, 0:1], g2[:, 0, :], ALU.max)
        p.reduce('dve', mx[:, 1:2], g2[:, 1, :], ALU.max)
        p.tt('dve', mx[:, 2:3], mx[:, 0:1], mx[:, 1:2], ALU.mult)
        p.act(mx[:, 3:4], mx[:, 2:3], AF.Sqrt, scale=128.0)
        p.ts('dve', mx[:, 3:4], mx[:, 3:4], -1.0, ALU.mult)
        negC = mx[:, 3:4]
        PT = [p.sb(f"PT{i}", [128, 512], BF16, st) for i in range(3)]
        rec = [p.sb(f"rec{i}", [128, 512], F32, st) for i in range(2)]
        og = [p.sb(f"og{i}", [128, 512], F32, st) for i in range(2)]
        groups = [(0, 256, [0, 1])] + [(256 + g * 512, 512, list(range(NTA))) for g in range(16)]
        it = 0
        for gi, (q0, W_, keys) in enumerate(groups):
            pO, pD = ps[4 + (gi % 2) * 2], ps[5 + (gi % 2) * 2]
            for n, kt in enumerate(keys):
                pS = ps[it % 3]
                pt_ = PT[it % 3]
                it += 1
                p.mm(pS[:, 0:W_], KaT[:, kt * 128:(kt + 1) * 128], QaT[:, q0:q0 + W_])
                p.act(pt_[:, 0:W_], pS[:, 0:W_], AF.Exp, bias=negC, scale=128.0 ** -0.5)
                p.mm(pO[:, 0:W_], Va[:, kt, :], pt_[:, 0:W_], start=(n == 0), stop=(n == len(keys) - 1))
                p.mm(pD[:, 0:W_], ones_bf, pt_[:, 0:W_], start=(n == 0), stop=(n == len(keys) - 1))
            r, o = rec[gi % 2], og[gi % 2]
            p.recip(r[:, 0:W_], pD[:, 0:W_])
            p.tt('dve', o[:, 0:W_], pO[:, 0:W_], r[:, 0:W_], ALU.mult)
            p.dma('sp', out_d[128:256, q0:q0 + W_], o[:, 0:W_])
    p.finalize()
    return nc


def _rope_tables(dim, rows=128, grid_w=64, theta=10000.0):
    row = np.repeat(np.arange(rows, dtype=np.float32), grid_w)
    col = np.tile(np.arange(grid_w, dtype=np.float32), rows)
    n_freq = dim // 4
    freqs = (np.float32(theta) ** (-np.arange(n_freq, dtype=np.float32) / np.float32(n_freq))).astype(np.float32)
    ang = np.concatenate([row[:, None] * freqs, col[:, None] * freqs], axis=-1).astype(np.float32)
    return np.cos(ang).astype(np.float32), np.sin(ang).astype(np.float32)


def _with_ctx_rope(cos, sin):
    n = cos.shape[1]
    c = np.concatenate([np.ones((256, n), np.float32), cos], 0)
    s = np.concatenate([np.zeros((256, n), np.float32), sin], 0)
    return np.ascontiguousarray(c), np.ascontiguousarray(s)


def _mix0_consts():
    ip = np.arange(128, dtype=np.float32)[:, None]
    i = np.arange(128, dtype=np.float32)[None, :]
    cst = np.zeros((6, 128, 128), np.float32)
    cst[0] = np.eye(128, dtype=np.float32)
    cst[1] = np.maximum(i - ip, 0)
    cst[2] = np.maximum(ip - i, 0)
    cst[3] = (i >= ip)
    cst[4] = (ip > i)
    cst[5, :, 0] = ip[:, 0] + 1
    cst[5, :, 1] = 128 - ip[:, 0]
    cst[5, :, 2] = 127 - ip[:, 0]
    cst[5, :, 3] = ip[:, 0]
    cst[5, :, 4] = 128
    return cst


def run_mix0(x, ctx, c, c_ctx, mod_w, mod_b, norm_g, w_in, decay_exp, ret_g, q_g, k_g):
    nc = build_mix0()
    cos, sin = _with_ctx_rope(*_rope_tables(128))
    cst = _mix0_consts()
    in_maps = []
    for core in range(8):
        b, j = core // 4, core % 4
        kv = j // 2
        colsel = np.concatenate([np.arange(j * 128, (j + 1) * 128) + off for off in (0, 512, 1024, 1536, 2048)] +
                                [np.arange(kv * 128, (kv + 1) * 128) + off for off in (2560, 2816)])
        gb = np.stack([np.broadcast_to(ret_g[j * 128:(j + 1) * 128], (128, 128)),
                       np.broadcast_to(q_g, (128, 128)), np.broadcast_to(k_g, (128, 128))]).astype(np.float32)
        in_maps.append({
            "x": np.ascontiguousarray(np.concatenate([ctx[b], x[b]], 0), dtype=np.float32),
            "cT": _cT(c[b], c_ctx), "mod_w": np.ascontiguousarray(mod_w), "mod_b": np.ascontiguousarray(np.stack([mod_b] * 2)),
            "norm_g": np.ascontiguousarray(norm_g), "w_in": np.ascontiguousarray(w_in[:, colsel]),
            "cos": cos, "sin": sin,
            "dec": np.ascontiguousarray(np.broadcast_to(decay_exp[:, j], (128, 2)), dtype=np.float32),
            "gb": np.ascontiguousarray(gb), "cst": cst,
        })
    res = run_bass_kernel_spmd(nc, in_maps, core_ids=list(range(8)), trace=TRACE)
    if TRACE:
        print("DEV_NS", res.exec_time_ns)
    mix = np.empty((2, NTA * 128, 1024), np.float32)
    for core in range(8):
        b, j = core // 4, core % 4
        o = res.results[core]["mixT"]
        mix[b, :, j * 128:(j + 1) * 128] = o[0:128].T
        mix[b, :, 512 + j * 128:512 + (j + 1) * 128] = o[128:256].T
    return mix[:, 256:], mix[:, :256]


LAM_INIT1 = 0.8 - 0.6 * math.exp(-0.3 * 1)
RWKV_LN_EPS = 64e-5
C_ID, C_TRI0, C_TRI1, C_MS0, C_MI0, C_MS1, C_SH, C_SHP, C_SHN, C_ONE = range(10)
B_W00, B_W01, B_A00, B_A01, B_KK, B_KA, B_RK, B_LNG, B_LNB = range(9)


def build_mix1():
    nc = bass.Bass("TRN2", target_bir_lowering=False)
    p = Prog(nc)
    NTOK = NTA * 128
    NLAT = 64 * 128
    x_d = p.dram("x", [NTOK, D], F32, "ExternalInput")
    cT_d = p.dram("cT", [128, 8, 2], F32, "ExternalInput")
    modw_d = p.dram("mod_w", [D, 6 * D], F32, "ExternalInput")
    modb_d = p.dram("mod_b", [2, 6 * D], F32, "ExternalInput")
    ng_d = p.dram("norm_g", [4, D], F32, "ExternalInput")
    win_d = p.dram("w_in", [D, 1152], F32, "ExternalInput")
    mu_d = p.dram("mu", [128, 768], F32, "ExternalInput")
    cos_d = p.dram("cos", [NTOK, 32], F32, "ExternalInput")
    sin_d = p.dram("sin", [NTOK, 32], F32, "ExternalInput")
    lam_d = p.dram("lamv", [128, 4, 64], F32, "ExternalInput")
    sg_d = p.dram("subg", [128, 1], F32, "ExternalInput")
    bc_d = p.dram("bc", [9, 128, 128], F32, "ExternalInput")
    mat_d = p.dram("mats", [3, 128, 128], F32, "ExternalInput")
    cst_d = p.dram("cst", [10, 128, 128], F32, "ExternalInput")
    out_d = p.dram("mixT", [256, NLAT], F32, "ExternalOutput")
    mod_s = p.dram("mod_s", [2, 6 * D], F32, "Internal")
    qd_s = p.dram("qd_s", [128, NTOK], BF16, "Internal")
    kd_s = p.dram("kd_s", [128, NTOK], BF16, "Internal")
    vd_s = p.dram("vd_s", [128, NTA, 128], BF16, "Internal")
    st_s = p.dram("st_s", [NTA, 128, 768], F32, "Internal")

    ps = [p.ps(f"ps{i}", [128, 512]) for i in range(8)]
    cst = p.sb("cst", [128, 10, 128], F32)
    p.dma('sp', cst, cst_d.with_ap(cst_d.ap.rearrange("c p n -> p c n")))
    cstb = p.sb("cstb", [128, 10, 128], BF16)
    p.copy('dve', cstb, cst)
    ident = cst[:, C_ID, :]
    bc = p.sb("bc", [128, 9, 128], F32)
    p.dma('sp', bc, bc_d.with_ap(bc_d.ap.rearrange("c p n -> p c n")))
    mats = p.sb("mats", [128, 3, 128], F32)
    p.dma('sp', mats, mat_d.with_ap(mat_d.ap.rearrange("c p n -> p c n")))
    epsc = p.sb("epsc", [128, 2], F32)
    p.memset('dve', epsc[:, 0:1], NORM_EPS)
    p.memset('dve', epsc[:, 1:2], RWKV_LN_EPS)
    ones_bf = cstb[:, C_ONE, :]
    Yacc = p.sb("Yacc", [128, NTA, 128], F32)
    Vst = p.sb("Vst", [128, NTA, 128], BF16)
    Gst = p.sb("Gst", [128, NTA, 128], BF16)
    BCf = p.sb("BCf", [128, NTA, 2], F32)
    ST = [[p.sb(f"ST{d}{h}", [64, 64], F32) for h in range(2)] for d in range(2)]
    YH = [Yacc, Yacc]

    _tmpn = [0]

    def scan_step(st, d, t, r, kd, v, kk, b, logw, first):
        T_ = TMP
        TRI = cst[:, C_TRI0 + d, :]
        MS = cst[:, C_MS0 if d == 0 else C_MS1, :]
        MST = cst[:, C_MS1 if d == 0 else C_MS0, :]
        MA = cst[:, C_MI0 if d == 0 else C_MS1, :]
        pc, pt = ps[0], ps[1]
        p.mm(pc[:, 0:128], TRI, logw)
        p.mm(pt[:, 0:128], cst[:, C_ONE, :], logw)
        cum = T_['cum']
        p.copy('act', cum, pc[:, 0:128])
        e_neg, e_x, e_in, e_end = T_['e_neg'], T_['e_x'], T_['e_in'], T_['e_end']
        p.act(e_neg, cum, AF.Exp, scale=-1.0)
        p.tt('dve', e_x, cum, logw, ALU.subtract)
        p.act(e_x, e_x, AF.Exp)
        if d == 0:
            p.act(e_in, cum, AF.Exp)
        p.tt('dve', e_end, pt[:, 0:128], cum, ALU.subtract)
        p.act(e_end, e_end, AF.Exp)
        at, bt, kt, rt, Bp, Kp = T_['at'], T_['bt'], T_['kt'], T_['rt'], T_['Bp'], T_['Kp']
        p.stt('dve', at, kk, -1.0, e_x, ALU.mult, ALU.mult)
        p.tt('pool', bt, b, e_neg, ALU.mult)
        p.tt('pool', kt, kd, e_neg, ALU.mult)
        p.tt('dve', rt, r, e_in if d == 0 else e_x, ALU.mult)
        p.tt('pool', Bp, b, e_end, ALU.mult)
        p.tt('pool', Kp, kd, e_end, ALU.mult)
        def chain(hh):
            banks = [ps[2 + hh], ps[4 + hh], ps[6 + hh]]
            bi = [0]

            def nb():
                bk = banks[bi[0] % 3]
                bi[0] += 1
                return bk

            hs_ = slice(hh * 64, (hh + 1) * 64)
            fm = {}
            for i, (nm, src) in enumerate((('at', at), ('bt', bt), ('kt', kt), ('rt', rt))):
                pp = nb()
                p.tr(pp[0:64, 0:128], src[:, hs_], ident)
                dst = T_[f'{nm}T{hh}']
                p.copy('act' if i % 2 else 'dve', dst, pp[0:64, 0:128])
                fm[nm] = dst
                if i % 2:
                    yield
            wc = T_[f'wc{hh}']
            pw_ = nb()
            p.mm(pw_[0:64, 0:1], logw[:, hs_], cst[:, C_ONE, 0:1])
            p.act(wc, pw_[0:64, 0:1], AF.Exp)
            LT, L, LakT, ArbT, ArkT = (T_[f'{n_}{hh}'] for n_ in ('LT', 'L', 'LakT', 'ArbT', 'ArkT'))
            for i, (dst, lhs, rhs, msk) in enumerate(((LT, fm['bt'], fm['at'], MS), (L, fm['at'], fm['bt'], MST),
                                                       (LakT, fm['kt'], fm['at'], MS), (ArbT, fm['bt'], fm['rt'], MA),
                                                       (ArkT, fm['kt'], fm['rt'], MA))):
                pp = nb()
                p.mm(pp[:, 0:128], lhs, rhs)
                p.tt('dve', dst, pp[:, 0:128], msk, ALU.mult)
                if i in (1, 4):
                    yield
            G, Pa, PaT, Pb, PbT = (T_[f'{n_}{hh}'] for n_ in ('G', 'Pa', 'PaT', 'Pb', 'PbT'))
            p.tt('pool', G, LT, ident, ALU.add)
            cur, curT = L, LT
            nxt = [(Pa, PaT), (Pb, PbT)]
            for lev in range(1, 7):
                Pn, PnT = nxt[lev % 2]
                pp = nb()
                p.mm(pp[:, 0:128], curT, cur)
                if lev < 6:
                    pp2 = nb()
                    p.mm(pp2[:, 0:128], cur, curT)
                p.copy('act', Pn, pp[:, 0:128])
                if lev < 6:
                    p.copy('dve', PnT, pp2[:, 0:128])
                yield
                pq = nb()
                p.mm(pq[:, 0:128], Pn, G)
                p.tt('dve', G, pq[:, 0:128], G, ALU.add)
                cur, curT = Pn, PnT
                yield
            S = ST[d][hh]
            X, U = T_[f'X{hh}'], T_[f'U{hh}']
            px = nb()
            vh = v[:, hs_]
            p.mm(px[:, 0:64], fm['at'], S, start=True, stop=False)
            p.mm(px[:, 0:64], LakT, vh, start=False, stop=True)
            p.copy('act', X, px[:, 0:64])
            yield
            pu = nb()
            p.mm(pu[:, 0:64], G, X)
            p.copy('dve', U, pu[:, 0:64])
            yield
            py = nb()
            p.mm(py[:, 0:64], fm['rt'], S, start=True, stop=False)
            p.mm(py[:, 0:64], ArbT, U, start=False, stop=False)
            p.mm(py[:, 0:64], ArkT, vh, start=False, stop=True)
            pss = nb()
            p.mm(pss[0:64, 0:64], Bp[:, hs_], U, start=True, stop=False)
            p.mm(pss[0:64, 0:64], Kp[:, hs_], vh, start=False, stop=True)
            ydst = YH[hh][:, t, hs_]
            if first:
                p.copy('act', ydst, py[:, 0:64])
            else:
                p.tt('dve', ydst, py[:, 0:64], ydst, ALU.add)
            p.stt('dve', S, S, wc, pss[0:64, 0:64], ALU.mult, ALU.add)
            yield

        live = [chain(0), chain(1)]
        while live:
            for g_ in list(live):
                try:
                    next(g_)
                except StopIteration:
                    live.remove(g_)

    with p.scope() as st0:
        with p.scope() as st:
            mod_rows(p, ps, st, cT_d, modw_d, modb_d, mod_s, [0, 1, 2, 3])
        st = st0
        TMP = {}
        for nm in ('cum', 'e_neg', 'e_x', 'e_in', 'e_end', 'at', 'bt', 'kt', 'rt', 'Bp', 'Kp'):
            TMP[nm] = p.sb("t_" + nm, [128, 128], F32, st)
        for hh in range(2):
            for nm in ('LT', 'L', 'LakT', 'ArbT', 'ArkT', 'G', 'Pa', 'PaT', 'Pb', 'PbT'):
                TMP[f'{nm}{hh}'] = p.sb(f"t_{nm}{hh}", [128, 128], F32, st)
            for nm in ('X', 'U'):
                TMP[f'{nm}{hh}'] = p.sb(f"t_{nm}{hh}", [128, 64], F32, st)
        for hh in range(2):
            for nm in ('at', 'bt', 'kt', 'rt'):
                TMP[f'{nm}T{hh}'] = p.sb(f"t_{nm}T{hh}", [64, 128], F32, st)
            TMP[f'wc{hh}'] = p.sb(f"t_wc{hh}", [64, 1], F32, st)
        for d in range(2):
            for hh in range(2):
                p.memset('dve', ST[d][hh], 0.0)
        with p.scope() as st:
            A1 = [p.sb(f"A1_{v}", [128, D], F32, st) for v in range(2)]
            B1 = [p.sb(f"B1_{v}", [128, D], F32, st) for v in range(2)]
            ngb = p.sb("ngb", [128, D], F32, st)
            p.dma('sp', ngb, ng_d.with_ap(ng_d.ap[0:1, :].partition_broadcast(128)))
            for v in range(2):
                p.dma('sp', B1[v], mod_s.with_ap(mod_s.ap[v:v + 1, 0:D].partition_broadcast(128)))
                p.dma('sp', A1[v], mod_s.with_ap(mod_s.ap[v:v + 1, D:2 * D].partition_broadcast(128)))
                p.ts('dve', A1[v], A1[v], 1.0, ALU.add)
                p.tt('dve', A1[v], A1[v], ngb, ALU.mult)
            Wd = p.sb("Wd", [128, 8, 384], BF16, st)
            W1 = p.sb("W1", [128, 8, 768], BF16, st)
            W2 = p.sb("W2", [128, 8, 768], BF16, st)
            mu = p.sb("mu", [128, 2, 768], F32, st)
            p.dma('sp', mu[:, 0, :], mu_d)
            p.ts('dve', mu[:, 1, :], mu[:, 0, :], 0.5, ALU.mult)
            p.ts('dve', mu[:, 0, :], mu[:, 0, :], -1.0, ALU.mult, 1.0, ALU.add)
            wv = win_d.ap.rearrange("(k p) n -> p k n", p=128)
            wst = [p.sb(f"wst{i}", [128, 768], F32, st) for i in range(2)]
            for k in range(8):
                p.dma('pool', Wd[:, k, :], win_d.with_ap(wv[:, k, 0:384]))
                w_ = wst[k % 2]
                p.dma('sp', w_, win_d.with_ap(wv[:, k, 384:1152]))
                p.tt('dve', W1[:, k, :], w_, mu[:, 0, :], ALU.mult)
                p.tt('pool', W2[:, k, :], w_, mu[:, 1, :], ALU.mult)
            xt = [p.sb(f"xt{i}", [128, D], F32, st) for i in range(2)]
            tm = p.sb("tm", [128, D], F32, st)
            sq = p.sb("sq", [128, D], F32, st)
            hb = [p.sb(f"hb{i}", [128, D], BF16, st) for i in range(3)]
            hT = p.sb("hT", [128, 8, 128], BF16, st)
            hsT = p.sb("hsT", [128, 8, 128], BF16, st)
            cs = [p.sb(f"cs{i}", [128, 2, 32], F32, st) for i in range(2)]
            Pd = p.sb("Pd", [128, 384], F32, st)
            RKV = p.sb("RKV", [128, 384], F32, st)
            WAG = p.sb("WAG", [128, 384], F32, st)
            Rr = p.sb("Rr", [128, 2, 128], F32, st)
            rtmp = p.sb("rtmp", [128, 8, 32], F32, st)
            cols = p.sb("cols", [128, NTA, 8], F32, st)
            stg = [p.sb(f"stg{i}", [128, 3, 128], BF16, st) for i in range(2)]
            thT = p.sb("thT", [128, 3, 128], F32, st)
            Q = {}
            for nm in ('th', 'kk', 'tq', 'a0', 'a1', 'lw0', 'kd0', 'b0'):
                Q[nm] = p.sb("q_" + nm, [128, 128], F32, st)
            stash = wst

            def stage1(t):
                v = 1 if t < 2 else 0
                x_t = xt[t % 2]
                p.dma('sp', x_t, x_d[t * 128:(t + 1) * 128, :])
                rc = cols[:, t, 0:1]
                p.act(sq, x_t, AF.Square)
                p.reduce('dve', rc, sq, ALU.add)
                p.act(rc, rc, AF.Sqrt, bias=epsc[:, 0:1], scale=1.0 / D)
                p.recip(rc, rc)
                p.stt('dve', tm, x_t, rc, A1[v], ALU.mult, ALU.mult)
                p.tt('pool', hb[t % 3], tm, B1[v], ALU.add)

            stage1(0)
            for t in range(NTA):
                if t + 1 < NTA:
                    stage1(t + 1)
                has_prev = t not in (0, 2)
                has_next = t not in (1, NTA - 1)
                h = hb[t % 3]
                for k in range(8):
                    ks = slice(k * 128, (k + 1) * 128)
                    pa = ps[k // 4][:, (k % 4) * 128:(k % 4 + 1) * 128]
                    p.mm(pa, h[:, ks], cstb[:, C_ID, :])
                    pb = ps[2 + k // 4][:, (k % 4) * 128:(k % 4 + 1) * 128]
                    p.mm(pb, h[:, ks], cstb[:, C_SH, :], start=True, stop=not (has_prev or has_next))
                    if has_prev:
                        p.mm(pb, hb[(t - 1) % 3][:, ks], cstb[:, C_SHP, :], start=False, stop=not has_next)
                    if has_next:
                        p.mm(pb, hb[(t + 1) % 3][:, ks], cstb[:, C_SHN, :], start=False, stop=True)
                for half in range(2):
                    p.copy('act', hT[:, half * 4:(half + 1) * 4, :], ps[half][:, :].with_ap(ps[half].ap.rearrange("p (k n) -> p k n", k=4)))
                    p.copy('dve', hsT[:, half * 4:(half + 1) * 4, :], ps[2 + half].with_ap(ps[2 + half].ap.rearrange("p (k n) -> p k n", k=4)))
                for k in range(8):
                    p.mm(ps[4][:, 0:384], hT[:, k, :], Wd[:, k, :], start=(k == 0), stop=(k == 7))
                for c0, pp in ((0, ps[5]), (384, ps[6])):
                    for k in range(8):
                        p.mm(pp[:, 0:384], hT[:, k, :], W1[:, k, c0:c0 + 384], start=(k == 0), stop=False)
                    for k in range(8):
                        p.mm(pp[:, 0:384], hsT[:, k, :], W2[:, k, c0:c0 + 384], start=False, stop=(k == 7))
                p.copy('act', Pd, ps[4][:, 0:384])
                p.copy('dve', RKV, ps[5][:, 0:384])
                p.copy('act', WAG, ps[6][:, 0:384])
                c_t = cs[t % 2]
                p.dma('sp', c_t[:, 0, :], cos_d[t * 128:(t + 1) * 128, :])
                p.dma('sp', c_t[:, 1, :], sin_d[t * 128:(t + 1) * 128, :])
                for i in range(4):
                    c0 = i * 64
                    rope_tm(p, Rr[:, i // 2, (i % 2) * 64:(i % 2) * 64 + 32], Rr[:, i // 2, (i % 2) * 64 + 32:(i % 2) * 64 + 64],
                            Pd[:, c0:c0 + 32], Pd[:, c0 + 32:c0 + 64], c_t[:, 0, :], c_t[:, 1, :],
                            rtmp[:, 2 * i, :], rtmp[:, 2 * i + 1, :],
                            e1='dve' if i % 2 == 0 else 'pool', e2='pool' if i % 2 == 0 else 'dve')
                sg = stg[t % 2]
                ts_ = slice(t * 128, (t + 1) * 128)
                for i in range(2):
                    pp = ps[i]
                    p.tr(pp[:, 0:128], Rr[:, i, :], ident)
                    p.copy('act' if i else 'dve', sg[:, i, :], pp[:, 0:128])
                p.copy('pool', sg[:, 2, :], Pd[:, 256:384])
                p.dma('sp', qd_s[:, ts_], sg[:, 0, :])
                p.dma('sp', kd_s[:, ts_], sg[:, 1, :])
                p.dma('sp', vd_s[:, t, :], sg[:, 2, :])
                r_, k_, v_ = RKV[:, 0:128], RKV[:, 128:256], RKV[:, 256:384]
                p.copy('pool', Vst[:, t, :], v_)
                th = Q['th']
                p.act(th, WAG[:, 0:128], AF.Tanh)
                p.tr(ps[0][:, 0:128], th, ident)
                p.copy('dve', thT[:, 0, :], ps[0][:, 0:128])
                p.tr(ps[1][:, 0:128], WAG[:, 128:256], ident)
                p.copy('act', thT[:, 1, :], ps[1][:, 0:128])
                p.act(th, WAG[:, 256:384], AF.Sigmoid)
                p.tr(ps[2][:, 0:128], th, ident)
                p.copy('dve', thT[:, 2, :], ps[2][:, 0:128])
                p.mm(ps[3][:, 0:128], thT[:, 2, :], mats[:, 2, :])
                p.copy('act', Gst[:, t, :], ps[3][:, 0:128])
                kk = Q['kk']
                tq = Q['tq']
                p.tt('dve', kk, k_, bc[:, B_KK, :], ALU.mult)
                p.act(tq, kk, AF.Square)
                nrm = cols[:, t, 2:4]
                p.reduce('dve', nrm, tq.with_ap(tq.ap.rearrange("p (h c) -> p h c", h=2)), ALU.add)
                p.act(nrm, nrm, AF.Sqrt)
                p.ts('dve', nrm, nrm, 1e-12, ALU.max)
                p.recip(nrm, nrm)
                for hh in range(2):
                    p.ts('dve', kk[:, hh * 64:(hh + 1) * 64], kk[:, hh * 64:(hh + 1) * 64], cols[:, t, 2 + hh:3 + hh], ALU.mult)
                p.tt('pool', tq, r_, k_, ALU.mult)
                p.tt('pool', tq, tq, bc[:, B_RK, :], ALU.mult)
                p.reduce('dve', BCf[:, t, :], tq.with_ap(tq.ap.rearrange("p (h c) -> p h c", h=2)), ALU.add)
                sth = stash[t % 2]
                res = []
                for d in range(2):
                    ds_ = slice(d * 64, (d + 1) * 64)
                    a_d = Q['a0'] if d == 0 else sth[:, 512:640]
                    lw = Q['lw0'] if d == 0 else sth[:, 640:768]
                    kd = Q['kd0'] if d == 0 else sth[:, 128:256]
                    pw, pa_ = ps[4 + d], ps[6 + d]
                    p.mm(pw[:, 0:128], thT[ds_, 0, :], mats[ds_, 0, :])
                    p.mm(pa_[:, 0:128], thT[ds_, 1, :], mats[ds_, 1, :])
                    p.tt('dve', lw, pw[:, 0:128], bc[:, B_W00 + d, :], ALU.add)
                    p.act(lw, lw, AF.Sigmoid)
                    p.ts('pool', lw, lw, -math.exp(-0.5), ALU.mult)
                    p.tt('dve', a_d, pa_[:, 0:128], bc[:, B_A00 + d, :], ALU.add)
                    p.act(a_d, a_d, AF.Sigmoid)
                    p.stt('dve', kd, a_d, -1.0, bc[:, B_KA, :], ALU.add, ALU.mult)
                    p.ts('pool', kd, kd, 1.0, ALU.add)
                    p.tt('pool', kd, kd, k_, ALU.mult)
                    b_d = Q['b0'] if d == 0 else sth[:, 512:640]
                    p.tt('dve', b_d, kk, a_d, ALU.mult)
                    res.append((kd, b_d, lw))
                p.copy('pool', sth[:, 0:128], r_)
                p.copy('pool', sth[:, 256:384], v_)
                p.copy('pool', sth[:, 384:512], kk)
                p.dma('sp', st_s[t], sth)
                scan_step(st, 0, t, r_, res[0][0], v_, kk, res[0][1], res[0][2], True)
        with p.scope() as st:
            stash = [p.sb(f"stashb{i}", [128, 768], F32, st) for i in range(2)]
            order = [1, 0] + list(range(NTA - 1, 1, -1))
            for n, t in enumerate(order):
                sth = stash[n % 2]
                p.dma('sp', sth, st_s[t])
                scan_step(st, 1, t, sth[:, 0:128], sth[:, 128:256], sth[:, 256:384], sth[:, 384:512], sth[:, 512:640], sth[:, 640:768], False)
            OUT = p.sb("OUTr", [128, NLAT], F32, st)
            fqs = [[p.sb(f"fq{j}_{i}", [128, 128], F32, st) for i in range(3)] for j in range(3)]
            fcol = p.sb("fcol", [128, NTA, 8], F32, st)
            p.barrier()

            def ftile(t):
                fq = fqs[t % 3]
                y = p.sub(Yacc[:, t, :], f"Ya_{t}")
                fc = p.sub(fcol[:, t, :], f"fc_{t}")
                y3 = y.with_ap(y.ap.rearrange("p (h c) -> p h c", h=2))
                mean = fc[:, 0:2]
                p.reduce('dve', mean, y3, ALU.add)
                p.ts('dve', mean, mean, 1.0 / 64, ALU.mult)
                yc = fq[0]
                for hh in range(2):
                    p.ts('dve', yc[:, hh * 64:(hh + 1) * 64], y[:, hh * 64:(hh + 1) * 64], fc[:, hh:hh + 1], ALU.subtract)
                p.act(fq[1], yc, AF.Square)
                yield
                var = fc[:, 2:4]
                p.reduce('dve', var, fq[1].with_ap(fq[1].ap.rearrange("p (h c) -> p h c", h=2)), ALU.add)
                p.act(var, var, AF.Sqrt, bias=epsc[:, 1:2], scale=1.0 / 64)
                yield
                p.recip(var, var)
                for hh in range(2):
                    hs_ = slice(hh * 64, (hh + 1) * 64)
                    p.stt('dve', yc[:, hs_], yc[:, hs_], fc[:, 2 + hh:3 + hh], bc[:, B_LNG, hs_], ALU.mult, ALU.mult)
                p.tt('pool', yc, yc, bc[:, B_LNB, :], ALU.add)
                yield
                for hh in range(2):
                    hs_ = slice(hh * 64, (hh + 1) * 64)
                    p.stt('dve', yc[:, hs_], Vst[:, t, hs_], BCf[:, t, hh:hh + 1], yc[:, hs_], ALU.mult, ALU.add)
                p.tt('pool', fq[2], yc, Gst[:, t, :], ALU.mult)
                yield
                pp = ps[t % 4]
                p.tr(pp[:, 0:128], fq[2], ident)
                p.copy('act', OUT[:, (t - 2) * 128:(t - 1) * 128], pp[:, 0:128])
                yield

            interleave([(lambda t=t: ftile(t)) for t in range(2, NTA)], window=3, skew=1)
            p.dma('sp', out_d[128:256, :], OUT)

    with p.scope() as st:
        QT = p.sb("QdT", [128, NTOK], BF16, st)
        KT = p.sb("KdT", [128, NTOK], BF16, st)
        Vd = p.sb("Vd", [128, NTA, 128], BF16, st)
        p.dma('sp', QT, qd_s)
        p.dma('sp', KT, kd_s)
        p.dma('sp', Vd, vd_s)
        lamv = p.sb("lamv", [128, 4, 64], F32, st)
        p.dma('sp', lamv, lam_d)
        lc = p.sb("lc", [128, 8], F32, st)
        lt = p.sb("lt", [128, 2, 64], F32, st)
        p.tt('dve', lt[:, 0, :], lamv[:, 0, :], lamv[:, 1, :], ALU.mult)
        p.tt('dve', lt[:, 1, :], lamv[:, 2, :], lamv[:, 3, :], ALU.mult)
        p.reduce('dve', lc[:, 0:2], lt, ALU.add)
        p.act(lc[:, 2:4], lc[:, 0:2], AF.Exp)
        p.tt('dve', lc[:, 4:5], lc[:, 2:3], lc[:, 3:4], ALU.subtract)
        p.ts('dve', lc[:, 5:6], lc[:, 4:5], LAM_INIT1, ALU.add, -1.0, ALU.mult)
        neglam = lc[:, 5:6]
        subg = p.sb("subg", [128, 1], F32, st)
        p.dma('sp', subg, sg_d)
        p.ts('dve', subg, subg, 1.0 - LAM_INIT1, ALU.mult)
        PT = [p.sb(f"PTd{i}", [128, 512], BF16, st) for i in range(3)]
        rec = [p.sb(f"recd{i}", [128, 512], F32, st) for i in range(2)]
        o0 = p.sb("o0", [128, 512], F32, st)
        o1 = p.sb("o1", [128, 512], F32, st)
        osq = p.sb("osq", [128, 512], F32, st)
        it = 0
        for g in range(16):
            q0 = 256 + g * 512
            for m in range(2):
                ms = slice(m * 64, (m + 1) * 64)
                pO, pD = ps[4 + m * 2], ps[5 + m * 2]
                for kt in range(NTA):
                    pS = ps[it % 3]
                    pt_ = PT[it % 3]
                    it += 1
                    p.mm(pS, KT[ms, kt * 128:(kt + 1) * 128], QT[ms, q0:q0 + 512])
                    p.act(pt_, pS, AF.Exp, scale=64.0 ** -0.5)
                    p.mm(pO, Vd[:, kt, :], pt_, start=(kt == 0), stop=(kt == NTA - 1))
                    p.mm(pD, ones_bf, pt_, start=(kt == 0), stop=(kt == NTA - 1))
                p.recip(rec[m], pD)
                p.tt('dve', o0 if m == 0 else o1, pO, rec[m], ALU.mult)
            p.stt('dve', o0, o1, neglam, o0, ALU.mult, ALU.add)
            p.act(osq, o0, AF.Square)
            pn = ps[3]
            p.mm(pn, cst[:, C_ONE, :], osq)
            p.act(osq, pn, AF.Sqrt, bias=epsc[:, 0:1], scale=1.0 / 128)
            p.recip(osq, osq)
            p.stt('dve', o0, o0, subg, osq, ALU.mult, ALU.mult)
            p.dma('sp', out_d[0:128, g * 512:(g + 1) * 512], o0)
    p.finalize()
    return nc


def _mix1_consts():
    s = np.arange(128, dtype=np.float32)[:, None]
    t = np.arange(128, dtype=np.float32)[None, :]
    c = np.zeros((10, 128, 128), np.float32)
    c[C_ID] = np.eye(128, dtype=np.float32)
    c[C_TRI0] = (s <= t)
    c[C_TRI1] = (s >= t)
    c[C_MS0] = (s < t)
    c[C_MI0] = (s <= t)
    c[C_MS1] = (s > t)
    c[C_SH] = (np.abs(s - t) == 1)
    c[C_SHP] = (s == 127) & (t == 0)
    c[C_SHN] = (s == 0) & (t == 127)
    c[C_ONE] = 1.0
    return c


def run_mix1(x, ctx, c, c_ctx, mod_w, mod_b, norm_g, w_in, lamv, subg, mu, w0, w2, a0, a2, g2, k_k, k_a, r_k, ln_g, ln_b):
    nc = build_mix1()
    cos, sin = _with_ctx_rope(*_rope_tables(64))
    cst = _mix1_consts()
    in_maps = []
    for core in range(8):
        b, j = core // 4, core % 4
        hc = np.arange(j * 128, (j + 1) * 128)
        a128 = np.arange(128)
        colsel = np.concatenate([hc, 512 + hc, 1024 + hc, 1536 + hc, 2048 + hc, 2560 + hc, 3072 + a128, 3200 + a128, 3328 + a128])
        musel = np.concatenate([hc, 512 + hc, 1024 + hc, 1536 + a128, 1664 + a128, 1792 + a128])
        rows = [w0[0][hc], w0[1][hc], a0[0][hc], a0[1][hc], k_k[hc], k_a[hc], r_k.reshape(-1)[hc], ln_g[hc], ln_b[hc]]
        bc = np.stack([np.broadcast_to(r, (128, 128)) for r in rows]).astype(np.float32)
        mats = np.stack([np.concatenate([w2[0][:, hc], w2[1][:, hc]], 0), np.concatenate([a2[0][:, hc], a2[1][:, hc]], 0), g2[:, hc]]).astype(np.float32)
        in_maps.append({
            "x": np.ascontiguousarray(np.concatenate([ctx[b], x[b]], 0), dtype=np.float32),
            "cT": _cT(c[b], c_ctx), "mod_w": np.ascontiguousarray(mod_w), "mod_b": np.ascontiguousarray(np.stack([mod_b] * 2)),
            "norm_g": np.ascontiguousarray(norm_g), "w_in": np.ascontiguousarray(w_in[:, colsel]),
            "mu": np.ascontiguousarray(np.broadcast_to(mu[musel], (128, 768)), dtype=np.float32),
            "cos": cos, "sin": sin,
            "lamv": np.ascontiguousarray(np.broadcast_to(lamv, (128, 4, 64)), dtype=np.float32),
            "subg": np.ascontiguousarray(subg.reshape(128, 1), dtype=np.float32),
            "bc": np.ascontiguousarray(bc), "mats": np.ascontiguousarray(mats), "cst": cst,
        })
    res = run_bass_kernel_spmd(nc, in_maps, core_ids=list(range(8)), trace=TRACE)
    if TRACE:
        print("DEV_NS", res.exec_time_ns)
    mix = np.empty((2, 8192, 1024), np.float32)
    for core in range(8):
        b, j = core // 4, core % 4
        o = res.results[core]["mixT"]
        mix[b, :, j * 128:(j + 1) * 128] = o[0:128].T
        mix[b, :, 512 + j * 128:512 + (j + 1) * 128] = o[128:256].T
    return mix


def kernel(x, c, ctx, c_ctx, mod_w, mod_b, norm_g, even_w_in, even_w_out, ret_decay_exp, ret_norm_g,
           gqa_q_norm, gqa_k_norm, ffn_w_gate, ffn_w_up, ffn_w_down, odd_w_in, odd_w_out, diff_lambda,
           diff_subln_g, rwkv_mu, rwkv_w0, rwkv_w2, rwkv_a0, rwkv_a2, rwkv_g2, rwkv_k_k, rwkv_k_a, rwkv_r_k,
           rwkv_ln_g, rwkv_ln_b, moe_router, moe_w_gate, moe_w_up, moe_w_down):
    f = lambda a: np.asarray(a, dtype=np.float32)
    x, c, ctx, c_ctx, mod_w, mod_b, norm_g = map(f, (x, c, ctx, c_ctx, mod_w, mod_b, norm_g))
    mix_lat, mix_ctx = run_mix0(x, ctx, c, c_ctx, mod_w[0], mod_b[0], norm_g[0], f(even_w_in)[0], f(ret_decay_exp)[0],
                                f(ret_norm_g)[0], f(gqa_q_norm)[0], f(gqa_k_norm)[0])
    x1, ctx1 = run_post(0, x, ctx, mix_lat, mix_ctx, c, c_ctx, mod_w, mod_b, norm_g, f(even_w_out)[0],
                        f(ffn_w_gate), f(ffn_w_up), f(ffn_w_down), np.zeros((1024, 8), np.float32), True, 1, 2816, 2)
    mix1 = run_mix1(x1, ctx1, c, c_ctx, mod_w[1], mod_b[1], norm_g[1], f(odd_w_in)[0], f(diff_lambda)[0], f(diff_subln_g)[0],
                    f(rwkv_mu)[0], f(rwkv_w0)[0], f(rwkv_w2)[0], f(rwkv_a0)[0], f(rwkv_a2)[0], f(rwkv_g2)[0], f(rwkv_k_k)[0],
                    f(rwkv_k_a)[0], f(rwkv_r_k)[0], f(rwkv_ln_g)[0], f(rwkv_ln_b)[0])
    x2 = run_post1(x1, mix1, c, c_ctx, mod_w[1], mod_b[1], norm_g[1], f(odd_w_out)[0],
                   f(moe_w_gate)[0], f(moe_w_up)[0], f(moe_w_down)[0], f(moe_router)[0])
    return x2


def build_d1(NT=16):
    nc = bass.Bass("TRN2", target_bir_lowering=False)
    p = Prog(nc)
    NTOK = NT * 128
    x_d = p.dram("x", [NTOK, D], F32, "ExternalInput")
    mixT_d = p.dram("mixT", [D, NTOK], F32, "ExternalInput")
    cT_d = p.dram("cT", [128, 8, 2], F32, "ExternalInput")
    modw_d = p.dram("mod_w", [D, 6 * D], F32, "ExternalInput")
    modb_d = p.dram("mod_b", [2, 6 * D], F32, "ExternalInput")
    ng_d = p.dram("norm_g", [4, D], F32, "ExternalInput")
    wout_d = p.dram("w_out", [D, D], F32, "ExternalInput")
    rt_d = p.dram("router", [D, 128], F32, "ExternalInput")
    id_d = p.dram("ident", [128, 128], F32, "ExternalInput")
    x1_d = p.dram("x1", [NTOK, D], F32, "ExternalOutput")
    h2T_d = p.dram("h2T", [D, NTOK], BF16, "ExternalOutput")
    gates_d = p.dram("gates", [NTOK, 8], F32, "ExternalOutput")
    mod_s = p.dram("mod_s", [2, 6 * D], F32, "Internal")
    ps = [p.ps(f"ps{i}", [128, 512]) for i in range(8)]
    ident = p.sb("ident", [128, 128], F32)
    p.dma('sp', ident, id_d)
    epsc = p.sb("epsc", [128, 1], F32)
    p.memset('dve', epsc, NORM_EPS)
    h2T = p.sb("h2T", [128, 8, NTOK], BF16)
    gates = p.sb("gates", [128, NT, 8], F32)
    with p.scope() as st:
        mod_rows(p, ps, st, cT_d, modw_d, modb_d, mod_s, [4, 5, 6, 7, 8, 9])

    def rms_rstd(dst_col, src, sq_tmp):
        p.act(sq_tmp, src, AF.Square)
        p.reduce('dve', dst_col, sq_tmp, ALU.add)
        p.act(dst_col, dst_col, AF.Sqrt, bias=epsc, scale=1.0 / D)
        p.recip(dst_col, dst_col)

    with p.scope() as st:
        G1 = p.sb("G1", [128, D], F32, st)
        A2 = p.sb("A2", [128, D], F32, st)
        B2 = p.sb("B2", [128, D], F32, st)
        ngb = p.sb("ngb", [128, 2, D], F32, st)
        p.dma('sp', ngb[:, 0, :], ng_d.with_ap(ng_d.ap[1:2, :].partition_broadcast(128)))
        p.dma('sp', ngb[:, 1, :], ng_d.with_ap(ng_d.ap[2:3, :].partition_broadcast(128)))
        p.dma('sp', G1, mod_s.with_ap(mod_s.ap[0:1, 2 * D:3 * D].partition_broadcast(128)))
        p.tt('dve', G1, G1, ngb[:, 0, :], ALU.mult)
        p.dma('sp', B2, mod_s.with_ap(mod_s.ap[0:1, 3 * D:4 * D].partition_broadcast(128)))
        p.dma('sp', A2, mod_s.with_ap(mod_s.ap[0:1, 4 * D:5 * D].partition_broadcast(128)))
        p.ts('dve', A2, A2, 1.0, ALU.add)
        p.tt('dve', A2, A2, ngb[:, 1, :], ALU.mult)
        mixT = p.sb("mixT", [128, 8, NTOK], BF16, st)
        mview = mixT_d.ap.rearrange("(k p) n -> p k n", p=128)
        for k in range(8):
            p.dma('pool', mixT[:, k, :], mixT_d.with_ap(mview[:, k, :]))
        wout = p.sb("wout", [128, 8, D], BF16, st)
        wv = wout_d.ap.rearrange("(k p) n -> p k n", p=128)
        for k in range(8):
            p.dma('pool', wout[:, k, :], wout_d.with_ap(wv[:, k, :]))
        rt = p.sb("rt", [128, 8, 128], F32, st)
        if DBG != 'd1c':
            p.dma('sp', rt, rt_d.with_ap(rt_d.ap.rearrange("(k p) e -> p k e", p=128)))
        h2Tf = p.sb("h2Tf", [128, 8, 128], F32, st)
        xt = [p.sb(f"xt{i}", [128, D], F32, st) for i in range(2)]
        tmp = [p.sb(f"tmp{i}", [128, D], F32, st) for i in range(2)]
        sq = p.sb("sq", [128, D], F32, st)
        cols = p.sb("cols", [128, NT, 8], F32, st)
        lg = p.sb("lg", [128, 4, 8], F32, st)
        for t in range(NT):
            x_t, tm = xt[t % 2], tmp[t % 2]
            p.dma('sp', x_t, x_d[t * 128:(t + 1) * 128, :])
            py = [ps[0], ps[1]]
            for fh in range(2):
                for k in range(8):
                    p.mm(py[fh], mixT[:, k, t * 128:(t + 1) * 128], wout[:, k, fh * 512:(fh + 1) * 512], start=(k == 0), stop=(k == 7))
            for fh in range(2):
                p.copy('act', tm[:, fh * 512:(fh + 1) * 512], py[fh])
            rc = cols[:, t, 0:1]
            rms_rstd(rc, tm, sq)
            p.stt('dve', tm, tm, rc, G1, ALU.mult, ALU.mult)
            p.tt('pool', x_t, x_t, tm, ALU.add)
            p.dma('sp', x1_d[t * 128:(t + 1) * 128, :], x_t)
            rc2 = cols[:, t, 1:2]
            rms_rstd(rc2, x_t, sq)
            p.stt('dve', tm, x_t, rc2, A2, ALU.mult, ALU.mult)
            p.tt('pool', tm, tm, B2, ALU.add)
            for k in range(8):
                pt = ps[2 + (k % 4)]
                p.tr(pt[:, 0:128], tm[:, k * 128:(k + 1) * 128], ident)
                p.copy('act' if k % 2 else 'dve', h2Tf[:, k, :], pt[:, 0:128])
                p.copy('pool', h2T[:, k, t * 128:(t + 1) * 128], h2Tf[:, k, :])
            if DBG in ('d1a', 'd1b', 'd1c'):
                p.memset('dve', gates[:, t, :], 0.125)
                continue
            pl = ps[6]
            for k in range(8):
                p.mm(pl[:, 0:128], h2Tf[:, k, :], rt[:, k, :], start=(k == 0), stop=(k == 7))
            L = lg[:, 0, :]
            p.copy('dve', L, pl[:, 0:8])
            m1 = cols[:, t, 2:3]
            m2 = cols[:, t, 3:4]
            p.reduce('dve', m1, L, ALU.max)
            mk1 = lg[:, 1, :]
            p.ts('dve', mk1, L, m1, ALU.is_equal)
            L2 = lg[:, 2, :]
            p.stt('dve', L2, mk1, -1e30, L, ALU.mult, ALU.add)
            p.reduce('dve', m2, L2, ALU.max)
            mk2 = lg[:, 3, :]
            p.ts('dve', mk2, L2, m2, ALU.is_equal)
            w1 = cols[:, t, 4:5]
            w2 = cols[:, t, 5:6]
            p.tt('dve', w1, m1, m2, ALU.subtract)
            p.act(w1, w1, AF.Sigmoid)
            p.ts('dve', w2, w1, -1.0, ALU.mult, 1.0, ALU.add)
            p.ts('dve', gates[:, t, :], mk1, w1, ALU.mult)
            p.stt('dve', gates[:, t, :], mk2, w2, gates[:, t, :], ALU.mult, ALU.add)
        hv = h2T_d.ap.rearrange("(k p) n -> p k n", p=128)
        for k in range(8):
            p.dma('sp', h2T_d.with_ap(hv[:, k, :]), h2T[:, k, :])
        p.dma('sp', gates_d.with_ap(gates_d.ap.rearrange("(t p) e -> p t e", p=128)), gates)
    p.finalize()
    return nc


def build_d2(NCH=8, H=3584, BLK=4):
    nc = bass.Bass("TRN2", target_bir_lowering=False)
    p = Prog(nc)
    CT = 16
    NTOK = NCH * CT * 128
    HC = H // 128
    NB = HC // BLK
    h2T_d = p.dram("h2T", [D, NTOK], BF16, "ExternalInput")
    g_d = p.dram("gate", [128, NCH * CT], F32, "ExternalInput")
    wg_d = p.dram("wg", [D, H], F32, "ExternalInput")
    wu_d = p.dram("wu", [D, H], F32, "ExternalInput")
    wd_d = p.dram("wd", [H, D], F32, "ExternalInput")
    out_d = p.dram("y", [NTOK, D], F32, "ExternalOutput")
    ps = [p.ps(f"ps{i}", [128, 512]) for i in range(8)]
    gt = p.sb("gt", [128, NCH * CT], F32)
    p.dma('sp', gt, g_d)
    h2T = [p.sb(f"h2T{i}", [128, 8, CT * 128], BF16) for i in range(2)]
    acc = p.sb("acc", [128, CT, D], F32)
    wgb = [p.sb(f"wg{i}", [128, 8, BLK * 128], BF16) for i in range(2)]
    wub = [p.sb(f"wu{i}", [128, 8, BLK * 128], BF16) for i in range(2)]
    wdb = [p.sb(f"wd{i}", [128, BLK, D], BF16) for i in range(2)]
    actT = [p.sb(f"actT{i}", [128, BLK, 512], BF16) for i in range(2)]
    sg = [p.sb(f"sg{i}", [128, 512], F32) for i in range(2)]
    yo = [p.sb(f"yo{i}", [128, D], F32) for i in range(2)]
    wgv = wg_d.ap.rearrange("(k p) h -> p k h", p=128)
    wuv = wu_d.ap.rearrange("(k p) h -> p k h", p=128)
    wdv = wd_d.ap.rearrange("(c p) f -> p c f", p=128)
    hv = h2T_d.ap.rearrange("(k p) n -> p k n", p=128)
    it = 0
    gi = 0
    for ch in range(NCH):
        hb = h2T[ch % 2]
        for k in range(8):
            p.dma('sp', hb[:, k, :], h2T_d.with_ap(hv[:, k, ch * CT * 128:(ch + 1) * CT * 128]))
        for b in range(NB):
            s = it % 2
            it += 1
            hs = slice(b * BLK * 128, (b + 1) * BLK * 128)
            for k in range(8):
                p.dma('pool', wgb[s][:, k, :], wg_d.with_ap(wgv[:, k, hs]))
                p.dma('pool', wub[s][:, k, :], wu_d.with_ap(wuv[:, k, hs]))
            for c in range(BLK):
                p.dma('pool', wdb[s][:, c, :], wd_d.with_ap(wdv[:, b * BLK + c, :]))
            for t0 in range(0, CT, 4):
                ts_ = slice(t0 * 128, (t0 + 4) * 128)
                a = actT[gi % 2]
                gi += 1
                for c in range(BLK):
                    pg, pu = ps[(c % 2) * 2], ps[(c % 2) * 2 + 1]
                    for k in range(8):
                        p.mm(pg, wgb[s][:, k, c * 128:(c + 1) * 128], hb[:, k, ts_], start=(k == 0), stop=(k == 7))
                    for k in range(8):
                        p.mm(pu, wub[s][:, k, c * 128:(c + 1) * 128], hb[:, k, ts_], start=(k == 0), stop=(k == 7))
                    sgt = sg[c % 2]
                    p.act(sgt, pg, AF.Silu)
                    p.tt('dve', a[:, c, :], sgt, pu, ALU.mult)
                for j in range(4):
                    t = t0 + j
                    for fh in range(2):
                        pd = ps[4 + ((j * 2 + fh) % 4)]
                        for c in range(BLK):
                            p.mm(pd, a[:, c, j * 128:(j + 1) * 128], wdb[s][:, c, fh * 512:(fh + 1) * 512], start=(c == 0), stop=(c == BLK - 1))
                        dst = acc[:, t, fh * 512:(fh + 1) * 512]
                        if b == 0:
                            p.copy('dve', dst, pd)
                        else:
                            p.tt('dve', dst, pd, dst, ALU.add)
        for t in range(CT):
            gt_ = ch * CT + t
            y = yo[t % 2]
            p.ts('pool', y, acc[:, t, :], gt[:, gt_:gt_ + 1], ALU.mult)
            p.dma('sp', out_d[gt_ * 128:(gt_ + 1) * 128, :], y)
    p.finalize()
    return nc


def build_d3(NT=16, NE=8):
    nc = bass.Bass("TRN2", target_bir_lowering=False)
    p = Prog(nc)
    NTOK = NT * 128
    ys_d = p.dram("ys", [NE, NTOK, D], F32, "ExternalInput")
    x1_d = p.dram("x1", [NTOK, D], F32, "ExternalInput")
    cT_d = p.dram("cT", [128, 8, 2], F32, "ExternalInput")
    modw_d = p.dram("mod_w", [D, 6 * D], F32, "ExternalInput")
    modb_d = p.dram("mod_b", [2, 6 * D], F32, "ExternalInput")
    ng_d = p.dram("norm_g", [4, D], F32, "ExternalInput")
    out_d = p.dram("out", [NTOK, D], F32, "ExternalOutput")
    mod_s = p.dram("mod_s", [2, 6 * D], F32, "Internal")
    ps = [p.ps(f"ps{i}", [128, 512]) for i in range(8)]
    epsc = p.sb("epsc", [128, 1], F32)
    p.memset('dve', epsc, NORM_EPS)
    with p.scope() as st:
        mod_rows(p, ps, st, cT_d, modw_d, modb_d, mod_s, [10, 11])
    with p.scope() as st:
        G2 = p.sb("G2", [128, D], F32, st)
        ng3 = p.sb("ng3", [128, D], F32, st)
        p.dma('sp', ng3, ng_d.with_ap(ng_d.ap[3:4, :].partition_broadcast(128)))
        p.dma('sp', G2, mod_s.with_ap(mod_s.ap[0:1, 5 * D:6 * D].partition_broadcast(128)))
        p.tt('dve', G2, G2, ng3, ALU.mult)
        yt = [p.sb(f"yt{i}", [128, NE, D], F32, st) for i in range(2)]
        x1t = [p.sb(f"x1t{i}", [128, D], F32, st) for i in range(2)]
        sq = p.sb("sq", [128, D], F32, st)
        cols = p.sb("cols", [128, NT], F32, st)
        for t in range(NT):
            y = yt[t % 2]
            x1 = x1t[t % 2]
            for e in range(NE):
                p.dma('sp', y[:, e, :], ys_d[e, t * 128:(t + 1) * 128, :])
            p.dma('sp', x1, x1_d[t * 128:(t + 1) * 128, :])
            f = y[:, 0, :]
            for e in range(1, NE):
                p.tt('dve' if e % 2 else 'pool', f, f, y[:, e, :], ALU.add)
            rc = cols[:, t:t + 1]
            p.act(sq, f, AF.Square)
            p.reduce('dve', rc, sq, ALU.add)
            p.act(rc, rc, AF.Sqrt, bias=epsc, scale=1.0 / D)
            p.recip(rc, rc)
            p.stt('dve', f, f, rc, G2, ALU.mult, ALU.mult)
            p.tt('pool', x1, x1, f, ALU.add)
            p.dma('sp', out_d[t * 128:(t + 1) * 128, :], x1)
    p.finalize()
    return nc


def run_post1(x_lat, mix_lat, c, c_ctx, mod_w, mod_b, norm_g, w_out, wg, wu, wd, router):
    import ml_dtypes
    cores = list(range(8))
    nc = build_d1()
    rpad = np.ascontiguousarray(np.concatenate([router, np.zeros((1024, 120), np.float32)], 1))
    in_maps = []
    for core in cores:
        b, q = core // 4, core % 4
        in_maps.append({
            "x": np.ascontiguousarray(x_lat[b, q * 2048:(q + 1) * 2048]),
            "mixT": np.ascontiguousarray(mix_lat[b, q * 2048:(q + 1) * 2048].T),
            "cT": _cT(c[b], c_ctx), "mod_w": np.ascontiguousarray(mod_w), "mod_b": np.ascontiguousarray(np.stack([mod_b] * 2)),
            "norm_g": np.ascontiguousarray(norm_g), "w_out": np.ascontiguousarray(w_out), "router": rpad, "ident": _IDENT,
        })
    r1 = run_bass_kernel_spmd(nc, in_maps, core_ids=cores, trace=TRACE).results
    if TRACE:
        pass
    h2T_all = np.ascontiguousarray(np.concatenate([np.asarray(r1[k]["h2T"]) for k in cores], axis=1))
    gates_all = np.concatenate([np.asarray(r1[k]["gates"]) for k in cores], axis=0)
    nc = build_d2()
    in_maps = []
    for e in cores:
        in_maps.append({
            "h2T": h2T_all,
            "gate": np.ascontiguousarray(gates_all[:, e].reshape(128, 128).T),
            "wg": np.ascontiguousarray(wg[e]), "wu": np.ascontiguousarray(wu[e]), "wd": np.ascontiguousarray(wd[e]),
        })
    r2 = run_bass_kernel_spmd(nc, in_maps, core_ids=cores, trace=TRACE).results
    nc = build_d3()
    in_maps = []
    for core in cores:
        b, q = core // 4, core % 4
        ys = np.ascontiguousarray(np.stack([np.asarray(r2[e]["y"])[core * 2048:(core + 1) * 2048] for e in cores]))
        in_maps.append({
            "ys": ys, "x1": np.asarray(r1[core]["x1"]),
            "cT": _cT(c[b], c_ctx), "mod_w": np.ascontiguousarray(mod_w), "mod_b": np.ascontiguousarray(np.stack([mod_b] * 2)),
            "norm_g": np.ascontiguousarray(norm_g),
        })
    r3 = run_bass_kernel_spmd(nc, in_maps, core_ids=cores, trace=TRACE).results
    x2 = np.empty_like(x_lat)
    for core in cores:
        b, q = core // 4, core % 4
        x2[b, q * 2048:(q + 1) * 2048] = r3[core]["out"]
    return x2
```
